# Optimizing a Trainium2 kernel written in Bass

```python
import math
import jax, jax.numpy as jnp
from jax import lax
import numpy as np

D_MODEL = 1024
BATCH = 8
SEQ = 4096
DEPTH = 1

CHUNK = 64
S5_WIDTH = 512
S5_GROUP = 16
S5_GROUPS = S5_WIDTH // S5_GROUP
S5_STATE = 64
DA_HEADS = 8
DA_HEAD_DIM = 64
DA_WIDTH = DA_HEADS * 2 * DA_HEAD_DIM
Q_BLOCK = 128
N_BRANCH = 2
IN_WIDTH = S5_WIDTH + 3 * DA_WIDTH + N_BRANCH * D_MODEL
N_GROUPS = 4
EXP_PER_GROUP = 8
N_EXPERTS = N_GROUPS * EXP_PER_GROUP
D_EXPERT = 256
TOP_K = 2
N_MOD = 6
EPS = 1e-6

kernel_name = "hybrid_s5_diffattn_hmoe_block"


def rms_norm(x, g):
    xf = x.astype(jnp.float32)
    y = xf * lax.rsqrt(jnp.mean(xf * xf, axis=-1, keepdims=True) + EPS)
    return (y * g.astype(jnp.float32)).astype(x.dtype)


def _complex_affine_combine(e1, e2):
    a1r, a1i, b1r, b1i = e1
    a2r, a2i, b2r, b2i = e2
    ar = a2r * a1r - a2i * a1i
    ai = a2r * a1i + a2i * a1r
    br = a2r * b1r - a2i * b1i + b2r
    bi = a2r * b1i + a2i * b1r + b2i
    return (ar, ai, br, bi)


def s5_branch(u, a_re, a_im, b_re, b_im, c_re, c_im, d_skip, log_dt, w_glu, b_glu):
    nb, L, _ = u.shape
    uf = u.astype(jnp.float32)
    ug = uf.reshape(nb, L, S5_GROUPS, S5_GROUP)
    dt = jnp.exp(log_dt.astype(jnp.float32))[:, None]
    lr = a_re.astype(jnp.float32)
    li = a_im.astype(jnp.float32)
    mag = jnp.exp(lr * dt)
    abar_re = mag * jnp.cos(li * dt)
    abar_im = mag * jnp.sin(li * dt)
    den = lr * lr + li * li
    nr = abar_re - 1.0
    f_re = (nr * lr + abar_im * li) / den
    f_im = (abar_im * lr - nr * li) / den
    bb_re = f_re[..., None] * b_re - f_im[..., None] * b_im
    bb_im = f_re[..., None] * b_im + f_im[..., None] * b_re
    bu_re = jnp.einsum('blgc,gpc->blgp', ug, bb_re)
    bu_im = jnp.einsum('blgc,gpc->blgp', ug, bb_im)
    a_seq_re = jnp.broadcast_to(abar_re[None, None], (1, L, S5_GROUPS, S5_STATE))
    a_seq_im = jnp.broadcast_to(abar_im[None, None], (1, L, S5_GROUPS, S5_STATE))
    _, _, x_re, x_im = lax.associative_scan(
        _complex_affine_combine, (a_seq_re, a_seq_im, bu_re, bu_im), axis=1)
    y = (jnp.einsum('blgp,gcp->blgc', x_re, c_re)
         - jnp.einsum('blgp,gcp->blgc', x_im, c_im))
    y = y.reshape(nb, L, S5_WIDTH) + d_skip * uf
    y = jax.nn.gelu(y)
    y = y * jax.nn.sigmoid(y @ w_glu + b_glu)
    return y.astype(u.dtype)


def diff_attention(q, k, v, lq1, lk1, lq2, lk2, g_subln, lambda_init):
    nb, L, _ = q.shape
    q = q.reshape(nb, L, DA_HEADS, 2, DA_HEAD_DIM)
    k = k.reshape(nb, L, DA_HEADS, 2, DA_HEAD_DIM)
    v = v.reshape(nb, L, DA_HEADS, 2 * DA_HEAD_DIM)
    lam = (jnp.exp(jnp.sum(lq1.astype(jnp.float32) * lk1.astype(jnp.float32)))
           - jnp.exp(jnp.sum(lq2.astype(jnp.float32) * lk2.astype(jnp.float32)))
           + lambda_init)
    n_blk = L // Q_BLOCK
    qb = q.reshape(nb, n_blk, Q_BLOCK, DA_HEADS, 2, DA_HEAD_DIM).swapaxes(0, 1)
    k_chunk = jnp.arange(L) // CHUNK
    scale = DA_HEAD_DIM ** -0.5

    def one_block(args):
        q_blk, i = args
        s = jnp.einsum('bqhmd,bkhmd->bhmqk', q_blk, k).astype(jnp.float32) * scale
        q_chunk = (i * Q_BLOCK + jnp.arange(Q_BLOCK)) // CHUNK
        mask = k_chunk[None, :] <= q_chunk[:, None]
        s = jnp.where(mask, s, -jnp.inf)
        p = jax.nn.softmax(s, axis=-1)
        attn = p[:, :, 0] - lam * p[:, :, 1]
        return jnp.einsum('bhqk,bkhe->bqhe', attn.astype(v.dtype), v)

    o = lax.map(one_block, (qb, jnp.arange(n_blk)))
    o = o.swapaxes(0, 1).reshape(nb, L, DA_HEADS, 2 * DA_HEAD_DIM)
    o = rms_norm(o, g_subln) * (1.0 - lambda_init)
    return o.reshape(nb, L, DA_WIDTH)


def hier_moe(u, w_rg, b_rg, w_re, b_re, w_g, w_u, w_d):
    nb, L, D = u.shape
    t = u.reshape(nb * L, D)
    grp_prob = jax.nn.softmax((t @ w_rg + b_rg).astype(jnp.float32), axis=-1)
    gp, gi = lax.top_k(grp_prob, 1)
    exp_logits = (t @ w_re + b_re).astype(jnp.float32).reshape(-1, N_GROUPS, EXP_PER_GROUP)
    in_grp = jnp.take_along_axis(exp_logits, gi[:, :, None], axis=1)[:, 0]
    top_v, top_i = lax.top_k(in_grp, TOP_K)
    w = jax.nn.softmax(top_v, axis=-1) * gp
    eid = gi * EXP_PER_GROUP + top_i
    combine = jnp.einsum('tk,tke->te', w,
                         jax.nn.one_hot(eid, N_EXPERTS, dtype=jnp.float32)).astype(u.dtype)

    def expert_step(acc, params):
        wg, wu, wd, cw = params
        hdn = jax.nn.silu(t @ wg) * (t @ wu)
        return acc + cw[:, None] * (hdn @ wd), None

    y, _ = lax.scan(expert_step, jnp.zeros_like(t), (w_g, w_u, w_d, combine.T))
    return y.reshape(nb, L, D)


def setup_inputs(seed: int = 0) -> dict:
    key = jax.random.key(seed)
    ks = iter(jax.random.split(key, 40))
    f32 = jnp.float32

    def nrm(shape, scale):
        return jax.random.normal(next(ks), shape, f32) * scale

    def gain(shape):
        return 1.0 + nrm(shape, 0.01)

    n_idx = jnp.arange(S5_STATE, dtype=f32)
    return {
        "x": nrm((BATCH, SEQ, D_MODEL), 1.0),
        "c": nrm((BATCH, D_MODEL), 1.0),
        "w_ada": nrm((DEPTH, D_MODEL, N_MOD * D_MODEL), 0.5 * D_MODEL ** -0.5),
        "b_ada": nrm((DEPTH, N_MOD * D_MODEL), 0.01),
        "g_norm_mix": gain((DEPTH, D_MODEL)),
        "w_in": nrm((DEPTH, D_MODEL, IN_WIDTH), D_MODEL ** -0.5),
        "b_in": nrm((DEPTH, IN_WIDTH), 0.01),
        "s5_a_re": -0.5 + nrm((DEPTH, S5_GROUPS, S5_STATE), 0.01),
        "s5_a_im": math.pi * n_idx + nrm((DEPTH, S5_GROUPS, S5_STATE), 0.01),
        "s5_b_re": nrm((DEPTH, S5_GROUPS, S5_STATE, S5_GROUP), (2 * S5_GROUP) ** -0.5),
        "s5_b_im": nrm((DEPTH, S5_GROUPS, S5_STATE, S5_GROUP), (2 * S5_GROUP) ** -0.5),
        "s5_c_re": nrm((DEPTH, S5_GROUPS, S5_GROUP, S5_STATE), S5_STATE ** -0.5),
        "s5_c_im": nrm((DEPTH, S5_GROUPS, S5_GROUP, S5_STATE), S5_STATE ** -0.5),
        "s5_d": nrm((DEPTH, S5_WIDTH), 1.0),
        "s5_log_dt": jax.random.uniform(next(ks), (DEPTH, S5_GROUPS), f32,
                                        minval=math.log(1e-3), maxval=math.log(1e-1)),
        "w_glu": nrm((DEPTH, S5_WIDTH, S5_WIDTH), S5_WIDTH ** -0.5),
        "b_glu": nrm((DEPTH, S5_WIDTH), 0.01),
        "lambda_q1": nrm((DEPTH, DA_HEAD_DIM), 0.1),
        "lambda_k1": nrm((DEPTH, DA_HEAD_DIM), 0.1),
        "lambda_q2": nrm((DEPTH, DA_HEAD_DIM), 0.1),
        "lambda_k2": nrm((DEPTH, DA_HEAD_DIM), 0.1),
        "g_subln": gain((DEPTH, 2 * DA_HEAD_DIM)),
        "w_br_ssm": nrm((DEPTH, S5_WIDTH, D_MODEL), S5_WIDTH ** -0.5),
        "w_br_attn": nrm((DEPTH, DA_WIDTH, D_MODEL), DA_WIDTH ** -0.5),
        "w_out": nrm((DEPTH, D_MODEL, D_MODEL), D_MODEL ** -0.5),
        "g_norm_ffn": gain((DEPTH, D_MODEL)),
        "w_router_grp": nrm((DEPTH, D_MODEL, N_GROUPS), D_MODEL ** -0.5),
        "b_router_grp": nrm((DEPTH, N_GROUPS), 0.01),
        "w_router_exp": nrm((DEPTH, D_MODEL, N_EXPERTS), D_MODEL ** -0.5),
        "b_router_exp": nrm((DEPTH, N_EXPERTS), 0.01),
        "w_exp_gate": nrm((DEPTH, N_EXPERTS, D_MODEL, D_EXPERT), D_MODEL ** -0.5),
        "w_exp_up": nrm((DEPTH, N_EXPERTS, D_MODEL, D_EXPERT), D_MODEL ** -0.5),
        "w_exp_down": nrm((DEPTH, N_EXPERTS, D_EXPERT, D_MODEL), D_EXPERT ** -0.5),
        "g_final": gain((D_MODEL,)),
    }


def reference(x, c, w_ada, b_ada, g_norm_mix, w_in, b_in,
              s5_a_re, s5_a_im, s5_b_re, s5_b_im, s5_c_re, s5_c_im, s5_d, s5_log_dt,
              w_glu, b_glu, lambda_q1, lambda_k1, lambda_q2, lambda_k2, g_subln,
              w_br_ssm, w_br_attn, w_out, g_norm_ffn,
              w_router_grp, b_router_grp, w_router_exp, b_router_exp,
              w_exp_gate, w_exp_up, w_exp_down, g_final):
    h = x
    cs = jax.nn.silu(c)
    splits = [S5_WIDTH, S5_WIDTH + DA_WIDTH, S5_WIDTH + 2 * DA_WIDTH, S5_WIDTH + 3 * DA_WIDTH]
    for l in range(DEPTH):
        lambda_init = 0.8 - 0.6 * math.exp(-0.3 * l)
        mod = (cs @ w_ada[l] + b_ada[l])[:, None, :]
        sh_m, sc_m, gt_m, sh_f, sc_f, gt_f = jnp.split(mod, N_MOD, axis=-1)

        u = rms_norm(h, g_norm_mix[l]) * (1.0 + sc_m) + sh_m
        proj = u @ w_in[l] + b_in[l]
        s5_in, q, k, v, gates = jnp.split(proj, splits, axis=-1)
        y_ssm = s5_branch(s5_in, s5_a_re[l], s5_a_im[l], s5_b_re[l], s5_b_im[l],
                          s5_c_re[l], s5_c_im[l], s5_d[l], s5_log_dt[l],
                          w_glu[l], b_glu[l]) @ w_br_ssm[l]
        y_att = diff_attention(q, k, v, lambda_q1[l], lambda_k1[l], lambda_q2[l],
                               lambda_k2[l], g_subln[l], lambda_init) @ w_br_attn[l]
        g_ssm, g_att = jnp.split(jax.nn.sigmoid(gates), N_BRANCH, axis=-1)
        mix = (g_ssm * y_ssm + g_att * y_att) @ w_out[l]
        h = h + gt_m * mix

        u = rms_norm(h, g_norm_ffn[l]) * (1.0 + sc_f) + sh_f
        ffn = hier_moe(u, w_router_grp[l], b_router_grp[l], w_router_exp[l], b_router_exp[l],
                       w_exp_gate[l], w_exp_up[l], w_exp_down[l])
        h = h + gt_f * ffn
    return rms_norm(h, g_final)
```

```python
import math
from contextlib import ExitStack
import numpy as np
import concourse.bass as bass
import concourse.mybir as mybir
from concourse.bass_utils import run_bass_kernel_spmd

F32 = mybir.dt.float32
BF16 = mybir.dt.bfloat16
AF = mybir.ActivationFunctionType
ALU = mybir.AluOpType
AX = mybir.AxisListType

L = 4096
D = 1024
NTT = 8
EPS = 1e-6
LAMBDA_INIT = 0.8 - 0.6 * math.exp(-0.3 * 0)
NEXP = 32
ENGS = ("pe", "act", "dve", "pool", "sp")
ENG_ATTR = {"pe": "tensor", "act": "scalar", "dve": "vector", "pool": "gpsimd", "sp": "sync"}


class Buf:
    __slots__ = ("name", "w", "r")

    def __init__(self, name=""):
        self.name = name
        self.w = None
        self.r = []


class Op:
    __slots__ = ("eng", "idx", "fn", "waits", "signal", "count", "dma", "dsem", "dval", "snap", "bg")

    def __init__(self, eng, idx, fn, dma):
        self.eng = eng
        self.idx = idx
        self.fn = fn
        self.waits = []
        self.signal = False
        self.count = None
        self.dma = dma
        self.dsem = None
        self.dval = None
        self.snap = None
        self.bg = False


class Sched:
    def __init__(self, nc, n_dma_sems=48):
        self.nc = nc
        self.pending = {e: [] for e in ENGS}
        self.nops = {e: 0 for e in ENGS}
        self.last = {e: None for e in ENGS}
        self.seen = {e: {} for e in ENGS}
        self.n_dma_sems = n_dma_sems
        self.dma_rr = 0
        self.dma_last = [None] * n_dma_sems
        self.dma_last_nb = [None] * n_dma_sems
        self.dma_val = [0] * n_dma_sems
        self.cnt = {e: 0 for e in ENGS}
        self.sems = {e: nc.alloc_semaphore(name=f"sem_{e}") for e in ENGS}
        self.dsems = [nc.alloc_semaphore(name=f"dsem_{i}") for i in range(n_dma_sems)]

    def _need(self, o, d):
        e = o.eng
        if d.dma:
            key = ("d", d.dsem)
            if self.seen[e].get(key, 0) >= d.dval:
                return
            self.seen[e][key] = d.dval
            o.waits.append(d)
        else:
            if d.eng == e and e == "pe":
                return
            key = d.eng
            if self.seen[e].get(key, -1) >= d.idx:
                return
            self.seen[e][key] = d.idx
            d.signal = True
            o.waits.append(d)
        if d.snap is not None:
            se = self.seen[e]
            for k, v in d.snap.items():
                if k == e:
                    continue
                if se.get(k, -1) < v:
                    se[k] = v

    def add(self, eng, fn, reads=(), writes=(), dma=False, bg=False):
        o = Op(eng, self.nops[eng], fn, dma)
        o.bg = bg
        self.nops[eng] += 1
        deps = []
        for b in reads:
            if b.w is not None:
                deps.append(b.w)
        for b in writes:
            if b.w is not None:
                deps.append(b.w)
            deps.extend(b.r)
        if dma:
            s = self.dma_rr
            self.dma_rr = (self.dma_rr + 1) % self.n_dma_sems
            prev = self.dma_last[s]
            if prev is not None:
                deps.append(prev)
            self.dma_val[s] += 16
            o.dsem = s
            o.dval = self.dma_val[s]
            self.dma_last[s] = o
            if not bg:
                self.dma_last_nb[s] = o
        for d in deps:
            if d is not o:
                self._need(o, d)
        for b in reads:
            b.r.append(o)
        for b in writes:
            b.w = o
            b.r = []
        self.pending[eng].append(o)
        if not dma:
            self.last[eng] = o
        o.snap = dict(self.seen[eng])
        return o

    def flush(self):
        lasts = [self.last[e] for e in ENGS if self.last[e] is not None]
        dlasts = [(self.dma_last_nb[i] if d.bg else d) for i, d in enumerate(self.dma_last) if d is not None]
        dlasts = [d for d in dlasts if d is not None]
        for e in ENGS:
            o = Op(e, self.nops[e], None, False)
            self.nops[e] += 1
            for d in lasts + dlasts:
                self._need(o, d)
            self.pending[e].append(o)
            o.snap = dict(self.seen[e])
        nc = self.nc
        for e in ENGS:
            for o in self.pending[e]:
                if o.signal and not o.dma:
                    self.cnt[e] += 1
                    o.count = self.cnt[e]
        sems, dsems = self.sems, self.dsems
        with nc.Block() as block:
            for e in ENGS:
                ops = self.pending[e]

                def body(eng, ops=ops, e=e):
                    for o in ops:
                        for d in o.waits:
                            if d.dma:
                                eng.wait_ge(dsems[d.dsem], d.dval)
                            else:
                                eng.wait_ge(sems[d.eng], d.count)
                        if o.fn is None:
                            continue
                        ins = o.fn(eng)
                        if o.dma:
                            ins.then_inc(dsems[o.dsem], 16)
                        elif o.signal:
                            ins.then_inc(sems[e], 1)

                getattr(block, ENG_ATTR[e])(body)
        self.pending = {e: [] for e in ENGS}


class Ctx:
    pass


def dma(S, q, out, in_, reads=(), writes=(), bg=False):
    return S.add(q, lambda e: e.dma_start(out=out, in_=in_), reads, writes, dma=True, bg=bg)


VEC_ROWS = {}


def build(debug=()):
    nc = bass.Bass("TRN2", target_bir_lowering=False)
    C = Ctx()
    C.nc = nc
    C.debug = set(debug)
    C.S = S = Sched(nc)

    def din(name, shape):
        return nc.dram_tensor(name, list(shape), F32, kind="ExternalInput").ap()

    I = C.I = {}
    for name, shape in [
        ("x", (L, D)), ("c", (D,)), ("w_ada", (D, 6 * D)), ("b_ada", (6 * D,)), ("g_norm_mix", (D,)),
        ("w_in", (D, 5632)), ("b_in", (5632,)),
        ("s5_a_re", (32, 64)), ("s5_a_im", (32, 64)), ("s5_b_re", (32, 64, 16)), ("s5_b_im", (32, 64, 16)),
        ("s5_c_re", (32, 16, 64)), ("s5_c_im", (32, 16, 64)), ("s5_d", (512,)), ("s5_log_dt", (32,)),
        ("w_glu", (512, 512)), ("b_glu", (512,)),
        ("lambda_q1", (64,)), ("lambda_k1", (64,)), ("lambda_q2", (64,)), ("lambda_k2", (64,)), ("g_subln", (128,)),
        ("w_br_ssm", (512, D)), ("w_br_attn", (D, D)), ("w_out", (D, D)), ("g_norm_ffn", (D,)),
        ("w_router_grp", (D, 4)), ("b_router_grp", (4,)), ("w_router_exp", (D, 32)), ("b_router_exp", (32,)),
        ("w_exp_gate", (NEXP, D, 256)), ("w_exp_up", (NEXP, D, 256)), ("w_exp_down", (NEXP, 256, D)),
        ("g_final", (D,)),
    ]:
        I[name] = din(name, shape)
    C.out = nc.dram_tensor("out", [L, D], F32, kind="ExternalOutput").ap()

    def scratch(name, shape, dt):
        if name in C.debug:
            return nc.dram_tensor(name, list(shape), dt, kind="ExternalOutput").ap()
        return nc.dram_tensor(name, list(shape), dt, kind="Internal").ap()

    C.scratch = scratch
    W = C.W = {}
    W["qT"] = scratch("qT", (D, L), BF16)
    W["kT"] = scratch("kT", (D, L), BF16)
    W["vS"] = scratch("vS", (L, D), BF16)
    W["gT"] = scratch("gT", (2 * D, L), BF16)
    W["s5S"] = scratch("s5S", (L, 512), F32)
    W["y2T"] = scratch("y2T", (512, L), BF16)
    W["onT"] = scratch("onT", (D, L), BF16)
    W["hS"] = scratch("hS", (L, D), F32)
    W["u2T"] = scratch("u2T", (D, L), BF16)
    W["combS"] = scratch("combS", (L, NEXP), F32)
    W["wgS"] = scratch("wgS", (NEXP, D, 256), BF16)
    W["wuS"] = scratch("wuS", (NEXP, D, 256), BF16)
    W["wdS"] = scratch("wdS", (NEXP, 256, D), BF16)
    NSLOT = 6144
    W["XN"] = scratch("XN", (L, D), BF16)
    W["CG"] = scratch("CG", (L, 8), F32)
    W["XS"] = scratch("XS", (NSLOT, D), BF16)
    W["CGS"] = scratch("CGS", (NSLOT, 8), F32)
    W["YS"] = scratch("YS", (NSLOT, D), F32)
    W["WALL"] = scratch("WALL", (NEXP * 128, 6144), BF16)
    C.WB = {k: Buf(k) for k in W}
    C.sparse = "dense" not in C.debug
    if "dbg_cols" in C.debug:
        C.dbg_cols = nc.dram_tensor("dbg_cols", [128, 256], F32, kind="ExternalOutput").ap()
    if "dbg_s5y" in C.debug:
        C.dbg_s5y = nc.dram_tensor("dbg_s5y", [L, 512], F32, kind="ExternalOutput").ap()

    with ExitStack() as _es:
        p0 = _es.enter_context(nc.psum_tensor("ps0", [128, 512], F32))
        p1 = _es.enter_context(nc.psum_tensor("ps1", [128, 512], F32))
        p2 = _es.enter_context(nc.psum_tensor("ps2", [128, 512], F32))
        p3 = _es.enter_context(nc.psum_tensor("ps3", [128, 512], F32))
        p4 = _es.enter_context(nc.psum_tensor("ps4", [128, 512], F32))
        p5 = _es.enter_context(nc.psum_tensor("ps5", [128, 512], F32))
        p6 = _es.enter_context(nc.psum_tensor("ps6", [128, 512], F32))
        p7 = _es.enter_context(nc.psum_tensor("ps7", [128, 512], F32))
        ident_f = _es.enter_context(nc.sbuf_tensor("ident_f", [128, 128], F32))
        ident_b = _es.enter_context(nc.sbuf_tensor("ident_b", [128, 128], BF16))
        ones_b = _es.enter_context(nc.sbuf_tensor("ones_b", [128, 128], BF16))
        cols = _es.enter_context(nc.sbuf_tensor("cols", [128, 256], F32))
        gtm_bc = _es.enter_context(nc.sbuf_tensor("gtm_bc", [128, D], F32))
        gtf_bc = _es.enter_context(nc.sbuf_tensor("gtf_bc", [128, D], F32))
        C.RK = _es.enter_context(nc.sbuf_tensor("RK", [128, 32], F32))
        C.OH = _es.enter_context(nc.sbuf_tensor("OH", [128, 32, 4], F32))
        C.tot = _es.enter_context(nc.sbuf_tensor("tot", [128, 4], F32))
        C.SLOT_I = _es.enter_context(nc.sbuf_tensor("SLOT_I", [128, 32], mybir.dt.int32))
        C.TG_I = _es.enter_context(nc.sbuf_tensor("TG_I", [128, 12], mybir.dt.int32))
        C.WIDX = _es.enter_context(nc.sbuf_tensor("WIDX", [128, 96], mybir.dt.int32))
        C.b_w4 = Buf("w4g")
        C.b_rk = Buf("rk")
        C.PS = [p0, p1, p2, p3, p4, p5, p6, p7]
        C.PB = [Buf(f"ps{i}") for i in range(8)]
        C.ident_f, C.ident_b, C.ones_b, C.cols = ident_f, ident_b, ones_b, cols
        C.gtm_bc, C.gtf_bc = gtm_bc, gtf_bc
        C.b_const = Buf("const")
        C.b_cols = Buf("cols")
        C.b_gt = Buf("gt")
        with ExitStack() as _es01:
            C.w_in = _es01.enter_context(nc.sbuf_tensor("sb_w_in", [128, 8, 5632], BF16))
            C.b_win = [Buf(f"win{i}") for i in range(11)]
            phase0(C)
            S.flush()
            if "stop0" not in C.debug and "skip1" not in C.debug:
                phase1(C)
        if "stop0" not in C.debug:
            if "skip2" not in C.debug and "stop1" not in C.debug:
                phase2(C)
            if "heads" in C.debug:
                C.heads = [0, 5]
            with ExitStack() as _es34:
                C.wbs = _es34.enter_context(nc.sbuf_tensor("wbs", [128, 4, D], BF16))
                C.wba = _es34.enter_context(nc.sbuf_tensor("wba", [128, 8, D], BF16))
                C.wout = _es34.enter_context(nc.sbuf_tensor("wout", [128, 8, D], BF16))
                if "skip3" not in C.debug and "stop1" not in C.debug:
                    phase3(C)
                if "stop3" not in C.debug and "stop1" not in C.debug:
                    phase4a(C)
            if "stop3" not in C.debug and "stop1" not in C.debug:
                if "n_exp" in C.debug:
                    C.n_exp = 2
                if "stop4a" not in C.debug:
                    if C.sparse:
                        phase4p(C)
                        phase4b_sparse(C)
                    else:
                        phase4b(C)
        if "dbg_cols" in C.debug:
            dma(S, "sp", C.dbg_cols, cols[:], reads=[C.b_cols])
            S.flush()
    return nc


COL_C = 0
COL_BADA = 8
COL_BIN = 56
COL_GNM = 100
COL_GNF = 108
COL_BGLU = 116
COL_GSUB = 120
COL_MOD = 128
COL_G1 = 176
COL_G2 = 184
COL_GSUBS = 192


def phase0(C):
    nc, S, I = C.nc, C.S, C.I
    cols = C.cols
    bc = C.b_const
    S.add("pool", lambda e: e.memset(C.ident_f[:], 0.0), writes=[bc])
    S.add("pool", lambda e: e.affine_select(out=C.ident_f[:], in_=C.ident_f[:], pattern=[[-1, 128]],
                                            compare_op=ALU.not_equal, fill=1.0, base=0, channel_multiplier=1),
          reads=[bc], writes=[bc])
    S.add("dve", lambda e: e.tensor_copy(out=C.ident_b[:], in_=C.ident_f[:]), reads=[bc], writes=[bc])
    S.add("dve", lambda e: e.memset(C.ones_b[:], 1.0), writes=[bc])
    with ExitStack() as _es:
        rows = _es.enter_context(nc.sbuf_tensor("rows", [128, 128], F32))
        cs_b = _es.enter_context(nc.sbuf_tensor("cs_b", [128, 8], BF16))
        cs_rep = _es.enter_context(nc.sbuf_tensor("cs_rep", [128, 8, 128], BF16))
        wa0 = _es.enter_context(nc.sbuf_tensor("wa0", [128, 8, 512], BF16))
        wa1 = _es.enter_context(nc.sbuf_tensor("wa1", [128, 8, 512], BF16))
        bada_bc = _es.enter_context(nc.sbuf_tensor("bada_bc", [128, 2, D], F32))
        b_rows = Buf("rows")
        S.add("dve", lambda e: e.memset(rows[:], 0.0), writes=[b_rows])
        for name, base, n in [("c", COL_C, 8), ("b_ada", COL_BADA, 48), ("b_in", COL_BIN, 44), ("g_norm_mix", COL_GNM, 8),
                              ("g_norm_ffn", COL_GNF, 8), ("b_glu", COL_BGLU, 4), ("g_subln", COL_GSUB, 1)]:
            dma(S, "sp", rows[base:base + n, :], I[name].rearrange("(k p) -> k p", p=128), writes=[b_rows])
        b_bada = Buf("bada_bc")
        dma(S, "sp", bada_bc[:, 0, :], I["b_ada"][2 * D:3 * D].partition_broadcast(128), writes=[b_bada])
        dma(S, "sp", bada_bc[:, 1, :], I["b_ada"][5 * D:6 * D].partition_broadcast(128), writes=[b_bada])
        pT = C.PS[0]
        S.add("pe", lambda e: e.transpose(out=pT[:, 0:128], in_=rows[:], identity=C.ident_f[:]), reads=[b_rows, bc], writes=[C.PB[0]])
        S.add("dve", lambda e: e.tensor_copy(out=cols[:, 0:128], in_=pT[:, 0:128]), reads=[C.PB[0]], writes=[C.b_cols])
        b_cs = Buf("cs")
        S.add("act", lambda e: e.activation(out=cols[:, COL_C:COL_C + 8], in_=cols[:, COL_C:COL_C + 8], func=AF.Silu),
              reads=[C.b_cols], writes=[C.b_cols])
        S.add("dve", lambda e: e.tensor_copy(out=cs_b[:], in_=cols[:, COL_C:COL_C + 8]), reads=[C.b_cols], writes=[b_cs])
        S.add("dve", lambda e: e.tensor_copy(out=cs_rep[:], in_=cs_b[:].unsqueeze(2).broadcast_to([128, 8, 128])),
              reads=[b_cs], writes=[b_cs])
        was = [wa0, wa1]
        waf = [_es.enter_context(nc.sbuf_tensor(f"waf{i}", [128, 8, 512], F32)) for i in range(2)]
        b_waf = [Buf("waf0"), Buf("waf1")]
        b_wa = [Buf("wa0"), Buf("wa1")]
        pcol = C.PS[1]
        for n in range(12):
            wa = was[n % 2]
            bw = b_wa[n % 2]
            waf_ = waf[n % 2]
            dma(S, "sp", waf_[:], I["w_ada"][:, n * 512:(n + 1) * 512].rearrange("(k p) n -> p k n", p=128), writes=[b_waf[n % 2]])
            S.add("act", lambda e, wa=wa, waf_=waf_: e.activation(out=wa[:], in_=waf_[:], func=AF.Copy), reads=[b_waf[n % 2]], writes=[bw])
            if n >= 1:
                i_ = n - 1
                dma(S, "pool", C.w_in[:, :, i_ * 512:(i_ + 1) * 512], I["w_in"][:, i_ * 512:(i_ + 1) * 512].rearrange("(k p) n -> p k n", p=128),
                    writes=[C.b_win[i_]], bg=True)
            for s in range(4):
                j = n * 4 + s
                for k in range(8):
                    S.add("pe", lambda e, wa=wa, s=s, k=k, j=j: e.matmul(pcol[:, j:j + 1], lhsT=wa[:, k, s * 128:(s + 1) * 128],
                                                                      rhs=cs_b[:, k:k + 1], start=(k == 0), stop=(k == 7)),
                          reads=[bw, b_cs], writes=[C.PB[1]])
            if n in (4, 5, 10, 11):
                prow = C.PS[2 + (n % 2)]
                pbb = C.PB[2 + (n % 2)]
                for k in range(8):
                    S.add("pe", lambda e, wa=wa, k=k, prow=prow: e.matmul(prow[:], lhsT=cs_rep[:, k, :], rhs=wa[:, k, :],
                                                                       start=(k == 0), stop=(k == 7)),
                          reads=[bw, b_cs], writes=[pbb])
                dst = C.gtm_bc if n < 6 else C.gtf_bc
                hh = n % 2
                wi = 0 if n < 6 else 1
                S.add("dve", lambda e, dst=dst, hh=hh, wi=wi, prow=prow: e.tensor_tensor(
                    out=dst[:, hh * 512:(hh + 1) * 512], in0=prow[:], in1=bada_bc[:, wi, hh * 512:(hh + 1) * 512], op=ALU.add),
                    reads=[pbb, b_bada], writes=[C.b_gt])
        S.add("dve", lambda e: e.tensor_tensor(out=cols[:, COL_MOD:COL_MOD + 48], in0=pcol[:, 0:48],
                                               in1=cols[:, COL_BADA:COL_BADA + 48], op=ALU.add),
              reads=[C.PB[1], C.b_cols], writes=[C.b_cols])
        S.add("dve", lambda e: e.scalar_tensor_tensor(out=cols[:, COL_G1:COL_G1 + 8], in0=cols[:, COL_MOD + 8:COL_MOD + 16], scalar=1.0,
                                                      in1=cols[:, COL_GNM:COL_GNM + 8], op0=ALU.add, op1=ALU.mult),
              reads=[C.b_cols], writes=[C.b_cols])
        S.add("dve", lambda e: e.scalar_tensor_tensor(out=cols[:, COL_G2:COL_G2 + 8], in0=cols[:, COL_MOD + 32:COL_MOD + 40], scalar=1.0,
                                                      in1=cols[:, COL_GNF:COL_GNF + 8], op0=ALU.add, op1=ALU.mult),
              reads=[C.b_cols], writes=[C.b_cols])
        S.add("dve", lambda e: e.tensor_scalar(out=cols[:, COL_GSUBS:COL_GSUBS + 1], in0=cols[:, COL_GSUB:COL_GSUB + 1],
                                               scalar1=(1.0 - LAMBDA_INIT), scalar2=None, op0=ALU.mult),
              reads=[C.b_cols], writes=[C.b_cols])
        S.flush()


def phase1(C):
    nc, S, I, W, WB = C.nc, C.S, C.I, C.W, C.WB
    cols = C.cols
    with ExitStack() as _es:
        w_in = C.w_in
        xt0 = _es.enter_context(nc.sbuf_tensor("xt0", [128, 4, D], F32))
        xt1 = _es.enter_context(nc.sbuf_tensor("xt1", [128, 4, D], F32))
        xn = _es.enter_context(nc.sbuf_tensor("xn", [128, 4, D], BF16))
        junk = _es.enter_context(nc.sbuf_tensor("junk", [128, D], BF16))
        ssq = _es.enter_context(nc.sbuf_tensor("ssq", [128, 8], F32))
        uT0 = _es.enter_context(nc.sbuf_tensor("uT0", [128, 8, 512], BF16))
        uT1 = _es.enter_context(nc.sbuf_tensor("uT1", [128, 8, 512], BF16))
        stg0 = _es.enter_context(nc.sbuf_tensor("stg0", [128, 4, 512], BF16))
        stg1 = _es.enter_context(nc.sbuf_tensor("stg1", [128, 4, 512], BF16))
        vstg = _es.enter_context(nc.sbuf_tensor("vstg", [128, 4, D], BF16))
        s5stg = _es.enter_context(nc.sbuf_tensor("s5stg", [128, 4, 512], F32))
        bias_bc = _es.enter_context(nc.sbuf_tensor("bias_bc", [128, 1536], F32))
        b_win = C.b_win
        b_bias = Buf("bias_bc")
        dma(S, "sp", bias_bc[:, 0:512], I["b_in"][0:512].partition_broadcast(128), writes=[b_bias])
        dma(S, "sp", bias_bc[:, 512:1536], I["b_in"][2560:3584].partition_broadcast(128), writes=[b_bias])
        xts = [xt0, xt1]
        b_xt = [Buf("xt0"), Buf("xt1")]
        uTs = [uT0, uT1]
        b_uT = [Buf("uT0"), Buf("uT1")]
        stgs = [stg0, stg1]
        b_stg = [Buf("stg0"), Buf("stg1")]
        b_xn, b_ssq, b_junk, b_vstg, b_s5stg = Buf("xn"), Buf("ssq"), Buf("junk"), Buf("vstg"), Buf("s5stg")
        PS, PB = C.PS, C.PB

        def load_x(t):
            dma(S, "sp", xts[t % 2][:], I["x"][t * 512:(t + 1) * 512, :].rearrange("(s p) d -> p s d", p=128), writes=[b_xt[t % 2]])

        load_x(0)
        rot = 0
        stg_i = 0
        for t in range(NTT):
            if t + 1 < NTT:
                load_x(t + 1)
            xt = xts[t % 2]
            bx = b_xt[t % 2]
            uT = uTs[t % 2]
            bu = b_uT[t % 2]
            S.add("dve", lambda e: e.memset(ssq[:], 0.0), writes=[b_ssq])
            for s in range(4):
                S.add("act", lambda e, s=s, xt=xt: e.activation(out=junk[:], in_=xt[:, s, :], func=AF.Square, accum_out=ssq[:, s:s + 1]),
                      reads=[bx, b_ssq], writes=[b_junk, b_ssq])
            S.add("dve", lambda e: e.tensor_scalar(out=ssq[:, 4:8], in0=ssq[:, 0:4], scalar1=1.0 / D, scalar2=EPS, op0=ALU.mult, op1=ALU.add),
                  reads=[b_ssq], writes=[b_ssq])
            S.add("act", lambda e: e.activation(out=ssq[:, 4:8], in_=ssq[:, 4:8], func=AF.Sqrt), reads=[b_ssq], writes=[b_ssq])
            S.add("dve", lambda e: e.reciprocal(out=ssq[:, 4:8], in_=ssq[:, 4:8]), reads=[b_ssq], writes=[b_ssq])
            for s in range(4):
                S.add("dve", lambda e, s=s, xt=xt: e.tensor_scalar(out=xn[:, s, :], in0=xt[:, s, :], scalar1=ssq[:, 4 + s:5 + s], scalar2=None,
                                                                op0=ALU.mult),
                      reads=[bx, b_ssq], writes=[b_xn])
            for k in range(8):
                pt = PS[k % 2]
                ptb = pt[:].bitcast(BF16)
                for s in range(4):
                    S.add("pe", lambda e, s=s, k=k, ptb=ptb: e.transpose(out=ptb[:, s * 128:(s + 1) * 128], in_=xn[:, s, k * 128:(k + 1) * 128],
                                                                      identity=C.ident_b[:]),
                          reads=[b_xn, C.b_const], writes=[PB[k % 2]])
                S.add("dve", lambda e, k=k, ptb=ptb, uT=uT: e.tensor_scalar(out=uT[:, k, :], in0=ptb[:, 0:512], scalar1=cols[:, COL_G1 + k:COL_G1 + k + 1],
                                                                         scalar2=cols[:, COL_MOD + k:COL_MOD + k + 1], op0=ALU.mult, op1=ALU.add),
                      reads=[PB[k % 2], C.b_cols], writes=[bu])
            groups = [("qT", 4, 0), ("qT", 8, 4), ("kT", 12, 0), ("kT", 16, 4), ("gT", 28, 0), ("gT", 32, 4), ("gT", 36, 8), ("gT", 40, 12)]
            for (dst, ct0, r0) in groups:
                stg = stgs[stg_i % 2]
                bs = b_stg[stg_i % 2]
                stg_i += 1
                for cc in range(4):
                    ct = ct0 + cc
                    bank = 2 + (rot % 4)
                    rot += 1
                    pp = PS[bank]
                    for k in range(8):
                        S.add("pe", lambda e, pp=pp, k=k, ct=ct, uT=uT: e.matmul(pp[:], lhsT=w_in[:, k, ct * 128:(ct + 1) * 128], rhs=uT[:, k, :],
                                                                             start=(k == 0), stop=(k == 7)),
                              reads=[b_win[ct // 4], bu], writes=[PB[bank]])
                    fn = AF.Sigmoid if dst == "gT" else AF.Identity
                    S.add("act", lambda e, pp=pp, cc=cc, ct=ct, stg=stg, fn=fn: e.activation(out=stg[:, cc, :], in_=pp[:], func=fn,
                                                                                        bias=cols[:, COL_BIN + ct:COL_BIN + ct + 1], scale=1.0),
                          reads=[PB[bank], C.b_cols], writes=[bs])
                dma(S, "sp", W[dst][r0 * 128:(r0 + 4) * 128, t * 512:(t + 1) * 512].rearrange("(c p) n -> p c n", p=128), stg[:],
                    reads=[bs])
            for s in range(4):
                for hh in range(3):
                    bank = 6 + (rot % 2)
                    rot += 1
                    pp = PS[bank]
                    c0 = 0 if hh == 2 else 2560 + hh * 512
                    for k in range(8):
                        S.add("pe", lambda e, pp=pp, k=k, s=s, c0=c0, uT=uT: e.matmul(pp[:], lhsT=uT[:, k, s * 128:(s + 1) * 128], rhs=w_in[:, k, c0:c0 + 512],
                                                                                  start=(k == 0), stop=(k == 7)),
                              reads=[b_win[c0 // 512], bu], writes=[PB[bank]])
                    if hh < 2:
                        S.add("dve", lambda e, pp=pp, s=s, hh=hh: e.tensor_tensor(out=vstg[:, s, hh * 512:(hh + 1) * 512], in0=pp[:],
                                                                               in1=bias_bc[:, 512 + hh * 512:1024 + hh * 512], op=ALU.add),
                              reads=[PB[bank], b_bias], writes=[b_vstg])
                    else:
                        S.add("dve", lambda e, pp=pp, s=s: e.tensor_tensor(out=s5stg[:, s, :], in0=pp[:], in1=bias_bc[:, 0:512], op=ALU.add),
                              reads=[PB[bank], b_bias], writes=[b_s5stg])
            dma(S, "sp", W["vS"][t * 512:(t + 1) * 512, :].rearrange("(s p) d -> p s d", p=128), vstg[:], reads=[b_vstg])
            dma(S, "sp", W["s5S"][t * 512:(t + 1) * 512, :].rearrange("(s p) d -> p s d", p=128), s5stg[:], reads=[b_s5stg])
        S.flush()


def phase4p(C):
    nc, S, W = C.nc, C.S, C.W
    with ExitStack() as _es:
        sb = lambda name, shape, dt: _es.enter_context(nc.sbuf_tensor(name, shape, dt))
        NB4 = 6
        xb = [sb(f"xb{i}", [128, D], BF16) for i in range(NB4)]
        cgb = [sb(f"cgb{i}", [128, 8], F32) for i in range(NB4)]
        b_xb = [Buf(f"xb{i}") for i in range(NB4)]
        b_cgb = [Buf(f"cgb{i}") for i in range(NB4)]
        for blk in range(32):
            sl = blk % NB4
            dma(S, "sp", xb[sl][:], W["XN"][blk * 128:(blk + 1) * 128, :], writes=[b_xb[sl]])
            dma(S, "sp", cgb[sl][:], W["CG"][blk * 128:(blk + 1) * 128, :], writes=[b_cgb[sl]])
            S.add("pool", lambda e, sl=sl, blk=blk: e.indirect_dma_start(
                out=W["XS"], out_offset=bass.IndirectOffsetOnAxis(ap=C.SLOT_I[:, blk:blk + 1], axis=0), in_=xb[sl][:], in_offset=None), reads=[b_xb[sl]], writes=[], dma=True)
            S.add("pool", lambda e, sl=sl, blk=blk: e.indirect_dma_start(
                out=W["CGS"], out_offset=bass.IndirectOffsetOnAxis(ap=C.SLOT_I[:, blk:blk + 1], axis=0), in_=cgb[sl][:], in_offset=None), reads=[b_cgb[sl]], writes=[], dma=True)
        S.flush()


def phase4b_sparse(C):
    nc, S, I, W = C.nc, C.S, C.I, C.W
    PS, PB = C.PS, C.PB
    cols = C.cols
    bc = C.b_const
    NT = 11
    regs = {}
    with ExitStack() as _es:
        sb = lambda name, shape, dt: _es.enter_context(nc.sbuf_tensor(name, shape, dt))
        xs = [sb(f"xs{i}", [128, 4, D], BF16) for i in range(2)]
        cgs = [sb(f"cgs{i}", [128, 4, 8], F32) for i in range(2)]
        u2t = sb("u2ts", [128, 8, 512], BF16)
        yacc = [sb(f"yaccs{i}", [128, 4, D], F32) for i in range(2)]
        wall = [sb(f"wall{i}", [128, 6144], BF16) for i in range(3)]
        wg = [wall[i][:, 0:2048].rearrange("p (k f) -> p k f", f=256) for i in range(3)]
        wu = [wall[i][:, 2048:4096].rearrange("p (k f) -> p k f", f=256) for i in range(3)]
        wd = [wall[i][:, 4096:6144].rearrange("p (k d) -> p k d", d=D) for i in range(3)]
        sg = [sb(f"sgs{i}", [128, 2, 512], BF16) for i in range(2)]
        hd = [sb(f"hds{i}", [128, 2, 512], BF16) for i in range(2)]
        b_xs = [Buf("xs0"), Buf("xs1")]
        b_cgs = [Buf("cgs0"), Buf("cgs1")]
        b_u2t = Buf("u2ts")
        b_ya = [Buf("ya0"), Buf("ya1")]
        b_wg = [Buf("wg0"), Buf("wg1"), Buf("wg2")]
        b_wu = [Buf("wu0"), Buf("wu1"), Buf("wu2")]
        b_wd = [Buf("wd0"), Buf("wd1"), Buf("wd2")]
        b_sg = [Buf("sg0"), Buf("sg1")]
        b_hd = [Buf("hd0"), Buf("hd1")]

        def load_tile(st):
            sl = st % 2
            dma(S, "sp", xs[sl][:], W["XS"][st * 512:(st + 1) * 512, :].rearrange("(s p) d -> p s d", p=128), writes=[b_xs[sl]])
            dma(S, "sp", cgs[sl][:], W["CGS"][st * 512:(st + 1) * 512, :].rearrange("(s p) e -> p s e", p=128), writes=[b_cgs[sl]])

        def load_w(st, j, sl):
            q = st * 8 + j
            S.add("pool", lambda e, q=q, sl=sl: e.indirect_dma_start(out=wall[sl][:], out_offset=None, in_=W["WALL"],
                                                                  in_offset=bass.IndirectOffsetOnAxis(ap=C.WIDX[:, q:q + 1], axis=0)),
                  reads=[C.b_rk], writes=[b_wg[sl], b_wu[sl], b_wd[sl]], dma=True)

        seq = [(st, j) for st in range(NT) for j in range(8)]
        deferred = []
        load_tile(0)
        load_w(0, 0, 0)
        load_w(0, 1, 1)
        rot = 0
        cnt = 0
        for qi, (st, j) in enumerate(seq):
            sl = qi % 3
            tsl = st % 2
            if j == 0:
                for k in range(8):
                    bk = 6 + k % 2
                    ptb = PS[bk][:].bitcast(BF16)
                    for s_ in range(4):
                        S.add("pe", lambda e, s_=s_, k=k, ptb=ptb, tsl=tsl: e.transpose(out=ptb[:, s_ * 128:(s_ + 1) * 128], in_=xs[tsl][:, s_, k * 128:(k + 1) * 128], identity=C.ident_b[:]),
                              reads=[b_xs[tsl], bc], writes=[PB[bk]])
                    S.add("dve", lambda e, k=k, ptb=ptb: e.tensor_scalar(out=u2t[:, k, :], in0=ptb[:, 0:512], scalar1=cols[:, COL_G2 + k:COL_G2 + k + 1],
                                                                       scalar2=cols[:, COL_MOD + 24 + k:COL_MOD + 25 + k], op0=ALU.mult, op1=ALU.add),
                          reads=[PB[bk], C.b_cols], writes=[b_u2t])
                S.add("pool", lambda e, tsl=tsl: e.memset(yacc[tsl][:], 0.0), writes=[b_ya[tsl]])
            c2 = cnt % 2
            cnt += 1
            for f in range(2):
                for k in range(8):
                    S.add("pe", lambda e, f=f, k=k, sl=sl: e.matmul(PS[f][:], lhsT=wg[sl][:, k, f * 128:(f + 1) * 128], rhs=u2t[:, k, :], start=(k == 0), stop=(k == 7)),
                          reads=[b_wg[sl], b_u2t], writes=[PB[f]])
            for f in range(2):
                for k in range(8):
                    S.add("pe", lambda e, f=f, k=k, sl=sl: e.matmul(PS[2 + f][:], lhsT=wu[sl][:, k, f * 128:(f + 1) * 128], rhs=u2t[:, k, :], start=(k == 0), stop=(k == 7)),
                          reads=[b_wu[sl], b_u2t], writes=[PB[2 + f]])
            for f in range(2):
                S.add("act", lambda e, f=f, c2=c2: e.activation(out=sg[c2][:, f, :], in_=PS[f][:], func=AF.Silu), reads=[PB[f]], writes=[b_sg[c2]])
                S.add("dve", lambda e, f=f, c2=c2: e.tensor_tensor(out=hd[c2][:, f, :], in0=PS[2 + f][:], in1=sg[c2][:, f, :], op=ALU.mult),
                      reads=[PB[2 + f], b_sg[c2]], writes=[b_hd[c2]])

            def down(st=st, j=j, sl=sl, tsl=tsl, c2=c2):
                nonlocal rot
                for sub in range(4):
                    for dh in range(2):
                        bk = 4 + rot % 4
                        rot += 1
                        for f in range(2):
                            S.add("pe", lambda e, bk=bk, f=f, sub=sub, dh=dh: e.matmul(
                                PS[bk][:], lhsT=hd[c2][:, f, sub * 128:(sub + 1) * 128], rhs=wd[sl][:, f, dh * 512:(dh + 1) * 512], start=(f == 0), stop=(f == 1)),
                                reads=[b_hd[c2], b_wd[sl]], writes=[PB[bk]])
                        S.add("dve", lambda e, bk=bk, sub=sub, dh=dh: e.scalar_tensor_tensor(
                            out=yacc[tsl][:, sub, dh * 512:(dh + 1) * 512], in0=PS[bk][:], scalar=cgs[tsl][:, sub, j:j + 1], in1=yacc[tsl][:, sub, dh * 512:(dh + 1) * 512],
                            op0=ALU.mult, op1=ALU.add), reads=[PB[bk], b_cgs[tsl], b_ya[tsl]], writes=[b_ya[tsl]])
                if j == 7:
                    dma(S, "sp", W["YS"][st * 512:(st + 1) * 512, :].rearrange("(s p) d -> p s d", p=128), yacc[tsl][:], reads=[b_ya[tsl]])

            if "nodefer" in C.debug:
                down()
            else:
                if deferred:
                    deferred.pop()()
                deferred.append(down)
            if qi + 2 < len(seq):
                load_w(seq[qi + 2][0], seq[qi + 2][1], (qi + 2) % 3)
            if j == 0 and st + 1 < NT:
                load_tile(st + 1)
        while deferred:
            deferred.pop()()
        S.flush()
    with ExitStack() as _es:
        sb = lambda name, shape, dt: _es.enter_context(nc.sbuf_tensor(name, shape, dt))
        yg = [sb(f"yg{i}", [128, 4, D], F32) for i in range(3)]
        hp = [sb(f"hpf{i}", [128, 4, D], F32) for i in range(3)]
        gfin = sb("gfin_s", [128, D], F32)
        junk = sb("junk6", [128, D], BF16)
        ssq = sb("ssq6", [128, 8], F32)
        b_yg = [Buf("yg0"), Buf("yg1"), Buf("yg2")]
        b_hp = [Buf("hp0"), Buf("hp1"), Buf("hp2")]
        b_gfin, b_junk, b_ssq = Buf("gfin"), Buf("junk"), Buf("ssq")
        dma(S, "sp", gfin[:], I["g_final"].partition_broadcast(128), writes=[b_gfin])

        def fetch(t):
            sl = t % 3
            for s_ in range(4):
                blk = t * 4 + s_
                S.add("pool", lambda e, sl=sl, s_=s_, blk=blk: e.indirect_dma_start(
                    out=yg[sl][:, s_, :], out_offset=None, in_=W["YS"], in_offset=bass.IndirectOffsetOnAxis(ap=C.SLOT_I[:, blk:blk + 1], axis=0)),
                    reads=[C.b_rk], writes=[b_yg[sl]], dma=True)
            dma(S, "sp", hp[sl][:], W["hS"][t * 512:(t + 1) * 512, :].rearrange("(s p) d -> p s d", p=128), writes=[b_hp[sl]])

        fetch(0)
        fetch(1)
        for t in range(NTT):
            sl = t % 3
            if t + 2 < NTT:
                fetch(t + 2)
            r0 = t * 512
            S.add("dve", lambda e, sl=sl: e.tensor_tensor(out=yg[sl][:], in0=yg[sl][:], in1=C.gtf_bc[:].unsqueeze(1).broadcast_to([128, 4, D]), op=ALU.mult),
                  reads=[b_yg[sl], C.b_gt], writes=[b_yg[sl]])
            S.add("dve", lambda e, sl=sl: e.tensor_tensor(out=hp[sl][:], in0=hp[sl][:], in1=yg[sl][:], op=ALU.add), reads=[b_yg[sl], b_hp[sl]], writes=[b_hp[sl]])
            S.add("dve", lambda e: e.memset(ssq[:], 0.0), writes=[b_ssq])
            for s_ in range(4):
                S.add("act", lambda e, s_=s_, sl=sl: e.activation(out=junk[:], in_=hp[sl][:, s_, :], func=AF.Square, accum_out=ssq[:, s_:s_ + 1]),
                      reads=[b_hp[sl], b_ssq], writes=[b_junk, b_ssq])
            S.add("dve", lambda e: e.tensor_scalar(out=ssq[:, 4:8], in0=ssq[:, 0:4], scalar1=1.0 / D, scalar2=EPS, op0=ALU.mult, op1=ALU.add),
                  reads=[b_ssq], writes=[b_ssq])
            S.add("act", lambda e: e.activation(out=ssq[:, 4:8], in_=ssq[:, 4:8], func=AF.Sqrt), reads=[b_ssq], writes=[b_ssq])
            S.add("dve", lambda e: e.reciprocal(out=ssq[:, 4:8], in_=ssq[:, 4:8]), reads=[b_ssq], writes=[b_ssq])
            for s_ in range(4):
                S.add("dve", lambda e, s_=s_, sl=sl: e.scalar_tensor_tensor(out=hp[sl][:, s_, :], in0=hp[sl][:, s_, :], scalar=ssq[:, 4 + s_:5 + s_], in1=gfin[:], op0=ALU.mult, op1=ALU.mult),
                      reads=[b_hp[sl], b_ssq, b_gfin], writes=[b_hp[sl]])
            dma(S, "sp", C.out[r0:r0 + 512, :].rearrange("(s p) d -> p s d", p=128), hp[sl][:], reads=[b_hp[sl]])
        S.flush()


_NC_CACHE = {}


def make_in_maps(inputs):
    maps = []
    shared = {}
    for k, v in inputs.items():
        if k in ("x", "c"):
            continue
        a = np.asarray(v)
        if k != "g_final":
            a = a[0]
        shared[k] = np.ascontiguousarray(a, dtype=np.float32)
    for b in range(8):
        m = dict(shared)
        m["x"] = np.ascontiguousarray(np.asarray(inputs["x"])[b], dtype=np.float32)
        m["c"] = np.ascontiguousarray(np.asarray(inputs["c"])[b], dtype=np.float32)
        maps.append(m)
    return maps


def kernel(**inputs):
    if "nc" not in _NC_CACHE:
        _NC_CACHE["nc"] = build()
    nc = _NC_CACHE["nc"]
    res = run_bass_kernel_spmd(nc, make_in_maps(inputs), core_ids=list(range(8)))
    return np.stack([np.asarray(r["out"], dtype=np.float32) for r in res.results], axis=0)


def _pstep(t):
    n = 1
    for s in list(t.shape)[1:]:
        n *= s
    return n


def view(t, off, dims, parts=128, p0=0):
    ps = _pstep(t)
    return bass.AP(t, p0 * ps + off, [[ps, parts]] + [list(d) for d in dims])


def phase2(C):
    nc, S, I, W = C.nc, C.S, C.I, C.W
    PS, PB = C.PS, C.PB
    cols = C.cols
    ident_f, ident_b = C.ident_f, C.ident_b
    bc = C.b_const
    with ExitStack() as _es:
        Btr = _es.enter_context(nc.sbuf_tensor("Btr", [128, 32, 64], BF16))
        Bti = _es.enter_context(nc.sbuf_tensor("Bti", [128, 32, 64], BF16))
        Ctr = _es.enter_context(nc.sbuf_tensor("Ctr", [64, 32, 128], F32))
        Cti = _es.enter_context(nc.sbuf_tensor("Cti", [64, 32, 128], F32))
        Dt = _es.enter_context(nc.sbuf_tensor("Dt", [128, 32, 128], BF16))
        P12 = _es.enter_context(nc.sbuf_tensor("P12", [64, 2, 64], F32))
        PP = _es.enter_context(nc.sbuf_tensor("PP", [64, 9, 2, 64], F32))
        apw = _es.enter_context(nc.sbuf_tensor("apw", [64, 2, 9, 32], F32))
        dbc = _es.enter_context(nc.sbuf_tensor("dbc", [128, 512], F32))
        wglu = _es.enter_context(nc.sbuf_tensor("wglu", [128, 4, 512], BF16))
        b_wt = Buf("s5w")
        b_wglu = Buf("wglu")
        b_dbc = Buf("dbc")
        dma(S, "pool", wglu[:], I["w_glu"].rearrange("(k p) n -> p k n", p=128), writes=[b_wglu])
        dma(S, "sp", dbc[:], I["s5_d"].partition_broadcast(128), writes=[b_dbc])
        with ExitStack() as _es:
            nat = _es.enter_context(nc.sbuf_tensor("nat", [128, 2, 64], F32))
            cnat = _es.enter_context(nc.sbuf_tensor("cnat", [128, 8, 64], F32))
            sm = _es.enter_context(nc.sbuf_tensor("sm", [64, 24, 32], F32))
            pw = _es.enter_context(nc.sbuf_tensor("pw", [64, 2, 9, 32], F32))
            Bn = _es.enter_context(nc.sbuf_tensor("Bn", [64, 2, 32, 16], F32))
            Bb = _es.enter_context(nc.sbuf_tensor("Bb", [64, 2, 32, 16], F32))
            CT = _es.enter_context(nc.sbuf_tensor("CT", [64, 3, 32, 16], F32))
            Ere = _es.enter_context(nc.sbuf_tensor("Ere", [64, 32, 15, 16], F32))
            Eim = _es.enter_context(nc.sbuf_tensor("Eim", [64, 32, 15, 16], F32))
            tmpB = _es.enter_context(nc.sbuf_tensor("tmpB", [64, 2, 32, 16], F32))
            halfpi = _es.enter_context(nc.sbuf_tensor("halfpi", [64, 1], F32))
            b_nat, b_cnat, b_sm, b_pw, b_Bn, b_Bb, b_CT, b_E, b_tmp = [Buf(n) for n in "nat cnat sm pw Bn Bb CT E tmp".split()]
            S.add("dve", lambda e: e.memset(nat[:], 0.0), writes=[b_nat])
            dma(S, "sp", nat[0:32, 0, :], I["s5_a_re"], writes=[b_nat])
            dma(S, "sp", nat[0:32, 1, :], I["s5_a_im"], writes=[b_nat])
            dma(S, "sp", cnat[:, 0:4, :], I["s5_c_re"].rearrange("g c p -> (g c) p").rearrange("(t q) p -> q t p", q=128), writes=[b_cnat])
            dma(S, "sp", cnat[:, 4:8, :], I["s5_c_im"].rearrange("g c p -> (g c) p").rearrange("(t q) p -> q t p", q=128), writes=[b_cnat])
            dma(S, "sp", Bn[:, 0, :, :], I["s5_b_re"].rearrange("g p c -> p g c"), writes=[b_Bn])
            dma(S, "sp", Bn[:, 1, :, :], I["s5_b_im"].rearrange("g p c -> p g c"), writes=[b_Bn])
            LR, LI, DT, AR, AI, T0, T1, T2, FR, FI, DEN, NR = range(12)
            dma(S, "sp", sm[:, DT, :], I["s5_log_dt"].partition_broadcast(64), writes=[b_sm])
            S.add("dve", lambda e: e.memset(halfpi[:], math.pi / 2), writes=[b_sm])
            for i, slot in ((0, LR), (1, LI)):
                S.add("pe", lambda e, i=i: e.transpose(out=PS[0][0:64, i * 128:(i + 1) * 128], in_=nat[:, i, :], identity=ident_f[:]),
                      reads=[b_nat, bc], writes=[PB[0]])
                S.add("dve", lambda e, i=i, slot=slot: e.tensor_copy(out=sm[:, slot, :], in_=PS[0][0:64, i * 128:i * 128 + 32]),
                      reads=[PB[0]], writes=[b_sm])
            for ri in range(2):
                for t in range(4):
                    S.add("pe", lambda e, ri=ri, t=t: e.transpose(out=PS[1][0:64, t * 128:(t + 1) * 128], in_=cnat[:, ri * 4 + t, :], identity=ident_f[:]),
                          reads=[b_cnat, bc], writes=[PB[1]])
                S.add("dve", lambda e, ri=ri: e.tensor_copy(out=CT[:, ri, :, :], in_=PS[1][0:64, :].rearrange("p (g c) -> p g c", c=16)),
                      reads=[PB[1]], writes=[b_CT])
            S.add("dve", lambda e: e.tensor_scalar(out=CT[:, 2, :, :], in0=CT[:, 1, :, :], scalar1=-1.0, scalar2=None, op0=ALU.mult),
                  reads=[b_CT], writes=[b_CT])

            def sv(i):
                return sm[:, i, :]

            def dv(fn, **kw):
                S.add("dve", lambda e: getattr(e, fn)(**kw), reads=[b_sm, b_pw], writes=[b_sm, b_pw])

            def av(**kw):
                S.add("act", lambda e: e.activation(**kw), reads=[b_sm, b_pw], writes=[b_sm, b_pw])

            av(out=sv(DT), in_=sv(DT), func=AF.Exp)
            dv("tensor_tensor", out=sv(T0), in0=sv(LR), in1=sv(DT), op=ALU.mult)
            av(out=sv(T0), in_=sv(T0), func=AF.Exp, scale=1.0 / 16)
            dv("tensor_tensor", out=sv(T1), in0=sv(LI), in1=sv(DT), op=ALU.mult)
            av(out=sv(AI), in_=sv(T1), func=AF.Sin, scale=1.0 / 16)
            av(out=sv(AR), in_=sv(T1), func=AF.Sin, scale=1.0 / 16, bias=halfpi[:, 0:1])
            dv("tensor_tensor", out=sv(AR), in0=sv(AR), in1=sv(T0), op=ALU.mult)
            dv("tensor_tensor", out=sv(AI), in0=sv(AI), in1=sv(T0), op=ALU.mult)
            for _ in range(4):
                dv("tensor_tensor", out=sv(T0), in0=sv(AR), in1=sv(AR), op=ALU.mult)
                dv("tensor_tensor", out=sv(T1), in0=sv(AI), in1=sv(AI), op=ALU.mult)
                dv("tensor_tensor", out=sv(T2), in0=sv(AR), in1=sv(AI), op=ALU.mult)
                dv("tensor_tensor", out=sv(AR), in0=sv(T0), in1=sv(T1), op=ALU.subtract)
                dv("tensor_scalar", out=sv(AI), in0=sv(T2), scalar1=2.0, scalar2=None, op0=ALU.mult)
            dv("tensor_tensor", out=sv(T0), in0=sv(LR), in1=sv(LR), op=ALU.mult)
            dv("tensor_tensor", out=sv(T1), in0=sv(LI), in1=sv(LI), op=ALU.mult)
            dv("tensor_tensor", out=sv(DEN), in0=sv(T0), in1=sv(T1), op=ALU.add)
            dv("reciprocal", out=sv(DEN), in_=sv(DEN))
            dv("tensor_scalar", out=sv(NR), in0=sv(AR), scalar1=-1.0, scalar2=None, op0=ALU.add)
            dv("tensor_tensor", out=sv(T0), in0=sv(NR), in1=sv(LR), op=ALU.mult)
            dv("tensor_tensor", out=sv(T1), in0=sv(AI), in1=sv(LI), op=ALU.mult)
            dv("tensor_tensor", out=sv(FR), in0=sv(T0), in1=sv(T1), op=ALU.add)
            dv("tensor_tensor", out=sv(FR), in0=sv(FR), in1=sv(DEN), op=ALU.mult)
            dv("tensor_tensor", out=sv(T0), in0=sv(AI), in1=sv(LR), op=ALU.mult)
            dv("tensor_tensor", out=sv(T1), in0=sv(NR), in1=sv(LI), op=ALU.mult)
            dv("tensor_tensor", out=sv(FI), in0=sv(T0), in1=sv(T1), op=ALU.subtract)
            dv("tensor_tensor", out=sv(FI), in0=sv(FI), in1=sv(DEN), op=ALU.mult)
            dv("memset", ap=pw[:, 0, 0, :], constant=1.0)
            dv("memset", ap=pw[:, 1, 0, :], constant=0.0)
            for t in range(8):
                dv("tensor_tensor", out=sv(T0), in0=pw[:, 0, t, :], in1=sv(AR), op=ALU.mult)
                dv("tensor_tensor", out=sv(T1), in0=pw[:, 1, t, :], in1=sv(AI), op=ALU.mult)
                dv("tensor_tensor", out=pw[:, 0, t + 1, :], in0=sv(T0), in1=sv(T1), op=ALU.subtract)
                dv("tensor_tensor", out=sv(T0), in0=pw[:, 0, t, :], in1=sv(AI), op=ALU.mult)
                dv("tensor_tensor", out=sv(T1), in0=pw[:, 1, t, :], in1=sv(AR), op=ALU.mult)
                dv("tensor_tensor", out=pw[:, 1, t + 1, :], in0=sv(T0), in1=sv(T1), op=ALU.add)
            dv("tensor_copy", out=P12[:, 0, 0:32], in_=pw[:, 0, 8, :])
            dv("tensor_copy", out=P12[:, 0, 32:64], in_=pw[:, 0, 8, :])
            dv("tensor_scalar", out=P12[:, 1, 0:32], in0=pw[:, 1, 8, :], scalar1=-1.0, scalar2=None, op0=ALU.mult)
            dv("tensor_copy", out=P12[:, 1, 32:64], in_=pw[:, 1, 8, :])
            dv("memset", ap=apw[:, 0, 0, :], constant=1.0)
            dv("memset", ap=apw[:, 1, 0, :], constant=0.0)
            for t in range(8):
                dv("tensor_tensor", out=sv(T0), in0=apw[:, 0, t, :], in1=pw[:, 0, 8, :], op=ALU.mult)
                dv("tensor_tensor", out=sv(T1), in0=apw[:, 1, t, :], in1=pw[:, 1, 8, :], op=ALU.mult)
                dv("tensor_tensor", out=apw[:, 0, t + 1, :], in0=sv(T0), in1=sv(T1), op=ALU.subtract)
                dv("tensor_tensor", out=sv(T0), in0=apw[:, 0, t, :], in1=pw[:, 1, 8, :], op=ALU.mult)
                dv("tensor_tensor", out=sv(T1), in0=apw[:, 1, t, :], in1=pw[:, 0, 8, :], op=ALU.mult)
                dv("tensor_tensor", out=apw[:, 1, t + 1, :], in0=sv(T0), in1=sv(T1), op=ALU.add)
            for i in range(9):
                dv("tensor_copy", out=PP[:, i, 0, 0:32], in_=apw[:, 0, i, :])
                dv("tensor_copy", out=PP[:, i, 0, 32:64], in_=apw[:, 0, i, :])
                dv("tensor_scalar", out=PP[:, i, 1, 0:32], in0=apw[:, 1, i, :], scalar1=-1.0, scalar2=None, op0=ALU.mult)
                dv("tensor_copy", out=PP[:, i, 1, 32:64], in_=apw[:, 1, i, :])

            def bcast_g(ap2):
                return ap2.unsqueeze(2).broadcast_to([64, 32, 16])

            def big(fn, reads, writes, **kw):
                S.add("dve", lambda e: getattr(e, fn)(**kw), reads=reads, writes=writes)

            RB = [b_sm, b_pw, b_Bn, b_Bb, b_tmp, b_CT]
            big("tensor_tensor", RB, [b_tmp], out=tmpB[:, 0], in0=Bn[:, 0], in1=bcast_g(sv(FR)), op=ALU.mult)
            big("tensor_tensor", RB, [b_tmp], out=tmpB[:, 1], in0=Bn[:, 1], in1=bcast_g(sv(FI)), op=ALU.mult)
            big("tensor_tensor", RB, [b_Bb], out=Bb[:, 0], in0=tmpB[:, 0], in1=tmpB[:, 1], op=ALU.subtract)
            big("tensor_tensor", RB, [b_tmp], out=tmpB[:, 0], in0=Bn[:, 1], in1=bcast_g(sv(FR)), op=ALU.mult)
            big("tensor_tensor", RB, [b_tmp], out=tmpB[:, 1], in0=Bn[:, 0], in1=bcast_g(sv(FI)), op=ALU.mult)
            big("tensor_tensor", RB, [b_Bb], out=Bb[:, 1], in0=tmpB[:, 0], in1=tmpB[:, 1], op=ALU.add)
            if "dbg_s5w" in C.debug:
                C.dbg_s5w = nc.dram_tensor("dbg_s5w", [64, 4 * 32 + 2 * 512], F32, kind="ExternalOutput").ap()
                dma(S, "sp", C.dbg_s5w[:, 0:32], pw[:, 0, 1, :], reads=[b_pw])
                dma(S, "sp", C.dbg_s5w[:, 32:64], pw[:, 1, 1, :], reads=[b_pw])
                dma(S, "sp", C.dbg_s5w[:, 64:96], pw[:, 0, 8, :], reads=[b_pw])
                dma(S, "sp", C.dbg_s5w[:, 96:128], pw[:, 1, 8, :], reads=[b_pw])
                dma(S, "sp", C.dbg_s5w[:, 128:640], Bb[:, 0].rearrange("p g c -> p (g c)"), reads=[b_Bb])
                dma(S, "sp", C.dbg_s5w[:, 640:1152], Bb[:, 1].rearrange("p g c -> p (g c)"), reads=[b_Bb])
            S.add("pool", lambda e: e.memset(Ere[:], 0.0), writes=[b_E])
            S.add("pool", lambda e: e.memset(Eim[:], 0.0), writes=[b_E])
            for m in range(8):
                t = 7 - m
                pr, pi = bcast_g(pw[:, 0, t, :]), bcast_g(pw[:, 1, t, :])
                big("tensor_tensor", RB, [b_tmp], out=tmpB[:, 0], in0=Bb[:, 0], in1=pr, op=ALU.mult)
                big("tensor_tensor", RB, [b_tmp], out=tmpB[:, 1], in0=Bb[:, 1], in1=pi, op=ALU.mult)
                big("tensor_tensor", RB + [b_E], [b_E], out=Ere[:, :, m, :], in0=tmpB[:, 0], in1=tmpB[:, 1], op=ALU.subtract)
                big("tensor_tensor", RB, [b_tmp], out=tmpB[:, 0], in0=Bb[:, 0], in1=pi, op=ALU.mult)
                big("tensor_tensor", RB, [b_tmp], out=tmpB[:, 1], in0=Bb[:, 1], in1=pr, op=ALU.mult)
                big("tensor_tensor", RB + [b_E], [b_E], out=Eim[:, :, m, :], in0=tmpB[:, 0], in1=tmpB[:, 1], op=ALU.add)
            Ctr4 = Ctr[:].rearrange("p g (j c) -> p g j c", c=16)
            Cti4 = Cti[:].rearrange("p g (j c) -> p g j c", c=16)
            for j in range(8):
                pr, pi = bcast_g(pw[:, 0, j + 1, :]), bcast_g(pw[:, 1, j + 1, :])
                big("tensor_tensor", RB, [b_tmp], out=tmpB[:, 0], in0=CT[:, 0], in1=pr, op=ALU.mult)
                big("tensor_tensor", RB, [b_tmp], out=tmpB[:, 1], in0=CT[:, 1], in1=pi, op=ALU.mult)
                big("tensor_tensor", RB + [b_wt], [b_wt], out=Ctr4[:, :, j, :], in0=tmpB[:, 0], in1=tmpB[:, 1], op=ALU.subtract)
                big("tensor_tensor", RB, [b_tmp], out=tmpB[:, 0], in0=CT[:, 0], in1=pi, op=ALU.mult)
                big("tensor_tensor", RB, [b_tmp], out=tmpB[:, 1], in0=CT[:, 2], in1=pr, op=ALU.mult)
                big("tensor_tensor", RB + [b_wt], [b_wt], out=Cti4[:, :, j, :], in0=tmpB[:, 1], in1=tmpB[:, 0], op=ALU.subtract)
            rot = 0
            for g0 in range(0, 32, 4):
                for ri, (Et, Bt) in enumerate(((Ere, Btr), (Eim, Bti))):
                    bank = 2 + (rot % 2)
                    rot += 1
                    for gg in range(4):
                        g = g0 + gg
                        S.add("pe", lambda e, Et=Et, g=g, gg=gg, bank=bank: e.transpose(
                            out=PS[bank][:, gg * 64:(gg + 1) * 64], in_=Et[:, g, 0:8, :].rearrange("p m c -> p (m c)"), identity=ident_f[0:64, 0:64]),
                            reads=[b_E, bc], writes=[PB[bank]])
                    S.add("dve", lambda e, Bt=Bt, g0=g0, bank=bank: e.tensor_copy(out=Bt[:, g0:g0 + 4, :],
                                                                             in_=PS[bank][:, 0:256].rearrange("p (g q) -> p g q", q=64)),
                          reads=[PB[bank]], writes=[b_wt])
                bank = 4 + ((g0 // 4) % 2)
                for gg in range(4):
                    g = g0 + gg
                    for j in range(8):
                        o = PS[bank][:, gg * 128 + j * 16: gg * 128 + (j + 1) * 16]
                        S.add("pe", lambda e, o=o, g=g, j=j: e.matmul(o, lhsT=Ere[:, g, 7 - j:15 - j, :].rearrange("p m c -> p (m c)"), rhs=CT[:, 0, g, :],
                                                                    start=True, stop=False),
                              reads=[b_E, b_CT], writes=[PB[bank]])
                        S.add("pe", lambda e, o=o, g=g, j=j: e.matmul(o, lhsT=Eim[:, g, 7 - j:15 - j, :].rearrange("p m c -> p (m c)"), rhs=CT[:, 2, g, :],
                                                                    start=False, stop=True),
                              reads=[b_E, b_CT], writes=[PB[bank]])
                S.add("act", lambda e, g0=g0, bank=bank: e.activation(out=Dt[:, g0:g0 + 4, :], in_=PS[bank][:].rearrange("p (g q) -> p g q", q=128), func=AF.Copy),
                      reads=[PB[bank]], writes=[b_wt])
            S.flush()
        if "stop_s5w" in C.debug:
            return
        with ExitStack() as _es:
            Uc = _es.enter_context(nc.sbuf_tensor("Uc", [128, 8, 512], F32))
            UT = _es.enter_context(nc.sbuf_tensor("UT", [128, 32, 128], BF16))
            Ug = _es.enter_context(nc.sbuf_tensor("Ug", [128, 32, 128], BF16))
            b_Ug = Buf("Ug")
            Xh = _es.enter_context(nc.sbuf_tensor("Xh", [64, 129, 64], F32))
            st = _es.enter_context(nc.sbuf_tensor("st", [64, 2, 64], F32))
            stw = [_es.enter_context(nc.sbuf_tensor(f"stw{i}", [64, 2, 16, 64], F32)) for i in range(2)]
            b_stw = [[Buf("stw00"), Buf("stw01")], [Buf("stw10"), Buf("stw11")]]
            Yc = _es.enter_context(nc.sbuf_tensor("Yc", [128, 8, 512], F32))
            Gc = _es.enter_context(nc.sbuf_tensor("Gc", [128, 8, 512], BF16))
            geT = _es.enter_context(nc.sbuf_tensor("geT", [128, 4, 1024], BF16))
            y2s = _es.enter_context(nc.sbuf_tensor("y2s", [128, 4, 1024], BF16))
            sig = _es.enter_context(nc.sbuf_tensor("sig", [128, 512], BF16))
            b_Uc, b_UT, b_Xh, b_st, b_Yc, b_Gc, b_geT, b_y2s, b_sig, b_st1 = [Buf(n) for n in "Uc UT Xh st Yc Gc geT y2s sig st1".split()]
            s5v = W["s5S"].rearrange("(c i) f -> c (i f)", i=8)
            S.add("dve", lambda e: e.memset(Xh[:, 0, :], 0.0), writes=[b_Xh])
            for T in range(4):
                dma(S, "sp", Uc[:].rearrange("p i f -> p (i f)"), s5v[T * 128:(T + 1) * 128, :], writes=[b_Uc])
                S.add("pool", lambda e: e.tensor_copy(out=Ug[:].rearrange("p g (i c) -> p g i c", c=16), in_=view(Uc, 0, [[16, 32], [512, 8], [1, 16]])),
                      reads=[b_Uc], writes=[b_Ug])
                for g0 in range(0, 32, 8):
                    bank = (g0 // 8) % 2
                    pb = PS[bank][:].bitcast(BF16)
                    for gg in range(8):
                        g = g0 + gg
                        S.add("pe", lambda e, g=g, gg=gg, pb=pb: e.transpose(out=pb[:, gg * 128:(gg + 1) * 128], in_=Ug[:, g, :], identity=ident_b[:]),
                              reads=[b_Ug, bc], writes=[PB[bank]])
                    S.add("act", lambda e, g0=g0, pb=pb: e.activation(out=UT[:, g0:g0 + 8, :], in_=pb[:, 0:1024].rearrange("p (g q) -> p g q", q=128), func=AF.Copy),
                          reads=[PB[bank]], writes=[b_UT])
                for g0 in range(0, 32, 4):
                    for ri, Bt in enumerate((Btr, Bti)):
                        bank = 2 + ri
                        for gg in range(4):
                            g = g0 + gg
                            S.add("pe", lambda e, Bt=Bt, g=g, gg=gg, bank=bank: e.matmul(PS[bank][0:64, gg * 128:(gg + 1) * 128], lhsT=Bt[:, g, :], rhs=UT[:, g, :],
                                                                                    start=True, stop=True),
                                  reads=[b_wt, b_UT], writes=[PB[bank]])
                        o = view(Xh, 64 + ri * 32 + g0, [[1, 4], [64, 128]], parts=64)
                        S.add("dve", lambda e, o=o, bank=bank: e.tensor_copy(out=o, in_=PS[bank][0:64, :].rearrange("p (g c) -> p g c", c=128)),
                              reads=[PB[bank]], writes=[b_Xh])
                def cmul_acc(dst0, src0, nb, i, k):
                    dst = view(Xh, dst0 * 64, [[512, nb], [1, 64]], parts=64)
                    src = view(Xh, src0 * 64, [[512, nb], [1, 64]], parts=64)
                    srcw = view(Xh, src0 * 64 + 32, [[512, nb], [-32, 2], [1, 32]], parts=64)
                    t1 = stw[k][:, 0, 0:nb, :]
                    t2 = stw[k][:, 1, 0:nb, :]
                    S.add("dve", lambda e: e.tensor_tensor(out=t1, in0=src, in1=PP[:, i, 0, :].unsqueeze(1).broadcast_to([64, nb, 64]), op=ALU.mult),
                          reads=[b_Xh], writes=[b_stw[k][0]])
                    S.add("dve", lambda e: e.tensor_tensor(out=t2.rearrange("p b (a c) -> p b a c", a=2), in0=srcw,
                                                           in1=PP[:, i, 1, :].rearrange("p (a c) -> p a c", a=2).unsqueeze(1).broadcast_to([64, nb, 2, 32]), op=ALU.mult),
                          reads=[b_Xh], writes=[b_stw[k][1]])
                    S.add("dve", lambda e: e.tensor_tensor(out=t1, in0=t1, in1=t2, op=ALU.add), reads=[b_stw[k][0], b_stw[k][1]], writes=[b_stw[k][0]])
                    S.add("dve", lambda e: e.tensor_tensor(out=dst, in0=dst, in1=t1, op=ALU.add), reads=[b_stw[k][0], b_Xh], writes=[b_Xh])

                if "s5_noscan" not in C.debug:
                    for i in range(1, 8):
                        cmul_acc(i + 1, i, 16, 1, i % 2)
                    for b in range(16):
                        cmul_acc(b * 8 + 8, b * 8, 1, 8, b % 2)
                    for i in range(1, 8):
                        cmul_acc(i, 0, 16, i, i % 2)
                if "s5_noY" in C.debug:
                    continue
                S.add("pool", lambda e: e.tensor_tensor(out=Yc[:], in0=Uc[:], in1=dbc[:].unsqueeze(1).broadcast_to([128, 8, 512]), op=ALU.mult),
                      reads=[b_Uc, b_dbc], writes=[b_Yc])
                for g0 in range(0, 32, 4):
                    bank = 4 + (g0 // 4) % 2
                    for gg in range(4):
                        g = g0 + gg
                        o = PS[bank][:, gg * 128:(gg + 1) * 128]
                        S.add("pe", lambda e, o=o, g=g: e.matmul(o, lhsT=UT[:, g, :], rhs=Dt[:, g, :], start=True, stop=False),
                              reads=[b_UT, b_wt], writes=[PB[bank]])
                        xr = view(Xh, g, [[64, 128]], parts=64)
                        xi = view(Xh, 32 + g, [[64, 128]], parts=64)
                        S.add("pe", lambda e, o=o, g=g, xr=xr: e.matmul(o, lhsT=xr, rhs=Ctr[:, g, :], start=False, stop=False),
                              reads=[b_Xh, b_wt], writes=[PB[bank]])
                        S.add("pe", lambda e, o=o, g=g, xi=xi: e.matmul(o, lhsT=xi, rhs=Cti[:, g, :], start=False, stop=True),
                              reads=[b_Xh, b_wt], writes=[PB[bank]])
                    yv = view(Yc, g0 * 16, [[16, 4], [512, 8], [1, 16]])
                    S.add("dve", lambda e, yv=yv, bank=bank: e.tensor_tensor(out=yv, in0=PS[bank][:].rearrange("p (g j c) -> p g j c", g=4, j=8), in1=yv, op=ALU.add),
                          reads=[PB[bank], b_Yc], writes=[b_Yc])
                if T < 3:
                    S.add("dve", lambda e: e.tensor_copy(out=Xh[:, 0, :], in_=Xh[:, 128, :]), reads=[b_Xh], writes=[b_Xh])
                if "dbg_s5y" in C.debug:
                    for i in range(8):
                        dma(S, "sp", C.dbg_s5y.rearrange("(c i) f -> c i f", i=8)[T * 128:(T + 1) * 128, i, :], Yc[:, i, :], reads=[b_Yc])
                for i in range(8):
                    S.add("act", lambda e, i=i: e.activation(out=Gc[:, i, :], in_=Yc[:, i, :], func=AF.Gelu_apprx_tanh), reads=[b_Yc], writes=[b_Gc])
                for ct in range(4):
                    bank = 6 + (ct % 2)
                    pb = PS[bank][:].bitcast(BF16)
                    for j in range(8):
                        S.add("pe", lambda e, pb=pb, j=j, ct=ct: e.transpose(out=pb[:, j * 128:(j + 1) * 128], in_=Gc[:, j, ct * 128:(ct + 1) * 128], identity=ident_b[:]),
                              reads=[b_Gc, bc], writes=[PB[bank]])
                    o = view(geT, ct * 1024, [[1, 8], [8, 128]])
                    S.add("dve", lambda e, o=o, pb=pb: e.tensor_copy(out=o, in_=pb[:, 0:1024].rearrange("p (j c) -> p j c", j=8)), reads=[PB[bank]], writes=[b_geT])
                for co in range(4):
                    for hh in range(2):
                        bank = 6 + ((co * 2 + hh) % 2)
                        for ci in range(4):
                            S.add("pe", lambda e, co=co, hh=hh, ci=ci, bank=bank: e.matmul(PS[bank][:], lhsT=wglu[:, ci, co * 128:(co + 1) * 128],
                                                                                      rhs=geT[:, ci, hh * 512:(hh + 1) * 512], start=(ci == 0), stop=(ci == 3)),
                                  reads=[b_wglu, b_geT], writes=[PB[bank]])
                        S.add("act", lambda e, co=co, bank=bank: e.activation(out=sig[:], in_=PS[bank][:], func=AF.Sigmoid,
                                                                          bias=cols[:, COL_BGLU + co:COL_BGLU + co + 1], scale=1.0),
                              reads=[PB[bank], C.b_cols], writes=[b_sig])
                        S.add("dve", lambda e, co=co, hh=hh: e.tensor_tensor(out=y2s[:, co, hh * 512:(hh + 1) * 512], in0=geT[:, co, hh * 512:(hh + 1) * 512], in1=sig[:],
                                                                          op=ALU.mult),
                              reads=[b_geT, b_sig], writes=[b_y2s])
                dma(S, "sp", W["y2T"][:, T * 1024:(T + 1) * 1024].rearrange("(c p) n -> p c n", p=128), y2s[:], reads=[b_y2s])
            S.flush()


_dummy = {}


def b_sm_dummy(C):
    if "b" not in _dummy:
        _dummy["b"] = Buf("dummy")
    return _dummy["b"]


def phase3(C):
    nc, S, I, W = C.nc, C.S, C.I, C.W
    PS, PB = C.PS, C.PB
    cols = C.cols
    bc = C.b_const
    NQ = 8
    heads = C.heads if hasattr(C, "heads") else range(8)
    with ExitStack() as _es:
        sb = lambda name, shape, dt: _es.enter_context(nc.sbuf_tensor(name, shape, dt))
        lqk = sb("lqk", [128, 4, 64], F32)
        lsc = sb("lsc", [128, 8], F32)
        masks = sb("masks", [128, 4, 512], BF16)
        kTh = [sb(f"kTh{i}", [128, L], BF16) for i in range(2)]
        qTh = [sb(f"qTh{i}", [128, L], BF16) for i in range(2)]
        Vh = [sb(f"Vh{i}", [128, 32, 128], BF16) for i in range(2)]
        Et = sb("Et", [128, 8, 512], BF16)
        rr = sb("rr", [128, 2, 512], F32)
        oo = sb("oo", [128, 2, 512], F32)
        sq = sb("sq", [128, 512], BF16)
        ons = [sb(f"ons{i}", [128, 512], BF16) for i in range(2)]
        b_l, b_mask = Buf("lam"), Buf("mask")
        b_k = [Buf("k0"), Buf("k1")]
        b_q = [Buf("q0"), Buf("q1")]
        b_v = [Buf("v0"), Buf("v1")]
        b_E = [Buf(f"E{i}") for i in range(8)]
        b_rr, b_oo, b_sq = Buf("rr"), Buf("oo"), Buf("sq")
        b_ons = [Buf("ons0"), Buf("ons1")]
        for i, n in enumerate(["lambda_q1", "lambda_k1", "lambda_q2", "lambda_k2"]):
            dma(S, "sp", lqk[:, i, :], I[n].partition_broadcast(128), writes=[b_l])
        S.add("dve", lambda e: e.tensor_tensor(out=lqk[:, 0, :], in0=lqk[:, 0, :], in1=lqk[:, 1, :], op=ALU.mult), reads=[b_l], writes=[b_l])
        S.add("dve", lambda e: e.tensor_tensor(out=lqk[:, 2, :], in0=lqk[:, 2, :], in1=lqk[:, 3, :], op=ALU.mult), reads=[b_l], writes=[b_l])
        S.add("dve", lambda e: e.reduce_sum(out=lsc[:, 0:1], in_=lqk[:, 0, :], axis=AX.X), reads=[b_l], writes=[b_l])
        S.add("dve", lambda e: e.reduce_sum(out=lsc[:, 1:2], in_=lqk[:, 2, :], axis=AX.X), reads=[b_l], writes=[b_l])
        S.add("act", lambda e: e.activation(out=lsc[:, 2:4], in_=lsc[:, 0:2], func=AF.Exp), reads=[b_l], writes=[b_l])
        S.add("dve", lambda e: e.tensor_tensor(out=lsc[:, 4:5], in0=lsc[:, 3:4], in1=lsc[:, 2:3], op=ALU.subtract), reads=[b_l], writes=[b_l])
        S.add("dve", lambda e: e.tensor_scalar(out=lsc[:, 5:6], in0=lsc[:, 4:5], scalar1=-LAMBDA_INIT, scalar2=None, op0=ALU.add), reads=[b_l], writes=[b_l])
        nlam = lsc[:, 5:6]
        S.add("pool", lambda e: e.memset(masks[:], 0.0), writes=[b_mask])
        for r in range(4):
            S.add("pool", lambda e, r=r: e.memset(masks[0:64, r, 128 * r:512], 1.0), writes=[b_mask])
            S.add("pool", lambda e, r=r: e.memset(masks[64:128, r, 128 * r + 64:512], 1.0), writes=[b_mask])

        def load_head(h, slot):
            dma(S, "sp", kTh[slot][:], W["kT"][h * 128:(h + 1) * 128, :], writes=[b_k[slot]])
            dma(S, "sp", qTh[slot][:], W["qT"][h * 128:(h + 1) * 128, :], writes=[b_q[slot]])
            dma(S, "sp", Vh[slot][:], W["vS"][:, h * 128:(h + 1) * 128].rearrange("(j p) e -> p j e", p=128), writes=[b_v[slot]])

        hl = list(heads)
        load_head(hl[0], 0)
        ecnt = 0
        ocnt = 0
        lnt = sb("lnt", [128, 2, 512], F32)
        b_ln = Buf("lnt")
        wst = [sb(f"wst{i}", [128, 6144], BF16) for i in range(2)]
        b_wst = [Buf("wst0"), Buf("wst1")]

        def precast_load(ex):
            sl = ex % 2
            dma(S, "pool", wst[sl][:, 0:2048].rearrange("p (k f) -> p k f", f=256), I["w_exp_gate"][ex].rearrange("(k p) f -> p k f", p=128), writes=[b_wst[sl]])
            dma(S, "pool", wst[sl][:, 2048:4096].rearrange("p (k f) -> p k f", f=256), I["w_exp_up"][ex].rearrange("(k p) f -> p k f", p=128), writes=[b_wst[sl]])
            dma(S, "pool", wst[sl][:, 4096:6144].rearrange("p (k d) -> p k d", d=D), I["w_exp_down"][ex].rearrange("(k p) d -> p k d", p=128), writes=[b_wst[sl]])

        def precast_store(ex):
            sl = ex % 2
            for c3 in range(3):
                dma(S, "sp", W["WALL"][ex * 128:(ex + 1) * 128, c3 * 2048:(c3 + 1) * 2048], wst[sl][:, c3 * 2048:(c3 + 1) * 2048], reads=[b_wst[sl]])

        unit = 0
        e2cnt = 0
        Es2 = sb("Es2", [128, 4, 512], BF16)
        b_E2 = [Buf(f"E2_{i}") for i in range(4)]
        pending = []

        def epi_fast(h, Q):
            S.add("act", lambda e: e.activation(out=oo[:, 0, :], in_=PS[4][:], func=AF.Copy), reads=[PB[4]], writes=[b_oo])
            S.add("act", lambda e: e.activation(out=oo[:, 1, :], in_=PS[5][:], func=AF.Copy), reads=[PB[5]], writes=[b_oo])
            S.add("dve", lambda e: e.tensor_copy(out=lnt[:, 0, :], in_=PS[6][:]), reads=[PB[6]], writes=[b_ln])
            S.add("dve", lambda e: e.tensor_copy(out=lnt[:, 1, :], in_=PS[7][:]), reads=[PB[7]], writes=[b_ln])
            S.add("dve", lambda e: e.reciprocal(out=rr[:, 0, :], in_=lnt[:, 1, :]), reads=[b_ln], writes=[b_rr])
            S.add("dve", lambda e: e.tensor_tensor(out=rr[:, 0, :], in0=rr[:, 0, :], in1=lnt[:, 0, :], op=ALU.mult), reads=[b_rr, b_ln], writes=[b_rr])
            S.add("dve", lambda e: e.tensor_tensor(out=oo[:, 1, :], in0=oo[:, 1, :], in1=rr[:, 0, :], op=ALU.mult), reads=[b_oo, b_rr], writes=[b_oo])
            S.add("dve", lambda e: e.scalar_tensor_tensor(out=oo[:, 0, :], in0=oo[:, 1, :], scalar=nlam, in1=oo[:, 0, :], op0=ALU.mult, op1=ALU.add),
                  reads=[b_oo, b_l], writes=[b_oo])
            S.add("pool", lambda e: e.tensor_tensor(out=sq[:], in0=oo[:, 0, :], in1=oo[:, 0, :], op=ALU.mult), reads=[b_oo], writes=[b_sq])
            S.add("dve", lambda e: e.scalar_tensor_tensor(out=rr[:, 1, :], in0=lnt[:, 0, :], scalar=EPS, in1=lnt[:, 0, :], op0=ALU.mult, op1=ALU.mult),
                  reads=[b_ln], writes=[b_rr])

        def epi_slow(h, Q):
            nonlocal ocnt
            S.add("pe", lambda e: e.matmul(PS[0][:], lhsT=C.ones_b[:], rhs=sq[:], start=True, stop=True), reads=[bc, b_sq], writes=[PB[0]])
            S.add("dve", lambda e: e.scalar_tensor_tensor(out=rr[:, 1, :], in0=PS[0][:], scalar=1.0 / 128, in1=rr[:, 1, :], op0=ALU.mult, op1=ALU.add),
                  reads=[PB[0], b_rr], writes=[b_rr])
            S.add("act", lambda e: e.activation(out=rr[:, 1, :], in_=rr[:, 1, :], func=AF.Ln), reads=[b_rr], writes=[b_rr])
            S.add("act", lambda e: e.activation(out=rr[:, 1, :], in_=rr[:, 1, :], func=AF.Exp, scale=-0.5), reads=[b_rr], writes=[b_rr])
            on = ons[ocnt % 2]
            bo = b_ons[ocnt % 2]
            ocnt += 1
            S.add("dve", lambda e, on=on: e.scalar_tensor_tensor(out=on[:], in0=oo[:, 0, :], scalar=cols[:, COL_GSUBS:COL_GSUBS + 1], in1=rr[:, 1, :],
                                                              op0=ALU.mult, op1=ALU.mult),
                  reads=[b_oo, b_rr, C.b_cols], writes=[bo])
            dma(S, "sp", W["onT"][h * 128:(h + 1) * 128, Q * 512:(Q + 1) * 512], on[:], reads=[bo])

        for hi, h in enumerate(hl):
            slot = hi % 2
            if hi + 1 < len(hl):
                load_head(hl[hi + 1], (hi + 1) % 2)
            kt, qt, vt = kTh[slot], qTh[slot], Vh[slot]
            bk, bq, bv = b_k[slot], b_q[slot], b_v[slot]
            for Q in range(NQ):
                nJ = 4 * (Q + 1)
                eslot = {}
                lready, lnext = [], []
                if C.sparse:
                    if 1 <= unit <= NEXP:
                        precast_store(unit - 1)
                    if unit < NEXP:
                        precast_load(unit)
                    if unit == NEXP + 1:
                        dma(S, "pool", C.wbs[:], I["w_br_ssm"].rearrange("(k p) n -> p k n", p=128), writes=[C.b_w4])
                        dma(S, "pool", C.wba[:], I["w_br_attn"].rearrange("(k p) n -> p k n", p=128), writes=[C.b_w4])
                        dma(S, "pool", C.wout[:], I["w_out"].rearrange("(k p) n -> p k n", p=128), writes=[C.b_w4])
                        C.w4_loaded = True
                    unit += 1
                for step in range(nJ + 1):
                    if step < nJ:
                        J = step
                        c0 = 0
                        for m in range(2):
                            bank = 2 * m + (J % 2)
                            S.add("pe", lambda e, bank=bank, m=m, J=J, Q=Q, kt=kt, qt=qt, c0=c0: e.matmul(
                                PS[bank][:, c0:512], lhsT=kt[m * 64:(m + 1) * 64, J * 128:(J + 1) * 128], rhs=qt[m * 64:(m + 1) * 64, Q * 512 + c0:(Q + 1) * 512],
                                start=True, stop=True), reads=[bk, bq], writes=[PB[bank]])
                            es = ecnt % 8
                            ecnt += 1
                            eslot[(J, m)] = es
                            S.add("act", lambda e, bank=bank, es=es, c0=c0: e.activation(out=Et[:, es, c0:512], in_=PS[bank][:, c0:512], func=AF.Exp, scale=0.125),
                                  reads=[PB[bank]], writes=[b_E[es]])
                            if J >= 4 * Q:
                                r = J - 4 * Q
                                S.add("dve", lambda e, es=es, r=r: e.tensor_tensor(out=Et[:, es, :], in0=Et[:, es, :], in1=masks[:, r, :], op=ALU.mult),
                                      reads=[b_E[es], b_mask], writes=[b_E[es]])
                    if step >= 1:
                        J = step - 1
                        c0 = 0
                        for m in range(2):
                            es = eslot[(J, m)]
                            S.add("pe", lambda e, m=m, J=J, es=es, vt=vt, nJ=nJ, c0=c0: e.matmul(PS[4 + m][:, c0:512], lhsT=vt[:, J, :], rhs=Et[:, es, c0:512],
                                                                                     start=(J == 0), stop=(J == nJ - 1)),
                                  reads=[bv, b_E[es]], writes=[PB[4 + m]])
                            if J % 2 == 1:
                                esp = eslot[(J - 1, m)]
                                s2 = e2cnt % 4
                                e2cnt += 1
                                S.add("pool", lambda e, es=es, esp=esp, s2=s2: e.tensor_tensor(out=Es2[:, s2, :], in0=Et[:, esp, :], in1=Et[:, es, :], op=ALU.add),
                                      reads=[b_E[es], b_E[esp]], writes=[b_E2[s2]])

                                def lmm(m=m, J=J, s2=s2, nJ=nJ):
                                    S.add("pe", lambda e: e.matmul(PS[6 + m][:], lhsT=C.ones_b[:], rhs=Es2[:, s2, :], start=(J == 1), stop=(J == nJ - 1)),
                                          reads=[bc, b_E2[s2]], writes=[PB[6 + m]])
                                lnext.append(lmm)
                        for f_ in lready:
                            f_()
                        lready = lnext
                        lnext = []
                    if step == min(3, nJ) and pending:
                        epi_slow(*pending.pop())
                for f_ in lready + lnext:
                    f_()
                epi_fast(h, Q)
                pending.append((h, Q))
        while pending:
            epi_slow(*pending.pop())
        S.flush()


def phase4a(C):
    nc, S, I, W = C.nc, C.S, C.I, C.W
    PS, PB = C.PS, C.PB
    cols = C.cols
    bc = C.b_const
    with ExitStack() as _es:
        sb = lambda name, shape, dt: _es.enter_context(nc.sbuf_tensor(name, shape, dt))
        wbs, wba, wout = C.wbs, C.wba, C.wout
        wr = sb("wr", [128, 8, 36], BF16)
        brt = sb("brt", [128, 36], F32)
        y2t = [sb(f"y2t{i}", [128, 4, 512], BF16) for i in range(2)]
        ont = [sb(f"ont{i}", [128, 8, 512], BF16) for i in range(2)]
        gtt = [sb(f"gtt{i}", [128, 16, 512], BF16) for i in range(2)]
        xt = sb("xt4", [128, 4, D], F32)
        mT = sb("mT", [128, 8, 512], BF16)
        tA = sb("tA", [128, 512], F32)
        tB = sb("tB", [128, 512], F32)
        hh_ = sb("h4", [128, 4, D], F32)
        xn2 = sb("xn2", [128, 4, D], BF16)
        junk = sb("junk4", [128, D], BF16)
        u2 = [sb(f"u2_{i}", [128, 8, 512], BF16) for i in range(2)]
        ssq = sb("ssq4", [128, 8], F32)
        rt = sb("rt", [128, 4, 36], F32)
        rw = sb("rw", [128, 24, 4], F32)
        ohg = sb("ohg", [128, 4, 4], F32)
        tE = sb("tE", [128, 4, 4, 8], F32)
        es = sb("es", [128, 6, 4, 8], F32)
        comb = [sb(f"comb{i}", [128, 4, 32], F32) for i in range(2)]
        ustr_f = sb("ustr_f", [128, 128], F32)
        ustr = sb("ustr", [128, 128], BF16)
        ohgb = sb("ohgb", [128, 4, 4], BF16)
        base = sb("base4", [128, 4, 4], F32)
        b_ohgb, b_base, b_tot = Buf("ohgb"), Buf("base"), Buf("tot")
        b_w = Buf("w4")
        if C.sparse:
            S.add("pool", lambda e: e.memset(ustr_f[:], 1.0), writes=[b_ohgb])
            S.add("pool", lambda e: e.affine_select(out=ustr_f[:], in_=ustr_f[:], pattern=[[1, 128]], compare_op=ALU.is_gt, fill=0.0, base=0, channel_multiplier=-1),
                  reads=[b_ohgb], writes=[b_ohgb])
            S.add("dve", lambda e: e.tensor_copy(out=ustr[:], in_=ustr_f[:]), reads=[b_ohgb], writes=[b_ohgb])
            S.add("dve", lambda e: e.memset(C.tot[:], 0.0), writes=[b_tot])
        if not getattr(C, "w4_loaded", False):
            dma(S, "pool", wbs[:], I["w_br_ssm"].rearrange("(k p) n -> p k n", p=128), writes=[b_w])
            dma(S, "pool", wba[:], I["w_br_attn"].rearrange("(k p) n -> p k n", p=128), writes=[b_w])
            dma(S, "pool", wout[:], I["w_out"].rearrange("(k p) n -> p k n", p=128), writes=[b_w])
        dma(S, "pool", wr[:, :, 0:4], I["w_router_grp"].rearrange("(k p) n -> p k n", p=128), writes=[b_w])
        dma(S, "pool", wr[:, :, 4:36], I["w_router_exp"].rearrange("(k p) n -> p k n", p=128), writes=[b_w])
        dma(S, "sp", brt[:, 0:4], I["b_router_grp"].partition_broadcast(128), writes=[b_w])
        dma(S, "sp", brt[:, 4:36], I["b_router_exp"].partition_broadcast(128), writes=[b_w])
        b_y2 = [Buf("y2t0"), Buf("y2t1")]
        b_on = [Buf("ont0"), Buf("ont1")]
        b_gt = [Buf("gtt0"), Buf("gtt1")]
        b_u2 = [Buf("u2_0"), Buf("u2_1")]
        b_cb = [Buf("comb0"), Buf("comb1")]
        b_xt, b_mT, b_tA, b_tB, b_h, b_xn2, b_junk, b_ssq, b_rt, b_rw = [Buf(n) for n in "xt mT tA tB h xn2 junk ssq rt rw".split()]

        def load(t):
            sl = t % 2
            dma(S, "sp", y2t[sl][:], W["y2T"][:, t * 512:(t + 1) * 512].rearrange("(c p) n -> p c n", p=128), writes=[b_y2[sl]])
            dma(S, "sp", ont[sl][:], W["onT"][:, t * 512:(t + 1) * 512].rearrange("(c p) n -> p c n", p=128), writes=[b_on[sl]])
            dma(S, "sp", gtt[sl][:], W["gT"][:, t * 512:(t + 1) * 512].rearrange("(c p) n -> p c n", p=128), writes=[b_gt[sl]])

        load(0)
        rot = 0
        for t in range(NTT):
            sl = t % 2
            if t + 1 < NTT:
                load(t + 1)
            dma(S, "sp", xt[:], I["x"][t * 512:(t + 1) * 512, :].rearrange("(s p) d -> p s d", p=128), writes=[b_xt])
            for ct in range(8):
                ba = rot % 2
                bb = 2 + rot % 2
                rot += 1
                for k in range(4):
                    S.add("pe", lambda e, ba=ba, k=k, ct=ct, sl=sl: e.matmul(PS[ba][:], lhsT=wbs[:, k, ct * 128:(ct + 1) * 128], rhs=y2t[sl][:, k, :],
                                                                        start=(k == 0), stop=(k == 3)), reads=[b_w, b_y2[sl]], writes=[PB[ba]])
                for k in range(8):
                    S.add("pe", lambda e, bb=bb, k=k, ct=ct, sl=sl: e.matmul(PS[bb][:], lhsT=wba[:, k, ct * 128:(ct + 1) * 128], rhs=ont[sl][:, k, :],
                                                                        start=(k == 0), stop=(k == 7)), reads=[b_w, b_on[sl]], writes=[PB[bb]])
                S.add("dve", lambda e, ba=ba, ct=ct, sl=sl: e.tensor_tensor(out=tA[:], in0=PS[ba][:], in1=gtt[sl][:, ct, :], op=ALU.mult),
                      reads=[PB[ba], b_gt[sl]], writes=[b_tA])
                S.add("dve", lambda e, bb=bb, ct=ct, sl=sl: e.tensor_tensor(out=tB[:], in0=PS[bb][:], in1=gtt[sl][:, 8 + ct, :], op=ALU.mult),
                      reads=[PB[bb], b_gt[sl]], writes=[b_tB])
                S.add("pool", lambda e, ct=ct: e.tensor_tensor(out=mT[:, ct, :], in0=tA[:], in1=tB[:], op=ALU.add), reads=[b_tA, b_tB], writes=[b_mT])
            for s in range(4):
                for hf in range(2):
                    bk = 4 + rot % 2
                    rot += 1
                    for k in range(8):
                        S.add("pe", lambda e, bk=bk, k=k, s=s, hf=hf: e.matmul(PS[bk][:], lhsT=mT[:, k, s * 128:(s + 1) * 128], rhs=wout[:, k, hf * 512:(hf + 1) * 512],
                                                                          start=(k == 0), stop=(k == 7)), reads=[b_mT, b_w], writes=[PB[bk]])
                    S.add("dve", lambda e, bk=bk, hf=hf: e.tensor_tensor(out=tA[:], in0=PS[bk][:], in1=C.gtm_bc[:, hf * 512:(hf + 1) * 512], op=ALU.mult),
                          reads=[PB[bk], C.b_gt], writes=[b_tA])
                    S.add("pool", lambda e, s=s, hf=hf: e.tensor_tensor(out=hh_[:, s, hf * 512:(hf + 1) * 512], in0=tA[:], in1=xt[:, s, hf * 512:(hf + 1) * 512], op=ALU.add),
                          reads=[b_tA, b_xt], writes=[b_h])
            dma(S, "sp", W["hS"][t * 512:(t + 1) * 512, :].rearrange("(s p) d -> p s d", p=128), hh_[:], reads=[b_h])
            S.add("dve", lambda e: e.memset(ssq[:], 0.0), writes=[b_ssq])
            for s in range(4):
                S.add("act", lambda e, s=s: e.activation(out=junk[:], in_=hh_[:, s, :], func=AF.Square, accum_out=ssq[:, s:s + 1]),
                      reads=[b_h, b_ssq], writes=[b_junk, b_ssq])
            S.add("dve", lambda e: e.tensor_scalar(out=ssq[:, 4:8], in0=ssq[:, 0:4], scalar1=1.0 / D, scalar2=EPS, op0=ALU.mult, op1=ALU.add),
                  reads=[b_ssq], writes=[b_ssq])
            S.add("act", lambda e: e.activation(out=ssq[:, 4:8], in_=ssq[:, 4:8], func=AF.Sqrt), reads=[b_ssq], writes=[b_ssq])
            S.add("dve", lambda e: e.reciprocal(out=ssq[:, 4:8], in_=ssq[:, 4:8]), reads=[b_ssq], writes=[b_ssq])
            for s in range(4):
                S.add("dve", lambda e, s=s: e.tensor_scalar(out=xn2[:, s, :], in0=hh_[:, s, :], scalar1=ssq[:, 4 + s:5 + s], scalar2=None, op0=ALU.mult),
                      reads=[b_h, b_ssq], writes=[b_xn2])
            u2t = u2[sl]
            for k in range(8):
                bk = 6 + k % 2
                ptb = PS[bk][:].bitcast(BF16)
                for s in range(4):
                    S.add("pe", lambda e, s=s, k=k, ptb=ptb: e.transpose(out=ptb[:, s * 128:(s + 1) * 128], in_=xn2[:, s, k * 128:(k + 1) * 128], identity=C.ident_b[:]),
                          reads=[b_xn2, bc], writes=[PB[bk]])
                S.add("dve", lambda e, k=k, ptb=ptb, u2t=u2t: e.tensor_scalar(out=u2t[:, k, :], in0=ptb[:, 0:512], scalar1=cols[:, COL_G2 + k:COL_G2 + k + 1],
                                                                           scalar2=cols[:, COL_MOD + 24 + k:COL_MOD + 25 + k], op0=ALU.mult, op1=ALU.add),
                      reads=[PB[bk], C.b_cols], writes=[b_u2[sl]])
            dma(S, "sp", W["u2T"][:, t * 512:(t + 1) * 512].rearrange("(c p) n -> p c n", p=128), u2t[:], reads=[b_u2[sl]])
            bk = 4 + rot % 2
            rot += 1
            for s in range(4):
                for k in range(8):
                    S.add("pe", lambda e, bk=bk, k=k, s=s, u2t=u2t: e.matmul(PS[bk][:, s * 36:(s + 1) * 36], lhsT=u2t[:, k, s * 128:(s + 1) * 128], rhs=wr[:, k, :],
                                                                        start=(k == 0), stop=(k == 7)), reads=[b_u2[sl], b_w], writes=[PB[bk]])
            S.add("dve", lambda e, bk=bk: e.tensor_tensor(out=rt[:], in0=PS[bk][:, 0:144].rearrange("p (s n) -> p s n", n=36),
                                                         in1=brt[:].unsqueeze(1).broadcast_to([128, 4, 36]), op=ALU.add),
                  reads=[PB[bk], b_w], writes=[b_rt])
            RR = [b_rt, b_rw]

            def dv(fn, **kw):
                S.add("dve", lambda e: getattr(e, fn)(**kw), reads=RR, writes=[b_rw])

            def av(**kw):
                S.add("act", lambda e: e.activation(**kw), reads=RR, writes=[b_rw])

            G = rt[:, :, 0:4]
            E4 = rt[:, :, 4:36].rearrange("p s (g e) -> p s g e", e=8)
            w_ = lambda i: rw[:, i, :]
            b4 = lambda a: a.unsqueeze(2).broadcast_to([128, 4, 4])
            b8 = lambda a: a.unsqueeze(2).broadcast_to([128, 4, 8])
            GMAX, GSUM, GP, M1, M2, DD, ED, W1, W1G, W2G = range(10)
            dv("tensor_reduce", out=w_(GMAX), in_=G, axis=AX.X, op=ALU.max)
            dv("tensor_tensor", out=ohg[:], in0=G, in1=b4(w_(GMAX)), op=ALU.subtract)
            av(out=tE[:, 0, :, 0:4], in_=ohg[:], func=AF.Exp)
            dv("tensor_reduce", out=w_(GSUM), in_=tE[:, 0, :, 0:4], axis=AX.X, op=ALU.add)
            dv("reciprocal", out=w_(GP), in_=w_(GSUM))
            dv("tensor_tensor", out=ohg[:], in0=G, in1=b4(w_(GMAX)), op=ALU.is_equal)
            dv("tensor_tensor", out=tE[:], in0=E4, in1=ohg[:].unsqueeze(3).broadcast_to([128, 4, 4, 8]), op=ALU.mult)
            dv("tensor_reduce", out=es[:, 0], in_=tE[:].rearrange("p s g e -> p s e g"), axis=AX.X, op=ALU.add)
            dv("tensor_reduce", out=w_(M1), in_=es[:, 0], axis=AX.X, op=ALU.max)
            dv("tensor_tensor", out=es[:, 1], in0=es[:, 0], in1=b8(w_(M1)), op=ALU.is_equal)
            dv("scalar_tensor_tensor", out=es[:, 2], in0=es[:, 1], scalar=-1e30, in1=es[:, 0], op0=ALU.mult, op1=ALU.add)
            dv("tensor_reduce", out=w_(M2), in_=es[:, 2], axis=AX.X, op=ALU.max)
            dv("tensor_tensor", out=es[:, 3], in0=es[:, 2], in1=b8(w_(M2)), op=ALU.is_equal)
            dv("tensor_tensor", out=w_(DD), in0=w_(M2), in1=w_(M1), op=ALU.subtract)
            av(out=w_(ED), in_=w_(DD), func=AF.Exp)
            dv("tensor_scalar", out=w_(W1), in0=w_(ED), scalar1=1.0, scalar2=None, op0=ALU.add)
            dv("reciprocal", out=w_(W1), in_=w_(W1))
            dv("tensor_tensor", out=w_(W1G), in0=w_(W1), in1=w_(GP), op=ALU.mult)
            dv("tensor_tensor", out=w_(W2G), in0=w_(W1G), in1=w_(ED), op=ALU.mult)
            dv("tensor_tensor", out=es[:, 4], in0=es[:, 1], in1=b8(w_(W1G)), op=ALU.mult)
            dv("tensor_tensor", out=es[:, 5], in0=es[:, 3], in1=b8(w_(W2G)), op=ALU.mult)
            dv("tensor_tensor", out=es[:, 4], in0=es[:, 4], in1=es[:, 5], op=ALU.add)
            if C.sparse:
                dma(S, "sp", W["XN"][t * 512:(t + 1) * 512, :].rearrange("(s p) d -> p s d", p=128), xn2[:], reads=[b_xn2])
                dma(S, "sp", W["CG"][t * 512:(t + 1) * 512, :].rearrange("(s p) e -> p s e", p=128), es[:, 4], reads=[b_rw])
                S.add("dve", lambda e: e.tensor_copy(out=ohgb[:], in_=ohg[:]), reads=RR, writes=[b_ohgb])
                bk2 = 4 + rot % 2
                rot += 1
                S.add("pe", lambda e, bk2=bk2: e.matmul(PS[bk2][:, 0:16], lhsT=ustr[:], rhs=ohgb[:].rearrange("p s g -> p (s g)"), start=True, stop=True),
                      reads=[b_ohgb], writes=[PB[bk2]])
                S.add("pe", lambda e, bk2=bk2: e.matmul(PS[bk2][:, 16:32], lhsT=C.ones_b[:], rhs=ohgb[:].rearrange("p s g -> p (s g)"), start=True, stop=True),
                      reads=[b_ohgb, bc], writes=[PB[bk2]])
                S.add("dve", lambda e: e.tensor_copy(out=base[:, 0, :], in_=C.tot[:]), reads=[b_tot], writes=[b_base])
                for s_ in range(1, 4):
                    S.add("dve", lambda e, s_=s_, bk2=bk2: e.tensor_tensor(out=base[:, s_, :], in0=base[:, s_ - 1, :], in1=PS[bk2][:, 16 + 4 * (s_ - 1):16 + 4 * s_], op=ALU.add),
                          reads=[b_base, PB[bk2]], writes=[b_base])
                S.add("dve", lambda e, bk2=bk2: e.tensor_tensor(out=C.tot[:], in0=base[:, 3, :], in1=PS[bk2][:, 28:32], op=ALU.add),
                      reads=[b_base, PB[bk2]], writes=[b_tot])
                S.add("dve", lambda e, bk2=bk2: e.tensor_tensor(out=base[:], in0=base[:], in1=PS[bk2][:, 0:16].rearrange("p (s g) -> p s g", g=4), op=ALU.add),
                      reads=[b_base, PB[bk2]], writes=[b_base])
                S.add("dve", lambda e: e.tensor_tensor(out=base[:], in0=base[:], in1=ohg[:], op=ALU.mult), reads=[b_base] + RR, writes=[b_base])
                S.add("dve", lambda e, t=t: e.tensor_reduce(out=C.RK[:, t * 4:(t + 1) * 4], in_=base[:], axis=AX.X, op=ALU.add), reads=[b_base], writes=[C.b_rk])
                S.add("dve", lambda e, t=t: e.tensor_copy(out=C.OH[:, t * 4:(t + 1) * 4, :], in_=ohg[:]), reads=RR, writes=[C.b_rk])
            cb = comb[sl]
            S.add("dve", lambda e, cb=cb: e.tensor_tensor(out=cb[:].rearrange("p s (g e) -> p s g e", e=8), in0=ohg[:].unsqueeze(3).broadcast_to([128, 4, 4, 8]),
                                                         in1=es[:, 4].unsqueeze(2).broadcast_to([128, 4, 4, 8]), op=ALU.mult),
                  reads=RR, writes=[b_cb[sl]])
            dma(S, "sp", W["combS"][t * 512:(t + 1) * 512, :].rearrange("(s p) e -> p s e", p=128), cb[:], reads=[b_cb[sl]])
        if C.sparse:
            TH = sb("TH", [128, 4, 8], F32)
            cmpt = sb("cmpt", [128, 4, 8], F32)
            kg = sb("kg", [128, 4], F32)
            ck = sb("ck", [128, 5], F32)
            bsl = sb("bsl", [128, 4], F32)
            tmp3 = sb("tmp3", [128, 32, 4], F32)
            slf = sb("slf", [128, 32], F32)
            SI = sb("SI", [128, 12], F32)
            cmp2 = sb("cmp2", [128, 12, 3], F32)
            tgf = sb("tgf", [128, 12], F32)
            bq_ = Buf("slotcalc")
            RQ = [bq_, b_tot, C.b_rk]

            def dq(fn, **kw):
                S.add("dve", lambda e: getattr(e, fn)(**kw), reads=RQ, writes=[bq_])

            for j in range(8):
                dq("memset", ap=TH[:, :, j:j + 1], constant=512.0 * j)
            for j in range(12):
                dq("memset", ap=SI[:, j:j + 1], constant=float(j))
            dq("tensor_tensor", out=cmpt[:], in0=C.tot[:].unsqueeze(2).broadcast_to([128, 4, 8]), in1=TH[:], op=ALU.is_gt)
            dq("tensor_reduce", out=kg[:], in_=cmpt[:], axis=AX.X, op=ALU.add)
            dq("memset", ap=ck[:, 0:1], constant=0.0)
            for g in range(4):
                dq("tensor_tensor", out=ck[:, g + 1:g + 2], in0=ck[:, g:g + 1], in1=kg[:, g:g + 1], op=ALU.add)
            dq("tensor_scalar", out=bsl[:], in0=ck[:, 0:4], scalar1=512.0, scalar2=None, op0=ALU.mult)
            dq("tensor_tensor", out=tmp3[:], in0=C.OH[:], in1=bsl[:].unsqueeze(1).broadcast_to([128, 32, 4]), op=ALU.mult)
            dq("tensor_reduce", out=slf[:], in_=tmp3[:], axis=AX.X, op=ALU.add)
            dq("tensor_tensor", out=slf[:], in0=slf[:], in1=C.RK[:], op=ALU.add)
            dq("tensor_copy", out=C.SLOT_I[:], in_=slf[:])
            dq("tensor_tensor", out=cmp2[:], in0=ck[:, 1:4].unsqueeze(1).broadcast_to([128, 12, 3]), in1=SI[:].unsqueeze(2).broadcast_to([128, 12, 3]), op=ALU.is_le)
            dq("tensor_reduce", out=tgf[:], in_=cmp2[:], axis=AX.X, op=ALU.add)
            dq("tensor_copy", out=C.TG_I[:], in_=tgf[:])
            pidx_i = sb("pidx_i", [128, 1], mybir.dt.int32)
            pidx = sb("pidx", [128, 1], F32)
            j128 = sb("j128", [128, 8], F32)
            widf = sb("widf", [128, 12, 8], F32)
            S.add("pool", lambda e: e.iota(pidx_i[:], [[0, 1]], base=0, channel_multiplier=1), writes=[bq_])
            dq("tensor_copy", out=pidx[:], in_=pidx_i[:])
            for j in range(8):
                dq("memset", ap=j128[:, j:j + 1], constant=128.0 * j)
            dq("tensor_scalar", out=j128[:], in0=j128[:], scalar1=pidx[:, 0:1], scalar2=None, op0=ALU.add)
            dq("scalar_tensor_tensor", out=widf[:], in0=tgf[:].unsqueeze(2).broadcast_to([128, 12, 8]), scalar=1024.0,
               in1=j128[:].unsqueeze(1).broadcast_to([128, 12, 8]), op0=ALU.mult, op1=ALU.add)
            dq("tensor_copy", out=C.WIDX[:].rearrange("p (s j) -> p s j", j=8), in_=widf[:])
            if "dbg_slot" in C.debug:
                C.dbg_slot = nc.dram_tensor("dbg_slot", [128, 64], F32, kind="ExternalOutput").ap()
                dma(S, "sp", C.dbg_slot[:, 0:32], slf[:], reads=[bq_])
                dma(S, "sp", C.dbg_slot[:, 32:44], tgf[:], reads=[bq_])
                dma(S, "sp", C.dbg_slot[:, 44:48], C.tot[:], reads=[bq_])
        S.flush()


def phase4b(C):
    nc, S, I, W = C.nc, C.S, C.I, C.W
    PS, PB = C.PS, C.PB
    n_exp = C.n_exp if hasattr(C, "n_exp") else NEXP
    with ExitStack() as _es:
        sb = lambda name, shape, dt: _es.enter_context(nc.sbuf_tensor(name, shape, dt))
        u2t = sb("u2tt", [128, 8, 1024], BF16)
        cbt = sb("cbt", [128, 8, 32], F32)
        yacc = sb("yacc", [128, 8, D], F32)
        wg = [sb(f"wg{i}", [128, 8, 256], BF16) for i in range(2)]
        wu = [sb(f"wu{i}", [128, 8, 256], BF16) for i in range(2)]
        wd = [sb(f"wd{i}", [128, 2, D], BF16) for i in range(2)]
        sg = [sb(f"sg{i}", [128, 2, 512], BF16) for i in range(2)]
        hd = [sb(f"hd{i}", [128, 2, 512], BF16) for i in range(2)]
        hp = sb("hp", [128, 4, D], F32)
        gfin = sb("gfin", [128, D], F32)
        junk = sb("junk5", [128, D], BF16)
        ssq = sb("ssq5", [128, 8], F32)
        b_u2t, b_cbt, b_yacc, b_hp, b_gfin, b_junk, b_ssq = [Buf(n) for n in "u2t cbt yacc hp gfin junk ssq".split()]
        b_wg = [Buf("wg0"), Buf("wg1")]
        b_wu = [Buf("wu0"), Buf("wu1")]
        b_wd = [Buf("wd0"), Buf("wd1")]
        b_sg = [Buf("sg0"), Buf("sg1")]
        b_hd = [Buf("hd0"), Buf("hd1")]
        dma(S, "sp", gfin[:], I["g_final"].partition_broadcast(128), writes=[b_gfin])

        def load_w(TT, e, sl):
            if TT == 0:
                dma(S, "pool", wg[sl][:], I["w_exp_gate"][e].rearrange("(k p) f -> p k f", p=128), writes=[b_wg[sl]])
                dma(S, "pool", wu[sl][:], I["w_exp_up"][e].rearrange("(k p) f -> p k f", p=128), writes=[b_wu[sl]])
                dma(S, "pool", wd[sl][:], I["w_exp_down"][e].rearrange("(k p) d -> p k d", p=128), writes=[b_wd[sl]])
                dma(S, "sp", W["wgS"][e].rearrange("(k p) f -> p k f", p=128), wg[sl][:], reads=[b_wg[sl]])
                dma(S, "sp", W["wuS"][e].rearrange("(k p) f -> p k f", p=128), wu[sl][:], reads=[b_wu[sl]])
                dma(S, "sp", W["wdS"][e].rearrange("(k p) d -> p k d", p=128), wd[sl][:], reads=[b_wd[sl]])
            else:
                dma(S, "sp", wg[sl][:], W["wgS"][e].rearrange("(k p) f -> p k f", p=128), writes=[b_wg[sl]])
                dma(S, "sp", wu[sl][:], W["wuS"][e].rearrange("(k p) f -> p k f", p=128), writes=[b_wu[sl]])
                dma(S, "sp", wd[sl][:], W["wdS"][e].rearrange("(k p) d -> p k d", p=128), writes=[b_wd[sl]])

        rot = 0
        cnt = 0
        for TT in range(4):
            dma(S, "sp", u2t[:], W["u2T"][:, TT * 1024:(TT + 1) * 1024].rearrange("(c p) n -> p c n", p=128), writes=[b_u2t])
            dma(S, "sp", cbt[:], W["combS"][TT * 1024:(TT + 1) * 1024, :].rearrange("(s p) e -> p s e", p=128), writes=[b_cbt])
            S.add("pool", lambda e: e.memset(yacc[:], 0.0), writes=[b_yacc])
            load_w(TT, 0, 0)
            for ex in range(n_exp):
                sl = ex % 2
                if ex + 1 < n_exp:
                    load_w(TT, ex + 1, (ex + 1) % 2)
                for half in range(2):
                    c2 = cnt % 2
                    cnt += 1
                    for f in range(2):
                        for k in range(8):
                            S.add("pe", lambda e, f=f, k=k, sl=sl, half=half: e.matmul(PS[f][:], lhsT=wg[sl][:, k, f * 128:(f + 1) * 128], rhs=u2t[:, k, half * 512:(half + 1) * 512],
                                                                                  start=(k == 0), stop=(k == 7)), reads=[b_wg[sl], b_u2t], writes=[PB[f]])
                    for f in range(2):
                        for k in range(8):
                            S.add("pe", lambda e, f=f, k=k, sl=sl, half=half: e.matmul(PS[2 + f][:], lhsT=wu[sl][:, k, f * 128:(f + 1) * 128], rhs=u2t[:, k, half * 512:(half + 1) * 512],
                                                                                  start=(k == 0), stop=(k == 7)), reads=[b_wu[sl], b_u2t], writes=[PB[2 + f]])
                    for f in range(2):
                        S.add("act", lambda e, f=f, c2=c2: e.activation(out=sg[c2][:, f, :], in_=PS[f][:], func=AF.Silu), reads=[PB[f]], writes=[b_sg[c2]])
                        S.add("dve", lambda e, f=f, c2=c2: e.tensor_tensor(out=hd[c2][:, f, :], in0=PS[2 + f][:], in1=sg[c2][:, f, :], op=ALU.mult),
                              reads=[PB[2 + f], b_sg[c2]], writes=[b_hd[c2]])
                    for sub in range(4):
                        for dh in range(2):
                            bk = 4 + rot % 4
                            rot += 1
                            for f in range(2):
                                S.add("pe", lambda e, bk=bk, f=f, sub=sub, dh=dh, c2=c2, sl=sl: e.matmul(
                                    PS[bk][:], lhsT=hd[c2][:, f, sub * 128:(sub + 1) * 128], rhs=wd[sl][:, f, dh * 512:(dh + 1) * 512], start=(f == 0), stop=(f == 1)),
                                    reads=[b_hd[c2], b_wd[sl]], writes=[PB[bk]])
                            s8 = half * 4 + sub
                            S.add("dve", lambda e, bk=bk, s8=s8, dh=dh, ex=ex: e.scalar_tensor_tensor(
                                out=yacc[:, s8, dh * 512:(dh + 1) * 512], in0=PS[bk][:], scalar=cbt[:, s8, ex:ex + 1], in1=yacc[:, s8, dh * 512:(dh + 1) * 512],
                                op0=ALU.mult, op1=ALU.add), reads=[PB[bk], b_cbt, b_yacc], writes=[b_yacc])
            for half in range(2):
                r0 = TT * 1024 + half * 512
                dma(S, "sp", hp[:], W["hS"][r0:r0 + 512, :].rearrange("(s p) d -> p s d", p=128), writes=[b_hp])
                ya = yacc[:, half * 4:(half + 1) * 4, :]
                S.add("dve", lambda e, ya=ya: e.tensor_tensor(out=ya, in0=ya, in1=C.gtf_bc[:].unsqueeze(1).broadcast_to([128, 4, D]), op=ALU.mult),
                      reads=[b_yacc, C.b_gt], writes=[b_yacc])
                S.add("pool", lambda e, ya=ya: e.tensor_tensor(out=hp[:], in0=hp[:], in1=ya, op=ALU.add), reads=[b_yacc, b_hp], writes=[b_hp])
                S.add("dve", lambda e: e.memset(ssq[:], 0.0), writes=[b_ssq])
                for s in range(4):
                    S.add("act", lambda e, s=s: e.activation(out=junk[:], in_=hp[:, s, :], func=AF.Square, accum_out=ssq[:, s:s + 1]),
                          reads=[b_hp, b_ssq], writes=[b_junk, b_ssq])
                S.add("dve", lambda e: e.tensor_scalar(out=ssq[:, 4:8], in0=ssq[:, 0:4], scalar1=1.0 / D, scalar2=EPS, op0=ALU.mult, op1=ALU.add),
                      reads=[b_ssq], writes=[b_ssq])
                S.add("act", lambda e: e.activation(out=ssq[:, 4:8], in_=ssq[:, 4:8], func=AF.Sqrt), reads=[b_ssq], writes=[b_ssq])
                S.add("dve", lambda e: e.reciprocal(out=ssq[:, 4:8], in_=ssq[:, 4:8]), reads=[b_ssq], writes=[b_ssq])
                for s in range(4):
                    S.add("dve", lambda e, s=s: e.scalar_tensor_tensor(out=hp[:, s, :], in0=hp[:, s, :], scalar=ssq[:, 4 + s:5 + s], in1=gfin[:], op0=ALU.mult, op1=ALU.mult),
                          reads=[b_hp, b_ssq, b_gfin], writes=[b_hp])
                dma(S, "sp", C.out[r0:r0 + 512, :].rearrange("(s p) d -> p s d", p=128), hp[:], reads=[b_hp])
        S.flush()
```

```python
import math
from contextlib import ExitStack
import numpy as np
import concourse.bass as bass
import concourse.mybir as mybir
from concourse.bass_utils import run_bass_kernel_spmd

F32 = mybir.dt.float32
BF16 = mybir.dt.bfloat16
AF = mybir.ActivationFunctionType
ALU = mybir.AluOpType
AX = mybir.AxisListType

L = 4096
D = 1024
NTT = 8
EPS = 1e-6
LAMBDA_INIT = 0.8 - 0.6 * math.exp(-0.3 * 0)
NEXP = 32
ENGS = ("pe", "act", "dve", "pool", "sp")
ENG_ATTR = {"pe": "tensor", "act": "scalar", "dve": "vector", "pool": "gpsimd", "sp": "sync"}


class Buf:
    __slots__ = ("name", "w", "r")

    def __init__(self, name=""):
        self.name = name
        self.w = None
        self.r = []


class Op:
    __slots__ = ("eng", "idx", "fn", "waits", "signal", "count", "dma", "dsem", "dval", "snap", "bg")

    def __init__(self, eng, idx, fn, dma):
        self.eng = eng
        self.idx = idx
        self.fn = fn
        self.waits = []
        self.signal = False
        self.count = None
        self.dma = dma
        self.dsem = None
        self.dval = None
        self.snap = None
        self.bg = False


class Sched:
    def __init__(self, nc, n_dma_sems=48):
        self.nc = nc
        self.pending = {e: [] for e in ENGS}
        self.nops = {e: 0 for e in ENGS}
        self.last = {e: None for e in ENGS}
        self.seen = {e: {} for e in ENGS}
        self.n_dma_sems = n_dma_sems
        self.dma_rr = 0
        self.dma_rr_sw = 0
        self.n_hw = 28
        self.dma_last = [None] * n_dma_sems
        self.dma_last_nb = [None] * n_dma_sems
        self.dma_val = [0] * n_dma_sems
        self.cnt = {e: 0 for e in ENGS}
        self.sems = {e: nc.alloc_semaphore(name=f"sem_{e}") for e in ENGS}
        self.dsems = [nc.alloc_semaphore(name=f"dsem_{i}") for i in range(n_dma_sems)]

    def _need(self, o, d):
        e = o.eng
        if d.dma:
            key = ("d", d.dsem)
            if self.seen[e].get(key, 0) >= d.dval:
                return
            self.seen[e][key] = d.dval
            o.waits.append(d)
        else:
            if d.eng == e and e == "pe":
                return
            key = d.eng
            if self.seen[e].get(key, -1) >= d.idx:
                return
            self.seen[e][key] = d.idx
            d.signal = True
            o.waits.append(d)
        if d.snap is not None:
            se = self.seen[e]
            for k, v in d.snap.items():
                if k == e:
                    continue
                if se.get(k, -1) < v:
                    se[k] = v

    def add(self, eng, fn, reads=(), writes=(), dma=False, bg=False):
        o = Op(eng, self.nops[eng], fn, dma)
        o.bg = bg
        self.nops[eng] += 1
        deps = []
        for b in reads:
            if b.w is not None:
                deps.append(b.w)
        for b in writes:
            if b.w is not None:
                deps.append(b.w)
            deps.extend(b.r)
        if dma:
            if eng == "pool":
                s = self.n_hw + self.dma_rr_sw
                self.dma_rr_sw = (self.dma_rr_sw + 1) % (self.n_dma_sems - self.n_hw)
            else:
                s = self.dma_rr
                self.dma_rr = (self.dma_rr + 1) % self.n_hw
            prev = self.dma_last[s]
            if prev is not None:
                deps.append(prev)
            self.dma_val[s] += 16
            o.dsem = s
            o.dval = self.dma_val[s]
            self.dma_last[s] = o
            if not bg:
                self.dma_last_nb[s] = o
        for d in deps:
            if d is not o:
                self._need(o, d)
        for b in reads:
            b.r.append(o)
        for b in writes:
            b.w = o
            b.r = []
        self.pending[eng].append(o)
        if not dma:
            self.last[eng] = o
        o.snap = dict(self.seen[eng])
        return o

    def flush(self):
        lasts = [self.last[e] for e in ENGS if self.last[e] is not None]
        dlasts = [(self.dma_last_nb[i] if d.bg else d) for i, d in enumerate(self.dma_last) if d is not None]
        dlasts = [d for d in dlasts if d is not None]
        for e in ENGS:
            o = Op(e, self.nops[e], None, False)
            self.nops[e] += 1
            for d in lasts + dlasts:
                self._need(o, d)
            self.pending[e].append(o)
            o.snap = dict(self.seen[e])
        nc = self.nc
        for e in ENGS:
            for o in self.pending[e]:
                if o.signal and not o.dma:
                    self.cnt[e] += 1
                    o.count = self.cnt[e]
        sems, dsems = self.sems, self.dsems
        with nc.Block() as block:
            for e in ENGS:
                ops = self.pending[e]

                def body(eng, ops=ops, e=e):
                    for o in ops:
                        for d in o.waits:
                            if d.dma:
                                eng.wait_ge(dsems[d.dsem], d.dval)
                            else:
                                eng.wait_ge(sems[d.eng], d.count)
                        if o.fn is None:
                            continue
                        ins = o.fn(eng)
                        if o.dma:
                            ins.then_inc(dsems[o.dsem], 16)
                        elif o.signal:
                            ins.then_inc(sems[e], 1)

                getattr(block, ENG_ATTR[e])(body)
        self.pending = {e: [] for e in ENGS}


class Ctx:
    pass


def dma(S, q, out, in_, reads=(), writes=(), bg=False):
    return S.add(q, lambda e: e.dma_start(out=out, in_=in_), reads, writes, dma=True, bg=bg)


VEC_ROWS = {}


def build(debug=()):
    nc = bass.Bass("TRN2", target_bir_lowering=False)
    C = Ctx()
    C.nc = nc
    C.debug = set(debug)
    C.S = S = Sched(nc)

    def din(name, shape):
        return nc.dram_tensor(name, list(shape), F32, kind="ExternalInput").ap()

    I = C.I = {}
    for name, shape in [
        ("x", (L, D)), ("c", (D,)), ("w_ada", (D, 6 * D)), ("b_ada", (6 * D,)), ("g_norm_mix", (D,)),
        ("w_in", (D, 5632)), ("b_in", (5632,)),
        ("s5_a_re", (32, 64)), ("s5_a_im", (32, 64)), ("s5_b_re", (32, 64, 16)), ("s5_b_im", (32, 64, 16)),
        ("s5_c_re", (32, 16, 64)), ("s5_c_im", (32, 16, 64)), ("s5_d", (512,)), ("s5_log_dt", (32,)),
        ("w_glu", (512, 512)), ("b_glu", (512,)),
        ("lambda_q1", (64,)), ("lambda_k1", (64,)), ("lambda_q2", (64,)), ("lambda_k2", (64,)), ("g_subln", (128,)),
        ("w_br_ssm", (512, D)), ("w_br_attn", (D, D)), ("w_out", (D, D)), ("g_norm_ffn", (D,)),
        ("w_router_grp", (D, 4)), ("b_router_grp", (4,)), ("w_router_exp", (D, 32)), ("b_router_exp", (32,)),
        ("w_exp_gate", (NEXP, D, 256)), ("w_exp_up", (NEXP, D, 256)), ("w_exp_down", (NEXP, 256, D)),
        ("g_final", (D,)),
    ]:
        I[name] = din(name, shape)
    C.out = nc.dram_tensor("out", [L, D], F32, kind="ExternalOutput").ap()

    def scratch(name, shape, dt):
        if name in C.debug:
            return nc.dram_tensor(name, list(shape), dt, kind="ExternalOutput").ap()
        return nc.dram_tensor(name, list(shape), dt, kind="Internal").ap()

    C.scratch = scratch
    W = C.W = {}
    W["qT"] = scratch("qT", (D, L), BF16)
    W["kT"] = scratch("kT", (D, L), BF16)
    W["vS"] = scratch("vS", (L, D), BF16)
    W["gT"] = scratch("gT", (2 * D, L), BF16)
    W["s5S"] = scratch("s5S", (L, 512), F32)
    W["y2T"] = scratch("y2T", (512, L), BF16)
    W["onT"] = scratch("onT", (D, L), BF16)
    W["hS"] = scratch("hS", (L, D), F32)
    W["u2T"] = scratch("u2T", (D, L), BF16)
    W["combS"] = scratch("combS", (L, NEXP), F32)
    W["wgS"] = scratch("wgS", (NEXP, D, 256), BF16)
    W["wuS"] = scratch("wuS", (NEXP, D, 256), BF16)
    W["wdS"] = scratch("wdS", (NEXP, 256, D), BF16)
    NSLOT = 6144
    W["XN"] = scratch("XN", (L, D), BF16)
    W["CG"] = scratch("CG", (L, 8), F32)
    W["XS"] = scratch("XS", (NSLOT, D), BF16)
    W["CGS"] = scratch("CGS", (NSLOT, 8), F32)
    W["YS"] = scratch("YS", (NSLOT, D), F32)
    W["WALL"] = scratch("WALL", (NEXP * 128, 6144), BF16)
    C.WB = {k: Buf(k) for k in W}
    C.sparse = "dense" not in C.debug
    if "dbg_cols" in C.debug:
        C.dbg_cols = nc.dram_tensor("dbg_cols", [128, 256], F32, kind="ExternalOutput").ap()
    if "dbg_s5y" in C.debug:
        C.dbg_s5y = nc.dram_tensor("dbg_s5y", [L, 512], F32, kind="ExternalOutput").ap()

    with ExitStack() as _es:
        p0 = _es.enter_context(nc.psum_tensor("ps0", [128, 512], F32))
        p1 = _es.enter_context(nc.psum_tensor("ps1", [128, 512], F32))
        p2 = _es.enter_context(nc.psum_tensor("ps2", [128, 512], F32))
        p3 = _es.enter_context(nc.psum_tensor("ps3", [128, 512], F32))
        p4 = _es.enter_context(nc.psum_tensor("ps4", [128, 512], F32))
        p5 = _es.enter_context(nc.psum_tensor("ps5", [128, 512], F32))
        p6 = _es.enter_context(nc.psum_tensor("ps6", [128, 512], F32))
        p7 = _es.enter_context(nc.psum_tensor("ps7", [128, 512], F32))
        ident_f = _es.enter_context(nc.sbuf_tensor("ident_f", [128, 128], F32))
        ident_b = _es.enter_context(nc.sbuf_tensor("ident_b", [128, 128], BF16))
        ones_b = _es.enter_context(nc.sbuf_tensor("ones_b", [128, 128], BF16))
        cols = _es.enter_context(nc.sbuf_tensor("cols", [128, 256], F32))
        gtm_bc = _es.enter_context(nc.sbuf_tensor("gtm_bc", [128, D], F32))
        gtf_bc = _es.enter_context(nc.sbuf_tensor("gtf_bc", [128, D], F32))
        C.RK = _es.enter_context(nc.sbuf_tensor("RK", [128, 32], F32))
        C.OH = _es.enter_context(nc.sbuf_tensor("OH", [128, 32, 4], F32))
        C.tot = _es.enter_context(nc.sbuf_tensor("tot", [128, 4], F32))
        C.SLOT_I = _es.enter_context(nc.sbuf_tensor("SLOT_I", [128, 32], mybir.dt.int32))
        C.TG_I = _es.enter_context(nc.sbuf_tensor("TG_I", [128, 12], mybir.dt.int32))
        C.WIDX = _es.enter_context(nc.sbuf_tensor("WIDX", [128, 96], mybir.dt.int32))
        C.b_w4 = Buf("w4g")
        C.b_rk = Buf("rk")
        C.PS = [p0, p1, p2, p3, p4, p5, p6, p7]
        C.PB = [Buf(f"ps{i}") for i in range(8)]
        C.ident_f, C.ident_b, C.ones_b, C.cols = ident_f, ident_b, ones_b, cols
        C.gtm_bc, C.gtf_bc = gtm_bc, gtf_bc
        C.b_const = Buf("const")
        C.b_cols = Buf("cols")
        C.b_gt = Buf("gt")
        with ExitStack() as _es01:
            C.w_in = _es01.enter_context(nc.sbuf_tensor("sb_w_in", [128, 8, 5632], BF16))
            C.b_win = [Buf(f"win{i}") for i in range(11)]
            phase0(C)
            S.flush()
            if "stop0" not in C.debug and "skip1" not in C.debug:
                phase1(C)
        if "stop0" not in C.debug:
            if "skip2" not in C.debug and "stop1" not in C.debug:
                phase2(C)
            if "heads" in C.debug:
                C.heads = [0, 5]
            for f_ in C.debug:
                if f_.startswith("epi="):
                    C.epi_step = int(f_[4:])
            with ExitStack() as _es34:
                C.wbs = _es34.enter_context(nc.sbuf_tensor("wbs", [128, 4, D], BF16))
                C.wba = _es34.enter_context(nc.sbuf_tensor("wba", [128, 8, D], BF16))
                C.wout = _es34.enter_context(nc.sbuf_tensor("wout", [128, 8, D], BF16))
                if "skip3" not in C.debug and "stop1" not in C.debug:
                    phase3(C)
                if "stop3" not in C.debug and "stop1" not in C.debug:
                    phase4a(C)
            if "stop3" not in C.debug and "stop1" not in C.debug:
                if "n_exp" in C.debug:
                    C.n_exp = 2
                if "stop4a" not in C.debug:
                    if C.sparse:
                        phase4p(C)
                        phase4b_sparse(C)
                    else:
                        phase4b(C)
        if "dbg_cols" in C.debug:
            dma(S, "sp", C.dbg_cols, cols[:], reads=[C.b_cols])
            S.flush()
    return nc


COL_C = 0
COL_BADA = 8
COL_BIN = 56
COL_GNM = 100
COL_GNF = 108
COL_BGLU = 116
COL_GSUB = 120
COL_MOD = 128
COL_G1 = 176
COL_G2 = 184
COL_GSUBS = 192


def phase0(C):
    nc, S, I = C.nc, C.S, C.I
    cols = C.cols
    bc = C.b_const
    S.add("pool", lambda e: e.memset(C.ident_f[:], 0.0), writes=[bc])
    S.add("pool", lambda e: e.affine_select(out=C.ident_f[:], in_=C.ident_f[:], pattern=[[-1, 128]],
                                            compare_op=ALU.not_equal, fill=1.0, base=0, channel_multiplier=1),
          reads=[bc], writes=[bc])
    S.add("dve", lambda e: e.tensor_copy(out=C.ident_b[:], in_=C.ident_f[:]), reads=[bc], writes=[bc])
    S.add("dve", lambda e: e.memset(C.ones_b[:], 1.0), writes=[bc])
    with ExitStack() as _es:
        rows = _es.enter_context(nc.sbuf_tensor("rows", [128, 128], F32))
        cs_b = _es.enter_context(nc.sbuf_tensor("cs_b", [128, 8], BF16))
        cs_rep = _es.enter_context(nc.sbuf_tensor("cs_rep", [128, 8, 128], BF16))
        wa0 = _es.enter_context(nc.sbuf_tensor("wa0", [128, 8, 512], BF16))
        wa1 = _es.enter_context(nc.sbuf_tensor("wa1", [128, 8, 512], BF16))
        bada_bc = _es.enter_context(nc.sbuf_tensor("bada_bc", [128, 2, D], F32))
        b_rows = Buf("rows")
        S.add("dve", lambda e: e.memset(rows[:], 0.0), writes=[b_rows])
        for name, base, n in [("c", COL_C, 8), ("b_ada", COL_BADA, 48), ("b_in", COL_BIN, 44), ("g_norm_mix", COL_GNM, 8),
                              ("g_norm_ffn", COL_GNF, 8), ("b_glu", COL_BGLU, 4), ("g_subln", COL_GSUB, 1)]:
            dma(S, "sp", rows[base:base + n, :], I[name].rearrange("(k p) -> k p", p=128), writes=[b_rows])
        b_bada = Buf("bada_bc")
        dma(S, "sp", bada_bc[:, 0, :], I["b_ada"][2 * D:3 * D].partition_broadcast(128), writes=[b_bada])
        dma(S, "sp", bada_bc[:, 1, :], I["b_ada"][5 * D:6 * D].partition_broadcast(128), writes=[b_bada])
        pT = C.PS[0]
        S.add("pe", lambda e: e.transpose(out=pT[:, 0:128], in_=rows[:], identity=C.ident_f[:]), reads=[b_rows, bc], writes=[C.PB[0]])
        S.add("dve", lambda e: e.tensor_copy(out=cols[:, 0:128], in_=pT[:, 0:128]), reads=[C.PB[0]], writes=[C.b_cols])
        b_cs = Buf("cs")
        S.add("act", lambda e: e.activation(out=cols[:, COL_C:COL_C + 8], in_=cols[:, COL_C:COL_C + 8], func=AF.Silu),
              reads=[C.b_cols], writes=[C.b_cols])
        S.add("dve", lambda e: e.tensor_copy(out=cs_b[:], in_=cols[:, COL_C:COL_C + 8]), reads=[C.b_cols], writes=[b_cs])
        S.add("dve", lambda e: e.tensor_copy(out=cs_rep[:], in_=cs_b[:].unsqueeze(2).broadcast_to([128, 8, 128])),
              reads=[b_cs], writes=[b_cs])
        was = [wa0, wa1]
        waf = [_es.enter_context(nc.sbuf_tensor(f"waf{i}", [128, 8, 512], F32)) for i in range(2)]
        b_waf = [Buf("waf0"), Buf("waf1")]
        b_wa = [Buf("wa0"), Buf("wa1")]
        pcol = C.PS[1]
        for n in range(12):
            wa = was[n % 2]
            bw = b_wa[n % 2]
            waf_ = waf[n % 2]
            dma(S, "sp", waf_[:], I["w_ada"][:, n * 512:(n + 1) * 512].rearrange("(k p) n -> p k n", p=128), writes=[b_waf[n % 2]])
            S.add("act", lambda e, wa=wa, waf_=waf_: e.activation(out=wa[:], in_=waf_[:], func=AF.Copy), reads=[b_waf[n % 2]], writes=[bw])
            if n >= 1:
                i_ = n - 1
                dma(S, "pool", C.w_in[:, :, i_ * 512:(i_ + 1) * 512], I["w_in"][:, i_ * 512:(i_ + 1) * 512].rearrange("(k p) n -> p k n", p=128),
                    writes=[C.b_win[i_]], bg=True)
            for s in range(4):
                j = n * 4 + s
                for k in range(8):
                    S.add("pe", lambda e, wa=wa, s=s, k=k, j=j: e.matmul(pcol[:, j:j + 1], lhsT=wa[:, k, s * 128:(s + 1) * 128],
                                                                      rhs=cs_b[:, k:k + 1], start=(k == 0), stop=(k == 7)),
                          reads=[bw, b_cs], writes=[C.PB[1]])
            if n in (4, 5, 10, 11):
                prow = C.PS[2 + (n % 2)]
                pbb = C.PB[2 + (n % 2)]
                for k in range(8):
                    S.add("pe", lambda e, wa=wa, k=k, prow=prow: e.matmul(prow[:], lhsT=cs_rep[:, k, :], rhs=wa[:, k, :],
                                                                       start=(k == 0), stop=(k == 7)),
                          reads=[bw, b_cs], writes=[pbb])
                dst = C.gtm_bc if n < 6 else C.gtf_bc
                hh = n % 2
                wi = 0 if n < 6 else 1
                S.add("dve", lambda e, dst=dst, hh=hh, wi=wi, prow=prow: e.tensor_tensor(
                    out=dst[:, hh * 512:(hh + 1) * 512], in0=prow[:], in1=bada_bc[:, wi, hh * 512:(hh + 1) * 512], op=ALU.add),
                    reads=[pbb, b_bada], writes=[C.b_gt])
        S.add("dve", lambda e: e.tensor_tensor(out=cols[:, COL_MOD:COL_MOD + 48], in0=pcol[:, 0:48],
                                               in1=cols[:, COL_BADA:COL_BADA + 48], op=ALU.add),
              reads=[C.PB[1], C.b_cols], writes=[C.b_cols])
        S.add("dve", lambda e: e.scalar_tensor_tensor(out=cols[:, COL_G1:COL_G1 + 8], in0=cols[:, COL_MOD + 8:COL_MOD + 16], scalar=1.0,
                                                      in1=cols[:, COL_GNM:COL_GNM + 8], op0=ALU.add, op1=ALU.mult),
              reads=[C.b_cols], writes=[C.b_cols])
        S.add("dve", lambda e: e.scalar_tensor_tensor(out=cols[:, COL_G2:COL_G2 + 8], in0=cols[:, COL_MOD + 32:COL_MOD + 40], scalar=1.0,
                                                      in1=cols[:, COL_GNF:COL_GNF + 8], op0=ALU.add, op1=ALU.mult),
              reads=[C.b_cols], writes=[C.b_cols])
        S.add("dve", lambda e: e.tensor_scalar(out=cols[:, COL_GSUBS:COL_GSUBS + 1], in0=cols[:, COL_GSUB:COL_GSUB + 1],
                                               scalar1=(1.0 - LAMBDA_INIT), scalar2=None, op0=ALU.mult),
              reads=[C.b_cols], writes=[C.b_cols])
        S.flush()


def phase1(C):
    nc, S, I, W, WB = C.nc, C.S, C.I, C.W, C.WB
    cols = C.cols
    with ExitStack() as _es:
        w_in = C.w_in
        xt0 = _es.enter_context(nc.sbuf_tensor("xt0", [128, 4, D], F32))
        xt1 = _es.enter_context(nc.sbuf_tensor("xt1", [128, 4, D], F32))
        xn = _es.enter_context(nc.sbuf_tensor("xn", [128, 4, D], BF16))
        junk = _es.enter_context(nc.sbuf_tensor("junk", [128, D], BF16))
        ssq = _es.enter_context(nc.sbuf_tensor("ssq", [128, 8], F32))
        uT0 = _es.enter_context(nc.sbuf_tensor("uT0", [128, 8, 512], BF16))
        uT1 = _es.enter_context(nc.sbuf_tensor("uT1", [128, 8, 512], BF16))
        stg0 = _es.enter_context(nc.sbuf_tensor("stg0", [128, 4, 512], BF16))
        stg1 = _es.enter_context(nc.sbuf_tensor("stg1", [128, 4, 512], BF16))
        vstg = _es.enter_context(nc.sbuf_tensor("vstg", [128, 4, D], BF16))
        s5stg = _es.enter_context(nc.sbuf_tensor("s5stg", [128, 4, 512], F32))
        bias_bc = _es.enter_context(nc.sbuf_tensor("bias_bc", [128, 1536], F32))
        b_win = C.b_win
        b_bias = Buf("bias_bc")
        dma(S, "sp", bias_bc[:, 0:512], I["b_in"][0:512].partition_broadcast(128), writes=[b_bias])
        dma(S, "sp", bias_bc[:, 512:1536], I["b_in"][2560:3584].partition_broadcast(128), writes=[b_bias])
        xts = [xt0, xt1]
        b_xt = [Buf("xt0"), Buf("xt1")]
        uTs = [uT0, uT1]
        b_uT = [Buf("uT0"), Buf("uT1")]
        stgs = [stg0, stg1]
        b_stg = [Buf("stg0"), Buf("stg1")]
        b_xn, b_ssq, b_junk, b_vstg, b_s5stg = Buf("xn"), Buf("ssq"), Buf("junk"), Buf("vstg"), Buf("s5stg")
        PS, PB = C.PS, C.PB

        def load_x(t):
            dma(S, "sp", xts[t % 2][:], I["x"][t * 512:(t + 1) * 512, :].rearrange("(s p) d -> p s d", p=128), writes=[b_xt[t % 2]])

        load_x(0)
        rot = 0
        stg_i = 0
        for t in range(NTT):
            if t + 1 < NTT:
                load_x(t + 1)
            xt = xts[t % 2]
            bx = b_xt[t % 2]
            uT = uTs[t % 2]
            bu = b_uT[t % 2]
            S.add("dve", lambda e: e.memset(ssq[:], 0.0), writes=[b_ssq])
            for s in range(4):
                S.add("act", lambda e, s=s, xt=xt: e.activation(out=junk[:], in_=xt[:, s, :], func=AF.Square, accum_out=ssq[:, s:s + 1]),
                      reads=[bx, b_ssq], writes=[b_junk, b_ssq])
            S.add("dve", lambda e: e.tensor_scalar(out=ssq[:, 4:8], in0=ssq[:, 0:4], scalar1=1.0 / D, scalar2=EPS, op0=ALU.mult, op1=ALU.add),
                  reads=[b_ssq], writes=[b_ssq])
            S.add("act", lambda e: e.activation(out=ssq[:, 4:8], in_=ssq[:, 4:8], func=AF.Sqrt), reads=[b_ssq], writes=[b_ssq])
            S.add("dve", lambda e: e.reciprocal(out=ssq[:, 4:8], in_=ssq[:, 4:8]), reads=[b_ssq], writes=[b_ssq])
            for s in range(4):
                S.add("dve", lambda e, s=s, xt=xt: e.tensor_scalar(out=xn[:, s, :], in0=xt[:, s, :], scalar1=ssq[:, 4 + s:5 + s], scalar2=None,
                                                                op0=ALU.mult),
                      reads=[bx, b_ssq], writes=[b_xn])
            for k in range(8):
                pt = PS[k % 2]
                ptb = pt[:].bitcast(BF16)
                for s in range(4):
                    S.add("pe", lambda e, s=s, k=k, ptb=ptb: e.transpose(out=ptb[:, s * 128:(s + 1) * 128], in_=xn[:, s, k * 128:(k + 1) * 128],
                                                                      identity=C.ident_b[:]),
                          reads=[b_xn, C.b_const], writes=[PB[k % 2]])
                S.add("dve", lambda e, k=k, ptb=ptb, uT=uT: e.tensor_scalar(out=uT[:, k, :], in0=ptb[:, 0:512], scalar1=cols[:, COL_G1 + k:COL_G1 + k + 1],
                                                                         scalar2=cols[:, COL_MOD + k:COL_MOD + k + 1], op0=ALU.mult, op1=ALU.add),
                      reads=[PB[k % 2], C.b_cols], writes=[bu])
            groups = [("qT", 4, 0), ("qT", 8, 4), ("kT", 12, 0), ("kT", 16, 4), ("gT", 28, 0), ("gT", 32, 4), ("gT", 36, 8), ("gT", 40, 12)]
            for (dst, ct0, r0) in groups:
                stg = stgs[stg_i % 2]
                bs = b_stg[stg_i % 2]
                stg_i += 1
                for cc in range(4):
                    ct = ct0 + cc
                    bank = 2 + (rot % 4)
                    rot += 1
                    pp = PS[bank]
                    for k in range(8):
                        S.add("pe", lambda e, pp=pp, k=k, ct=ct, uT=uT: e.matmul(pp[:], lhsT=w_in[:, k, ct * 128:(ct + 1) * 128], rhs=uT[:, k, :],
                                                                             start=(k == 0), stop=(k == 7)),
                              reads=[b_win[ct // 4], bu], writes=[PB[bank]])
                    fn = AF.Sigmoid if dst == "gT" else AF.Identity
                    S.add("act", lambda e, pp=pp, cc=cc, ct=ct, stg=stg, fn=fn: e.activation(out=stg[:, cc, :], in_=pp[:], func=fn,
                                                                                        bias=cols[:, COL_BIN + ct:COL_BIN + ct + 1], scale=1.0),
                          reads=[PB[bank], C.b_cols], writes=[bs])
                dma(S, "sp", W[dst][r0 * 128:(r0 + 4) * 128, t * 512:(t + 1) * 512].rearrange("(c p) n -> p c n", p=128), stg[:],
                    reads=[bs])
            for s in range(4):
                for hh in range(3):
                    bank = 6 + (rot % 2)
                    rot += 1
                    pp = PS[bank]
                    c0 = 0 if hh == 2 else 2560 + hh * 512
                    for k in range(8):
                        S.add("pe", lambda e, pp=pp, k=k, s=s, c0=c0, uT=uT: e.matmul(pp[:], lhsT=uT[:, k, s * 128:(s + 1) * 128], rhs=w_in[:, k, c0:c0 + 512],
                                                                                  start=(k == 0), stop=(k == 7)),
                              reads=[b_win[c0 // 512], bu], writes=[PB[bank]])
                    if hh < 2:
                        S.add("dve", lambda e, pp=pp, s=s, hh=hh: e.tensor_tensor(out=vstg[:, s, hh * 512:(hh + 1) * 512], in0=pp[:],
                                                                               in1=bias_bc[:, 512 + hh * 512:1024 + hh * 512], op=ALU.add),
                              reads=[PB[bank], b_bias], writes=[b_vstg])
                    else:
                        S.add("dve", lambda e, pp=pp, s=s: e.tensor_tensor(out=s5stg[:, s, :], in0=pp[:], in1=bias_bc[:, 0:512], op=ALU.add),
                              reads=[PB[bank], b_bias], writes=[b_s5stg])
            dma(S, "sp", W["vS"][t * 512:(t + 1) * 512, :].rearrange("(s p) d -> p s d", p=128), vstg[:], reads=[b_vstg])
            dma(S, "sp", W["s5S"][t * 512:(t + 1) * 512, :].rearrange("(s p) d -> p s d", p=128), s5stg[:], reads=[b_s5stg])
        S.flush()


def phase4p(C):
    nc, S, W = C.nc, C.S, C.W
    with ExitStack() as _es:
        sb = lambda name, shape, dt: _es.enter_context(nc.sbuf_tensor(name, shape, dt))
        NB4 = 6
        xb = [sb(f"xb{i}", [128, D], BF16) for i in range(NB4)]
        cgb = [sb(f"cgb{i}", [128, 8], F32) for i in range(NB4)]
        b_xb = [Buf(f"xb{i}") for i in range(NB4)]
        b_cgb = [Buf(f"cgb{i}") for i in range(NB4)]
        for blk in range(32):
            sl = blk % NB4
            dma(S, "sp", xb[sl][:], W["XN"][blk * 128:(blk + 1) * 128, :], writes=[b_xb[sl]])
            dma(S, "sp", cgb[sl][:], W["CG"][blk * 128:(blk + 1) * 128, :], writes=[b_cgb[sl]])
            S.add("pool", lambda e, sl=sl, blk=blk: e.indirect_dma_start(
                out=W["XS"], out_offset=bass.IndirectOffsetOnAxis(ap=C.SLOT_I[:, blk:blk + 1], axis=0), in_=xb[sl][:], in_offset=None), reads=[b_xb[sl]], writes=[], dma=True)
            S.add("pool", lambda e, sl=sl, blk=blk: e.indirect_dma_start(
                out=W["CGS"], out_offset=bass.IndirectOffsetOnAxis(ap=C.SLOT_I[:, blk:blk + 1], axis=0), in_=cgb[sl][:], in_offset=None), reads=[b_cgb[sl]], writes=[], dma=True)
        S.flush()


def phase4b_sparse(C):
    nc, S, I, W = C.nc, C.S, C.I, C.W
    PS, PB = C.PS, C.PB
    cols = C.cols
    bc = C.b_const
    NT = 11
    regs = {}
    with ExitStack() as _es:
        sb = lambda name, shape, dt: _es.enter_context(nc.sbuf_tensor(name, shape, dt))
        xs = [sb(f"xs{i}", [128, 4, D], BF16) for i in range(2)]
        cgs = [sb(f"cgs{i}", [128, 4, 8], F32) for i in range(2)]
        u2t = sb("u2ts", [128, 8, 512], BF16)
        yacc = [sb(f"yaccs{i}", [128, 4, D], F32) for i in range(2)]
        wall = [sb(f"wall{i}", [128, 6144], BF16) for i in range(3)]
        wg = [wall[i][:, 0:2048].rearrange("p (k f) -> p k f", f=256) for i in range(3)]
        wu = [wall[i][:, 2048:4096].rearrange("p (k f) -> p k f", f=256) for i in range(3)]
        wd = [wall[i][:, 4096:6144].rearrange("p (k d) -> p k d", d=D) for i in range(3)]
        sg = [sb(f"sgs{i}", [128, 2, 512], BF16) for i in range(2)]
        hd = [sb(f"hds{i}", [128, 2, 512], BF16) for i in range(2)]
        b_xs = [Buf("xs0"), Buf("xs1")]
        b_cgs = [Buf("cgs0"), Buf("cgs1")]
        b_u2t = Buf("u2ts")
        b_ya = [Buf("ya0"), Buf("ya1")]
        b_wg = [Buf("wg0"), Buf("wg1"), Buf("wg2")]
        b_wu = [Buf("wu0"), Buf("wu1"), Buf("wu2")]
        b_wd = [Buf("wd0"), Buf("wd1"), Buf("wd2")]
        b_sg = [Buf("sg0"), Buf("sg1")]
        b_hd = [Buf("hd0"), Buf("hd1")]

        def load_tile(st):
            sl = st % 2
            dma(S, "sp", xs[sl][:], W["XS"][st * 512:(st + 1) * 512, :].rearrange("(s p) d -> p s d", p=128), writes=[b_xs[sl]])
            dma(S, "sp", cgs[sl][:], W["CGS"][st * 512:(st + 1) * 512, :].rearrange("(s p) e -> p s e", p=128), writes=[b_cgs[sl]])

        def load_w(st, j, sl):
            q = st * 8 + j
            S.add("pool", lambda e, q=q, sl=sl: e.indirect_dma_start(out=wall[sl][:], out_offset=None, in_=W["WALL"],
                                                                  in_offset=bass.IndirectOffsetOnAxis(ap=C.WIDX[:, q:q + 1], axis=0)),
                  reads=[C.b_rk], writes=[b_wg[sl], b_wu[sl], b_wd[sl]], dma=True)

        seq = [(st, j) for st in range(NT) for j in range(8)]
        deferred = []
        load_tile(0)
        load_w(0, 0, 0)
        load_w(0, 1, 1)
        rot = 0
        cnt = 0
        for qi, (st, j) in enumerate(seq):
            sl = qi % 3
            tsl = st % 2
            if j == 0:
                for k in range(8):
                    bk = 6 + k % 2
                    ptb = PS[bk][:].bitcast(BF16)
                    for s_ in range(4):
                        S.add("pe", lambda e, s_=s_, k=k, ptb=ptb, tsl=tsl: e.transpose(out=ptb[:, s_ * 128:(s_ + 1) * 128], in_=xs[tsl][:, s_, k * 128:(k + 1) * 128], identity=C.ident_b[:]),
                              reads=[b_xs[tsl], bc], writes=[PB[bk]])
                    S.add("dve", lambda e, k=k, ptb=ptb: e.tensor_scalar(out=u2t[:, k, :], in0=ptb[:, 0:512], scalar1=cols[:, COL_G2 + k:COL_G2 + k + 1],
                                                                       scalar2=cols[:, COL_MOD + 24 + k:COL_MOD + 25 + k], op0=ALU.mult, op1=ALU.add),
                          reads=[PB[bk], C.b_cols], writes=[b_u2t])
                S.add("pool", lambda e, tsl=tsl: e.memset(yacc[tsl][:], 0.0), writes=[b_ya[tsl]])
            c2 = cnt % 2
            cnt += 1
            for f in range(2):
                for k in range(8):
                    S.add("pe", lambda e, f=f, k=k, sl=sl: e.matmul(PS[f][:], lhsT=wg[sl][:, k, f * 128:(f + 1) * 128], rhs=u2t[:, k, :], start=(k == 0), stop=(k == 7)),
                          reads=[b_wg[sl], b_u2t], writes=[PB[f]])
            for f in range(2):
                for k in range(8):
                    S.add("pe", lambda e, f=f, k=k, sl=sl: e.matmul(PS[2 + f][:], lhsT=wu[sl][:, k, f * 128:(f + 1) * 128], rhs=u2t[:, k, :], start=(k == 0), stop=(k == 7)),
                          reads=[b_wu[sl], b_u2t], writes=[PB[2 + f]])
            for f in range(2):
                S.add("act", lambda e, f=f, c2=c2: e.activation(out=sg[c2][:, f, :], in_=PS[f][:], func=AF.Silu), reads=[PB[f]], writes=[b_sg[c2]])
                S.add("dve", lambda e, f=f, c2=c2: e.tensor_tensor(out=hd[c2][:, f, :], in0=PS[2 + f][:], in1=sg[c2][:, f, :], op=ALU.mult),
                      reads=[PB[2 + f], b_sg[c2]], writes=[b_hd[c2]])

            def down(st=st, j=j, sl=sl, tsl=tsl, c2=c2):
                nonlocal rot
                for sub in range(4):
                    for dh in range(2):
                        bk = 4 + rot % 4
                        rot += 1
                        for f in range(2):
                            S.add("pe", lambda e, bk=bk, f=f, sub=sub, dh=dh: e.matmul(
                                PS[bk][:], lhsT=hd[c2][:, f, sub * 128:(sub + 1) * 128], rhs=wd[sl][:, f, dh * 512:(dh + 1) * 512], start=(f == 0), stop=(f == 1)),
                                reads=[b_hd[c2], b_wd[sl]], writes=[PB[bk]])
                        S.add("dve", lambda e, bk=bk, sub=sub, dh=dh: e.scalar_tensor_tensor(
                            out=yacc[tsl][:, sub, dh * 512:(dh + 1) * 512], in0=PS[bk][:], scalar=cgs[tsl][:, sub, j:j + 1], in1=yacc[tsl][:, sub, dh * 512:(dh + 1) * 512],
                            op0=ALU.mult, op1=ALU.add), reads=[PB[bk], b_cgs[tsl], b_ya[tsl]], writes=[b_ya[tsl]])
                if j == 7:
                    dma(S, "sp", W["YS"][st * 512:(st + 1) * 512, :].rearrange("(s p) d -> p s d", p=128), yacc[tsl][:], reads=[b_ya[tsl]])

            if "nodefer" in C.debug:
                down()
            else:
                if deferred:
                    deferred.pop()()
                deferred.append(down)
            if qi + 2 < len(seq):
                load_w(seq[qi + 2][0], seq[qi + 2][1], (qi + 2) % 3)
            if j == 0 and st + 1 < NT:
                load_tile(st + 1)
        while deferred:
            deferred.pop()()
        S.flush()
    with ExitStack() as _es:
        sb = lambda name, shape, dt: _es.enter_context(nc.sbuf_tensor(name, shape, dt))
        yg = [sb(f"yg{i}", [128, 4, D], F32) for i in range(3)]
        hp = [sb(f"hpf{i}", [128, 4, D], F32) for i in range(3)]
        gfin = sb("gfin_s", [128, D], F32)
        junk = sb("junk6", [128, D], BF16)
        ssq = sb("ssq6", [128, 8], F32)
        b_yg = [Buf("yg0"), Buf("yg1"), Buf("yg2")]
        b_hp = [Buf("hp0"), Buf("hp1"), Buf("hp2")]
        b_gfin, b_junk, b_ssq = Buf("gfin"), Buf("junk"), Buf("ssq")
        dma(S, "sp", gfin[:], I["g_final"].partition_broadcast(128), writes=[b_gfin])

        def fetch(t):
            sl = t % 3
            for s_ in range(4):
                blk = t * 4 + s_
                S.add("pool", lambda e, sl=sl, s_=s_, blk=blk: e.indirect_dma_start(
                    out=yg[sl][:, s_, :], out_offset=None, in_=W["YS"], in_offset=bass.IndirectOffsetOnAxis(ap=C.SLOT_I[:, blk:blk + 1], axis=0)),
                    reads=[C.b_rk], writes=[b_yg[sl]], dma=True)
            dma(S, "sp", hp[sl][:], W["hS"][t * 512:(t + 1) * 512, :].rearrange("(s p) d -> p s d", p=128), writes=[b_hp[sl]])

        fetch(0)
        fetch(1)
        for t in range(NTT):
            sl = t % 3
            if t + 2 < NTT:
                fetch(t + 2)
            r0 = t * 512
            S.add("dve", lambda e, sl=sl: e.tensor_tensor(out=yg[sl][:], in0=yg[sl][:], in1=C.gtf_bc[:].unsqueeze(1).broadcast_to([128, 4, D]), op=ALU.mult),
                  reads=[b_yg[sl], C.b_gt], writes=[b_yg[sl]])
            S.add("dve", lambda e, sl=sl: e.tensor_tensor(out=hp[sl][:], in0=hp[sl][:], in1=yg[sl][:], op=ALU.add), reads=[b_yg[sl], b_hp[sl]], writes=[b_hp[sl]])
            S.add("dve", lambda e: e.memset(ssq[:], 0.0), writes=[b_ssq])
            for s_ in range(4):
                S.add("act", lambda e, s_=s_, sl=sl: e.activation(out=junk[:], in_=hp[sl][:, s_, :], func=AF.Square, accum_out=ssq[:, s_:s_ + 1]),
                      reads=[b_hp[sl], b_ssq], writes=[b_junk, b_ssq])
            S.add("dve", lambda e: e.tensor_scalar(out=ssq[:, 4:8], in0=ssq[:, 0:4], scalar1=1.0 / D, scalar2=EPS, op0=ALU.mult, op1=ALU.add),
                  reads=[b_ssq], writes=[b_ssq])
            S.add("act", lambda e: e.activation(out=ssq[:, 4:8], in_=ssq[:, 4:8], func=AF.Sqrt), reads=[b_ssq], writes=[b_ssq])
            S.add("dve", lambda e: e.reciprocal(out=ssq[:, 4:8], in_=ssq[:, 4:8]), reads=[b_ssq], writes=[b_ssq])
            for s_ in range(4):
                S.add("dve", lambda e, s_=s_, sl=sl: e.scalar_tensor_tensor(out=hp[sl][:, s_, :], in0=hp[sl][:, s_, :], scalar=ssq[:, 4 + s_:5 + s_], in1=gfin[:], op0=ALU.mult, op1=ALU.mult),
                      reads=[b_hp[sl], b_ssq, b_gfin], writes=[b_hp[sl]])
            dma(S, "sp", C.out[r0:r0 + 512, :].rearrange("(s p) d -> p s d", p=128), hp[sl][:], reads=[b_hp[sl]])
        S.flush()


_NC_CACHE = {}


def make_in_maps(inputs):
    maps = []
    shared = {}
    for k, v in inputs.items():
        if k in ("x", "c"):
            continue
        a = np.asarray(v)
        if k != "g_final":
            a = a[0]
        shared[k] = np.ascontiguousarray(a, dtype=np.float32)
    for b in range(8):
        m = dict(shared)
        m["x"] = np.ascontiguousarray(np.asarray(inputs["x"])[b], dtype=np.float32)
        m["c"] = np.ascontiguousarray(np.asarray(inputs["c"])[b], dtype=np.float32)
        maps.append(m)
    return maps


def kernel(**inputs):
    if "nc" not in _NC_CACHE:
        _NC_CACHE["nc"] = build()
    nc = _NC_CACHE["nc"]
    res = run_bass_kernel_spmd(nc, make_in_maps(inputs), core_ids=list(range(8)))
    return np.stack([np.asarray(r["out"], dtype=np.float32) for r in res.results], axis=0)


def _pstep(t):
    n = 1
    for s in list(t.shape)[1:]:
        n *= s
    return n


def view(t, off, dims, parts=128, p0=0):
    ps = _pstep(t)
    return bass.AP(t, p0 * ps + off, [[ps, parts]] + [list(d) for d in dims])


def phase2(C):
    nc, S, I, W = C.nc, C.S, C.I, C.W
    PS, PB = C.PS, C.PB
    cols = C.cols
    ident_f, ident_b = C.ident_f, C.ident_b
    bc = C.b_const
    with ExitStack() as _es:
        Btr = _es.enter_context(nc.sbuf_tensor("Btr", [128, 32, 64], BF16))
        Bti = _es.enter_context(nc.sbuf_tensor("Bti", [128, 32, 64], BF16))
        Ctr = _es.enter_context(nc.sbuf_tensor("Ctr", [64, 32, 128], F32))
        Cti = _es.enter_context(nc.sbuf_tensor("Cti", [64, 32, 128], F32))
        Dt = _es.enter_context(nc.sbuf_tensor("Dt", [128, 32, 128], BF16))
        P12 = _es.enter_context(nc.sbuf_tensor("P12", [64, 2, 64], F32))
        PP = _es.enter_context(nc.sbuf_tensor("PP", [64, 9, 2, 64], F32))
        apw = _es.enter_context(nc.sbuf_tensor("apw", [64, 2, 9, 32], F32))
        dbc = _es.enter_context(nc.sbuf_tensor("dbc", [128, 512], F32))
        wglu = _es.enter_context(nc.sbuf_tensor("wglu", [128, 4, 512], BF16))
        b_wt = Buf("s5w")
        b_wglu = Buf("wglu")
        b_dbc = Buf("dbc")
        dma(S, "pool", wglu[:], I["w_glu"].rearrange("(k p) n -> p k n", p=128), writes=[b_wglu])
        dma(S, "sp", dbc[:], I["s5_d"].partition_broadcast(128), writes=[b_dbc])
        with ExitStack() as _es:
            nat = _es.enter_context(nc.sbuf_tensor("nat", [128, 2, 64], F32))
            cnat = _es.enter_context(nc.sbuf_tensor("cnat", [128, 8, 64], F32))
            sm = _es.enter_context(nc.sbuf_tensor("sm", [64, 24, 32], F32))
            pw = _es.enter_context(nc.sbuf_tensor("pw", [64, 2, 9, 32], F32))
            Bn = _es.enter_context(nc.sbuf_tensor("Bn", [64, 2, 32, 16], F32))
            Bb = _es.enter_context(nc.sbuf_tensor("Bb", [64, 2, 32, 16], F32))
            CT = _es.enter_context(nc.sbuf_tensor("CT", [64, 3, 32, 16], F32))
            Ere = _es.enter_context(nc.sbuf_tensor("Ere", [64, 32, 15, 16], F32))
            Eim = _es.enter_context(nc.sbuf_tensor("Eim", [64, 32, 15, 16], F32))
            tmpB = _es.enter_context(nc.sbuf_tensor("tmpB", [64, 2, 32, 16], F32))
            halfpi = _es.enter_context(nc.sbuf_tensor("halfpi", [64, 1], F32))
            b_nat, b_cnat, b_sm, b_pw, b_Bn, b_Bb, b_CT, b_E, b_tmp = [Buf(n) for n in "nat cnat sm pw Bn Bb CT E tmp".split()]
            S.add("dve", lambda e: e.memset(nat[:], 0.0), writes=[b_nat])
            dma(S, "sp", nat[0:32, 0, :], I["s5_a_re"], writes=[b_nat])
            dma(S, "sp", nat[0:32, 1, :], I["s5_a_im"], writes=[b_nat])
            dma(S, "sp", cnat[:, 0:4, :], I["s5_c_re"].rearrange("g c p -> (g c) p").rearrange("(t q) p -> q t p", q=128), writes=[b_cnat])
            dma(S, "sp", cnat[:, 4:8, :], I["s5_c_im"].rearrange("g c p -> (g c) p").rearrange("(t q) p -> q t p", q=128), writes=[b_cnat])
            dma(S, "sp", Bn[:, 0, :, :], I["s5_b_re"].rearrange("g p c -> p g c"), writes=[b_Bn])
            dma(S, "sp", Bn[:, 1, :, :], I["s5_b_im"].rearrange("g p c -> p g c"), writes=[b_Bn])
            LR, LI, DT, AR, AI, T0, T1, T2, FR, FI, DEN, NR = range(12)
            dma(S, "sp", sm[:, DT, :], I["s5_log_dt"].partition_broadcast(64), writes=[b_sm])
            S.add("dve", lambda e: e.memset(halfpi[:], math.pi / 2), writes=[b_sm])
            for i, slot in ((0, LR), (1, LI)):
                S.add("pe", lambda e, i=i: e.transpose(out=PS[0][0:64, i * 128:(i + 1) * 128], in_=nat[:, i, :], identity=ident_f[:]),
                      reads=[b_nat, bc], writes=[PB[0]])
                S.add("dve", lambda e, i=i, slot=slot: e.tensor_copy(out=sm[:, slot, :], in_=PS[0][0:64, i * 128:i * 128 + 32]),
                      reads=[PB[0]], writes=[b_sm])
            for ri in range(2):
                for t in range(4):
                    S.add("pe", lambda e, ri=ri, t=t: e.transpose(out=PS[1][0:64, t * 128:(t + 1) * 128], in_=cnat[:, ri * 4 + t, :], identity=ident_f[:]),
                          reads=[b_cnat, bc], writes=[PB[1]])
                S.add("dve", lambda e, ri=ri: e.tensor_copy(out=CT[:, ri, :, :], in_=PS[1][0:64, :].rearrange("p (g c) -> p g c", c=16)),
                      reads=[PB[1]], writes=[b_CT])
            S.add("dve", lambda e: e.tensor_scalar(out=CT[:, 2, :, :], in0=CT[:, 1, :, :], scalar1=-1.0, scalar2=None, op0=ALU.mult),
                  reads=[b_CT], writes=[b_CT])

            def sv(i):
                return sm[:, i, :]

            def dv(fn, **kw):
                S.add("dve", lambda e: getattr(e, fn)(**kw), reads=[b_sm, b_pw], writes=[b_sm, b_pw])

            def av(**kw):
                S.add("act", lambda e: e.activation(**kw), reads=[b_sm, b_pw], writes=[b_sm, b_pw])

            av(out=sv(DT), in_=sv(DT), func=AF.Exp)
            dv("tensor_tensor", out=sv(T0), in0=sv(LR), in1=sv(DT), op=ALU.mult)
            av(out=sv(T0), in_=sv(T0), func=AF.Exp, scale=1.0 / 16)
            dv("tensor_tensor", out=sv(T1), in0=sv(LI), in1=sv(DT), op=ALU.mult)
            av(out=sv(AI), in_=sv(T1), func=AF.Sin, scale=1.0 / 16)
            av(out=sv(AR), in_=sv(T1), func=AF.Sin, scale=1.0 / 16, bias=halfpi[:, 0:1])
            dv("tensor_tensor", out=sv(AR), in0=sv(AR), in1=sv(T0), op=ALU.mult)
            dv("tensor_tensor", out=sv(AI), in0=sv(AI), in1=sv(T0), op=ALU.mult)
            for _ in range(4):
                dv("tensor_tensor", out=sv(T0), in0=sv(AR), in1=sv(AR), op=ALU.mult)
                dv("tensor_tensor", out=sv(T1), in0=sv(AI), in1=sv(AI), op=ALU.mult)
                dv("tensor_tensor", out=sv(T2), in0=sv(AR), in1=sv(AI), op=ALU.mult)
                dv("tensor_tensor", out=sv(AR), in0=sv(T0), in1=sv(T1), op=ALU.subtract)
                dv("tensor_scalar", out=sv(AI), in0=sv(T2), scalar1=2.0, scalar2=None, op0=ALU.mult)
            dv("tensor_tensor", out=sv(T0), in0=sv(LR), in1=sv(LR), op=ALU.mult)
            dv("tensor_tensor", out=sv(T1), in0=sv(LI), in1=sv(LI), op=ALU.mult)
            dv("tensor_tensor", out=sv(DEN), in0=sv(T0), in1=sv(T1), op=ALU.add)
            dv("reciprocal", out=sv(DEN), in_=sv(DEN))
            dv("tensor_scalar", out=sv(NR), in0=sv(AR), scalar1=-1.0, scalar2=None, op0=ALU.add)
            dv("tensor_tensor", out=sv(T0), in0=sv(NR), in1=sv(LR), op=ALU.mult)
            dv("tensor_tensor", out=sv(T1), in0=sv(AI), in1=sv(LI), op=ALU.mult)
            dv("tensor_tensor", out=sv(FR), in0=sv(T0), in1=sv(T1), op=ALU.add)
            dv("tensor_tensor", out=sv(FR), in0=sv(FR), in1=sv(DEN), op=ALU.mult)
            dv("tensor_tensor", out=sv(T0), in0=sv(AI), in1=sv(LR), op=ALU.mult)
            dv("tensor_tensor", out=sv(T1), in0=sv(NR), in1=sv(LI), op=ALU.mult)
            dv("tensor_tensor", out=sv(FI), in0=sv(T0), in1=sv(T1), op=ALU.subtract)
            dv("tensor_tensor", out=sv(FI), in0=sv(FI), in1=sv(DEN), op=ALU.mult)
            dv("memset", ap=pw[:, 0, 0, :], constant=1.0)
            dv("memset", ap=pw[:, 1, 0, :], constant=0.0)
            for t in range(8):
                dv("tensor_tensor", out=sv(T0), in0=pw[:, 0, t, :], in1=sv(AR), op=ALU.mult)
                dv("tensor_tensor", out=sv(T1), in0=pw[:, 1, t, :], in1=sv(AI), op=ALU.mult)
                dv("tensor_tensor", out=pw[:, 0, t + 1, :], in0=sv(T0), in1=sv(T1), op=ALU.subtract)
                dv("tensor_tensor", out=sv(T0), in0=pw[:, 0, t, :], in1=sv(AI), op=ALU.mult)
                dv("tensor_tensor", out=sv(T1), in0=pw[:, 1, t, :], in1=sv(AR), op=ALU.mult)
                dv("tensor_tensor", out=pw[:, 1, t + 1, :], in0=sv(T0), in1=sv(T1), op=ALU.add)
            dv("tensor_copy", out=P12[:, 0, 0:32], in_=pw[:, 0, 8, :])
            dv("tensor_copy", out=P12[:, 0, 32:64], in_=pw[:, 0, 8, :])
            dv("tensor_scalar", out=P12[:, 1, 0:32], in0=pw[:, 1, 8, :], scalar1=-1.0, scalar2=None, op0=ALU.mult)
            dv("tensor_copy", out=P12[:, 1, 32:64], in_=pw[:, 1, 8, :])
            dv("memset", ap=apw[:, 0, 0, :], constant=1.0)
            dv("memset", ap=apw[:, 1, 0, :], constant=0.0)
            for t in range(8):
                dv("tensor_tensor", out=sv(T0), in0=apw[:, 0, t, :], in1=pw[:, 0, 8, :], op=ALU.mult)
                dv("tensor_tensor", out=sv(T1), in0=apw[:, 1, t, :], in1=pw[:, 1, 8, :], op=ALU.mult)
                dv("tensor_tensor", out=apw[:, 0, t + 1, :], in0=sv(T0), in1=sv(T1), op=ALU.subtract)
                dv("tensor_tensor", out=sv(T0), in0=apw[:, 0, t, :], in1=pw[:, 1, 8, :], op=ALU.mult)
                dv("tensor_tensor", out=sv(T1), in0=apw[:, 1, t, :], in1=pw[:, 0, 8, :], op=ALU.mult)
                dv("tensor_tensor", out=apw[:, 1, t + 1, :], in0=sv(T0), in1=sv(T1), op=ALU.add)
            for i in range(9):
                dv("tensor_copy", out=PP[:, i, 0, 0:32], in_=apw[:, 0, i, :])
                dv("tensor_copy", out=PP[:, i, 0, 32:64], in_=apw[:, 0, i, :])
                dv("tensor_scalar", out=PP[:, i, 1, 0:32], in0=apw[:, 1, i, :], scalar1=-1.0, scalar2=None, op0=ALU.mult)
                dv("tensor_copy", out=PP[:, i, 1, 32:64], in_=apw[:, 1, i, :])

            def bcast_g(ap2):
                return ap2.unsqueeze(2).broadcast_to([64, 32, 16])

            def big(fn, reads, writes, **kw):
                S.add("dve", lambda e: getattr(e, fn)(**kw), reads=reads, writes=writes)

            RB = [b_sm, b_pw, b_Bn, b_Bb, b_tmp, b_CT]
            big("tensor_tensor", RB, [b_tmp], out=tmpB[:, 0], in0=Bn[:, 0], in1=bcast_g(sv(FR)), op=ALU.mult)
            big("tensor_tensor", RB, [b_tmp], out=tmpB[:, 1], in0=Bn[:, 1], in1=bcast_g(sv(FI)), op=ALU.mult)
            big("tensor_tensor", RB, [b_Bb], out=Bb[:, 0], in0=tmpB[:, 0], in1=tmpB[:, 1], op=ALU.subtract)
            big("tensor_tensor", RB, [b_tmp], out=tmpB[:, 0], in0=Bn[:, 1], in1=bcast_g(sv(FR)), op=ALU.mult)
            big("tensor_tensor", RB, [b_tmp], out=tmpB[:, 1], in0=Bn[:, 0], in1=bcast_g(sv(FI)), op=ALU.mult)
            big("tensor_tensor", RB, [b_Bb], out=Bb[:, 1], in0=tmpB[:, 0], in1=tmpB[:, 1], op=ALU.add)
            if "dbg_s5w" in C.debug:
                C.dbg_s5w = nc.dram_tensor("dbg_s5w", [64, 4 * 32 + 2 * 512], F32, kind="ExternalOutput").ap()
                dma(S, "sp", C.dbg_s5w[:, 0:32], pw[:, 0, 1, :], reads=[b_pw])
                dma(S, "sp", C.dbg_s5w[:, 32:64], pw[:, 1, 1, :], reads=[b_pw])
                dma(S, "sp", C.dbg_s5w[:, 64:96], pw[:, 0, 8, :], reads=[b_pw])
                dma(S, "sp", C.dbg_s5w[:, 96:128], pw[:, 1, 8, :], reads=[b_pw])
                dma(S, "sp", C.dbg_s5w[:, 128:640], Bb[:, 0].rearrange("p g c -> p (g c)"), reads=[b_Bb])
                dma(S, "sp", C.dbg_s5w[:, 640:1152], Bb[:, 1].rearrange("p g c -> p (g c)"), reads=[b_Bb])
            S.add("pool", lambda e: e.memset(Ere[:], 0.0), writes=[b_E])
            S.add("pool", lambda e: e.memset(Eim[:], 0.0), writes=[b_E])
            for m in range(8):
                t = 7 - m
                pr, pi = bcast_g(pw[:, 0, t, :]), bcast_g(pw[:, 1, t, :])
                big("tensor_tensor", RB, [b_tmp], out=tmpB[:, 0], in0=Bb[:, 0], in1=pr, op=ALU.mult)
                big("tensor_tensor", RB, [b_tmp], out=tmpB[:, 1], in0=Bb[:, 1], in1=pi, op=ALU.mult)
                big("tensor_tensor", RB + [b_E], [b_E], out=Ere[:, :, m, :], in0=tmpB[:, 0], in1=tmpB[:, 1], op=ALU.subtract)
                big("tensor_tensor", RB, [b_tmp], out=tmpB[:, 0], in0=Bb[:, 0], in1=pi, op=ALU.mult)
                big("tensor_tensor", RB, [b_tmp], out=tmpB[:, 1], in0=Bb[:, 1], in1=pr, op=ALU.mult)
                big("tensor_tensor", RB + [b_E], [b_E], out=Eim[:, :, m, :], in0=tmpB[:, 0], in1=tmpB[:, 1], op=ALU.add)
            Ctr4 = Ctr[:].rearrange("p g (j c) -> p g j c", c=16)
            Cti4 = Cti[:].rearrange("p g (j c) -> p g j c", c=16)
            for j in range(8):
                pr, pi = bcast_g(pw[:, 0, j + 1, :]), bcast_g(pw[:, 1, j + 1, :])
                big("tensor_tensor", RB, [b_tmp], out=tmpB[:, 0], in0=CT[:, 0], in1=pr, op=ALU.mult)
                big("tensor_tensor", RB, [b_tmp], out=tmpB[:, 1], in0=CT[:, 1], in1=pi, op=ALU.mult)
                big("tensor_tensor", RB + [b_wt], [b_wt], out=Ctr4[:, :, j, :], in0=tmpB[:, 0], in1=tmpB[:, 1], op=ALU.subtract)
                big("tensor_tensor", RB, [b_tmp], out=tmpB[:, 0], in0=CT[:, 0], in1=pi, op=ALU.mult)
                big("tensor_tensor", RB, [b_tmp], out=tmpB[:, 1], in0=CT[:, 2], in1=pr, op=ALU.mult)
                big("tensor_tensor", RB + [b_wt], [b_wt], out=Cti4[:, :, j, :], in0=tmpB[:, 1], in1=tmpB[:, 0], op=ALU.subtract)
            rot = 0
            for g0 in range(0, 32, 4):
                for ri, (Et, Bt) in enumerate(((Ere, Btr), (Eim, Bti))):
                    bank = 2 + (rot % 2)
                    rot += 1
                    for gg in range(4):
                        g = g0 + gg
                        S.add("pe", lambda e, Et=Et, g=g, gg=gg, bank=bank: e.transpose(
                            out=PS[bank][:, gg * 64:(gg + 1) * 64], in_=Et[:, g, 0:8, :].rearrange("p m c -> p (m c)"), identity=ident_f[0:64, 0:64]),
                            reads=[b_E, bc], writes=[PB[bank]])
                    S.add("dve", lambda e, Bt=Bt, g0=g0, bank=bank: e.tensor_copy(out=Bt[:, g0:g0 + 4, :],
                                                                             in_=PS[bank][:, 0:256].rearrange("p (g q) -> p g q", q=64)),
                          reads=[PB[bank]], writes=[b_wt])
                bank = 4 + ((g0 // 4) % 2)
                for gg in range(4):
                    g = g0 + gg
                    for j in range(8):
                        o = PS[bank][:, gg * 128 + j * 16: gg * 128 + (j + 1) * 16]
                        S.add("pe", lambda e, o=o, g=g, j=j: e.matmul(o, lhsT=Ere[:, g, 7 - j:15 - j, :].rearrange("p m c -> p (m c)"), rhs=CT[:, 0, g, :],
                                                                    start=True, stop=False),
                              reads=[b_E, b_CT], writes=[PB[bank]])
                        S.add("pe", lambda e, o=o, g=g, j=j: e.matmul(o, lhsT=Eim[:, g, 7 - j:15 - j, :].rearrange("p m c -> p (m c)"), rhs=CT[:, 2, g, :],
                                                                    start=False, stop=True),
                              reads=[b_E, b_CT], writes=[PB[bank]])
                S.add("act", lambda e, g0=g0, bank=bank: e.activation(out=Dt[:, g0:g0 + 4, :], in_=PS[bank][:].rearrange("p (g q) -> p g q", q=128), func=AF.Copy),
                      reads=[PB[bank]], writes=[b_wt])
            S.flush()
        if "stop_s5w" in C.debug:
            return
        with ExitStack() as _es:
            Uc = _es.enter_context(nc.sbuf_tensor("Uc", [128, 8, 512], F32))
            UT = _es.enter_context(nc.sbuf_tensor("UT", [128, 32, 128], BF16))
            Ug = _es.enter_context(nc.sbuf_tensor("Ug", [128, 32, 128], BF16))
            b_Ug = Buf("Ug")
            Xh = _es.enter_context(nc.sbuf_tensor("Xh", [64, 129, 64], F32))
            st = _es.enter_context(nc.sbuf_tensor("st", [64, 2, 64], F32))
            stw = [_es.enter_context(nc.sbuf_tensor(f"stw{i}", [64, 2, 16, 64], F32)) for i in range(2)]
            b_stw = [[Buf("stw00"), Buf("stw01")], [Buf("stw10"), Buf("stw11")]]
            Yc = _es.enter_context(nc.sbuf_tensor("Yc", [128, 8, 512], F32))
            Gc = _es.enter_context(nc.sbuf_tensor("Gc", [128, 8, 512], BF16))
            geT = _es.enter_context(nc.sbuf_tensor("geT", [128, 4, 1024], BF16))
            y2s = _es.enter_context(nc.sbuf_tensor("y2s", [128, 4, 1024], BF16))
            sig = _es.enter_context(nc.sbuf_tensor("sig", [128, 512], BF16))
            b_Uc, b_UT, b_Xh, b_st, b_Yc, b_Gc, b_geT, b_y2s, b_sig, b_st1 = [Buf(n) for n in "Uc UT Xh st Yc Gc geT y2s sig st1".split()]
            s5v = W["s5S"].rearrange("(c i) f -> c (i f)", i=8)
            S.add("dve", lambda e: e.memset(Xh[:, 0, :], 0.0), writes=[b_Xh])
            prev_rest = []
            sig2 = [sig, _es.enter_context(nc.sbuf_tensor("sigB", [128, 512], BF16))]
            b_sig2 = [Buf("sig0"), Buf("sig1")]
            for T in range(4):
                dma(S, "sp", Uc[:].rearrange("p i f -> p (i f)"), s5v[T * 128:(T + 1) * 128, :], writes=[b_Uc])
                S.add("pool", lambda e: e.tensor_copy(out=Ug[:].rearrange("p g (i c) -> p g i c", c=16), in_=view(Uc, 0, [[16, 32], [512, 8], [1, 16]])),
                      reads=[b_Uc], writes=[b_Ug])
                for g0 in range(0, 32, 8):
                    bank = (g0 // 8) % 2
                    pb = PS[bank][:].bitcast(BF16)
                    for gg in range(8):
                        g = g0 + gg
                        S.add("pe", lambda e, g=g, gg=gg, pb=pb: e.transpose(out=pb[:, gg * 128:(gg + 1) * 128], in_=Ug[:, g, :], identity=ident_b[:]),
                              reads=[b_Ug, bc], writes=[PB[bank]])
                    S.add("act", lambda e, g0=g0, pb=pb: e.activation(out=UT[:, g0:g0 + 8, :], in_=pb[:, 0:1024].rearrange("p (g q) -> p g q", q=128), func=AF.Copy),
                          reads=[PB[bank]], writes=[b_UT])
                for g0 in range(0, 32, 4):
                    for ri, Bt in enumerate((Btr, Bti)):
                        bank = 2 + ri
                        for gg in range(4):
                            g = g0 + gg
                            S.add("pe", lambda e, Bt=Bt, g=g, gg=gg, bank=bank: e.matmul(PS[bank][0:64, gg * 128:(gg + 1) * 128], lhsT=Bt[:, g, :], rhs=UT[:, g, :],
                                                                                    start=True, stop=True),
                                  reads=[b_wt, b_UT], writes=[PB[bank]])
                        o = view(Xh, 64 + ri * 32 + g0, [[1, 4], [64, 128]], parts=64)
                        S.add("dve", lambda e, o=o, bank=bank: e.tensor_copy(out=o, in_=PS[bank][0:64, :].rearrange("p (g c) -> p g c", c=128)),
                              reads=[PB[bank]], writes=[b_Xh])
                if len(prev_rest) > 0:
                    prev_rest.pop()()
                def cmul_acc(dst0, src0, nb, i, k):
                    dst = view(Xh, dst0 * 64, [[512, nb], [1, 64]], parts=64)
                    src = view(Xh, src0 * 64, [[512, nb], [1, 64]], parts=64)
                    srcw = view(Xh, src0 * 64 + 32, [[512, nb], [-32, 2], [1, 32]], parts=64)
                    t1 = stw[k][:, 0, 0:nb, :]
                    t2 = stw[k][:, 1, 0:nb, :]
                    S.add("dve", lambda e: e.tensor_tensor(out=t1, in0=src, in1=PP[:, i, 0, :].unsqueeze(1).broadcast_to([64, nb, 64]), op=ALU.mult),
                          reads=[b_Xh], writes=[b_stw[k][0]])
                    S.add("dve", lambda e: e.tensor_tensor(out=t2.rearrange("p b (a c) -> p b a c", a=2), in0=srcw,
                                                           in1=PP[:, i, 1, :].rearrange("p (a c) -> p a c", a=2).unsqueeze(1).broadcast_to([64, nb, 2, 32]), op=ALU.mult),
                          reads=[b_Xh], writes=[b_stw[k][1]])
                    S.add("dve", lambda e: e.tensor_tensor(out=t1, in0=t1, in1=t2, op=ALU.add), reads=[b_stw[k][0], b_stw[k][1]], writes=[b_stw[k][0]])
                    S.add("dve", lambda e: e.tensor_tensor(out=dst, in0=dst, in1=t1, op=ALU.add), reads=[b_stw[k][0], b_Xh], writes=[b_Xh])

                if "s5_noscan" not in C.debug:
                    for i in range(1, 8):
                        cmul_acc(i + 1, i, 16, 1, i % 2)
                    for b in range(16):
                        cmul_acc(b * 8 + 8, b * 8, 1, 8, b % 2)
                    for i in range(1, 8):
                        cmul_acc(i, 0, 16, i, i % 2)
                if "s5_noY" in C.debug:
                    continue
                S.add("pool", lambda e: e.tensor_tensor(out=Yc[:], in0=Uc[:], in1=dbc[:].unsqueeze(1).broadcast_to([128, 8, 512]), op=ALU.mult),
                      reads=[b_Uc, b_dbc], writes=[b_Yc])
                for g0 in range(0, 32, 4):
                    bank = 4 + (g0 // 4) % 2
                    for gg in range(4):
                        g = g0 + gg
                        o = PS[bank][:, gg * 128:(gg + 1) * 128]
                        S.add("pe", lambda e, o=o, g=g: e.matmul(o, lhsT=UT[:, g, :], rhs=Dt[:, g, :], start=True, stop=False),
                              reads=[b_UT, b_wt], writes=[PB[bank]])
                        xr = view(Xh, g, [[64, 128]], parts=64)
                        xi = view(Xh, 32 + g, [[64, 128]], parts=64)
                        S.add("pe", lambda e, o=o, g=g, xr=xr: e.matmul(o, lhsT=xr, rhs=Ctr[:, g, :], start=False, stop=False),
                              reads=[b_Xh, b_wt], writes=[PB[bank]])
                        S.add("pe", lambda e, o=o, g=g, xi=xi: e.matmul(o, lhsT=xi, rhs=Cti[:, g, :], start=False, stop=True),
                              reads=[b_Xh, b_wt], writes=[PB[bank]])
                    yv = view(Yc, g0 * 16, [[16, 4], [512, 8], [1, 16]])
                    S.add("dve", lambda e, yv=yv, bank=bank: e.tensor_tensor(out=yv, in0=PS[bank][:].rearrange("p (g j c) -> p g j c", g=4, j=8), in1=yv, op=ALU.add),
                          reads=[PB[bank], b_Yc], writes=[b_Yc])
                if T < 3:
                    S.add("dve", lambda e: e.tensor_copy(out=Xh[:, 0, :], in_=Xh[:, 128, :]), reads=[b_Xh], writes=[b_Xh])
                if "dbg_s5y" in C.debug:
                    for i in range(8):
                        dma(S, "sp", C.dbg_s5y.rearrange("(c i) f -> c i f", i=8)[T * 128:(T + 1) * 128, i, :], Yc[:, i, :], reads=[b_Yc])
                def rest(T=T):
                    for i in range(8):
                        S.add("act", lambda e, i=i: e.activation(out=Gc[:, i, :], in_=Yc[:, i, :], func=AF.Gelu_apprx_tanh), reads=[b_Yc], writes=[b_Gc])
                    for ct in range(4):
                        bank = 6 + (ct % 2)
                        pb = PS[bank][:].bitcast(BF16)
                        for j in range(8):
                            S.add("pe", lambda e, pb=pb, j=j, ct=ct: e.transpose(out=pb[:, j * 128:(j + 1) * 128], in_=Gc[:, j, ct * 128:(ct + 1) * 128], identity=ident_b[:]),
                                  reads=[b_Gc, bc], writes=[PB[bank]])
                        o = view(geT, ct * 1024, [[1, 8], [8, 128]])
                        S.add("act", lambda e, o=o, pb=pb: e.activation(out=o, in_=pb[:, 0:1024].rearrange("p (j c) -> p j c", j=8), func=AF.Copy), reads=[PB[bank]], writes=[b_geT])
                    for co in range(4):
                        for hh in range(2):
                            q2 = (co * 2 + hh) % 2
                            bank = 6 + q2
                            for ci in range(4):
                                S.add("pe", lambda e, co=co, hh=hh, ci=ci, bank=bank: e.matmul(PS[bank][:], lhsT=wglu[:, ci, co * 128:(co + 1) * 128],
                                                                                          rhs=geT[:, ci, hh * 512:(hh + 1) * 512], start=(ci == 0), stop=(ci == 3)),
                                      reads=[b_wglu, b_geT], writes=[PB[bank]])
                            S.add("act", lambda e, co=co, bank=bank, q2=q2: e.activation(out=sig2[q2][:], in_=PS[bank][:], func=AF.Sigmoid,
                                                                                     bias=cols[:, COL_BGLU + co:COL_BGLU + co + 1], scale=1.0),
                                  reads=[PB[bank], C.b_cols], writes=[b_sig2[q2]])
                            S.add("pool", lambda e, co=co, hh=hh, q2=q2: e.tensor_tensor(out=y2s[:, co, hh * 512:(hh + 1) * 512], in0=geT[:, co, hh * 512:(hh + 1) * 512],
                                                                                      in1=sig2[q2][:], op=ALU.mult),
                                  reads=[b_geT, b_sig2[q2]], writes=[b_y2s])
                    dma(S, "sp", W["y2T"][:, T * 1024:(T + 1) * 1024].rearrange("(c p) n -> p c n", p=128), y2s[:], reads=[b_y2s])

                prev_rest.append(rest)
            while prev_rest:
                prev_rest.pop()()
            S.flush()


_dummy = {}


def b_sm_dummy(C):
    if "b" not in _dummy:
        _dummy["b"] = Buf("dummy")
    return _dummy["b"]


def phase3(C):
    nc, S, I, W = C.nc, C.S, C.I, C.W
    PS, PB = C.PS, C.PB
    cols = C.cols
    bc = C.b_const
    NQ = 8
    heads = C.heads if hasattr(C, "heads") else range(8)
    with ExitStack() as _es:
        sb = lambda name, shape, dt: _es.enter_context(nc.sbuf_tensor(name, shape, dt))
        lqk = sb("lqk", [128, 4, 64], F32)
        lsc = sb("lsc", [128, 8], F32)
        masks = sb("masks", [128, 4, 512], BF16)
        kTh = [sb(f"kTh{i}", [128, L], BF16) for i in range(2)]
        qTh = [sb(f"qTh{i}", [128, L], BF16) for i in range(2)]
        Vh = [sb(f"Vh{i}", [128, 32, 128], BF16) for i in range(2)]
        Et = sb("Et", [128, 8, 512], BF16)
        rr = sb("rr", [128, 2, 512], F32)
        oo = sb("oo", [128, 2, 512], F32)
        sq = sb("sq", [128, 512], BF16)
        ons = [sb(f"ons{i}", [128, 512], BF16) for i in range(2)]
        b_l, b_mask = Buf("lam"), Buf("mask")
        b_k = [Buf("k0"), Buf("k1")]
        b_q = [Buf("q0"), Buf("q1")]
        b_v = [Buf("v0"), Buf("v1")]
        b_E = [Buf(f"E{i}") for i in range(8)]
        b_rr, b_oo, b_sq = Buf("rr"), Buf("oo"), Buf("sq")
        b_ons = [Buf("ons0"), Buf("ons1")]
        for i, n in enumerate(["lambda_q1", "lambda_k1", "lambda_q2", "lambda_k2"]):
            dma(S, "sp", lqk[:, i, :], I[n].partition_broadcast(128), writes=[b_l])
        S.add("dve", lambda e: e.tensor_tensor(out=lqk[:, 0, :], in0=lqk[:, 0, :], in1=lqk[:, 1, :], op=ALU.mult), reads=[b_l], writes=[b_l])
        S.add("dve", lambda e: e.tensor_tensor(out=lqk[:, 2, :], in0=lqk[:, 2, :], in1=lqk[:, 3, :], op=ALU.mult), reads=[b_l], writes=[b_l])
        S.add("dve", lambda e: e.reduce_sum(out=lsc[:, 0:1], in_=lqk[:, 0, :], axis=AX.X), reads=[b_l], writes=[b_l])
        S.add("dve", lambda e: e.reduce_sum(out=lsc[:, 1:2], in_=lqk[:, 2, :], axis=AX.X), reads=[b_l], writes=[b_l])
        S.add("act", lambda e: e.activation(out=lsc[:, 2:4], in_=lsc[:, 0:2], func=AF.Exp), reads=[b_l], writes=[b_l])
        S.add("dve", lambda e: e.tensor_tensor(out=lsc[:, 4:5], in0=lsc[:, 3:4], in1=lsc[:, 2:3], op=ALU.subtract), reads=[b_l], writes=[b_l])
        S.add("dve", lambda e: e.tensor_scalar(out=lsc[:, 5:6], in0=lsc[:, 4:5], scalar1=-LAMBDA_INIT, scalar2=None, op0=ALU.add), reads=[b_l], writes=[b_l])
        nlam = lsc[:, 5:6]
        S.add("pool", lambda e: e.memset(masks[:], 0.0), writes=[b_mask])
        for r in range(4):
            S.add("pool", lambda e, r=r: e.memset(masks[0:64, r, 128 * r:512], 1.0), writes=[b_mask])
            S.add("pool", lambda e, r=r: e.memset(masks[64:128, r, 128 * r + 64:512], 1.0), writes=[b_mask])

        def load_head(h, slot):
            dma(S, "sp", kTh[slot][:], W["kT"][h * 128:(h + 1) * 128, :], writes=[b_k[slot]])
            dma(S, "sp", qTh[slot][:], W["qT"][h * 128:(h + 1) * 128, :], writes=[b_q[slot]])
            dma(S, "sp", Vh[slot][:], W["vS"][:, h * 128:(h + 1) * 128].rearrange("(j p) e -> p j e", p=128), writes=[b_v[slot]])

        hl = list(heads)
        load_head(hl[0], 0)
        ecnt = 0
        ocnt = 0
        lnt = sb("lnt", [128, 2, 512], F32)
        b_ln = Buf("lnt")
        wst = [sb(f"wst{i}", [128, 6144], BF16) for i in range(2)]
        b_wst = [Buf("wst0"), Buf("wst1")]

        def precast_load(ex):
            sl = ex % 2
            dma(S, "pool", wst[sl][:, 0:2048].rearrange("p (k f) -> p k f", f=256), I["w_exp_gate"][ex].rearrange("(k p) f -> p k f", p=128), writes=[b_wst[sl]])
            dma(S, "pool", wst[sl][:, 2048:4096].rearrange("p (k f) -> p k f", f=256), I["w_exp_up"][ex].rearrange("(k p) f -> p k f", p=128), writes=[b_wst[sl]])
            dma(S, "pool", wst[sl][:, 4096:6144].rearrange("p (k d) -> p k d", d=D), I["w_exp_down"][ex].rearrange("(k p) d -> p k d", p=128), writes=[b_wst[sl]])

        def precast_store(ex):
            sl = ex % 2
            for c3 in range(3):
                dma(S, "sp", W["WALL"][ex * 128:(ex + 1) * 128, c3 * 2048:(c3 + 1) * 2048], wst[sl][:, c3 * 2048:(c3 + 1) * 2048], reads=[b_wst[sl]])

        unit = 0
        e2cnt = 0
        EPI_STEP = int(C.epi_step) if hasattr(C, "epi_step") else 8
        Es2 = sb("Es2", [128, 4, 512], BF16)
        b_E2 = [Buf(f"E2_{i}") for i in range(4)]
        pending = []

        def epi_fast(h, Q):
            S.add("act", lambda e: e.activation(out=oo[:, 0, :], in_=PS[4][:], func=AF.Copy), reads=[PB[4]], writes=[b_oo])
            S.add("act", lambda e: e.activation(out=oo[:, 1, :], in_=PS[5][:], func=AF.Copy), reads=[PB[5]], writes=[b_oo])
            S.add("dve", lambda e: e.tensor_copy(out=lnt[:, 0, :], in_=PS[6][:]), reads=[PB[6]], writes=[b_ln])
            S.add("dve", lambda e: e.tensor_copy(out=lnt[:, 1, :], in_=PS[7][:]), reads=[PB[7]], writes=[b_ln])
            S.add("dve", lambda e: e.reciprocal(out=rr[:, 0, :], in_=lnt[:, 1, :]), reads=[b_ln], writes=[b_rr])
            S.add("dve", lambda e: e.tensor_tensor(out=rr[:, 0, :], in0=rr[:, 0, :], in1=lnt[:, 0, :], op=ALU.mult), reads=[b_rr, b_ln], writes=[b_rr])
            S.add("dve", lambda e: e.tensor_tensor(out=oo[:, 1, :], in0=oo[:, 1, :], in1=rr[:, 0, :], op=ALU.mult), reads=[b_oo, b_rr], writes=[b_oo])
            S.add("dve", lambda e: e.scalar_tensor_tensor(out=oo[:, 0, :], in0=oo[:, 1, :], scalar=nlam, in1=oo[:, 0, :], op0=ALU.mult, op1=ALU.add),
                  reads=[b_oo, b_l], writes=[b_oo])
            S.add("pool", lambda e: e.tensor_tensor(out=sq[:], in0=oo[:, 0, :], in1=oo[:, 0, :], op=ALU.mult), reads=[b_oo], writes=[b_sq])
            S.add("dve", lambda e: e.scalar_tensor_tensor(out=rr[:, 1, :], in0=lnt[:, 0, :], scalar=EPS, in1=lnt[:, 0, :], op0=ALU.mult, op1=ALU.mult),
                  reads=[b_ln], writes=[b_rr])

        def epi_slow(h, Q):
            nonlocal ocnt
            S.add("pe", lambda e: e.matmul(PS[0][:], lhsT=C.ones_b[:], rhs=sq[:], start=True, stop=True), reads=[bc, b_sq], writes=[PB[0]])
            S.add("dve", lambda e: e.scalar_tensor_tensor(out=rr[:, 1, :], in0=PS[0][:], scalar=1.0 / 128, in1=rr[:, 1, :], op0=ALU.mult, op1=ALU.add),
                  reads=[PB[0], b_rr], writes=[b_rr])
            S.add("act", lambda e: e.activation(out=rr[:, 1, :], in_=rr[:, 1, :], func=AF.Ln), reads=[b_rr], writes=[b_rr])
            S.add("act", lambda e: e.activation(out=rr[:, 1, :], in_=rr[:, 1, :], func=AF.Exp, scale=-0.5), reads=[b_rr], writes=[b_rr])
            on = ons[ocnt % 2]
            bo = b_ons[ocnt % 2]
            ocnt += 1
            S.add("dve", lambda e, on=on: e.scalar_tensor_tensor(out=on[:], in0=oo[:, 0, :], scalar=cols[:, COL_GSUBS:COL_GSUBS + 1], in1=rr[:, 1, :],
                                                              op0=ALU.mult, op1=ALU.mult),
                  reads=[b_oo, b_rr, C.b_cols], writes=[bo])
            dma(S, "sp", W["onT"][h * 128:(h + 1) * 128, Q * 512:(Q + 1) * 512], on[:], reads=[bo])

        for hi, h in enumerate(hl):
            slot = hi % 2
            if hi + 1 < len(hl):
                load_head(hl[hi + 1], (hi + 1) % 2)
            kt, qt, vt = kTh[slot], qTh[slot], Vh[slot]
            bk, bq, bv = b_k[slot], b_q[slot], b_v[slot]
            for Q in range(NQ):
                nJ = 4 * (Q + 1)
                eslot = {}
                lready, lnext = [], []
                if C.sparse:
                    if 1 <= unit <= NEXP:
                        precast_store(unit - 1)
                    if unit < NEXP:
                        precast_load(unit)
                    if unit == NEXP + 1:
                        dma(S, "pool", C.wbs[:], I["w_br_ssm"].rearrange("(k p) n -> p k n", p=128), writes=[C.b_w4])
                        dma(S, "pool", C.wba[:], I["w_br_attn"].rearrange("(k p) n -> p k n", p=128), writes=[C.b_w4])
                        dma(S, "pool", C.wout[:], I["w_out"].rearrange("(k p) n -> p k n", p=128), writes=[C.b_w4])
                        C.w4_loaded = True
                    unit += 1
                for step in range(nJ + 1):
                    if step < nJ:
                        J = step
                        c0 = 0
                        for m in range(2):
                            bank = 2 * m + (J % 2)
                            S.add("pe", lambda e, bank=bank, m=m, J=J, Q=Q, kt=kt, qt=qt, c0=c0: e.matmul(
                                PS[bank][:, c0:512], lhsT=kt[m * 64:(m + 1) * 64, J * 128:(J + 1) * 128], rhs=qt[m * 64:(m + 1) * 64, Q * 512 + c0:(Q + 1) * 512],
                                start=True, stop=True), reads=[bk, bq], writes=[PB[bank]])
                            es = ecnt % 8
                            ecnt += 1
                            eslot[(J, m)] = es
                            S.add("act", lambda e, bank=bank, es=es, c0=c0: e.activation(out=Et[:, es, c0:512], in_=PS[bank][:, c0:512], func=AF.Exp, scale=0.125),
                                  reads=[PB[bank]], writes=[b_E[es]])
                            if J >= 4 * Q:
                                r = J - 4 * Q
                                S.add("dve", lambda e, es=es, r=r: e.tensor_tensor(out=Et[:, es, :], in0=Et[:, es, :], in1=masks[:, r, :], op=ALU.mult),
                                      reads=[b_E[es], b_mask], writes=[b_E[es]])
                    if step >= 1:
                        J = step - 1
                        c0 = 0
                        for m in range(2):
                            es = eslot[(J, m)]
                            S.add("pe", lambda e, m=m, J=J, es=es, vt=vt, nJ=nJ, c0=c0: e.matmul(PS[4 + m][:, c0:512], lhsT=vt[:, J, :], rhs=Et[:, es, c0:512],
                                                                                     start=(J == 0), stop=(J == nJ - 1)),
                                  reads=[bv, b_E[es]], writes=[PB[4 + m]])
                            S.add("pe", lambda e, m=m, J=J, es=es, nJ=nJ, c0=c0: e.matmul(PS[6 + m][:, c0:512], lhsT=C.ones_b[:], rhs=Et[:, es, c0:512],
                                                                              start=(J == 0), stop=(J == nJ - 1)),
                                  reads=[bc, b_E[es]], writes=[PB[6 + m]])
                    if step == min(EPI_STEP, nJ) and pending:
                        epi_slow(*pending.pop())
                epi_fast(h, Q)
                pending.append((h, Q))
        while pending:
            epi_slow(*pending.pop())
        S.flush()


def phase4a(C):
    nc, S, I, W = C.nc, C.S, C.I, C.W
    PS, PB = C.PS, C.PB
    cols = C.cols
    bc = C.b_const
    with ExitStack() as _es:
        sb = lambda name, shape, dt: _es.enter_context(nc.sbuf_tensor(name, shape, dt))
        wbs, wba, wout = C.wbs, C.wba, C.wout
        wr = sb("wr", [128, 8, 36], BF16)
        brt = sb("brt", [128, 36], F32)
        y2t = [sb(f"y2t{i}", [128, 4, 512], BF16) for i in range(2)]
        ont = [sb(f"ont{i}", [128, 8, 512], BF16) for i in range(2)]
        gtt = [sb(f"gtt{i}", [128, 16, 512], BF16) for i in range(2)]
        xt = sb("xt4", [128, 4, D], F32)
        mT = sb("mT", [128, 8, 512], BF16)
        tA = sb("tA", [128, 512], F32)
        tB = sb("tB", [128, 512], F32)
        hh_ = sb("h4", [128, 4, D], F32)
        xn2 = sb("xn2", [128, 4, D], BF16)
        junk = sb("junk4", [128, D], BF16)
        u2 = [sb(f"u2_{i}", [128, 8, 512], BF16) for i in range(2)]
        ssq = sb("ssq4", [128, 8], F32)
        rt = sb("rt", [128, 4, 36], F32)
        rw = sb("rw", [128, 24, 4], F32)
        ohg = sb("ohg", [128, 4, 4], F32)
        tE = sb("tE", [128, 4, 4, 8], F32)
        es = sb("es", [128, 6, 4, 8], F32)
        comb = [sb(f"comb{i}", [128, 4, 32], F32) for i in range(2)]
        ustr_f = sb("ustr_f", [128, 128], F32)
        ustr = sb("ustr", [128, 128], BF16)
        ohgb = sb("ohgb", [128, 4, 4], BF16)
        base = sb("base4", [128, 4, 4], F32)
        b_ohgb, b_base, b_tot = Buf("ohgb"), Buf("base"), Buf("tot")
        b_w = Buf("w4")
        if C.sparse:
            S.add("pool", lambda e: e.memset(ustr_f[:], 1.0), writes=[b_ohgb])
            S.add("pool", lambda e: e.affine_select(out=ustr_f[:], in_=ustr_f[:], pattern=[[1, 128]], compare_op=ALU.is_gt, fill=0.0, base=0, channel_multiplier=-1),
                  reads=[b_ohgb], writes=[b_ohgb])
            S.add("dve", lambda e: e.tensor_copy(out=ustr[:], in_=ustr_f[:]), reads=[b_ohgb], writes=[b_ohgb])
            S.add("dve", lambda e: e.memset(C.tot[:], 0.0), writes=[b_tot])
        if not getattr(C, "w4_loaded", False):
            dma(S, "pool", wbs[:], I["w_br_ssm"].rearrange("(k p) n -> p k n", p=128), writes=[b_w])
            dma(S, "pool", wba[:], I["w_br_attn"].rearrange("(k p) n -> p k n", p=128), writes=[b_w])
            dma(S, "pool", wout[:], I["w_out"].rearrange("(k p) n -> p k n", p=128), writes=[b_w])
        dma(S, "pool", wr[:, :, 0:4], I["w_router_grp"].rearrange("(k p) n -> p k n", p=128), writes=[b_w])
        dma(S, "pool", wr[:, :, 4:36], I["w_router_exp"].rearrange("(k p) n -> p k n", p=128), writes=[b_w])
        dma(S, "sp", brt[:, 0:4], I["b_router_grp"].partition_broadcast(128), writes=[b_w])
        dma(S, "sp", brt[:, 4:36], I["b_router_exp"].partition_broadcast(128), writes=[b_w])
        b_y2 = [Buf("y2t0"), Buf("y2t1")]
        b_on = [Buf("ont0"), Buf("ont1")]
        b_gt = [Buf("gtt0"), Buf("gtt1")]
        b_u2 = [Buf("u2_0"), Buf("u2_1")]
        b_cb = [Buf("comb0"), Buf("comb1")]
        b_xt, b_mT, b_tA, b_tB, b_h, b_xn2, b_junk, b_ssq, b_rt, b_rw = [Buf(n) for n in "xt mT tA tB h xn2 junk ssq rt rw".split()]

        def load(t):
            sl = t % 2
            dma(S, "sp", y2t[sl][:], W["y2T"][:, t * 512:(t + 1) * 512].rearrange("(c p) n -> p c n", p=128), writes=[b_y2[sl]])
            dma(S, "sp", ont[sl][:], W["onT"][:, t * 512:(t + 1) * 512].rearrange("(c p) n -> p c n", p=128), writes=[b_on[sl]])
            dma(S, "sp", gtt[sl][:], W["gT"][:, t * 512:(t + 1) * 512].rearrange("(c p) n -> p c n", p=128), writes=[b_gt[sl]])

        load(0)
        rot = 0
        for t in range(NTT):
            sl = t % 2
            if t + 1 < NTT:
                load(t + 1)
            dma(S, "sp", xt[:], I["x"][t * 512:(t + 1) * 512, :].rearrange("(s p) d -> p s d", p=128), writes=[b_xt])
            for ct in range(8):
                ba = rot % 2
                bb = 2 + rot % 2
                rot += 1
                for k in range(4):
                    S.add("pe", lambda e, ba=ba, k=k, ct=ct, sl=sl: e.matmul(PS[ba][:], lhsT=wbs[:, k, ct * 128:(ct + 1) * 128], rhs=y2t[sl][:, k, :],
                                                                        start=(k == 0), stop=(k == 3)), reads=[b_w, b_y2[sl]], writes=[PB[ba]])
                for k in range(8):
                    S.add("pe", lambda e, bb=bb, k=k, ct=ct, sl=sl: e.matmul(PS[bb][:], lhsT=wba[:, k, ct * 128:(ct + 1) * 128], rhs=ont[sl][:, k, :],
                                                                        start=(k == 0), stop=(k == 7)), reads=[b_w, b_on[sl]], writes=[PB[bb]])
                S.add("dve", lambda e, ba=ba, ct=ct, sl=sl: e.tensor_tensor(out=tA[:], in0=PS[ba][:], in1=gtt[sl][:, ct, :], op=ALU.mult),
                      reads=[PB[ba], b_gt[sl]], writes=[b_tA])
                S.add("dve", lambda e, bb=bb, ct=ct, sl=sl: e.tensor_tensor(out=tB[:], in0=PS[bb][:], in1=gtt[sl][:, 8 + ct, :], op=ALU.mult),
                      reads=[PB[bb], b_gt[sl]], writes=[b_tB])
                S.add("pool", lambda e, ct=ct: e.tensor_tensor(out=mT[:, ct, :], in0=tA[:], in1=tB[:], op=ALU.add), reads=[b_tA, b_tB], writes=[b_mT])
            for s in range(4):
                for hf in range(2):
                    bk = 4 + rot % 2
                    rot += 1
                    for k in range(8):
                        S.add("pe", lambda e, bk=bk, k=k, s=s, hf=hf: e.matmul(PS[bk][:], lhsT=mT[:, k, s * 128:(s + 1) * 128], rhs=wout[:, k, hf * 512:(hf + 1) * 512],
                                                                          start=(k == 0), stop=(k == 7)), reads=[b_mT, b_w], writes=[PB[bk]])
                    S.add("dve", lambda e, bk=bk, hf=hf: e.tensor_tensor(out=tA[:], in0=PS[bk][:], in1=C.gtm_bc[:, hf * 512:(hf + 1) * 512], op=ALU.mult),
                          reads=[PB[bk], C.b_gt], writes=[b_tA])
                    S.add("pool", lambda e, s=s, hf=hf: e.tensor_tensor(out=hh_[:, s, hf * 512:(hf + 1) * 512], in0=tA[:], in1=xt[:, s, hf * 512:(hf + 1) * 512], op=ALU.add),
                          reads=[b_tA, b_xt], writes=[b_h])
            dma(S, "sp", W["hS"][t * 512:(t + 1) * 512, :].rearrange("(s p) d -> p s d", p=128), hh_[:], reads=[b_h])
            S.add("dve", lambda e: e.memset(ssq[:], 0.0), writes=[b_ssq])
            for s in range(4):
                S.add("act", lambda e, s=s: e.activation(out=junk[:], in_=hh_[:, s, :], func=AF.Square, accum_out=ssq[:, s:s + 1]),
                      reads=[b_h, b_ssq], writes=[b_junk, b_ssq])
            S.add("dve", lambda e: e.tensor_scalar(out=ssq[:, 4:8], in0=ssq[:, 0:4], scalar1=1.0 / D, scalar2=EPS, op0=ALU.mult, op1=ALU.add),
                  reads=[b_ssq], writes=[b_ssq])
            S.add("act", lambda e: e.activation(out=ssq[:, 4:8], in_=ssq[:, 4:8], func=AF.Sqrt), reads=[b_ssq], writes=[b_ssq])
            S.add("dve", lambda e: e.reciprocal(out=ssq[:, 4:8], in_=ssq[:, 4:8]), reads=[b_ssq], writes=[b_ssq])
            for s in range(4):
                S.add("dve", lambda e, s=s: e.tensor_scalar(out=xn2[:, s, :], in0=hh_[:, s, :], scalar1=ssq[:, 4 + s:5 + s], scalar2=None, op0=ALU.mult),
                      reads=[b_h, b_ssq], writes=[b_xn2])
            u2t = u2[sl]
            for k in range(8):
                bk = 6 + k % 2
                ptb = PS[bk][:].bitcast(BF16)
                for s in range(4):
                    S.add("pe", lambda e, s=s, k=k, ptb=ptb: e.transpose(out=ptb[:, s * 128:(s + 1) * 128], in_=xn2[:, s, k * 128:(k + 1) * 128], identity=C.ident_b[:]),
                          reads=[b_xn2, bc], writes=[PB[bk]])
                S.add("dve", lambda e, k=k, ptb=ptb, u2t=u2t: e.tensor_scalar(out=u2t[:, k, :], in0=ptb[:, 0:512], scalar1=cols[:, COL_G2 + k:COL_G2 + k + 1],
                                                                           scalar2=cols[:, COL_MOD + 24 + k:COL_MOD + 25 + k], op0=ALU.mult, op1=ALU.add),
                      reads=[PB[bk], C.b_cols], writes=[b_u2[sl]])
            dma(S, "sp", W["u2T"][:, t * 512:(t + 1) * 512].rearrange("(c p) n -> p c n", p=128), u2t[:], reads=[b_u2[sl]])
            bk = 4 + rot % 2
            rot += 1
            for s in range(4):
                for k in range(8):
                    S.add("pe", lambda e, bk=bk, k=k, s=s, u2t=u2t: e.matmul(PS[bk][:, s * 36:(s + 1) * 36], lhsT=u2t[:, k, s * 128:(s + 1) * 128], rhs=wr[:, k, :],
                                                                        start=(k == 0), stop=(k == 7)), reads=[b_u2[sl], b_w], writes=[PB[bk]])
            S.add("dve", lambda e, bk=bk: e.tensor_tensor(out=rt[:], in0=PS[bk][:, 0:144].rearrange("p (s n) -> p s n", n=36),
                                                         in1=brt[:].unsqueeze(1).broadcast_to([128, 4, 36]), op=ALU.add),
                  reads=[PB[bk], b_w], writes=[b_rt])
            RR = [b_rt, b_rw]

            def dv(fn, **kw):
                S.add("dve", lambda e: getattr(e, fn)(**kw), reads=RR, writes=[b_rw])

            def av(**kw):
                S.add("act", lambda e: e.activation(**kw), reads=RR, writes=[b_rw])

            G = rt[:, :, 0:4]
            E4 = rt[:, :, 4:36].rearrange("p s (g e) -> p s g e", e=8)
            w_ = lambda i: rw[:, i, :]
            b4 = lambda a: a.unsqueeze(2).broadcast_to([128, 4, 4])
            b8 = lambda a: a.unsqueeze(2).broadcast_to([128, 4, 8])
            GMAX, GSUM, GP, M1, M2, DD, ED, W1, W1G, W2G = range(10)
            dv("tensor_reduce", out=w_(GMAX), in_=G, axis=AX.X, op=ALU.max)
            dv("tensor_tensor", out=ohg[:], in0=G, in1=b4(w_(GMAX)), op=ALU.subtract)
            av(out=tE[:, 0, :, 0:4], in_=ohg[:], func=AF.Exp)
            dv("tensor_reduce", out=w_(GSUM), in_=tE[:, 0, :, 0:4], axis=AX.X, op=ALU.add)
            dv("reciprocal", out=w_(GP), in_=w_(GSUM))
            dv("tensor_tensor", out=ohg[:], in0=G, in1=b4(w_(GMAX)), op=ALU.is_equal)
            dv("tensor_tensor", out=tE[:], in0=E4, in1=ohg[:].unsqueeze(3).broadcast_to([128, 4, 4, 8]), op=ALU.mult)
            dv("tensor_reduce", out=es[:, 0], in_=tE[:].rearrange("p s g e -> p s e g"), axis=AX.X, op=ALU.add)
            dv("tensor_reduce", out=w_(M1), in_=es[:, 0], axis=AX.X, op=ALU.max)
            dv("tensor_tensor", out=es[:, 1], in0=es[:, 0], in1=b8(w_(M1)), op=ALU.is_equal)
            dv("scalar_tensor_tensor", out=es[:, 2], in0=es[:, 1], scalar=-1e30, in1=es[:, 0], op0=ALU.mult, op1=ALU.add)
            dv("tensor_reduce", out=w_(M2), in_=es[:, 2], axis=AX.X, op=ALU.max)
            dv("tensor_tensor", out=es[:, 3], in0=es[:, 2], in1=b8(w_(M2)), op=ALU.is_equal)
            dv("tensor_tensor", out=w_(DD), in0=w_(M2), in1=w_(M1), op=ALU.subtract)
            av(out=w_(ED), in_=w_(DD), func=AF.Exp)
            dv("tensor_scalar", out=w_(W1), in0=w_(ED), scalar1=1.0, scalar2=None, op0=ALU.add)
            dv("reciprocal", out=w_(W1), in_=w_(W1))
            dv("tensor_tensor", out=w_(W1G), in0=w_(W1), in1=w_(GP), op=ALU.mult)
            dv("tensor_tensor", out=w_(W2G), in0=w_(W1G), in1=w_(ED), op=ALU.mult)
            dv("tensor_tensor", out=es[:, 4], in0=es[:, 1], in1=b8(w_(W1G)), op=ALU.mult)
            dv("tensor_tensor", out=es[:, 5], in0=es[:, 3], in1=b8(w_(W2G)), op=ALU.mult)
            dv("tensor_tensor", out=es[:, 4], in0=es[:, 4], in1=es[:, 5], op=ALU.add)
            if C.sparse:
                dma(S, "sp", W["XN"][t * 512:(t + 1) * 512, :].rearrange("(s p) d -> p s d", p=128), xn2[:], reads=[b_xn2])
                dma(S, "sp", W["CG"][t * 512:(t + 1) * 512, :].rearrange("(s p) e -> p s e", p=128), es[:, 4], reads=[b_rw])
                S.add("dve", lambda e: e.tensor_copy(out=ohgb[:], in_=ohg[:]), reads=RR, writes=[b_ohgb])
                bk2 = 4 + rot % 2
                rot += 1
                S.add("pe", lambda e, bk2=bk2: e.matmul(PS[bk2][:, 0:16], lhsT=ustr[:], rhs=ohgb[:].rearrange("p s g -> p (s g)"), start=True, stop=True),
                      reads=[b_ohgb], writes=[PB[bk2]])
                S.add("pe", lambda e, bk2=bk2: e.matmul(PS[bk2][:, 16:32], lhsT=C.ones_b[:], rhs=ohgb[:].rearrange("p s g -> p (s g)"), start=True, stop=True),
                      reads=[b_ohgb, bc], writes=[PB[bk2]])
                S.add("dve", lambda e: e.tensor_copy(out=base[:, 0, :], in_=C.tot[:]), reads=[b_tot], writes=[b_base])
                for s_ in range(1, 4):
                    S.add("dve", lambda e, s_=s_, bk2=bk2: e.tensor_tensor(out=base[:, s_, :], in0=base[:, s_ - 1, :], in1=PS[bk2][:, 16 + 4 * (s_ - 1):16 + 4 * s_], op=ALU.add),
                          reads=[b_base, PB[bk2]], writes=[b_base])
                S.add("dve", lambda e, bk2=bk2: e.tensor_tensor(out=C.tot[:], in0=base[:, 3, :], in1=PS[bk2][:, 28:32], op=ALU.add),
                      reads=[b_base, PB[bk2]], writes=[b_tot])
                S.add("dve", lambda e, bk2=bk2: e.tensor_tensor(out=base[:], in0=base[:], in1=PS[bk2][:, 0:16].rearrange("p (s g) -> p s g", g=4), op=ALU.add),
                      reads=[b_base, PB[bk2]], writes=[b_base])
                S.add("dve", lambda e: e.tensor_tensor(out=base[:], in0=base[:], in1=ohg[:], op=ALU.mult), reads=[b_base] + RR, writes=[b_base])
                S.add("dve", lambda e, t=t: e.tensor_reduce(out=C.RK[:, t * 4:(t + 1) * 4], in_=base[:], axis=AX.X, op=ALU.add), reads=[b_base], writes=[C.b_rk])
                S.add("dve", lambda e, t=t: e.tensor_copy(out=C.OH[:, t * 4:(t + 1) * 4, :], in_=ohg[:]), reads=RR, writes=[C.b_rk])
            cb = comb[sl]
            S.add("dve", lambda e, cb=cb: e.tensor_tensor(out=cb[:].rearrange("p s (g e) -> p s g e", e=8), in0=ohg[:].unsqueeze(3).broadcast_to([128, 4, 4, 8]),
                                                         in1=es[:, 4].unsqueeze(2).broadcast_to([128, 4, 4, 8]), op=ALU.mult),
                  reads=RR, writes=[b_cb[sl]])
            dma(S, "sp", W["combS"][t * 512:(t + 1) * 512, :].rearrange("(s p) e -> p s e", p=128), cb[:], reads=[b_cb[sl]])
        if C.sparse:
            TH = sb("TH", [128, 4, 8], F32)
            cmpt = sb("cmpt", [128, 4, 8], F32)
            kg = sb("kg", [128, 4], F32)
            ck = sb("ck", [128, 5], F32)
            bsl = sb("bsl", [128, 4], F32)
            tmp3 = sb("tmp3", [128, 32, 4], F32)
            slf = sb("slf", [128, 32], F32)
            SI = sb("SI", [128, 12], F32)
            cmp2 = sb("cmp2", [128, 12, 3], F32)
            tgf = sb("tgf", [128, 12], F32)
            bq_ = Buf("slotcalc")
            RQ = [bq_, b_tot, C.b_rk]

            def dq(fn, **kw):
                S.add("dve", lambda e: getattr(e, fn)(**kw), reads=RQ, writes=[bq_])

            for j in range(8):
                dq("memset", ap=TH[:, :, j:j + 1], constant=512.0 * j)
            for j in range(12):
                dq("memset", ap=SI[:, j:j + 1], constant=float(j))
            dq("tensor_tensor", out=cmpt[:], in0=C.tot[:].unsqueeze(2).broadcast_to([128, 4, 8]), in1=TH[:], op=ALU.is_gt)
            dq("tensor_reduce", out=kg[:], in_=cmpt[:], axis=AX.X, op=ALU.add)
            dq("memset", ap=ck[:, 0:1], constant=0.0)
            for g in range(4):
                dq("tensor_tensor", out=ck[:, g + 1:g + 2], in0=ck[:, g:g + 1], in1=kg[:, g:g + 1], op=ALU.add)
            dq("tensor_scalar", out=bsl[:], in0=ck[:, 0:4], scalar1=512.0, scalar2=None, op0=ALU.mult)
            dq("tensor_tensor", out=tmp3[:], in0=C.OH[:], in1=bsl[:].unsqueeze(1).broadcast_to([128, 32, 4]), op=ALU.mult)
            dq("tensor_reduce", out=slf[:], in_=tmp3[:], axis=AX.X, op=ALU.add)
            dq("tensor_tensor", out=slf[:], in0=slf[:], in1=C.RK[:], op=ALU.add)
            dq("tensor_copy", out=C.SLOT_I[:], in_=slf[:])
            dq("tensor_tensor", out=cmp2[:], in0=ck[:, 1:4].unsqueeze(1).broadcast_to([128, 12, 3]), in1=SI[:].unsqueeze(2).broadcast_to([128, 12, 3]), op=ALU.is_le)
            dq("tensor_reduce", out=tgf[:], in_=cmp2[:], axis=AX.X, op=ALU.add)
            dq("tensor_copy", out=C.TG_I[:], in_=tgf[:])
            pidx_i = sb("pidx_i", [128, 1], mybir.dt.int32)
            pidx = sb("pidx", [128, 1], F32)
            j128 = sb("j128", [128, 8], F32)
            widf = sb("widf", [128, 12, 8], F32)
            S.add("pool", lambda e: e.iota(pidx_i[:], [[0, 1]], base=0, channel_multiplier=1), writes=[bq_])
            dq("tensor_copy", out=pidx[:], in_=pidx_i[:])
            for j in range(8):
                dq("memset", ap=j128[:, j:j + 1], constant=128.0 * j)
            dq("tensor_scalar", out=j128[:], in0=j128[:], scalar1=pidx[:, 0:1], scalar2=None, op0=ALU.add)
            dq("scalar_tensor_tensor", out=widf[:], in0=tgf[:].unsqueeze(2).broadcast_to([128, 12, 8]), scalar=1024.0,
               in1=j128[:].unsqueeze(1).broadcast_to([128, 12, 8]), op0=ALU.mult, op1=ALU.add)
            dq("tensor_copy", out=C.WIDX[:].rearrange("p (s j) -> p s j", j=8), in_=widf[:])
            if "dbg_slot" in C.debug:
                C.dbg_slot = nc.dram_tensor("dbg_slot", [128, 64], F32, kind="ExternalOutput").ap()
                dma(S, "sp", C.dbg_slot[:, 0:32], slf[:], reads=[bq_])
                dma(S, "sp", C.dbg_slot[:, 32:44], tgf[:], reads=[bq_])
                dma(S, "sp", C.dbg_slot[:, 44:48], C.tot[:], reads=[bq_])
        S.flush()


def phase4b(C):
    nc, S, I, W = C.nc, C.S, C.I, C.W
    PS, PB = C.PS, C.PB
    n_exp = C.n_exp if hasattr(C, "n_exp") else NEXP
    with ExitStack() as _es:
        sb = lambda name, shape, dt: _es.enter_context(nc.sbuf_tensor(name, shape, dt))
        u2t = sb("u2tt", [128, 8, 1024], BF16)
        cbt = sb("cbt", [128, 8, 32], F32)
        yacc = sb("yacc", [128, 8, D], F32)
        wg = [sb(f"wg{i}", [128, 8, 256], BF16) for i in range(2)]
        wu = [sb(f"wu{i}", [128, 8, 256], BF16) for i in range(2)]
        wd = [sb(f"wd{i}", [128, 2, D], BF16) for i in range(2)]
        sg = [sb(f"sg{i}", [128, 2, 512], BF16) for i in range(2)]
        hd = [sb(f"hd{i}", [128, 2, 512], BF16) for i in range(2)]
        hp = sb("hp", [128, 4, D], F32)
        gfin = sb("gfin", [128, D], F32)
        junk = sb("junk5", [128, D], BF16)
        ssq = sb("ssq5", [128, 8], F32)
        b_u2t, b_cbt, b_yacc, b_hp, b_gfin, b_junk, b_ssq = [Buf(n) for n in "u2t cbt yacc hp gfin junk ssq".split()]
        b_wg = [Buf("wg0"), Buf("wg1")]
        b_wu = [Buf("wu0"), Buf("wu1")]
        b_wd = [Buf("wd0"), Buf("wd1")]
        b_sg = [Buf("sg0"), Buf("sg1")]
        b_hd = [Buf("hd0"), Buf("hd1")]
        dma(S, "sp", gfin[:], I["g_final"].partition_broadcast(128), writes=[b_gfin])

        def load_w(TT, e, sl):
            if TT == 0:
                dma(S, "pool", wg[sl][:], I["w_exp_gate"][e].rearrange("(k p) f -> p k f", p=128), writes=[b_wg[sl]])
                dma(S, "pool", wu[sl][:], I["w_exp_up"][e].rearrange("(k p) f -> p k f", p=128), writes=[b_wu[sl]])
                dma(S, "pool", wd[sl][:], I["w_exp_down"][e].rearrange("(k p) d -> p k d", p=128), writes=[b_wd[sl]])
                dma(S, "sp", W["wgS"][e].rearrange("(k p) f -> p k f", p=128), wg[sl][:], reads=[b_wg[sl]])
                dma(S, "sp", W["wuS"][e].rearrange("(k p) f -> p k f", p=128), wu[sl][:], reads=[b_wu[sl]])
                dma(S, "sp", W["wdS"][e].rearrange("(k p) d -> p k d", p=128), wd[sl][:], reads=[b_wd[sl]])
            else:
                dma(S, "sp", wg[sl][:], W["wgS"][e].rearrange("(k p) f -> p k f", p=128), writes=[b_wg[sl]])
                dma(S, "sp", wu[sl][:], W["wuS"][e].rearrange("(k p) f -> p k f", p=128), writes=[b_wu[sl]])
                dma(S, "sp", wd[sl][:], W["wdS"][e].rearrange("(k p) d -> p k d", p=128), writes=[b_wd[sl]])

        rot = 0
        cnt = 0
        for TT in range(4):
            dma(S, "sp", u2t[:], W["u2T"][:, TT * 1024:(TT + 1) * 1024].rearrange("(c p) n -> p c n", p=128), writes=[b_u2t])
            dma(S, "sp", cbt[:], W["combS"][TT * 1024:(TT + 1) * 1024, :].rearrange("(s p) e -> p s e", p=128), writes=[b_cbt])
            S.add("pool", lambda e: e.memset(yacc[:], 0.0), writes=[b_yacc])
            load_w(TT, 0, 0)
            for ex in range(n_exp):
                sl = ex % 2
                if ex + 1 < n_exp:
                    load_w(TT, ex + 1, (ex + 1) % 2)
                for half in range(2):
                    c2 = cnt % 2
                    cnt += 1
                    for f in range(2):
                        for k in range(8):
                            S.add("pe", lambda e, f=f, k=k, sl=sl, half=half: e.matmul(PS[f][:], lhsT=wg[sl][:, k, f * 128:(f + 1) * 128], rhs=u2t[:, k, half * 512:(half + 1) * 512],
                                                                                  start=(k == 0), stop=(k == 7)), reads=[b_wg[sl], b_u2t], writes=[PB[f]])
                    for f in range(2):
                        for k in range(8):
                            S.add("pe", lambda e, f=f, k=k, sl=sl, half=half: e.matmul(PS[2 + f][:], lhsT=wu[sl][:, k, f * 128:(f + 1) * 128], rhs=u2t[:, k, half * 512:(half + 1) * 512],
                                                                                  start=(k == 0), stop=(k == 7)), reads=[b_wu[sl], b_u2t], writes=[PB[2 + f]])
                    for f in range(2):
                        S.add("act", lambda e, f=f, c2=c2: e.activation(out=sg[c2][:, f, :], in_=PS[f][:], func=AF.Silu), reads=[PB[f]], writes=[b_sg[c2]])
                        S.add("dve", lambda e, f=f, c2=c2: e.tensor_tensor(out=hd[c2][:, f, :], in0=PS[2 + f][:], in1=sg[c2][:, f, :], op=ALU.mult),
                              reads=[PB[2 + f], b_sg[c2]], writes=[b_hd[c2]])
                    for sub in range(4):
                        for dh in range(2):
                            bk = 4 + rot % 4
                            rot += 1
                            for f in range(2):
                                S.add("pe", lambda e, bk=bk, f=f, sub=sub, dh=dh, c2=c2, sl=sl: e.matmul(
                                    PS[bk][:], lhsT=hd[c2][:, f, sub * 128:(sub + 1) * 128], rhs=wd[sl][:, f, dh * 512:(dh + 1) * 512], start=(f == 0), stop=(f == 1)),
                                    reads=[b_hd[c2], b_wd[sl]], writes=[PB[bk]])
                            s8 = half * 4 + sub
                            S.add("dve", lambda e, bk=bk, s8=s8, dh=dh, ex=ex: e.scalar_tensor_tensor(
                                out=yacc[:, s8, dh * 512:(dh + 1) * 512], in0=PS[bk][:], scalar=cbt[:, s8, ex:ex + 1], in1=yacc[:, s8, dh * 512:(dh + 1) * 512],
                                op0=ALU.mult, op1=ALU.add), reads=[PB[bk], b_cbt, b_yacc], writes=[b_yacc])
            for half in range(2):
                r0 = TT * 1024 + half * 512
                dma(S, "sp", hp[:], W["hS"][r0:r0 + 512, :].rearrange("(s p) d -> p s d", p=128), writes=[b_hp])
                ya = yacc[:, half * 4:(half + 1) * 4, :]
                S.add("dve", lambda e, ya=ya: e.tensor_tensor(out=ya, in0=ya, in1=C.gtf_bc[:].unsqueeze(1).broadcast_to([128, 4, D]), op=ALU.mult),
                      reads=[b_yacc, C.b_gt], writes=[b_yacc])
                S.add("pool", lambda e, ya=ya: e.tensor_tensor(out=hp[:], in0=hp[:], in1=ya, op=ALU.add), reads=[b_yacc, b_hp], writes=[b_hp])
                S.add("dve", lambda e: e.memset(ssq[:], 0.0), writes=[b_ssq])
                for s in range(4):
                    S.add("act", lambda e, s=s: e.activation(out=junk[:], in_=hp[:, s, :], func=AF.Square, accum_out=ssq[:, s:s + 1]),
                          reads=[b_hp, b_ssq], writes=[b_junk, b_ssq])
                S.add("dve", lambda e: e.tensor_scalar(out=ssq[:, 4:8], in0=ssq[:, 0:4], scalar1=1.0 / D, scalar2=EPS, op0=ALU.mult, op1=ALU.add),
                      reads=[b_ssq], writes=[b_ssq])
                S.add("act", lambda e: e.activation(out=ssq[:, 4:8], in_=ssq[:, 4:8], func=AF.Sqrt), reads=[b_ssq], writes=[b_ssq])
                S.add("dve", lambda e: e.reciprocal(out=ssq[:, 4:8], in_=ssq[:, 4:8]), reads=[b_ssq], writes=[b_ssq])
                for s in range(4):
                    S.add("dve", lambda e, s=s: e.scalar_tensor_tensor(out=hp[:, s, :], in0=hp[:, s, :], scalar=ssq[:, 4 + s:5 + s], in1=gfin[:], op0=ALU.mult, op1=ALU.mult),
                          reads=[b_hp, b_ssq, b_gfin], writes=[b_hp])
                dma(S, "sp", C.out[r0:r0 + 512, :].rearrange("(s p) d -> p s d", p=128), hp[:], reads=[b_hp])
        S.flush()
```

```python
import math
from contextlib import ExitStack
import numpy as np
import concourse.bass as bass
import concourse.mybir as mybir
from concourse.bass_utils import run_bass_kernel_spmd

F32 = mybir.dt.float32
BF16 = mybir.dt.bfloat16
AF = mybir.ActivationFunctionType
ALU = mybir.AluOpType
AX = mybir.AxisListType

L = 4096
D = 1024
NTT = 8
EPS = 1e-6
LAMBDA_INIT = 0.8 - 0.6 * math.exp(-0.3 * 0)
NEXP = 32
ENGS = ("pe", "act", "dve", "pool", "sp")
ENG_ATTR = {"pe": "tensor", "act": "scalar", "dve": "vector", "pool": "gpsimd", "sp": "sync"}


class Buf:
    __slots__ = ("name", "w", "r")

    def __init__(self, name=""):
        self.name = name
        self.w = None
        self.r = []


class Op:
    __slots__ = ("eng", "idx", "fn", "waits", "signal", "count", "dma", "dsem", "dval", "snap", "bg")

    def __init__(self, eng, idx, fn, dma):
        self.eng = eng
        self.idx = idx
        self.fn = fn
        self.waits = []
        self.signal = False
        self.count = None
        self.dma = dma
        self.dsem = None
        self.dval = None
        self.snap = None
        self.bg = False


class Sched:
    def __init__(self, nc, n_dma_sems=48):
        self.nc = nc
        self.pending = {e: [] for e in ENGS}
        self.nops = {e: 0 for e in ENGS}
        self.last = {e: None for e in ENGS}
        self.seen = {e: {} for e in ENGS}
        self.n_dma_sems = n_dma_sems
        self.dma_rr = 0
        self.dma_rr_sw = 0
        self.n_hw = 28
        self.dma_last = [None] * n_dma_sems
        self.dma_last_nb = [None] * n_dma_sems
        self.dma_val = [0] * n_dma_sems
        self.cnt = {e: 0 for e in ENGS}
        self.sems = {e: nc.alloc_semaphore(name=f"sem_{e}") for e in ENGS}
        self.dsems = [nc.alloc_semaphore(name=f"dsem_{i}") for i in range(n_dma_sems)]

    def _need(self, o, d):
        e = o.eng
        if d.dma:
            key = ("d", d.dsem)
            if self.seen[e].get(key, 0) >= d.dval:
                return
            self.seen[e][key] = d.dval
            o.waits.append(d)
        else:
            if d.eng == e and e == "pe":
                return
            key = d.eng
            if self.seen[e].get(key, -1) >= d.idx:
                return
            self.seen[e][key] = d.idx
            d.signal = True
            o.waits.append(d)
        if d.snap is not None:
            se = self.seen[e]
            for k, v in d.snap.items():
                if k == e:
                    continue
                if se.get(k, -1) < v:
                    se[k] = v

    def add(self, eng, fn, reads=(), writes=(), dma=False, bg=False):
        o = Op(eng, self.nops[eng], fn, dma)
        o.bg = bg
        self.nops[eng] += 1
        deps = []
        for b in reads:
            if b.w is not None:
                deps.append(b.w)
        for b in writes:
            if b.w is not None:
                deps.append(b.w)
            deps.extend(b.r)
        if dma:
            if eng == "pool":
                s = self.n_hw + self.dma_rr_sw
                self.dma_rr_sw = (self.dma_rr_sw + 1) % (self.n_dma_sems - self.n_hw)
            else:
                s = self.dma_rr
                self.dma_rr = (self.dma_rr + 1) % self.n_hw
            prev = self.dma_last[s]
            if prev is not None:
                deps.append(prev)
            self.dma_val[s] += 16
            o.dsem = s
            o.dval = self.dma_val[s]
            self.dma_last[s] = o
            if not bg:
                self.dma_last_nb[s] = o
        for d in deps:
            if d is not o:
                self._need(o, d)
        for b in reads:
            b.r.append(o)
        for b in writes:
            b.w = o
            b.r = []
        self.pending[eng].append(o)
        if not dma:
            self.last[eng] = o
        o.snap = dict(self.seen[eng])
        return o

    def flush(self):
        lasts = [self.last[e] for e in ENGS if self.last[e] is not None]
        dlasts = [(self.dma_last_nb[i] if d.bg else d) for i, d in enumerate(self.dma_last) if d is not None]
        dlasts = [d for d in dlasts if d is not None]
        for e in ENGS:
            o = Op(e, self.nops[e], None, False)
            self.nops[e] += 1
            for d in lasts + dlasts:
                self._need(o, d)
            self.pending[e].append(o)
            o.snap = dict(self.seen[e])
        nc = self.nc
        for e in ENGS:
            for o in self.pending[e]:
                if o.signal and not o.dma:
                    self.cnt[e] += 1
                    o.count = self.cnt[e]
        sems, dsems = self.sems, self.dsems
        with nc.Block() as block:
            for e in ENGS:
                ops = self.pending[e]

                def body(eng, ops=ops, e=e):
                    for o in ops:
                        for d in o.waits:
                            if d.dma:
                                eng.wait_ge(dsems[d.dsem], d.dval)
                            else:
                                eng.wait_ge(sems[d.eng], d.count)
                        if o.fn is None:
                            continue
                        ins = o.fn(eng)
                        if o.dma:
                            ins.then_inc(dsems[o.dsem], 16)
                        elif o.signal:
                            ins.then_inc(sems[e], 1)

                getattr(block, ENG_ATTR[e])(body)
        self.pending = {e: [] for e in ENGS}


class Ctx:
    pass


def dma(S, q, out, in_, reads=(), writes=(), bg=False):
    return S.add(q, lambda e: e.dma_start(out=out, in_=in_), reads, writes, dma=True, bg=bg)


VEC_ROWS = {}


def build(debug=()):
    nc = bass.Bass("TRN2", target_bir_lowering=False)
    C = Ctx()
    C.nc = nc
    C.debug = set(debug)
    C.S = S = Sched(nc)

    def din(name, shape):
        return nc.dram_tensor(name, list(shape), F32, kind="ExternalInput").ap()

    I = C.I = {}
    for name, shape in [
        ("x", (L, D)), ("c", (D,)), ("w_ada", (D, 6 * D)), ("b_ada", (6 * D,)), ("g_norm_mix", (D,)),
        ("w_in", (D, 5632)), ("b_in", (5632,)),
        ("s5_a_re", (32, 64)), ("s5_a_im", (32, 64)), ("s5_b_re", (32, 64, 16)), ("s5_b_im", (32, 64, 16)),
        ("s5_c_re", (32, 16, 64)), ("s5_c_im", (32, 16, 64)), ("s5_d", (512,)), ("s5_log_dt", (32,)),
        ("w_glu", (512, 512)), ("b_glu", (512,)),
        ("lambda_q1", (64,)), ("lambda_k1", (64,)), ("lambda_q2", (64,)), ("lambda_k2", (64,)), ("g_subln", (128,)),
        ("w_br_ssm", (512, D)), ("w_br_attn", (D, D)), ("w_out", (D, D)), ("g_norm_ffn", (D,)),
        ("w_router_grp", (D, 4)), ("b_router_grp", (4,)), ("w_router_exp", (D, 32)), ("b_router_exp", (32,)),
        ("w_exp_gate", (NEXP, D, 256)), ("w_exp_up", (NEXP, D, 256)), ("w_exp_down", (NEXP, 256, D)),
        ("g_final", (D,)),
    ]:
        I[name] = din(name, shape)
    C.out = nc.dram_tensor("out", [L, D], F32, kind="ExternalOutput").ap()

    def scratch(name, shape, dt):
        if name in C.debug:
            return nc.dram_tensor(name, list(shape), dt, kind="ExternalOutput").ap()
        return nc.dram_tensor(name, list(shape), dt, kind="Internal").ap()

    C.scratch = scratch
    W = C.W = {}
    W["qT"] = scratch("qT", (D, L), BF16)
    W["kT"] = scratch("kT", (D, L), BF16)
    W["vS"] = scratch("vS", (L, D), BF16)
    W["gT"] = scratch("gT", (2 * D, L), BF16)
    W["s5S"] = scratch("s5S", (L, 512), F32)
    W["y2T"] = scratch("y2T", (512, L), BF16)
    W["onT"] = scratch("onT", (D, L), BF16)
    W["hS"] = scratch("hS", (L, D), F32)
    W["u2T"] = scratch("u2T", (D, L), BF16)
    W["combS"] = scratch("combS", (L, NEXP), F32)
    W["wgS"] = scratch("wgS", (NEXP, D, 256), BF16)
    W["wuS"] = scratch("wuS", (NEXP, D, 256), BF16)
    W["wdS"] = scratch("wdS", (NEXP, 256, D), BF16)
    NSLOT = 6144
    W["XN"] = scratch("XN", (L, D), BF16)
    W["CG"] = scratch("CG", (L, 8), F32)
    W["XS"] = scratch("XS", (NSLOT, D), BF16)
    W["CGS"] = scratch("CGS", (NSLOT, 8), F32)
    W["YS"] = scratch("YS", (NSLOT, D), F32)
    W["WALL"] = scratch("WALL", (NEXP * 128, 6144), BF16)
    C.WB = {k: Buf(k) for k in W}
    C.sparse = "dense" not in C.debug
    if "dbg_cols" in C.debug:
        C.dbg_cols = nc.dram_tensor("dbg_cols", [128, 256], F32, kind="ExternalOutput").ap()
    if "dbg_s5y" in C.debug:
        C.dbg_s5y = nc.dram_tensor("dbg_s5y", [L, 512], F32, kind="ExternalOutput").ap()

    with ExitStack() as _es:
        p0 = _es.enter_context(nc.psum_tensor("ps0", [128, 512], F32))
        p1 = _es.enter_context(nc.psum_tensor("ps1", [128, 512], F32))
        p2 = _es.enter_context(nc.psum_tensor("ps2", [128, 512], F32))
        p3 = _es.enter_context(nc.psum_tensor("ps3", [128, 512], F32))
        p4 = _es.enter_context(nc.psum_tensor("ps4", [128, 512], F32))
        p5 = _es.enter_context(nc.psum_tensor("ps5", [128, 512], F32))
        p6 = _es.enter_context(nc.psum_tensor("ps6", [128, 512], F32))
        p7 = _es.enter_context(nc.psum_tensor("ps7", [128, 512], F32))
        ident_f = _es.enter_context(nc.sbuf_tensor("ident_f", [128, 128], F32))
        ident_b = _es.enter_context(nc.sbuf_tensor("ident_b", [128, 128], BF16))
        ones_b = _es.enter_context(nc.sbuf_tensor("ones_b", [128, 128], BF16))
        cols = _es.enter_context(nc.sbuf_tensor("cols", [128, 256], F32))
        gtm_bc = _es.enter_context(nc.sbuf_tensor("gtm_bc", [128, D], F32))
        gtf_bc = _es.enter_context(nc.sbuf_tensor("gtf_bc", [128, D], F32))
        C.RK = _es.enter_context(nc.sbuf_tensor("RK", [128, 32], F32))
        C.OH = _es.enter_context(nc.sbuf_tensor("OH", [128, 32, 4], F32))
        C.tot = _es.enter_context(nc.sbuf_tensor("tot", [128, 4], F32))
        C.SLOT_I = _es.enter_context(nc.sbuf_tensor("SLOT_I", [128, 32], mybir.dt.int32))
        C.TG_I = _es.enter_context(nc.sbuf_tensor("TG_I", [128, 12], mybir.dt.int32))
        C.WIDX = _es.enter_context(nc.sbuf_tensor("WIDX", [128, 96], mybir.dt.int32))
        C.b_w4 = Buf("w4g")
        C.b_rk = Buf("rk")
        C.PS = [p0, p1, p2, p3, p4, p5, p6, p7]
        C.PB = [Buf(f"ps{i}") for i in range(8)]
        C.ident_f, C.ident_b, C.ones_b, C.cols = ident_f, ident_b, ones_b, cols
        C.gtm_bc, C.gtf_bc = gtm_bc, gtf_bc
        C.b_const = Buf("const")
        C.b_cols = Buf("cols")
        C.b_gt = Buf("gt")
        with ExitStack() as _es01:
            C.w_in = _es01.enter_context(nc.sbuf_tensor("sb_w_in", [128, 8, 5632], BF16))
            C.b_win = [Buf(f"win{i}") for i in range(11)]
            phase0(C)
            S.flush()
            if "stop0" not in C.debug and "skip1" not in C.debug:
                phase1(C)
        if "stop0" not in C.debug:
            if "skip2" not in C.debug and "stop1" not in C.debug:
                phase2(C)
            if "heads" in C.debug:
                C.heads = [0, 5]
            for f_ in C.debug:
                if f_.startswith("epi="):
                    C.epi_step = int(f_[4:])
            with ExitStack() as _es34:
                C.wbs = _es34.enter_context(nc.sbuf_tensor("wbs", [128, 4, D], BF16))
                C.wba = _es34.enter_context(nc.sbuf_tensor("wba", [128, 8, D], BF16))
                C.wout = _es34.enter_context(nc.sbuf_tensor("wout", [128, 8, D], BF16))
                if "skip3" not in C.debug and "stop1" not in C.debug:
                    phase3(C)
                if "stop3" not in C.debug and "stop1" not in C.debug:
                    phase4a(C)
            if "stop3" not in C.debug and "stop1" not in C.debug:
                if "n_exp" in C.debug:
                    C.n_exp = 2
                if "stop4a" not in C.debug:
                    if C.sparse:
                        phase4p(C)
                        phase4b_sparse(C)
                    else:
                        phase4b(C)
        if "dbg_cols" in C.debug:
            dma(S, "sp", C.dbg_cols, cols[:], reads=[C.b_cols])
            S.flush()
    return nc


COL_C = 0
COL_BADA = 8
COL_BIN = 56
COL_GNM = 100
COL_GNF = 108
COL_BGLU = 116
COL_GSUB = 120
COL_MOD = 128
COL_G1 = 176
COL_G2 = 184
COL_GSUBS = 192


def phase0(C):
    nc, S, I = C.nc, C.S, C.I
    cols = C.cols
    bc = C.b_const
    S.add("pool", lambda e: e.memset(C.ident_f[:], 0.0), writes=[bc])
    S.add("pool", lambda e: e.affine_select(out=C.ident_f[:], in_=C.ident_f[:], pattern=[[-1, 128]],
                                            compare_op=ALU.not_equal, fill=1.0, base=0, channel_multiplier=1),
          reads=[bc], writes=[bc])
    S.add("dve", lambda e: e.tensor_copy(out=C.ident_b[:], in_=C.ident_f[:]), reads=[bc], writes=[bc])
    S.add("dve", lambda e: e.memset(C.ones_b[:], 1.0), writes=[bc])
    with ExitStack() as _es:
        rows = _es.enter_context(nc.sbuf_tensor("rows", [128, 128], F32))
        cs_b = _es.enter_context(nc.sbuf_tensor("cs_b", [128, 8], BF16))
        cs_rep = _es.enter_context(nc.sbuf_tensor("cs_rep", [128, 8, 128], BF16))
        wa0 = _es.enter_context(nc.sbuf_tensor("wa0", [128, 8, 512], BF16))
        wa1 = _es.enter_context(nc.sbuf_tensor("wa1", [128, 8, 512], BF16))
        bada_bc = _es.enter_context(nc.sbuf_tensor("bada_bc", [128, 2, D], F32))
        b_rows = Buf("rows")
        S.add("dve", lambda e: e.memset(rows[:], 0.0), writes=[b_rows])
        for name, base, n in [("c", COL_C, 8), ("b_ada", COL_BADA, 48), ("b_in", COL_BIN, 44), ("g_norm_mix", COL_GNM, 8),
                              ("g_norm_ffn", COL_GNF, 8), ("b_glu", COL_BGLU, 4), ("g_subln", COL_GSUB, 1)]:
            dma(S, "sp", rows[base:base + n, :], I[name].rearrange("(k p) -> k p", p=128), writes=[b_rows])
        b_bada = Buf("bada_bc")
        dma(S, "sp", bada_bc[:, 0, :], I["b_ada"][2 * D:3 * D].partition_broadcast(128), writes=[b_bada])
        dma(S, "sp", bada_bc[:, 1, :], I["b_ada"][5 * D:6 * D].partition_broadcast(128), writes=[b_bada])
        pT = C.PS[0]
        S.add("pe", lambda e: e.transpose(out=pT[:, 0:128], in_=rows[:], identity=C.ident_f[:]), reads=[b_rows, bc], writes=[C.PB[0]])
        S.add("dve", lambda e: e.tensor_copy(out=cols[:, 0:128], in_=pT[:, 0:128]), reads=[C.PB[0]], writes=[C.b_cols])
        b_cs = Buf("cs")
        S.add("act", lambda e: e.activation(out=cols[:, COL_C:COL_C + 8], in_=cols[:, COL_C:COL_C + 8], func=AF.Silu),
              reads=[C.b_cols], writes=[C.b_cols])
        S.add("dve", lambda e: e.tensor_copy(out=cs_b[:], in_=cols[:, COL_C:COL_C + 8]), reads=[C.b_cols], writes=[b_cs])
        S.add("dve", lambda e: e.tensor_copy(out=cs_rep[:], in_=cs_b[:].unsqueeze(2).broadcast_to([128, 8, 128])),
              reads=[b_cs], writes=[b_cs])
        was = [wa0, wa1]
        waf = [_es.enter_context(nc.sbuf_tensor(f"waf{i}", [128, 8, 512], F32)) for i in range(2)]
        b_waf = [Buf("waf0"), Buf("waf1")]
        b_wa = [Buf("wa0"), Buf("wa1")]
        pcol = C.PS[1]
        for n in range(12):
            wa = was[n % 2]
            bw = b_wa[n % 2]
            waf_ = waf[n % 2]
            dma(S, "sp", waf_[:], I["w_ada"][:, n * 512:(n + 1) * 512].rearrange("(k p) n -> p k n", p=128), writes=[b_waf[n % 2]])
            S.add("act", lambda e, wa=wa, waf_=waf_: e.activation(out=wa[:], in_=waf_[:], func=AF.Copy), reads=[b_waf[n % 2]], writes=[bw])
            if n >= 1:
                i_ = n - 1
                dma(S, "pool", C.w_in[:, :, i_ * 512:(i_ + 1) * 512], I["w_in"][:, i_ * 512:(i_ + 1) * 512].rearrange("(k p) n -> p k n", p=128),
                    writes=[C.b_win[i_]], bg=True)
            for s in range(4):
                j = n * 4 + s
                for k in range(8):
                    S.add("pe", lambda e, wa=wa, s=s, k=k, j=j: e.matmul(pcol[:, j:j + 1], lhsT=wa[:, k, s * 128:(s + 1) * 128],
                                                                      rhs=cs_b[:, k:k + 1], start=(k == 0), stop=(k == 7)),
                          reads=[bw, b_cs], writes=[C.PB[1]])
            if n in (4, 5, 10, 11):
                prow = C.PS[2 + (n % 2)]
                pbb = C.PB[2 + (n % 2)]
                for k in range(8):
                    S.add("pe", lambda e, wa=wa, k=k, prow=prow: e.matmul(prow[:], lhsT=cs_rep[:, k, :], rhs=wa[:, k, :],
                                                                       start=(k == 0), stop=(k == 7)),
                          reads=[bw, b_cs], writes=[pbb])
                dst = C.gtm_bc if n < 6 else C.gtf_bc
                hh = n % 2
                wi = 0 if n < 6 else 1
                S.add("dve", lambda e, dst=dst, hh=hh, wi=wi, prow=prow: e.tensor_tensor(
                    out=dst[:, hh * 512:(hh + 1) * 512], in0=prow[:], in1=bada_bc[:, wi, hh * 512:(hh + 1) * 512], op=ALU.add),
                    reads=[pbb, b_bada], writes=[C.b_gt])
        S.add("dve", lambda e: e.tensor_tensor(out=cols[:, COL_MOD:COL_MOD + 48], in0=pcol[:, 0:48],
                                               in1=cols[:, COL_BADA:COL_BADA + 48], op=ALU.add),
              reads=[C.PB[1], C.b_cols], writes=[C.b_cols])
        S.add("dve", lambda e: e.scalar_tensor_tensor(out=cols[:, COL_G1:COL_G1 + 8], in0=cols[:, COL_MOD + 8:COL_MOD + 16], scalar=1.0,
                                                      in1=cols[:, COL_GNM:COL_GNM + 8], op0=ALU.add, op1=ALU.mult),
              reads=[C.b_cols], writes=[C.b_cols])
        S.add("dve", lambda e: e.scalar_tensor_tensor(out=cols[:, COL_G2:COL_G2 + 8], in0=cols[:, COL_MOD + 32:COL_MOD + 40], scalar=1.0,
                                                      in1=cols[:, COL_GNF:COL_GNF + 8], op0=ALU.add, op1=ALU.mult),
              reads=[C.b_cols], writes=[C.b_cols])
        S.add("dve", lambda e: e.tensor_scalar(out=cols[:, COL_GSUBS:COL_GSUBS + 1], in0=cols[:, COL_GSUB:COL_GSUB + 1],
                                               scalar1=(1.0 - LAMBDA_INIT), scalar2=None, op0=ALU.mult),
              reads=[C.b_cols], writes=[C.b_cols])
        S.flush()


def phase1(C):
    nc, S, I, W, WB = C.nc, C.S, C.I, C.W, C.WB
    cols = C.cols
    with ExitStack() as _es:
        w_in = C.w_in
        xt0 = _es.enter_context(nc.sbuf_tensor("xt0", [128, 4, D], F32))
        xt1 = _es.enter_context(nc.sbuf_tensor("xt1", [128, 4, D], F32))
        xn = _es.enter_context(nc.sbuf_tensor("xn", [128, 4, D], BF16))
        junk = _es.enter_context(nc.sbuf_tensor("junk", [128, D], BF16))
        ssq = _es.enter_context(nc.sbuf_tensor("ssq", [128, 8], F32))
        uT0 = _es.enter_context(nc.sbuf_tensor("uT0", [128, 8, 512], BF16))
        uT1 = _es.enter_context(nc.sbuf_tensor("uT1", [128, 8, 512], BF16))
        stg0 = _es.enter_context(nc.sbuf_tensor("stg0", [128, 4, 512], BF16))
        stg1 = _es.enter_context(nc.sbuf_tensor("stg1", [128, 4, 512], BF16))
        vstg = _es.enter_context(nc.sbuf_tensor("vstg", [128, 4, D], BF16))
        s5stg = _es.enter_context(nc.sbuf_tensor("s5stg", [128, 4, 512], F32))
        bias_bc = _es.enter_context(nc.sbuf_tensor("bias_bc", [128, 1536], F32))
        b_win = C.b_win
        b_bias = Buf("bias_bc")
        dma(S, "sp", bias_bc[:, 0:512], I["b_in"][0:512].partition_broadcast(128), writes=[b_bias])
        dma(S, "sp", bias_bc[:, 512:1536], I["b_in"][2560:3584].partition_broadcast(128), writes=[b_bias])
        xts = [xt0, xt1]
        b_xt = [Buf("xt0"), Buf("xt1")]
        uTs = [uT0, uT1]
        b_uT = [Buf("uT0"), Buf("uT1")]
        stgs = [stg0, stg1]
        b_stg = [Buf("stg0"), Buf("stg1")]
        b_xn, b_ssq, b_junk, b_vstg, b_s5stg = Buf("xn"), Buf("ssq"), Buf("junk"), Buf("vstg"), Buf("s5stg")
        PS, PB = C.PS, C.PB

        def load_x(t):
            dma(S, "sp", xts[t % 2][:], I["x"][t * 512:(t + 1) * 512, :].rearrange("(s p) d -> p s d", p=128), writes=[b_xt[t % 2]])

        load_x(0)
        rot = 0
        stg_i = 0
        for t in range(NTT):
            if t + 1 < NTT:
                load_x(t + 1)
            xt = xts[t % 2]
            bx = b_xt[t % 2]
            uT = uTs[t % 2]
            bu = b_uT[t % 2]
            S.add("dve", lambda e: e.memset(ssq[:], 0.0), writes=[b_ssq])
            for s in range(4):
                S.add("act", lambda e, s=s, xt=xt: e.activation(out=junk[:], in_=xt[:, s, :], func=AF.Square, accum_out=ssq[:, s:s + 1]),
                      reads=[bx, b_ssq], writes=[b_junk, b_ssq])
            S.add("dve", lambda e: e.tensor_scalar(out=ssq[:, 4:8], in0=ssq[:, 0:4], scalar1=1.0 / D, scalar2=EPS, op0=ALU.mult, op1=ALU.add),
                  reads=[b_ssq], writes=[b_ssq])
            S.add("act", lambda e: e.activation(out=ssq[:, 4:8], in_=ssq[:, 4:8], func=AF.Sqrt), reads=[b_ssq], writes=[b_ssq])
            S.add("dve", lambda e: e.reciprocal(out=ssq[:, 4:8], in_=ssq[:, 4:8]), reads=[b_ssq], writes=[b_ssq])
            for s in range(4):
                S.add("dve", lambda e, s=s, xt=xt: e.tensor_scalar(out=xn[:, s, :], in0=xt[:, s, :], scalar1=ssq[:, 4 + s:5 + s], scalar2=None,
                                                                op0=ALU.mult),
                      reads=[bx, b_ssq], writes=[b_xn])
            for k in range(8):
                pt = PS[k % 2]
                ptb = pt[:].bitcast(BF16)
                for s in range(4):
                    S.add("pe", lambda e, s=s, k=k, ptb=ptb: e.transpose(out=ptb[:, s * 128:(s + 1) * 128], in_=xn[:, s, k * 128:(k + 1) * 128],
                                                                      identity=C.ident_b[:]),
                          reads=[b_xn, C.b_const], writes=[PB[k % 2]])
                S.add("dve", lambda e, k=k, ptb=ptb, uT=uT: e.tensor_scalar(out=uT[:, k, :], in0=ptb[:, 0:512], scalar1=cols[:, COL_G1 + k:COL_G1 + k + 1],
                                                                         scalar2=cols[:, COL_MOD + k:COL_MOD + k + 1], op0=ALU.mult, op1=ALU.add),
                      reads=[PB[k % 2], C.b_cols], writes=[bu])
            groups = [("qT", 4, 0), ("qT", 8, 4), ("kT", 12, 0), ("kT", 16, 4), ("gT", 28, 0), ("gT", 32, 4), ("gT", 36, 8), ("gT", 40, 12)]
            for (dst, ct0, r0) in groups:
                stg = stgs[stg_i % 2]
                bs = b_stg[stg_i % 2]
                stg_i += 1
                for cc in range(4):
                    ct = ct0 + cc
                    bank = 2 + (rot % 4)
                    rot += 1
                    pp = PS[bank]
                    for k in range(8):
                        S.add("pe", lambda e, pp=pp, k=k, ct=ct, uT=uT: e.matmul(pp[:], lhsT=w_in[:, k, ct * 128:(ct + 1) * 128], rhs=uT[:, k, :],
                                                                             start=(k == 0), stop=(k == 7)),
                              reads=[b_win[ct // 4], bu], writes=[PB[bank]])
                    fn = AF.Sigmoid if dst == "gT" else AF.Identity
                    S.add("act", lambda e, pp=pp, cc=cc, ct=ct, stg=stg, fn=fn: e.activation(out=stg[:, cc, :], in_=pp[:], func=fn,
                                                                                        bias=cols[:, COL_BIN + ct:COL_BIN + ct + 1], scale=1.0),
                          reads=[PB[bank], C.b_cols], writes=[bs])
                dma(S, "sp", W[dst][r0 * 128:(r0 + 4) * 128, t * 512:(t + 1) * 512].rearrange("(c p) n -> p c n", p=128), stg[:],
                    reads=[bs])
            for s in range(4):
                for hh in range(3):
                    bank = 6 + (rot % 2)
                    rot += 1
                    pp = PS[bank]
                    c0 = 0 if hh == 2 else 2560 + hh * 512
                    for k in range(8):
                        S.add("pe", lambda e, pp=pp, k=k, s=s, c0=c0, uT=uT: e.matmul(pp[:], lhsT=uT[:, k, s * 128:(s + 1) * 128], rhs=w_in[:, k, c0:c0 + 512],
                                                                                  start=(k == 0), stop=(k == 7)),
                              reads=[b_win[c0 // 512], bu], writes=[PB[bank]])
                    if hh < 2:
                        S.add("dve", lambda e, pp=pp, s=s, hh=hh: e.tensor_tensor(out=vstg[:, s, hh * 512:(hh + 1) * 512], in0=pp[:],
                                                                               in1=bias_bc[:, 512 + hh * 512:1024 + hh * 512], op=ALU.add),
                              reads=[PB[bank], b_bias], writes=[b_vstg])
                    else:
                        S.add("dve", lambda e, pp=pp, s=s: e.tensor_tensor(out=s5stg[:, s, :], in0=pp[:], in1=bias_bc[:, 0:512], op=ALU.add),
                              reads=[PB[bank], b_bias], writes=[b_s5stg])
            dma(S, "sp", W["vS"][t * 512:(t + 1) * 512, :].rearrange("(s p) d -> p s d", p=128), vstg[:], reads=[b_vstg])
            dma(S, "sp", W["s5S"][t * 512:(t + 1) * 512, :].rearrange("(s p) d -> p s d", p=128), s5stg[:], reads=[b_s5stg])
        S.flush()


def phase4p(C):
    nc, S, W = C.nc, C.S, C.W
    with ExitStack() as _es:
        sb = lambda name, shape, dt: _es.enter_context(nc.sbuf_tensor(name, shape, dt))
        NB4 = 6
        xb = [sb(f"xb{i}", [128, D], BF16) for i in range(NB4)]
        cgb = [sb(f"cgb{i}", [128, 8], F32) for i in range(NB4)]
        b_xb = [Buf(f"xb{i}") for i in range(NB4)]
        b_cgb = [Buf(f"cgb{i}") for i in range(NB4)]
        for blk in range(32):
            sl = blk % NB4
            dma(S, "sp", xb[sl][:], W["XN"][blk * 128:(blk + 1) * 128, :], writes=[b_xb[sl]])
            dma(S, "sp", cgb[sl][:], W["CG"][blk * 128:(blk + 1) * 128, :], writes=[b_cgb[sl]])
            S.add("pool", lambda e, sl=sl, blk=blk: e.indirect_dma_start(
                out=W["XS"], out_offset=bass.IndirectOffsetOnAxis(ap=C.SLOT_I[:, blk:blk + 1], axis=0), in_=xb[sl][:], in_offset=None), reads=[b_xb[sl]], writes=[], dma=True)
            S.add("pool", lambda e, sl=sl, blk=blk: e.indirect_dma_start(
                out=W["CGS"], out_offset=bass.IndirectOffsetOnAxis(ap=C.SLOT_I[:, blk:blk + 1], axis=0), in_=cgb[sl][:], in_offset=None), reads=[b_cgb[sl]], writes=[], dma=True)
        S.flush()


def phase4b_sparse(C):
    nc, S, I, W = C.nc, C.S, C.I, C.W
    PS, PB = C.PS, C.PB
    cols = C.cols
    bc = C.b_const
    NT = 11
    regs = {}
    with ExitStack() as _es:
        sb = lambda name, shape, dt: _es.enter_context(nc.sbuf_tensor(name, shape, dt))
        xs = [sb(f"xs{i}", [128, 4, D], BF16) for i in range(2)]
        cgs = [sb(f"cgs{i}", [128, 4, 8], F32) for i in range(2)]
        u2t = sb("u2ts", [128, 8, 512], BF16)
        yacc = [sb(f"yaccs{i}", [128, 4, D], F32) for i in range(2)]
        wall = [sb(f"wall{i}", [128, 6144], BF16) for i in range(3)]
        wg = [wall[i][:, 0:2048].rearrange("p (k f) -> p k f", f=256) for i in range(3)]
        wu = [wall[i][:, 2048:4096].rearrange("p (k f) -> p k f", f=256) for i in range(3)]
        wd = [wall[i][:, 4096:6144].rearrange("p (k d) -> p k d", d=D) for i in range(3)]
        sg = [sb(f"sgs{i}", [128, 2, 512], BF16) for i in range(2)]
        hd = [sb(f"hds{i}", [128, 2, 512], BF16) for i in range(2)]
        b_xs = [Buf("xs0"), Buf("xs1")]
        b_cgs = [Buf("cgs0"), Buf("cgs1")]
        b_u2t = Buf("u2ts")
        b_ya = [Buf("ya0"), Buf("ya1")]
        b_wg = [Buf("wg0"), Buf("wg1"), Buf("wg2")]
        b_wu = [Buf("wu0"), Buf("wu1"), Buf("wu2")]
        b_wd = [Buf("wd0"), Buf("wd1"), Buf("wd2")]
        b_sg = [Buf("sg0"), Buf("sg1")]
        b_hd = [Buf("hd0"), Buf("hd1")]

        def load_tile(st):
            sl = st % 2
            dma(S, "sp", xs[sl][:], W["XS"][st * 512:(st + 1) * 512, :].rearrange("(s p) d -> p s d", p=128), writes=[b_xs[sl]])
            dma(S, "sp", cgs[sl][:], W["CGS"][st * 512:(st + 1) * 512, :].rearrange("(s p) e -> p s e", p=128), writes=[b_cgs[sl]])

        def load_w(st, j, sl):
            q = st * 8 + j
            S.add("pool", lambda e, q=q, sl=sl: e.indirect_dma_start(out=wall[sl][:], out_offset=None, in_=W["WALL"],
                                                                  in_offset=bass.IndirectOffsetOnAxis(ap=C.WIDX[:, q:q + 1], axis=0)),
                  reads=[C.b_rk], writes=[b_wg[sl], b_wu[sl], b_wd[sl]], dma=True)

        seq = [(st, j) for st in range(NT) for j in range(8)]
        deferred = []
        load_tile(0)
        load_w(0, 0, 0)
        load_w(0, 1, 1)
        rot = 0
        cnt = 0
        for qi, (st, j) in enumerate(seq):
            sl = qi % 3
            tsl = st % 2
            if j == 0:
                for k in range(8):
                    bk = 6 + k % 2
                    ptb = PS[bk][:].bitcast(BF16)
                    for s_ in range(4):
                        S.add("pe", lambda e, s_=s_, k=k, ptb=ptb, tsl=tsl: e.transpose(out=ptb[:, s_ * 128:(s_ + 1) * 128], in_=xs[tsl][:, s_, k * 128:(k + 1) * 128], identity=C.ident_b[:]),
                              reads=[b_xs[tsl], bc], writes=[PB[bk]])
                    S.add("dve", lambda e, k=k, ptb=ptb: e.tensor_scalar(out=u2t[:, k, :], in0=ptb[:, 0:512], scalar1=cols[:, COL_G2 + k:COL_G2 + k + 1],
                                                                       scalar2=cols[:, COL_MOD + 24 + k:COL_MOD + 25 + k], op0=ALU.mult, op1=ALU.add),
                          reads=[PB[bk], C.b_cols], writes=[b_u2t])
                S.add("pool", lambda e, tsl=tsl: e.memset(yacc[tsl][:], 0.0), writes=[b_ya[tsl]])
            c2 = cnt % 2
            cnt += 1
            for f in range(2):
                for k in range(8):
                    S.add("pe", lambda e, f=f, k=k, sl=sl: e.matmul(PS[f][:], lhsT=wg[sl][:, k, f * 128:(f + 1) * 128], rhs=u2t[:, k, :], start=(k == 0), stop=(k == 7)),
                          reads=[b_wg[sl], b_u2t], writes=[PB[f]])
            for f in range(2):
                for k in range(8):
                    S.add("pe", lambda e, f=f, k=k, sl=sl: e.matmul(PS[2 + f][:], lhsT=wu[sl][:, k, f * 128:(f + 1) * 128], rhs=u2t[:, k, :], start=(k == 0), stop=(k == 7)),
                          reads=[b_wu[sl], b_u2t], writes=[PB[2 + f]])
            for f in range(2):
                S.add("act", lambda e, f=f, c2=c2: e.activation(out=sg[c2][:, f, :], in_=PS[f][:], func=AF.Silu), reads=[PB[f]], writes=[b_sg[c2]])
                S.add("dve", lambda e, f=f, c2=c2: e.tensor_tensor(out=hd[c2][:, f, :], in0=PS[2 + f][:], in1=sg[c2][:, f, :], op=ALU.mult),
                      reads=[PB[2 + f], b_sg[c2]], writes=[b_hd[c2]])

            def down(st=st, j=j, sl=sl, tsl=tsl, c2=c2):
                nonlocal rot
                for sub in range(4):
                    for dh in range(2):
                        bk = 4 + rot % 4
                        rot += 1
                        for f in range(2):
                            S.add("pe", lambda e, bk=bk, f=f, sub=sub, dh=dh: e.matmul(
                                PS[bk][:], lhsT=hd[c2][:, f, sub * 128:(sub + 1) * 128], rhs=wd[sl][:, f, dh * 512:(dh + 1) * 512], start=(f == 0), stop=(f == 1)),
                                reads=[b_hd[c2], b_wd[sl]], writes=[PB[bk]])
                        S.add("dve", lambda e, bk=bk, sub=sub, dh=dh: e.scalar_tensor_tensor(
                            out=yacc[tsl][:, sub, dh * 512:(dh + 1) * 512], in0=PS[bk][:], scalar=cgs[tsl][:, sub, j:j + 1], in1=yacc[tsl][:, sub, dh * 512:(dh + 1) * 512],
                            op0=ALU.mult, op1=ALU.add), reads=[PB[bk], b_cgs[tsl], b_ya[tsl]], writes=[b_ya[tsl]])
                if j == 7:
                    dma(S, "sp", W["YS"][st * 512:(st + 1) * 512, :].rearrange("(s p) d -> p s d", p=128), yacc[tsl][:], reads=[b_ya[tsl]])

            if "nodefer" in C.debug:
                down()
            else:
                if deferred:
                    deferred.pop()()
                deferred.append(down)
            if qi + 2 < len(seq):
                load_w(seq[qi + 2][0], seq[qi + 2][1], (qi + 2) % 3)
            if j == 0 and st + 1 < NT:
                load_tile(st + 1)
        while deferred:
            deferred.pop()()
        S.flush()
    with ExitStack() as _es:
        sb = lambda name, shape, dt: _es.enter_context(nc.sbuf_tensor(name, shape, dt))
        yg = [sb(f"yg{i}", [128, 4, D], F32) for i in range(3)]
        hp = [sb(f"hpf{i}", [128, 4, D], F32) for i in range(3)]
        gfin = sb("gfin_s", [128, D], F32)
        junk = sb("junk6", [128, D], BF16)
        ssq = sb("ssq6", [128, 8], F32)
        b_yg = [Buf("yg0"), Buf("yg1"), Buf("yg2")]
        b_hp = [Buf("hp0"), Buf("hp1"), Buf("hp2")]
        b_gfin, b_junk, b_ssq = Buf("gfin"), Buf("junk"), Buf("ssq")
        dma(S, "sp", gfin[:], I["g_final"].partition_broadcast(128), writes=[b_gfin])

        def fetch(t):
            sl = t % 3
            for s_ in range(4):
                blk = t * 4 + s_
                S.add("pool", lambda e, sl=sl, s_=s_, blk=blk: e.indirect_dma_start(
                    out=yg[sl][:, s_, :], out_offset=None, in_=W["YS"], in_offset=bass.IndirectOffsetOnAxis(ap=C.SLOT_I[:, blk:blk + 1], axis=0)),
                    reads=[C.b_rk], writes=[b_yg[sl]], dma=True)
            dma(S, "sp", hp[sl][:], W["hS"][t * 512:(t + 1) * 512, :].rearrange("(s p) d -> p s d", p=128), writes=[b_hp[sl]])

        fetch(0)
        fetch(1)
        for t in range(NTT):
            sl = t % 3
            if t + 2 < NTT:
                fetch(t + 2)
            r0 = t * 512
            S.add("dve", lambda e, sl=sl: e.tensor_tensor(out=yg[sl][:], in0=yg[sl][:], in1=C.gtf_bc[:].unsqueeze(1).broadcast_to([128, 4, D]), op=ALU.mult),
                  reads=[b_yg[sl], C.b_gt], writes=[b_yg[sl]])
            S.add("dve", lambda e, sl=sl: e.tensor_tensor(out=hp[sl][:], in0=hp[sl][:], in1=yg[sl][:], op=ALU.add), reads=[b_yg[sl], b_hp[sl]], writes=[b_hp[sl]])
            S.add("dve", lambda e: e.memset(ssq[:], 0.0), writes=[b_ssq])
            for s_ in range(4):
                S.add("act", lambda e, s_=s_, sl=sl: e.activation(out=junk[:], in_=hp[sl][:, s_, :], func=AF.Square, accum_out=ssq[:, s_:s_ + 1]),
                      reads=[b_hp[sl], b_ssq], writes=[b_junk, b_ssq])
            S.add("dve", lambda e: e.tensor_scalar(out=ssq[:, 4:8], in0=ssq[:, 0:4], scalar1=1.0 / D, scalar2=EPS, op0=ALU.mult, op1=ALU.add),
                  reads=[b_ssq], writes=[b_ssq])
            S.add("act", lambda e: e.activation(out=ssq[:, 4:8], in_=ssq[:, 4:8], func=AF.Sqrt), reads=[b_ssq], writes=[b_ssq])
            S.add("dve", lambda e: e.reciprocal(out=ssq[:, 4:8], in_=ssq[:, 4:8]), reads=[b_ssq], writes=[b_ssq])
            for s_ in range(4):
                S.add("dve", lambda e, s_=s_, sl=sl: e.scalar_tensor_tensor(out=hp[sl][:, s_, :], in0=hp[sl][:, s_, :], scalar=ssq[:, 4 + s_:5 + s_], in1=gfin[:], op0=ALU.mult, op1=ALU.mult),
                      reads=[b_hp[sl], b_ssq, b_gfin], writes=[b_hp[sl]])
            dma(S, "sp", C.out[r0:r0 + 512, :].rearrange("(s p) d -> p s d", p=128), hp[sl][:], reads=[b_hp[sl]])
        S.flush()


_NC_CACHE = {}


def make_in_maps(inputs):
    maps = []
    shared = {}
    for k, v in inputs.items():
        if k in ("x", "c"):
            continue
        a = np.asarray(v)
        if k != "g_final":
            a = a[0]
        shared[k] = np.ascontiguousarray(a, dtype=np.float32)
    for b in range(8):
        m = dict(shared)
        m["x"] = np.ascontiguousarray(np.asarray(inputs["x"])[b], dtype=np.float32)
        m["c"] = np.ascontiguousarray(np.asarray(inputs["c"])[b], dtype=np.float32)
        maps.append(m)
    return maps


def kernel(**inputs):
    if "nc" not in _NC_CACHE:
        _NC_CACHE["nc"] = build()
    nc = _NC_CACHE["nc"]
    res = run_bass_kernel_spmd(nc, make_in_maps(inputs), core_ids=list(range(8)))
    return np.stack([np.asarray(r["out"], dtype=np.float32) for r in res.results], axis=0)


def _pstep(t):
    n = 1
    for s in list(t.shape)[1:]:
        n *= s
    return n


def view(t, off, dims, parts=128, p0=0):
    ps = _pstep(t)
    return bass.AP(t, p0 * ps + off, [[ps, parts]] + [list(d) for d in dims])


def phase2(C):
    nc, S, I, W = C.nc, C.S, C.I, C.W
    PS, PB = C.PS, C.PB
    cols = C.cols
    ident_f, ident_b = C.ident_f, C.ident_b
    bc = C.b_const
    with ExitStack() as _es:
        Btr = _es.enter_context(nc.sbuf_tensor("Btr", [128, 32, 64], BF16))
        Bti = _es.enter_context(nc.sbuf_tensor("Bti", [128, 32, 64], BF16))
        Ctr = _es.enter_context(nc.sbuf_tensor("Ctr", [64, 32, 128], F32))
        Cti = _es.enter_context(nc.sbuf_tensor("Cti", [64, 32, 128], F32))
        Dt = _es.enter_context(nc.sbuf_tensor("Dt", [128, 32, 128], BF16))
        P12 = _es.enter_context(nc.sbuf_tensor("P12", [64, 2, 64], F32))
        PP = _es.enter_context(nc.sbuf_tensor("PP", [64, 9, 2, 64], F32))
        apw = _es.enter_context(nc.sbuf_tensor("apw", [64, 2, 9, 32], F32))
        dbc = _es.enter_context(nc.sbuf_tensor("dbc", [128, 512], F32))
        wglu = _es.enter_context(nc.sbuf_tensor("wglu", [128, 4, 512], BF16))
        b_wt = Buf("s5w")
        b_wglu = Buf("wglu")
        b_dbc = Buf("dbc")
        dma(S, "pool", wglu[:], I["w_glu"].rearrange("(k p) n -> p k n", p=128), writes=[b_wglu])
        dma(S, "sp", dbc[:], I["s5_d"].partition_broadcast(128), writes=[b_dbc])
        with ExitStack() as _es:
            nat = _es.enter_context(nc.sbuf_tensor("nat", [128, 2, 64], F32))
            cnat = _es.enter_context(nc.sbuf_tensor("cnat", [128, 8, 64], F32))
            sm = _es.enter_context(nc.sbuf_tensor("sm", [64, 24, 32], F32))
            pw = _es.enter_context(nc.sbuf_tensor("pw", [64, 2, 9, 32], F32))
            Bn = _es.enter_context(nc.sbuf_tensor("Bn", [64, 2, 32, 16], F32))
            Bb = _es.enter_context(nc.sbuf_tensor("Bb", [64, 2, 32, 16], F32))
            CT = _es.enter_context(nc.sbuf_tensor("CT", [64, 3, 32, 16], F32))
            Ere = _es.enter_context(nc.sbuf_tensor("Ere", [64, 32, 15, 16], F32))
            Eim = _es.enter_context(nc.sbuf_tensor("Eim", [64, 32, 15, 16], F32))
            tmpB = _es.enter_context(nc.sbuf_tensor("tmpB", [64, 2, 32, 16], F32))
            halfpi = _es.enter_context(nc.sbuf_tensor("halfpi", [64, 1], F32))
            b_nat, b_cnat, b_sm, b_pw, b_Bn, b_Bb, b_CT, b_E, b_tmp = [Buf(n) for n in "nat cnat sm pw Bn Bb CT E tmp".split()]
            S.add("dve", lambda e: e.memset(nat[:], 0.0), writes=[b_nat])
            dma(S, "sp", nat[0:32, 0, :], I["s5_a_re"], writes=[b_nat])
            dma(S, "sp", nat[0:32, 1, :], I["s5_a_im"], writes=[b_nat])
            dma(S, "sp", cnat[:, 0:4, :], I["s5_c_re"].rearrange("g c p -> (g c) p").rearrange("(t q) p -> q t p", q=128), writes=[b_cnat])
            dma(S, "sp", cnat[:, 4:8, :], I["s5_c_im"].rearrange("g c p -> (g c) p").rearrange("(t q) p -> q t p", q=128), writes=[b_cnat])
            dma(S, "sp", Bn[:, 0, :, :], I["s5_b_re"].rearrange("g p c -> p g c"), writes=[b_Bn])
            dma(S, "sp", Bn[:, 1, :, :], I["s5_b_im"].rearrange("g p c -> p g c"), writes=[b_Bn])
            LR, LI, DT, AR, AI, T0, T1, T2, FR, FI, DEN, NR = range(12)
            dma(S, "sp", sm[:, DT, :], I["s5_log_dt"].partition_broadcast(64), writes=[b_sm])
            S.add("dve", lambda e: e.memset(halfpi[:], math.pi / 2), writes=[b_sm])
            for i, slot in ((0, LR), (1, LI)):
                S.add("pe", lambda e, i=i: e.transpose(out=PS[0][0:64, i * 128:(i + 1) * 128], in_=nat[:, i, :], identity=ident_f[:]),
                      reads=[b_nat, bc], writes=[PB[0]])
                S.add("dve", lambda e, i=i, slot=slot: e.tensor_copy(out=sm[:, slot, :], in_=PS[0][0:64, i * 128:i * 128 + 32]),
                      reads=[PB[0]], writes=[b_sm])
            for ri in range(2):
                for t in range(4):
                    S.add("pe", lambda e, ri=ri, t=t: e.transpose(out=PS[1][0:64, t * 128:(t + 1) * 128], in_=cnat[:, ri * 4 + t, :], identity=ident_f[:]),
                          reads=[b_cnat, bc], writes=[PB[1]])
                S.add("dve", lambda e, ri=ri: e.tensor_copy(out=CT[:, ri, :, :], in_=PS[1][0:64, :].rearrange("p (g c) -> p g c", c=16)),
                      reads=[PB[1]], writes=[b_CT])
            S.add("dve", lambda e: e.tensor_scalar(out=CT[:, 2, :, :], in0=CT[:, 1, :, :], scalar1=-1.0, scalar2=None, op0=ALU.mult),
                  reads=[b_CT], writes=[b_CT])

            def sv(i):
                return sm[:, i, :]

            def dv(fn, **kw):
                S.add("dve", lambda e: getattr(e, fn)(**kw), reads=[b_sm, b_pw], writes=[b_sm, b_pw])

            def av(**kw):
                S.add("act", lambda e: e.activation(**kw), reads=[b_sm, b_pw], writes=[b_sm, b_pw])

            av(out=sv(DT), in_=sv(DT), func=AF.Exp)
            dv("tensor_tensor", out=sv(T0), in0=sv(LR), in1=sv(DT), op=ALU.mult)
            av(out=sv(T0), in_=sv(T0), func=AF.Exp, scale=1.0 / 16)
            dv("tensor_tensor", out=sv(T1), in0=sv(LI), in1=sv(DT), op=ALU.mult)
            av(out=sv(AI), in_=sv(T1), func=AF.Sin, scale=1.0 / 16)
            av(out=sv(AR), in_=sv(T1), func=AF.Sin, scale=1.0 / 16, bias=halfpi[:, 0:1])
            dv("tensor_tensor", out=sv(AR), in0=sv(AR), in1=sv(T0), op=ALU.mult)
            dv("tensor_tensor", out=sv(AI), in0=sv(AI), in1=sv(T0), op=ALU.mult)
            for _ in range(4):
                dv("tensor_tensor", out=sv(T0), in0=sv(AR), in1=sv(AR), op=ALU.mult)
                dv("tensor_tensor", out=sv(T1), in0=sv(AI), in1=sv(AI), op=ALU.mult)
                dv("tensor_tensor", out=sv(T2), in0=sv(AR), in1=sv(AI), op=ALU.mult)
                dv("tensor_tensor", out=sv(AR), in0=sv(T0), in1=sv(T1), op=ALU.subtract)
                dv("tensor_scalar", out=sv(AI), in0=sv(T2), scalar1=2.0, scalar2=None, op0=ALU.mult)
            dv("tensor_tensor", out=sv(T0), in0=sv(LR), in1=sv(LR), op=ALU.mult)
            dv("tensor_tensor", out=sv(T1), in0=sv(LI), in1=sv(LI), op=ALU.mult)
            dv("tensor_tensor", out=sv(DEN), in0=sv(T0), in1=sv(T1), op=ALU.add)
            dv("reciprocal", out=sv(DEN), in_=sv(DEN))
            dv("tensor_scalar", out=sv(NR), in0=sv(AR), scalar1=-1.0, scalar2=None, op0=ALU.add)
            dv("tensor_tensor", out=sv(T0), in0=sv(NR), in1=sv(LR), op=ALU.mult)
            dv("tensor_tensor", out=sv(T1), in0=sv(AI), in1=sv(LI), op=ALU.mult)
            dv("tensor_tensor", out=sv(FR), in0=sv(T0), in1=sv(T1), op=ALU.add)
            dv("tensor_tensor", out=sv(FR), in0=sv(FR), in1=sv(DEN), op=ALU.mult)
            dv("tensor_tensor", out=sv(T0), in0=sv(AI), in1=sv(LR), op=ALU.mult)
            dv("tensor_tensor", out=sv(T1), in0=sv(NR), in1=sv(LI), op=ALU.mult)
            dv("tensor_tensor", out=sv(FI), in0=sv(T0), in1=sv(T1), op=ALU.subtract)
            dv("tensor_tensor", out=sv(FI), in0=sv(FI), in1=sv(DEN), op=ALU.mult)
            dv("memset", ap=pw[:, 0, 0, :], constant=1.0)
            dv("memset", ap=pw[:, 1, 0, :], constant=0.0)
            for t in range(8):
                dv("tensor_tensor", out=sv(T0), in0=pw[:, 0, t, :], in1=sv(AR), op=ALU.mult)
                dv("tensor_tensor", out=sv(T1), in0=pw[:, 1, t, :], in1=sv(AI), op=ALU.mult)
                dv("tensor_tensor", out=pw[:, 0, t + 1, :], in0=sv(T0), in1=sv(T1), op=ALU.subtract)
                dv("tensor_tensor", out=sv(T0), in0=pw[:, 0, t, :], in1=sv(AI), op=ALU.mult)
                dv("tensor_tensor", out=sv(T1), in0=pw[:, 1, t, :], in1=sv(AR), op=ALU.mult)
                dv("tensor_tensor", out=pw[:, 1, t + 1, :], in0=sv(T0), in1=sv(T1), op=ALU.add)
            dv("tensor_copy", out=P12[:, 0, 0:32], in_=pw[:, 0, 8, :])
            dv("tensor_copy", out=P12[:, 0, 32:64], in_=pw[:, 0, 8, :])
            dv("tensor_scalar", out=P12[:, 1, 0:32], in0=pw[:, 1, 8, :], scalar1=-1.0, scalar2=None, op0=ALU.mult)
            dv("tensor_copy", out=P12[:, 1, 32:64], in_=pw[:, 1, 8, :])
            dv("memset", ap=apw[:, 0, 0, :], constant=1.0)
            dv("memset", ap=apw[:, 1, 0, :], constant=0.0)
            for t in range(8):
                dv("tensor_tensor", out=sv(T0), in0=apw[:, 0, t, :], in1=pw[:, 0, 8, :], op=ALU.mult)
                dv("tensor_tensor", out=sv(T1), in0=apw[:, 1, t, :], in1=pw[:, 1, 8, :], op=ALU.mult)
                dv("tensor_tensor", out=apw[:, 0, t + 1, :], in0=sv(T0), in1=sv(T1), op=ALU.subtract)
                dv("tensor_tensor", out=sv(T0), in0=apw[:, 0, t, :], in1=pw[:, 1, 8, :], op=ALU.mult)
                dv("tensor_tensor", out=sv(T1), in0=apw[:, 1, t, :], in1=pw[:, 0, 8, :], op=ALU.mult)
                dv("tensor_tensor", out=apw[:, 1, t + 1, :], in0=sv(T0), in1=sv(T1), op=ALU.add)
            for i in range(9):
                dv("tensor_copy", out=PP[:, i, 0, 0:32], in_=apw[:, 0, i, :])
                dv("tensor_copy", out=PP[:, i, 0, 32:64], in_=apw[:, 0, i, :])
                dv("tensor_scalar", out=PP[:, i, 1, 0:32], in0=apw[:, 1, i, :], scalar1=-1.0, scalar2=None, op0=ALU.mult)
                dv("tensor_copy", out=PP[:, i, 1, 32:64], in_=apw[:, 1, i, :])

            def bcast_g(ap2):
                return ap2.unsqueeze(2).broadcast_to([64, 32, 16])

            def big(fn, reads, writes, **kw):
                S.add("dve", lambda e: getattr(e, fn)(**kw), reads=reads, writes=writes)

            RB = [b_sm, b_pw, b_Bn, b_Bb, b_tmp, b_CT]
            big("tensor_tensor", RB, [b_tmp], out=tmpB[:, 0], in0=Bn[:, 0], in1=bcast_g(sv(FR)), op=ALU.mult)
            big("tensor_tensor", RB, [b_tmp], out=tmpB[:, 1], in0=Bn[:, 1], in1=bcast_g(sv(FI)), op=ALU.mult)
            big("tensor_tensor", RB, [b_Bb], out=Bb[:, 0], in0=tmpB[:, 0], in1=tmpB[:, 1], op=ALU.subtract)
            big("tensor_tensor", RB, [b_tmp], out=tmpB[:, 0], in0=Bn[:, 1], in1=bcast_g(sv(FR)), op=ALU.mult)
            big("tensor_tensor", RB, [b_tmp], out=tmpB[:, 1], in0=Bn[:, 0], in1=bcast_g(sv(FI)), op=ALU.mult)
            big("tensor_tensor", RB, [b_Bb], out=Bb[:, 1], in0=tmpB[:, 0], in1=tmpB[:, 1], op=ALU.add)
            if "dbg_s5w" in C.debug:
                C.dbg_s5w = nc.dram_tensor("dbg_s5w", [64, 4 * 32 + 2 * 512], F32, kind="ExternalOutput").ap()
                dma(S, "sp", C.dbg_s5w[:, 0:32], pw[:, 0, 1, :], reads=[b_pw])
                dma(S, "sp", C.dbg_s5w[:, 32:64], pw[:, 1, 1, :], reads=[b_pw])
                dma(S, "sp", C.dbg_s5w[:, 64:96], pw[:, 0, 8, :], reads=[b_pw])
                dma(S, "sp", C.dbg_s5w[:, 96:128], pw[:, 1, 8, :], reads=[b_pw])
                dma(S, "sp", C.dbg_s5w[:, 128:640], Bb[:, 0].rearrange("p g c -> p (g c)"), reads=[b_Bb])
                dma(S, "sp", C.dbg_s5w[:, 640:1152], Bb[:, 1].rearrange("p g c -> p (g c)"), reads=[b_Bb])
            S.add("pool", lambda e: e.memset(Ere[:], 0.0), writes=[b_E])
            S.add("pool", lambda e: e.memset(Eim[:], 0.0), writes=[b_E])
            for m in range(8):
                t = 7 - m
                pr, pi = bcast_g(pw[:, 0, t, :]), bcast_g(pw[:, 1, t, :])
                big("tensor_tensor", RB, [b_tmp], out=tmpB[:, 0], in0=Bb[:, 0], in1=pr, op=ALU.mult)
                big("tensor_tensor", RB, [b_tmp], out=tmpB[:, 1], in0=Bb[:, 1], in1=pi, op=ALU.mult)
                big("tensor_tensor", RB + [b_E], [b_E], out=Ere[:, :, m, :], in0=tmpB[:, 0], in1=tmpB[:, 1], op=ALU.subtract)
                big("tensor_tensor", RB, [b_tmp], out=tmpB[:, 0], in0=Bb[:, 0], in1=pi, op=ALU.mult)
                big("tensor_tensor", RB, [b_tmp], out=tmpB[:, 1], in0=Bb[:, 1], in1=pr, op=ALU.mult)
                big("tensor_tensor", RB + [b_E], [b_E], out=Eim[:, :, m, :], in0=tmpB[:, 0], in1=tmpB[:, 1], op=ALU.add)
            Ctr4 = Ctr[:].rearrange("p g (j c) -> p g j c", c=16)
            Cti4 = Cti[:].rearrange("p g (j c) -> p g j c", c=16)
            for j in range(8):
                pr, pi = bcast_g(pw[:, 0, j + 1, :]), bcast_g(pw[:, 1, j + 1, :])
                big("tensor_tensor", RB, [b_tmp], out=tmpB[:, 0], in0=CT[:, 0], in1=pr, op=ALU.mult)
                big("tensor_tensor", RB, [b_tmp], out=tmpB[:, 1], in0=CT[:, 1], in1=pi, op=ALU.mult)
                big("tensor_tensor", RB + [b_wt], [b_wt], out=Ctr4[:, :, j, :], in0=tmpB[:, 0], in1=tmpB[:, 1], op=ALU.subtract)
                big("tensor_tensor", RB, [b_tmp], out=tmpB[:, 0], in0=CT[:, 0], in1=pi, op=ALU.mult)
                big("tensor_tensor", RB, [b_tmp], out=tmpB[:, 1], in0=CT[:, 2], in1=pr, op=ALU.mult)
                big("tensor_tensor", RB + [b_wt], [b_wt], out=Cti4[:, :, j, :], in0=tmpB[:, 1], in1=tmpB[:, 0], op=ALU.subtract)
            rot = 0
            for g0 in range(0, 32, 4):
                for ri, (Et, Bt) in enumerate(((Ere, Btr), (Eim, Bti))):
                    bank = 2 + (rot % 2)
                    rot += 1
                    for gg in range(4):
                        g = g0 + gg
                        S.add("pe", lambda e, Et=Et, g=g, gg=gg, bank=bank: e.transpose(
                            out=PS[bank][:, gg * 64:(gg + 1) * 64], in_=Et[:, g, 0:8, :].rearrange("p m c -> p (m c)"), identity=ident_f[0:64, 0:64]),
                            reads=[b_E, bc], writes=[PB[bank]])
                    S.add("dve", lambda e, Bt=Bt, g0=g0, bank=bank: e.tensor_copy(out=Bt[:, g0:g0 + 4, :],
                                                                             in_=PS[bank][:, 0:256].rearrange("p (g q) -> p g q", q=64)),
                          reads=[PB[bank]], writes=[b_wt])
                bank = 4 + ((g0 // 4) % 2)
                for gg in range(4):
                    g = g0 + gg
                    for j in range(8):
                        o = PS[bank][:, gg * 128 + j * 16: gg * 128 + (j + 1) * 16]
                        S.add("pe", lambda e, o=o, g=g, j=j: e.matmul(o, lhsT=Ere[:, g, 7 - j:15 - j, :].rearrange("p m c -> p (m c)"), rhs=CT[:, 0, g, :],
                                                                    start=True, stop=False),
                              reads=[b_E, b_CT], writes=[PB[bank]])
                        S.add("pe", lambda e, o=o, g=g, j=j: e.matmul(o, lhsT=Eim[:, g, 7 - j:15 - j, :].rearrange("p m c -> p (m c)"), rhs=CT[:, 2, g, :],
                                                                    start=False, stop=True),
                              reads=[b_E, b_CT], writes=[PB[bank]])
                S.add("act", lambda e, g0=g0, bank=bank: e.activation(out=Dt[:, g0:g0 + 4, :], in_=PS[bank][:].rearrange("p (g q) -> p g q", q=128), func=AF.Copy),
                      reads=[PB[bank]], writes=[b_wt])
            S.flush()
        if "stop_s5w" in C.debug:
            return
        with ExitStack() as _es:
            Uc = _es.enter_context(nc.sbuf_tensor("Uc", [128, 8, 512], F32))
            UT = _es.enter_context(nc.sbuf_tensor("UT", [128, 32, 128], BF16))
            Ug = _es.enter_context(nc.sbuf_tensor("Ug", [128, 32, 128], BF16))
            b_Ug = Buf("Ug")
            Xh = _es.enter_context(nc.sbuf_tensor("Xh", [64, 129, 64], F32))
            st = _es.enter_context(nc.sbuf_tensor("st", [64, 2, 64], F32))
            stw = [_es.enter_context(nc.sbuf_tensor(f"stw{i}", [64, 2, 16, 64], F32)) for i in range(2)]
            b_stw = [[Buf("stw00"), Buf("stw01")], [Buf("stw10"), Buf("stw11")]]
            Yc = _es.enter_context(nc.sbuf_tensor("Yc", [128, 8, 512], F32))
            Gc = _es.enter_context(nc.sbuf_tensor("Gc", [128, 8, 512], BF16))
            geT = _es.enter_context(nc.sbuf_tensor("geT", [128, 4, 1024], BF16))
            y2s = _es.enter_context(nc.sbuf_tensor("y2s", [128, 4, 1024], BF16))
            sig = _es.enter_context(nc.sbuf_tensor("sig", [128, 512], BF16))
            b_Uc, b_UT, b_Xh, b_st, b_Yc, b_Gc, b_geT, b_y2s, b_sig, b_st1 = [Buf(n) for n in "Uc UT Xh st Yc Gc geT y2s sig st1".split()]
            s5v = W["s5S"].rearrange("(c i) f -> c (i f)", i=8)
            S.add("dve", lambda e: e.memset(Xh[:, 0, :], 0.0), writes=[b_Xh])
            prev_rest = []
            sig2 = [sig, _es.enter_context(nc.sbuf_tensor("sigB", [128, 512], BF16))]
            b_sig2 = [Buf("sig0"), Buf("sig1")]
            for T in range(4):
                dma(S, "sp", Uc[:].rearrange("p i f -> p (i f)"), s5v[T * 128:(T + 1) * 128, :], writes=[b_Uc])
                S.add("pool", lambda e: e.tensor_copy(out=Ug[:].rearrange("p g (i c) -> p g i c", c=16), in_=view(Uc, 0, [[16, 32], [512, 8], [1, 16]])),
                      reads=[b_Uc], writes=[b_Ug])
                for g0 in range(0, 32, 8):
                    bank = (g0 // 8) % 2
                    pb = PS[bank][:].bitcast(BF16)
                    for gg in range(8):
                        g = g0 + gg
                        S.add("pe", lambda e, g=g, gg=gg, pb=pb: e.transpose(out=pb[:, gg * 128:(gg + 1) * 128], in_=Ug[:, g, :], identity=ident_b[:]),
                              reads=[b_Ug, bc], writes=[PB[bank]])
                    S.add("act", lambda e, g0=g0, pb=pb: e.activation(out=UT[:, g0:g0 + 8, :], in_=pb[:, 0:1024].rearrange("p (g q) -> p g q", q=128), func=AF.Copy),
                          reads=[PB[bank]], writes=[b_UT])
                for g0 in range(0, 32, 4):
                    for ri, Bt in enumerate((Btr, Bti)):
                        bank = 2 + ri
                        for gg in range(4):
                            g = g0 + gg
                            S.add("pe", lambda e, Bt=Bt, g=g, gg=gg, bank=bank: e.matmul(PS[bank][0:64, gg * 128:(gg + 1) * 128], lhsT=Bt[:, g, :], rhs=UT[:, g, :],
                                                                                    start=True, stop=True),
                                  reads=[b_wt, b_UT], writes=[PB[bank]])
                        o = view(Xh, 64 + ri * 32 + g0, [[1, 4], [64, 128]], parts=64)
                        S.add("dve", lambda e, o=o, bank=bank: e.tensor_copy(out=o, in_=PS[bank][0:64, :].rearrange("p (g c) -> p g c", c=128)),
                              reads=[PB[bank]], writes=[b_Xh])
                if len(prev_rest) > 0:
                    prev_rest.pop()()
                def cmul_acc(dst0, src0, nb, i, k):
                    dst = view(Xh, dst0 * 64, [[512, nb], [1, 64]], parts=64)
                    src = view(Xh, src0 * 64, [[512, nb], [1, 64]], parts=64)
                    srcw = view(Xh, src0 * 64 + 32, [[512, nb], [-32, 2], [1, 32]], parts=64)
                    t1 = stw[k][:, 0, 0:nb, :]
                    t2 = stw[k][:, 1, 0:nb, :]
                    S.add("dve", lambda e: e.tensor_tensor(out=t1, in0=src, in1=PP[:, i, 0, :].unsqueeze(1).broadcast_to([64, nb, 64]), op=ALU.mult),
                          reads=[b_Xh], writes=[b_stw[k][0]])
                    S.add("dve", lambda e: e.tensor_tensor(out=t2.rearrange("p b (a c) -> p b a c", a=2), in0=srcw,
                                                           in1=PP[:, i, 1, :].rearrange("p (a c) -> p a c", a=2).unsqueeze(1).broadcast_to([64, nb, 2, 32]), op=ALU.mult),
                          reads=[b_Xh], writes=[b_stw[k][1]])
                    S.add("dve", lambda e: e.tensor_tensor(out=t1, in0=t1, in1=t2, op=ALU.add), reads=[b_stw[k][0], b_stw[k][1]], writes=[b_stw[k][0]])
                    S.add("dve", lambda e: e.tensor_tensor(out=dst, in0=dst, in1=t1, op=ALU.add), reads=[b_stw[k][0], b_Xh], writes=[b_Xh])

                if "s5_noscan" not in C.debug:
                    for i in range(1, 8):
                        cmul_acc(i + 1, i, 16, 1, i % 2)
                    for b in range(16):
                        cmul_acc(b * 8 + 8, b * 8, 1, 8, b % 2)
                    for i in range(1, 8):
                        cmul_acc(i, 0, 16, i, i % 2)
                if "s5_noY" in C.debug:
                    continue
                S.add("pool", lambda e: e.tensor_tensor(out=Yc[:], in0=Uc[:], in1=dbc[:].unsqueeze(1).broadcast_to([128, 8, 512]), op=ALU.mult),
                      reads=[b_Uc, b_dbc], writes=[b_Yc])
                for g0 in range(0, 32, 4):
                    bank = 4 + (g0 // 4) % 2
                    for gg in range(4):
                        g = g0 + gg
                        o = PS[bank][:, gg * 128:(gg + 1) * 128]
                        S.add("pe", lambda e, o=o, g=g: e.matmul(o, lhsT=UT[:, g, :], rhs=Dt[:, g, :], start=True, stop=False),
                              reads=[b_UT, b_wt], writes=[PB[bank]])
                        xr = view(Xh, g, [[64, 128]], parts=64)
                        xi = view(Xh, 32 + g, [[64, 128]], parts=64)
                        S.add("pe", lambda e, o=o, g=g, xr=xr: e.matmul(o, lhsT=xr, rhs=Ctr[:, g, :], start=False, stop=False),
                              reads=[b_Xh, b_wt], writes=[PB[bank]])
                        S.add("pe", lambda e, o=o, g=g, xi=xi: e.matmul(o, lhsT=xi, rhs=Cti[:, g, :], start=False, stop=True),
                              reads=[b_Xh, b_wt], writes=[PB[bank]])
                    yv = view(Yc, g0 * 16, [[16, 4], [512, 8], [1, 16]])
                    S.add("dve", lambda e, yv=yv, bank=bank: e.tensor_tensor(out=yv, in0=PS[bank][:].rearrange("p (g j c) -> p g j c", g=4, j=8), in1=yv, op=ALU.add),
                          reads=[PB[bank], b_Yc], writes=[b_Yc])
                if T < 3:
                    S.add("dve", lambda e: e.tensor_copy(out=Xh[:, 0, :], in_=Xh[:, 128, :]), reads=[b_Xh], writes=[b_Xh])
                if "dbg_s5y" in C.debug:
                    for i in range(8):
                        dma(S, "sp", C.dbg_s5y.rearrange("(c i) f -> c i f", i=8)[T * 128:(T + 1) * 128, i, :], Yc[:, i, :], reads=[b_Yc])
                def rest(T=T):
                    for i in range(8):
                        S.add("act", lambda e, i=i: e.activation(out=Gc[:, i, :], in_=Yc[:, i, :], func=AF.Gelu_apprx_tanh), reads=[b_Yc], writes=[b_Gc])
                    for ct in range(4):
                        bank = 6 + (ct % 2)
                        pb = PS[bank][:].bitcast(BF16)
                        for j in range(8):
                            S.add("pe", lambda e, pb=pb, j=j, ct=ct: e.transpose(out=pb[:, j * 128:(j + 1) * 128], in_=Gc[:, j, ct * 128:(ct + 1) * 128], identity=ident_b[:]),
                                  reads=[b_Gc, bc], writes=[PB[bank]])
                        o = view(geT, ct * 1024, [[1, 8], [8, 128]])
                        S.add("act", lambda e, o=o, pb=pb: e.activation(out=o, in_=pb[:, 0:1024].rearrange("p (j c) -> p j c", j=8), func=AF.Copy), reads=[PB[bank]], writes=[b_geT])
                    for co in range(4):
                        for hh in range(2):
                            q2 = (co * 2 + hh) % 2
                            bank = 6 + q2
                            for ci in range(4):
                                S.add("pe", lambda e, co=co, hh=hh, ci=ci, bank=bank: e.matmul(PS[bank][:], lhsT=wglu[:, ci, co * 128:(co + 1) * 128],
                                                                                          rhs=geT[:, ci, hh * 512:(hh + 1) * 512], start=(ci == 0), stop=(ci == 3)),
                                      reads=[b_wglu, b_geT], writes=[PB[bank]])
                            S.add("act", lambda e, co=co, bank=bank, q2=q2: e.activation(out=sig2[q2][:], in_=PS[bank][:], func=AF.Sigmoid,
                                                                                     bias=cols[:, COL_BGLU + co:COL_BGLU + co + 1], scale=1.0),
                                  reads=[PB[bank], C.b_cols], writes=[b_sig2[q2]])
                            S.add("pool", lambda e, co=co, hh=hh, q2=q2: e.tensor_tensor(out=y2s[:, co, hh * 512:(hh + 1) * 512], in0=geT[:, co, hh * 512:(hh + 1) * 512],
                                                                                      in1=sig2[q2][:], op=ALU.mult),
                                  reads=[b_geT, b_sig2[q2]], writes=[b_y2s])
                    dma(S, "sp", W["y2T"][:, T * 1024:(T + 1) * 1024].rearrange("(c p) n -> p c n", p=128), y2s[:], reads=[b_y2s])

                prev_rest.append(rest)
            while prev_rest:
                prev_rest.pop()()
            S.flush()


_dummy = {}


def b_sm_dummy(C):
    if "b" not in _dummy:
        _dummy["b"] = Buf("dummy")
    return _dummy["b"]


def phase3(C):
    nc, S, I, W = C.nc, C.S, C.I, C.W
    PS, PB = C.PS, C.PB
    cols = C.cols
    bc = C.b_const
    NQ = 8
    heads = C.heads if hasattr(C, "heads") else range(8)
    with ExitStack() as _es:
        sb = lambda name, shape, dt: _es.enter_context(nc.sbuf_tensor(name, shape, dt))
        lqk = sb("lqk", [128, 4, 64], F32)
        lsc = sb("lsc", [128, 8], F32)
        masks = sb("masks", [128, 4, 512], BF16)
        kTh = [sb(f"kTh{i}", [128, L], BF16) for i in range(2)]
        qTh = [sb(f"qTh{i}", [128, L], BF16) for i in range(2)]
        Vh = [sb(f"Vh{i}", [128, 32, 128], BF16) for i in range(2)]
        Et = sb("Et", [128, 8, 512], BF16)
        rr = sb("rr", [128, 2, 512], F32)
        oo = sb("oo", [128, 2, 512], F32)
        sq = sb("sq", [128, 512], BF16)
        ons = [sb(f"ons{i}", [128, 512], BF16) for i in range(2)]
        b_l, b_mask = Buf("lam"), Buf("mask")
        b_k = [Buf("k0"), Buf("k1")]
        b_q = [Buf("q0"), Buf("q1")]
        b_v = [Buf("v0"), Buf("v1")]
        b_E = [Buf(f"E{i}") for i in range(8)]
        b_rr, b_oo, b_sq = Buf("rr"), Buf("oo"), Buf("sq")
        b_ons = [Buf("ons0"), Buf("ons1")]
        for i, n in enumerate(["lambda_q1", "lambda_k1", "lambda_q2", "lambda_k2"]):
            dma(S, "sp", lqk[:, i, :], I[n].partition_broadcast(128), writes=[b_l])
        S.add("dve", lambda e: e.tensor_tensor(out=lqk[:, 0, :], in0=lqk[:, 0, :], in1=lqk[:, 1, :], op=ALU.mult), reads=[b_l], writes=[b_l])
        S.add("dve", lambda e: e.tensor_tensor(out=lqk[:, 2, :], in0=lqk[:, 2, :], in1=lqk[:, 3, :], op=ALU.mult), reads=[b_l], writes=[b_l])
        S.add("dve", lambda e: e.reduce_sum(out=lsc[:, 0:1], in_=lqk[:, 0, :], axis=AX.X), reads=[b_l], writes=[b_l])
        S.add("dve", lambda e: e.reduce_sum(out=lsc[:, 1:2], in_=lqk[:, 2, :], axis=AX.X), reads=[b_l], writes=[b_l])
        S.add("act", lambda e: e.activation(out=lsc[:, 2:4], in_=lsc[:, 0:2], func=AF.Exp), reads=[b_l], writes=[b_l])
        S.add("dve", lambda e: e.tensor_tensor(out=lsc[:, 4:5], in0=lsc[:, 3:4], in1=lsc[:, 2:3], op=ALU.subtract), reads=[b_l], writes=[b_l])
        S.add("dve", lambda e: e.tensor_scalar(out=lsc[:, 5:6], in0=lsc[:, 4:5], scalar1=-LAMBDA_INIT, scalar2=None, op0=ALU.add), reads=[b_l], writes=[b_l])
        nlam = lsc[:, 5:6]
        S.add("pool", lambda e: e.memset(masks[:], 0.0), writes=[b_mask])
        for r in range(4):
            S.add("pool", lambda e, r=r: e.memset(masks[0:64, r, 128 * r:512], 1.0), writes=[b_mask])
            S.add("pool", lambda e, r=r: e.memset(masks[64:128, r, 128 * r + 64:512], 1.0), writes=[b_mask])

        def load_head(h, slot):
            dma(S, "sp", kTh[slot][:], W["kT"][h * 128:(h + 1) * 128, :], writes=[b_k[slot]])
            dma(S, "sp", qTh[slot][:], W["qT"][h * 128:(h + 1) * 128, :], writes=[b_q[slot]])
            dma(S, "sp", Vh[slot][:], W["vS"][:, h * 128:(h + 1) * 128].rearrange("(j p) e -> p j e", p=128), writes=[b_v[slot]])

        hl = list(heads)
        load_head(hl[0], 0)
        ecnt = 0
        ocnt = 0
        lnt = sb("lnt", [128, 2, 512], F32)
        b_ln = Buf("lnt")
        wst = [sb(f"wst{i}", [128, 6144], BF16) for i in range(2)]
        b_wst = [Buf("wst0"), Buf("wst1")]

        def precast_load(ex):
            sl = ex % 2
            dma(S, "pool", wst[sl][:, 0:2048].rearrange("p (k f) -> p k f", f=256), I["w_exp_gate"][ex].rearrange("(k p) f -> p k f", p=128), writes=[b_wst[sl]])
            dma(S, "pool", wst[sl][:, 2048:4096].rearrange("p (k f) -> p k f", f=256), I["w_exp_up"][ex].rearrange("(k p) f -> p k f", p=128), writes=[b_wst[sl]])
            dma(S, "pool", wst[sl][:, 4096:6144].rearrange("p (k d) -> p k d", d=D), I["w_exp_down"][ex].rearrange("(k p) d -> p k d", p=128), writes=[b_wst[sl]])

        def precast_store(ex):
            sl = ex % 2
            for c3 in range(3):
                dma(S, "sp", W["WALL"][ex * 128:(ex + 1) * 128, c3 * 2048:(c3 + 1) * 2048], wst[sl][:, c3 * 2048:(c3 + 1) * 2048], reads=[b_wst[sl]])

        unit = 0
        e2cnt = 0
        EPI_STEP = int(C.epi_step) if hasattr(C, "epi_step") else 8
        Es2 = sb("Es2", [128, 4, 512], BF16)
        b_E2 = [Buf(f"E2_{i}") for i in range(4)]
        pending = []

        def epi_fast(h, Q):
            S.add("act", lambda e: e.activation(out=oo[:, 0, :], in_=PS[4][:], func=AF.Copy), reads=[PB[4]], writes=[b_oo])
            S.add("act", lambda e: e.activation(out=oo[:, 1, :], in_=PS[5][:], func=AF.Copy), reads=[PB[5]], writes=[b_oo])
            S.add("dve", lambda e: e.tensor_copy(out=lnt[:, 0, :], in_=PS[6][:]), reads=[PB[6]], writes=[b_ln])
            S.add("dve", lambda e: e.tensor_copy(out=lnt[:, 1, :], in_=PS[7][:]), reads=[PB[7]], writes=[b_ln])
            S.add("dve", lambda e: e.reciprocal(out=rr[:, 0, :], in_=lnt[:, 1, :]), reads=[b_ln], writes=[b_rr])
            S.add("dve", lambda e: e.tensor_tensor(out=rr[:, 0, :], in0=rr[:, 0, :], in1=lnt[:, 0, :], op=ALU.mult), reads=[b_rr, b_ln], writes=[b_rr])
            S.add("dve", lambda e: e.tensor_tensor(out=oo[:, 1, :], in0=oo[:, 1, :], in1=rr[:, 0, :], op=ALU.mult), reads=[b_oo, b_rr], writes=[b_oo])
            S.add("dve", lambda e: e.scalar_tensor_tensor(out=oo[:, 0, :], in0=oo[:, 1, :], scalar=nlam, in1=oo[:, 0, :], op0=ALU.mult, op1=ALU.add),
                  reads=[b_oo, b_l], writes=[b_oo])
            S.add("pool", lambda e: e.tensor_tensor(out=sq[:], in0=oo[:, 0, :], in1=oo[:, 0, :], op=ALU.mult), reads=[b_oo], writes=[b_sq])
            S.add("dve", lambda e: e.scalar_tensor_tensor(out=rr[:, 1, :], in0=lnt[:, 0, :], scalar=EPS, in1=lnt[:, 0, :], op0=ALU.mult, op1=ALU.mult),
                  reads=[b_ln], writes=[b_rr])

        def epi_slow(h, Q):
            nonlocal ocnt
            S.add("pe", lambda e: e.matmul(PS[0][:], lhsT=C.ones_b[:], rhs=sq[:], start=True, stop=True), reads=[bc, b_sq], writes=[PB[0]])
            S.add("dve", lambda e: e.scalar_tensor_tensor(out=rr[:, 1, :], in0=PS[0][:], scalar=1.0 / 128, in1=rr[:, 1, :], op0=ALU.mult, op1=ALU.add),
                  reads=[PB[0], b_rr], writes=[b_rr])
            S.add("act", lambda e: e.activation(out=rr[:, 1, :], in_=rr[:, 1, :], func=AF.Ln), reads=[b_rr], writes=[b_rr])
            S.add("act", lambda e: e.activation(out=rr[:, 1, :], in_=rr[:, 1, :], func=AF.Exp, scale=-0.5), reads=[b_rr], writes=[b_rr])
            on = ons[ocnt % 2]
            bo = b_ons[ocnt % 2]
            ocnt += 1
            S.add("dve", lambda e, on=on: e.scalar_tensor_tensor(out=on[:], in0=oo[:, 0, :], scalar=cols[:, COL_GSUBS:COL_GSUBS + 1], in1=rr[:, 1, :],
                                                              op0=ALU.mult, op1=ALU.mult),
                  reads=[b_oo, b_rr, C.b_cols], writes=[bo])
            dma(S, "sp", W["onT"][h * 128:(h + 1) * 128, Q * 512:(Q + 1) * 512], on[:], reads=[bo])

        for hi, h in enumerate(hl):
            slot = hi % 2
            if hi + 1 < len(hl):
                load_head(hl[hi + 1], (hi + 1) % 2)
            kt, qt, vt = kTh[slot], qTh[slot], Vh[slot]
            bk, bq, bv = b_k[slot], b_q[slot], b_v[slot]
            for Q in range(NQ):
                nJ = 4 * (Q + 1)
                eslot = {}
                lready, lnext = [], []
                if C.sparse:
                    if 1 <= unit <= NEXP:
                        precast_store(unit - 1)
                    if unit < NEXP:
                        precast_load(unit)
                    if unit == NEXP + 1:
                        dma(S, "pool", C.wbs[:], I["w_br_ssm"].rearrange("(k p) n -> p k n", p=128), writes=[C.b_w4])
                        dma(S, "pool", C.wba[:], I["w_br_attn"].rearrange("(k p) n -> p k n", p=128), writes=[C.b_w4])
                        dma(S, "pool", C.wout[:], I["w_out"].rearrange("(k p) n -> p k n", p=128), writes=[C.b_w4])
                        C.w4_loaded = True
                    unit += 1
                for step in range(nJ + 1):
                    if step < nJ:
                        J = step
                        c0 = 0
                        for m in range(2):
                            bank = 2 * m + (J % 2)
                            S.add("pe", lambda e, bank=bank, m=m, J=J, Q=Q, kt=kt, qt=qt, c0=c0: e.matmul(
                                PS[bank][:, c0:512], lhsT=kt[m * 64:(m + 1) * 64, J * 128:(J + 1) * 128], rhs=qt[m * 64:(m + 1) * 64, Q * 512 + c0:(Q + 1) * 512],
                                start=True, stop=True), reads=[bk, bq], writes=[PB[bank]])
                            es = ecnt % 8
                            ecnt += 1
                            eslot[(J, m)] = es
                            S.add("act", lambda e, bank=bank, es=es, c0=c0: e.activation(out=Et[:, es, c0:512], in_=PS[bank][:, c0:512], func=AF.Exp, scale=0.125),
                                  reads=[PB[bank]], writes=[b_E[es]])
                            if J >= 4 * Q:
                                r = J - 4 * Q
                                S.add("dve", lambda e, es=es, r=r: e.tensor_tensor(out=Et[:, es, :], in0=Et[:, es, :], in1=masks[:, r, :], op=ALU.mult),
                                      reads=[b_E[es], b_mask], writes=[b_E[es]])
                    if step >= 1:
                        J = step - 1
                        c0 = 0
                        for m in range(2):
                            es = eslot[(J, m)]
                            S.add("pe", lambda e, m=m, J=J, es=es, vt=vt, nJ=nJ, c0=c0: e.matmul(PS[4 + m][:, c0:512], lhsT=vt[:, J, :], rhs=Et[:, es, c0:512],
                                                                                     start=(J == 0), stop=(J == nJ - 1)),
                                  reads=[bv, b_E[es]], writes=[PB[4 + m]])
                            S.add("pe", lambda e, m=m, J=J, es=es, nJ=nJ, c0=c0: e.matmul(PS[6 + m][:, c0:512], lhsT=C.ones_b[:], rhs=Et[:, es, c0:512],
                                                                              start=(J == 0), stop=(J == nJ - 1)),
                                  reads=[bc, b_E[es]], writes=[PB[6 + m]])
                    if step == min(EPI_STEP, nJ) and pending:
                        epi_slow(*pending.pop())
                epi_fast(h, Q)
                pending.append((h, Q))
        while pending:
            epi_slow(*pending.pop())
        S.flush()


def phase4a(C):
    nc, S, I, W = C.nc, C.S, C.I, C.W
    PS, PB = C.PS, C.PB
    cols = C.cols
    bc = C.b_const
    with ExitStack() as _es:
        sb = lambda name, shape, dt: _es.enter_context(nc.sbuf_tensor(name, shape, dt))
        wbs, wba, wout = C.wbs, C.wba, C.wout
        wr = sb("wr", [128, 8, 36], BF16)
        brt = sb("brt", [128, 36], F32)
        y2t = [sb(f"y2t{i}", [128, 4, 512], BF16) for i in range(2)]
        ont = [sb(f"ont{i}", [128, 8, 512], BF16) for i in range(2)]
        gtt = [sb(f"gtt{i}", [128, 16, 512], BF16) for i in range(2)]
        xt = sb("xt4", [128, 4, D], F32)
        mT = sb("mT", [128, 8, 512], BF16)
        tA = sb("tA", [128, 512], F32)
        tB = sb("tB", [128, 512], F32)
        hh_ = sb("h4", [128, 4, D], F32)
        xn2 = sb("xn2", [128, 4, D], BF16)
        junk = sb("junk4", [128, D], BF16)
        u2 = [sb(f"u2_{i}", [128, 8, 512], BF16) for i in range(2)]
        ssq = sb("ssq4", [128, 8], F32)
        rt = sb("rt", [128, 4, 36], F32)
        rw = sb("rw", [128, 24, 4], F32)
        ohg = sb("ohg", [128, 4, 4], F32)
        tE = sb("tE", [128, 4, 4, 8], F32)
        es = sb("es", [128, 6, 4, 8], F32)
        comb = [sb(f"comb{i}", [128, 4, 32], F32) for i in range(2)]
        ustr_f = sb("ustr_f", [128, 128], F32)
        ustr = sb("ustr", [128, 128], BF16)
        ohgb = sb("ohgb", [128, 4, 4], BF16)
        base = sb("base4", [128, 4, 4], F32)
        b_ohgb, b_base, b_tot = Buf("ohgb"), Buf("base"), Buf("tot")
        b_w = Buf("w4")
        if C.sparse:
            S.add("pool", lambda e: e.memset(ustr_f[:], 1.0), writes=[b_ohgb])
            S.add("pool", lambda e: e.affine_select(out=ustr_f[:], in_=ustr_f[:], pattern=[[1, 128]], compare_op=ALU.is_gt, fill=0.0, base=0, channel_multiplier=-1),
                  reads=[b_ohgb], writes=[b_ohgb])
            S.add("dve", lambda e: e.tensor_copy(out=ustr[:], in_=ustr_f[:]), reads=[b_ohgb], writes=[b_ohgb])
            S.add("dve", lambda e: e.memset(C.tot[:], 0.0), writes=[b_tot])
        if not getattr(C, "w4_loaded", False):
            dma(S, "pool", wbs[:], I["w_br_ssm"].rearrange("(k p) n -> p k n", p=128), writes=[b_w])
            dma(S, "pool", wba[:], I["w_br_attn"].rearrange("(k p) n -> p k n", p=128), writes=[b_w])
            dma(S, "pool", wout[:], I["w_out"].rearrange("(k p) n -> p k n", p=128), writes=[b_w])
        dma(S, "pool", wr[:, :, 0:4], I["w_router_grp"].rearrange("(k p) n -> p k n", p=128), writes=[b_w])
        dma(S, "pool", wr[:, :, 4:36], I["w_router_exp"].rearrange("(k p) n -> p k n", p=128), writes=[b_w])
        dma(S, "sp", brt[:, 0:4], I["b_router_grp"].partition_broadcast(128), writes=[b_w])
        dma(S, "sp", brt[:, 4:36], I["b_router_exp"].partition_broadcast(128), writes=[b_w])
        b_y2 = [Buf("y2t0"), Buf("y2t1")]
        b_on = [Buf("ont0"), Buf("ont1")]
        b_gt = [Buf("gtt0"), Buf("gtt1")]
        b_u2 = [Buf("u2_0"), Buf("u2_1")]
        b_cb = [Buf("comb0"), Buf("comb1")]
        b_xt, b_mT, b_tA, b_tB, b_h, b_xn2, b_junk, b_ssq, b_rt, b_rw = [Buf(n) for n in "xt mT tA tB h xn2 junk ssq rt rw".split()]

        def load(t):
            sl = t % 2
            dma(S, "sp", y2t[sl][:], W["y2T"][:, t * 512:(t + 1) * 512].rearrange("(c p) n -> p c n", p=128), writes=[b_y2[sl]])
            dma(S, "sp", ont[sl][:], W["onT"][:, t * 512:(t + 1) * 512].rearrange("(c p) n -> p c n", p=128), writes=[b_on[sl]])
            dma(S, "sp", gtt[sl][:], W["gT"][:, t * 512:(t + 1) * 512].rearrange("(c p) n -> p c n", p=128), writes=[b_gt[sl]])

        load(0)
        rot = 0
        routes = []
        for t in range(NTT):
            sl = t % 2
            if t + 1 < NTT:
                load(t + 1)
            dma(S, "sp", xt[:], I["x"][t * 512:(t + 1) * 512, :].rearrange("(s p) d -> p s d", p=128), writes=[b_xt])
            for ct in range(8):
                ba = rot % 2
                bb = 2 + rot % 2
                rot += 1
                for k in range(4):
                    S.add("pe", lambda e, ba=ba, k=k, ct=ct, sl=sl: e.matmul(PS[ba][:], lhsT=wbs[:, k, ct * 128:(ct + 1) * 128], rhs=y2t[sl][:, k, :],
                                                                        start=(k == 0), stop=(k == 3)), reads=[b_w, b_y2[sl]], writes=[PB[ba]])
                for k in range(8):
                    S.add("pe", lambda e, bb=bb, k=k, ct=ct, sl=sl: e.matmul(PS[bb][:], lhsT=wba[:, k, ct * 128:(ct + 1) * 128], rhs=ont[sl][:, k, :],
                                                                        start=(k == 0), stop=(k == 7)), reads=[b_w, b_on[sl]], writes=[PB[bb]])
                S.add("dve", lambda e, ba=ba, ct=ct, sl=sl: e.tensor_tensor(out=tA[:], in0=PS[ba][:], in1=gtt[sl][:, ct, :], op=ALU.mult),
                      reads=[PB[ba], b_gt[sl]], writes=[b_tA])
                S.add("dve", lambda e, bb=bb, ct=ct, sl=sl: e.tensor_tensor(out=tB[:], in0=PS[bb][:], in1=gtt[sl][:, 8 + ct, :], op=ALU.mult),
                      reads=[PB[bb], b_gt[sl]], writes=[b_tB])
                S.add("pool", lambda e, ct=ct: e.tensor_tensor(out=mT[:, ct, :], in0=tA[:], in1=tB[:], op=ALU.add), reads=[b_tA, b_tB], writes=[b_mT])
            if routes:
                routes.pop()()
            for s in range(4):
                for hf in range(2):
                    bk = 4 + rot % 2
                    rot += 1
                    for k in range(8):
                        S.add("pe", lambda e, bk=bk, k=k, s=s, hf=hf: e.matmul(PS[bk][:], lhsT=mT[:, k, s * 128:(s + 1) * 128], rhs=wout[:, k, hf * 512:(hf + 1) * 512],
                                                                          start=(k == 0), stop=(k == 7)), reads=[b_mT, b_w], writes=[PB[bk]])
                    S.add("dve", lambda e, bk=bk, hf=hf: e.tensor_tensor(out=tA[:], in0=PS[bk][:], in1=C.gtm_bc[:, hf * 512:(hf + 1) * 512], op=ALU.mult),
                          reads=[PB[bk], C.b_gt], writes=[b_tA])
                    S.add("pool", lambda e, s=s, hf=hf: e.tensor_tensor(out=hh_[:, s, hf * 512:(hf + 1) * 512], in0=tA[:], in1=xt[:, s, hf * 512:(hf + 1) * 512], op=ALU.add),
                          reads=[b_tA, b_xt], writes=[b_h])
            dma(S, "sp", W["hS"][t * 512:(t + 1) * 512, :].rearrange("(s p) d -> p s d", p=128), hh_[:], reads=[b_h])
            S.add("dve", lambda e: e.memset(ssq[:], 0.0), writes=[b_ssq])
            for s in range(4):
                S.add("act", lambda e, s=s: e.activation(out=junk[:], in_=hh_[:, s, :], func=AF.Square, accum_out=ssq[:, s:s + 1]),
                      reads=[b_h, b_ssq], writes=[b_junk, b_ssq])
            S.add("dve", lambda e: e.tensor_scalar(out=ssq[:, 4:8], in0=ssq[:, 0:4], scalar1=1.0 / D, scalar2=EPS, op0=ALU.mult, op1=ALU.add),
                  reads=[b_ssq], writes=[b_ssq])
            S.add("act", lambda e: e.activation(out=ssq[:, 4:8], in_=ssq[:, 4:8], func=AF.Sqrt), reads=[b_ssq], writes=[b_ssq])
            S.add("dve", lambda e: e.reciprocal(out=ssq[:, 4:8], in_=ssq[:, 4:8]), reads=[b_ssq], writes=[b_ssq])
            for s in range(4):
                S.add("dve", lambda e, s=s: e.tensor_scalar(out=xn2[:, s, :], in0=hh_[:, s, :], scalar1=ssq[:, 4 + s:5 + s], scalar2=None, op0=ALU.mult),
                      reads=[b_h, b_ssq], writes=[b_xn2])
            u2t = u2[sl]
            for k in range(8):
                bk = 6 + k % 2
                ptb = PS[bk][:].bitcast(BF16)
                for s in range(4):
                    S.add("pe", lambda e, s=s, k=k, ptb=ptb: e.transpose(out=ptb[:, s * 128:(s + 1) * 128], in_=xn2[:, s, k * 128:(k + 1) * 128], identity=C.ident_b[:]),
                          reads=[b_xn2, bc], writes=[PB[bk]])
                S.add("dve", lambda e, k=k, ptb=ptb, u2t=u2t: e.tensor_scalar(out=u2t[:, k, :], in0=ptb[:, 0:512], scalar1=cols[:, COL_G2 + k:COL_G2 + k + 1],
                                                                           scalar2=cols[:, COL_MOD + 24 + k:COL_MOD + 25 + k], op0=ALU.mult, op1=ALU.add),
                      reads=[PB[bk], C.b_cols], writes=[b_u2[sl]])
            dma(S, "sp", W["u2T"][:, t * 512:(t + 1) * 512].rearrange("(c p) n -> p c n", p=128), u2t[:], reads=[b_u2[sl]])
            bk = 4 + rot % 2
            rot += 1
            for s in range(4):
                for k in range(8):
                    S.add("pe", lambda e, bk=bk, k=k, s=s, u2t=u2t: e.matmul(PS[bk][:, s * 36:(s + 1) * 36], lhsT=u2t[:, k, s * 128:(s + 1) * 128], rhs=wr[:, k, :],
                                                                        start=(k == 0), stop=(k == 7)), reads=[b_u2[sl], b_w], writes=[PB[bk]])
            def route(t=t, sl=sl, bk=bk, u2t=u2t):
                nonlocal rot
                S.add("dve", lambda e, bk=bk: e.tensor_tensor(out=rt[:], in0=PS[bk][:, 0:144].rearrange("p (s n) -> p s n", n=36),
                                                             in1=brt[:].unsqueeze(1).broadcast_to([128, 4, 36]), op=ALU.add),
                      reads=[PB[bk], b_w], writes=[b_rt])
                RR = [b_rt, b_rw]

                def dv(fn, **kw):
                    S.add("dve", lambda e: getattr(e, fn)(**kw), reads=RR, writes=[b_rw])

                def av(**kw):
                    S.add("act", lambda e: e.activation(**kw), reads=RR, writes=[b_rw])

                G = rt[:, :, 0:4]
                E4 = rt[:, :, 4:36].rearrange("p s (g e) -> p s g e", e=8)
                w_ = lambda i: rw[:, i, :]
                b4 = lambda a: a.unsqueeze(2).broadcast_to([128, 4, 4])
                b8 = lambda a: a.unsqueeze(2).broadcast_to([128, 4, 8])
                GMAX, GSUM, GP, M1, M2, DD, ED, W1, W1G, W2G = range(10)
                dv("tensor_reduce", out=w_(GMAX), in_=G, axis=AX.X, op=ALU.max)
                dv("tensor_tensor", out=ohg[:], in0=G, in1=b4(w_(GMAX)), op=ALU.subtract)
                av(out=tE[:, 0, :, 0:4], in_=ohg[:], func=AF.Exp)
                dv("tensor_reduce", out=w_(GSUM), in_=tE[:, 0, :, 0:4], axis=AX.X, op=ALU.add)
                dv("reciprocal", out=w_(GP), in_=w_(GSUM))
                dv("tensor_tensor", out=ohg[:], in0=G, in1=b4(w_(GMAX)), op=ALU.is_equal)
                dv("tensor_tensor", out=tE[:], in0=E4, in1=ohg[:].unsqueeze(3).broadcast_to([128, 4, 4, 8]), op=ALU.mult)
                dv("tensor_reduce", out=es[:, 0], in_=tE[:].rearrange("p s g e -> p s e g"), axis=AX.X, op=ALU.add)
                dv("tensor_reduce", out=w_(M1), in_=es[:, 0], axis=AX.X, op=ALU.max)
                dv("tensor_tensor", out=es[:, 1], in0=es[:, 0], in1=b8(w_(M1)), op=ALU.is_equal)
                dv("scalar_tensor_tensor", out=es[:, 2], in0=es[:, 1], scalar=-1e30, in1=es[:, 0], op0=ALU.mult, op1=ALU.add)
                dv("tensor_reduce", out=w_(M2), in_=es[:, 2], axis=AX.X, op=ALU.max)
                dv("tensor_tensor", out=es[:, 3], in0=es[:, 2], in1=b8(w_(M2)), op=ALU.is_equal)
                dv("tensor_tensor", out=w_(DD), in0=w_(M2), in1=w_(M1), op=ALU.subtract)
                av(out=w_(ED), in_=w_(DD), func=AF.Exp)
                dv("tensor_scalar", out=w_(W1), in0=w_(ED), scalar1=1.0, scalar2=None, op0=ALU.add)
                dv("reciprocal", out=w_(W1), in_=w_(W1))
                dv("tensor_tensor", out=w_(W1G), in0=w_(W1), in1=w_(GP), op=ALU.mult)
                dv("tensor_tensor", out=w_(W2G), in0=w_(W1G), in1=w_(ED), op=ALU.mult)
                dv("tensor_tensor", out=es[:, 4], in0=es[:, 1], in1=b8(w_(W1G)), op=ALU.mult)
                dv("tensor_tensor", out=es[:, 5], in0=es[:, 3], in1=b8(w_(W2G)), op=ALU.mult)
                dv("tensor_tensor", out=es[:, 4], in0=es[:, 4], in1=es[:, 5], op=ALU.add)
                if C.sparse:
                    dma(S, "sp", W["XN"][t * 512:(t + 1) * 512, :].rearrange("(s p) d -> p s d", p=128), xn2[:], reads=[b_xn2])
                    dma(S, "sp", W["CG"][t * 512:(t + 1) * 512, :].rearrange("(s p) e -> p s e", p=128), es[:, 4], reads=[b_rw])
                    S.add("dve", lambda e: e.tensor_copy(out=ohgb[:], in_=ohg[:]), reads=RR, writes=[b_ohgb])
                    bk2 = 4 + rot % 2
                    rot += 1
                    S.add("pe", lambda e, bk2=bk2: e.matmul(PS[bk2][:, 0:16], lhsT=ustr[:], rhs=ohgb[:].rearrange("p s g -> p (s g)"), start=True, stop=True),
                          reads=[b_ohgb], writes=[PB[bk2]])
                    S.add("pe", lambda e, bk2=bk2: e.matmul(PS[bk2][:, 16:32], lhsT=C.ones_b[:], rhs=ohgb[:].rearrange("p s g -> p (s g)"), start=True, stop=True),
                          reads=[b_ohgb, bc], writes=[PB[bk2]])
                    S.add("dve", lambda e: e.tensor_copy(out=base[:, 0, :], in_=C.tot[:]), reads=[b_tot], writes=[b_base])
                    for s_ in range(1, 4):
                        S.add("dve", lambda e, s_=s_, bk2=bk2: e.tensor_tensor(out=base[:, s_, :], in0=base[:, s_ - 1, :], in1=PS[bk2][:, 16 + 4 * (s_ - 1):16 + 4 * s_], op=ALU.add),
                              reads=[b_base, PB[bk2]], writes=[b_base])
                    S.add("dve", lambda e, bk2=bk2: e.tensor_tensor(out=C.tot[:], in0=base[:, 3, :], in1=PS[bk2][:, 28:32], op=ALU.add),
                          reads=[b_base, PB[bk2]], writes=[b_tot])
                    S.add("dve", lambda e, bk2=bk2: e.tensor_tensor(out=base[:], in0=base[:], in1=PS[bk2][:, 0:16].rearrange("p (s g) -> p s g", g=4), op=ALU.add),
                          reads=[b_base, PB[bk2]], writes=[b_base])
                    S.add("dve", lambda e: e.tensor_tensor(out=base[:], in0=base[:], in1=ohg[:], op=ALU.mult), reads=[b_base] + RR, writes=[b_base])
                    S.add("dve", lambda e, t=t: e.tensor_reduce(out=C.RK[:, t * 4:(t + 1) * 4], in_=base[:], axis=AX.X, op=ALU.add), reads=[b_base], writes=[C.b_rk])
                    S.add("dve", lambda e, t=t: e.tensor_copy(out=C.OH[:, t * 4:(t + 1) * 4, :], in_=ohg[:]), reads=RR, writes=[C.b_rk])
                cb = comb[sl]
                S.add("dve", lambda e, cb=cb: e.tensor_tensor(out=cb[:].rearrange("p s (g e) -> p s g e", e=8), in0=ohg[:].unsqueeze(3).broadcast_to([128, 4, 4, 8]),
                                                             in1=es[:, 4].unsqueeze(2).broadcast_to([128, 4, 4, 8]), op=ALU.mult),
                      reads=RR, writes=[b_cb[sl]])
                dma(S, "sp", W["combS"][t * 512:(t + 1) * 512, :].rearrange("(s p) e -> p s e", p=128), cb[:], reads=[b_cb[sl]])
            routes.append(route)
        while routes:
            routes.pop()()
        if C.sparse:
            TH = sb("TH", [128, 4, 8], F32)
            cmpt = sb("cmpt", [128, 4, 8], F32)
            kg = sb("kg", [128, 4], F32)
            ck = sb("ck", [128, 5], F32)
            bsl = sb("bsl", [128, 4], F32)
            tmp3 = sb("tmp3", [128, 32, 4], F32)
            slf = sb("slf", [128, 32], F32)
            SI = sb("SI", [128, 12], F32)
            cmp2 = sb("cmp2", [128, 12, 3], F32)
            tgf = sb("tgf", [128, 12], F32)
            bq_ = Buf("slotcalc")
            RQ = [bq_, b_tot, C.b_rk]

            def dq(fn, **kw):
                S.add("dve", lambda e: getattr(e, fn)(**kw), reads=RQ, writes=[bq_])

            for j in range(8):
                dq("memset", ap=TH[:, :, j:j + 1], constant=512.0 * j)
            for j in range(12):
                dq("memset", ap=SI[:, j:j + 1], constant=float(j))
            dq("tensor_tensor", out=cmpt[:], in0=C.tot[:].unsqueeze(2).broadcast_to([128, 4, 8]), in1=TH[:], op=ALU.is_gt)
            dq("tensor_reduce", out=kg[:], in_=cmpt[:], axis=AX.X, op=ALU.add)
            dq("memset", ap=ck[:, 0:1], constant=0.0)
            for g in range(4):
                dq("tensor_tensor", out=ck[:, g + 1:g + 2], in0=ck[:, g:g + 1], in1=kg[:, g:g + 1], op=ALU.add)
            dq("tensor_scalar", out=bsl[:], in0=ck[:, 0:4], scalar1=512.0, scalar2=None, op0=ALU.mult)
            dq("tensor_tensor", out=tmp3[:], in0=C.OH[:], in1=bsl[:].unsqueeze(1).broadcast_to([128, 32, 4]), op=ALU.mult)
            dq("tensor_reduce", out=slf[:], in_=tmp3[:], axis=AX.X, op=ALU.add)
            dq("tensor_tensor", out=slf[:], in0=slf[:], in1=C.RK[:], op=ALU.add)
            dq("tensor_copy", out=C.SLOT_I[:], in_=slf[:])
            dq("tensor_tensor", out=cmp2[:], in0=ck[:, 1:4].unsqueeze(1).broadcast_to([128, 12, 3]), in1=SI[:].unsqueeze(2).broadcast_to([128, 12, 3]), op=ALU.is_le)
            dq("tensor_reduce", out=tgf[:], in_=cmp2[:], axis=AX.X, op=ALU.add)
            dq("tensor_copy", out=C.TG_I[:], in_=tgf[:])
            pidx_i = sb("pidx_i", [128, 1], mybir.dt.int32)
            pidx = sb("pidx", [128, 1], F32)
            j128 = sb("j128", [128, 8], F32)
            widf = sb("widf", [128, 12, 8], F32)
            S.add("pool", lambda e: e.iota(pidx_i[:], [[0, 1]], base=0, channel_multiplier=1), writes=[bq_])
            dq("tensor_copy", out=pidx[:], in_=pidx_i[:])
            for j in range(8):
                dq("memset", ap=j128[:, j:j + 1], constant=128.0 * j)
            dq("tensor_scalar", out=j128[:], in0=j128[:], scalar1=pidx[:, 0:1], scalar2=None, op0=ALU.add)
            dq("scalar_tensor_tensor", out=widf[:], in0=tgf[:].unsqueeze(2).broadcast_to([128, 12, 8]), scalar=1024.0,
               in1=j128[:].unsqueeze(1).broadcast_to([128, 12, 8]), op0=ALU.mult, op1=ALU.add)
            dq("tensor_copy", out=C.WIDX[:].rearrange("p (s j) -> p s j", j=8), in_=widf[:])
            if "dbg_slot" in C.debug:
                C.dbg_slot = nc.dram_tensor("dbg_slot", [128, 64], F32, kind="ExternalOutput").ap()
                dma(S, "sp", C.dbg_slot[:, 0:32], slf[:], reads=[bq_])
                dma(S, "sp", C.dbg_slot[:, 32:44], tgf[:], reads=[bq_])
                dma(S, "sp", C.dbg_slot[:, 44:48], C.tot[:], reads=[bq_])
        S.flush()


def phase4b(C):
    nc, S, I, W = C.nc, C.S, C.I, C.W
    PS, PB = C.PS, C.PB
    n_exp = C.n_exp if hasattr(C, "n_exp") else NEXP
    with ExitStack() as _es:
        sb = lambda name, shape, dt: _es.enter_context(nc.sbuf_tensor(name, shape, dt))
        u2t = sb("u2tt", [128, 8, 1024], BF16)
        cbt = sb("cbt", [128, 8, 32], F32)
        yacc = sb("yacc", [128, 8, D], F32)
        wg = [sb(f"wg{i}", [128, 8, 256], BF16) for i in range(2)]
        wu = [sb(f"wu{i}", [128, 8, 256], BF16) for i in range(2)]
        wd = [sb(f"wd{i}", [128, 2, D], BF16) for i in range(2)]
        sg = [sb(f"sg{i}", [128, 2, 512], BF16) for i in range(2)]
        hd = [sb(f"hd{i}", [128, 2, 512], BF16) for i in range(2)]
        hp = sb("hp", [128, 4, D], F32)
        gfin = sb("gfin", [128, D], F32)
        junk = sb("junk5", [128, D], BF16)
        ssq = sb("ssq5", [128, 8], F32)
        b_u2t, b_cbt, b_yacc, b_hp, b_gfin, b_junk, b_ssq = [Buf(n) for n in "u2t cbt yacc hp gfin junk ssq".split()]
        b_wg = [Buf("wg0"), Buf("wg1")]
        b_wu = [Buf("wu0"), Buf("wu1")]
        b_wd = [Buf("wd0"), Buf("wd1")]
        b_sg = [Buf("sg0"), Buf("sg1")]
        b_hd = [Buf("hd0"), Buf("hd1")]
        dma(S, "sp", gfin[:], I["g_final"].partition_broadcast(128), writes=[b_gfin])

        def load_w(TT, e, sl):
            if TT == 0:
                dma(S, "pool", wg[sl][:], I["w_exp_gate"][e].rearrange("(k p) f -> p k f", p=128), writes=[b_wg[sl]])
                dma(S, "pool", wu[sl][:], I["w_exp_up"][e].rearrange("(k p) f -> p k f", p=128), writes=[b_wu[sl]])
                dma(S, "pool", wd[sl][:], I["w_exp_down"][e].rearrange("(k p) d -> p k d", p=128), writes=[b_wd[sl]])
                dma(S, "sp", W["wgS"][e].rearrange("(k p) f -> p k f", p=128), wg[sl][:], reads=[b_wg[sl]])
                dma(S, "sp", W["wuS"][e].rearrange("(k p) f -> p k f", p=128), wu[sl][:], reads=[b_wu[sl]])
                dma(S, "sp", W["wdS"][e].rearrange("(k p) d -> p k d", p=128), wd[sl][:], reads=[b_wd[sl]])
            else:
                dma(S, "sp", wg[sl][:], W["wgS"][e].rearrange("(k p) f -> p k f", p=128), writes=[b_wg[sl]])
                dma(S, "sp", wu[sl][:], W["wuS"][e].rearrange("(k p) f -> p k f", p=128), writes=[b_wu[sl]])
                dma(S, "sp", wd[sl][:], W["wdS"][e].rearrange("(k p) d -> p k d", p=128), writes=[b_wd[sl]])

        rot = 0
        cnt = 0
        for TT in range(4):
            dma(S, "sp", u2t[:], W["u2T"][:, TT * 1024:(TT + 1) * 1024].rearrange("(c p) n -> p c n", p=128), writes=[b_u2t])
            dma(S, "sp", cbt[:], W["combS"][TT * 1024:(TT + 1) * 1024, :].rearrange("(s p) e -> p s e", p=128), writes=[b_cbt])
            S.add("pool", lambda e: e.memset(yacc[:], 0.0), writes=[b_yacc])
            load_w(TT, 0, 0)
            for ex in range(n_exp):
                sl = ex % 2
                if ex + 1 < n_exp:
                    load_w(TT, ex + 1, (ex + 1) % 2)
                for half in range(2):
                    c2 = cnt % 2
                    cnt += 1
                    for f in range(2):
                        for k in range(8):
                            S.add("pe", lambda e, f=f, k=k, sl=sl, half=half: e.matmul(PS[f][:], lhsT=wg[sl][:, k, f * 128:(f + 1) * 128], rhs=u2t[:, k, half * 512:(half + 1) * 512],
                                                                                  start=(k == 0), stop=(k == 7)), reads=[b_wg[sl], b_u2t], writes=[PB[f]])
                    for f in range(2):
                        for k in range(8):
                            S.add("pe", lambda e, f=f, k=k, sl=sl, half=half: e.matmul(PS[2 + f][:], lhsT=wu[sl][:, k, f * 128:(f + 1) * 128], rhs=u2t[:, k, half * 512:(half + 1) * 512],
                                                                                  start=(k == 0), stop=(k == 7)), reads=[b_wu[sl], b_u2t], writes=[PB[2 + f]])
                    for f in range(2):
                        S.add("act", lambda e, f=f, c2=c2: e.activation(out=sg[c2][:, f, :], in_=PS[f][:], func=AF.Silu), reads=[PB[f]], writes=[b_sg[c2]])
                        S.add("dve", lambda e, f=f, c2=c2: e.tensor_tensor(out=hd[c2][:, f, :], in0=PS[2 + f][:], in1=sg[c2][:, f, :], op=ALU.mult),
                              reads=[PB[2 + f], b_sg[c2]], writes=[b_hd[c2]])
                    for sub in range(4):
                        for dh in range(2):
                            bk = 4 + rot % 4
                            rot += 1
                            for f in range(2):
                                S.add("pe", lambda e, bk=bk, f=f, sub=sub, dh=dh, c2=c2, sl=sl: e.matmul(
                                    PS[bk][:], lhsT=hd[c2][:, f, sub * 128:(sub + 1) * 128], rhs=wd[sl][:, f, dh * 512:(dh + 1) * 512], start=(f == 0), stop=(f == 1)),
                                    reads=[b_hd[c2], b_wd[sl]], writes=[PB[bk]])
                            s8 = half * 4 + sub
                            S.add("dve", lambda e, bk=bk, s8=s8, dh=dh, ex=ex: e.scalar_tensor_tensor(
                                out=yacc[:, s8, dh * 512:(dh + 1) * 512], in0=PS[bk][:], scalar=cbt[:, s8, ex:ex + 1], in1=yacc[:, s8, dh * 512:(dh + 1) * 512],
                                op0=ALU.mult, op1=ALU.add), reads=[PB[bk], b_cbt, b_yacc], writes=[b_yacc])
            for half in range(2):
                r0 = TT * 1024 + half * 512
                dma(S, "sp", hp[:], W["hS"][r0:r0 + 512, :].rearrange("(s p) d -> p s d", p=128), writes=[b_hp])
                ya = yacc[:, half * 4:(half + 1) * 4, :]
                S.add("dve", lambda e, ya=ya: e.tensor_tensor(out=ya, in0=ya, in1=C.gtf_bc[:].unsqueeze(1).broadcast_to([128, 4, D]), op=ALU.mult),
                      reads=[b_yacc, C.b_gt], writes=[b_yacc])
                S.add("pool", lambda e, ya=ya: e.tensor_tensor(out=hp[:], in0=hp[:], in1=ya, op=ALU.add), reads=[b_yacc, b_hp], writes=[b_hp])
                S.add("dve", lambda e: e.memset(ssq[:], 0.0), writes=[b_ssq])
                for s in range(4):
                    S.add("act", lambda e, s=s: e.activation(out=junk[:], in_=hp[:, s, :], func=AF.Square, accum_out=ssq[:, s:s + 1]),
                          reads=[b_hp, b_ssq], writes=[b_junk, b_ssq])
                S.add("dve", lambda e: e.tensor_scalar(out=ssq[:, 4:8], in0=ssq[:, 0:4], scalar1=1.0 / D, scalar2=EPS, op0=ALU.mult, op1=ALU.add),
                      reads=[b_ssq], writes=[b_ssq])
                S.add("act", lambda e: e.activation(out=ssq[:, 4:8], in_=ssq[:, 4:8], func=AF.Sqrt), reads=[b_ssq], writes=[b_ssq])
                S.add("dve", lambda e: e.reciprocal(out=ssq[:, 4:8], in_=ssq[:, 4:8]), reads=[b_ssq], writes=[b_ssq])
                for s in range(4):
                    S.add("dve", lambda e, s=s: e.scalar_tensor_tensor(out=hp[:, s, :], in0=hp[:, s, :], scalar=ssq[:, 4 + s:5 + s], in1=gfin[:], op0=ALU.mult, op1=ALU.mult),
                          reads=[b_hp, b_ssq, b_gfin], writes=[b_hp])
                dma(S, "sp", C.out[r0:r0 + 512, :].rearrange("(s p) d -> p s d", p=128), hp[:], reads=[b_hp])
        S.flush()
```

```python
import math
from contextlib import ExitStack
import numpy as np
import concourse.bass as bass
import concourse.mybir as mybir
from concourse.bass_utils import run_bass_kernel_spmd

F32 = mybir.dt.float32
BF16 = mybir.dt.bfloat16
AF = mybir.ActivationFunctionType
ALU = mybir.AluOpType
AX = mybir.AxisListType

L = 4096
D = 1024
NTT = 8
EPS = 1e-6
LAMBDA_INIT = 0.8 - 0.6 * math.exp(-0.3 * 0)
NEXP = 32
ENGS = ("pe", "act", "dve", "pool", "sp")
ENG_ATTR = {"pe": "tensor", "act": "scalar", "dve": "vector", "pool": "gpsimd", "sp": "sync"}


class Buf:
    __slots__ = ("name", "w", "r")

    def __init__(self, name=""):
        self.name = name
        self.w = None
        self.r = []


class Op:
    __slots__ = ("eng", "idx", "fn", "waits", "signal", "count", "dma", "dsem", "dval", "snap", "bg")

    def __init__(self, eng, idx, fn, dma):
        self.eng = eng
        self.idx = idx
        self.fn = fn
        self.waits = []
        self.signal = False
        self.count = None
        self.dma = dma
        self.dsem = None
        self.dval = None
        self.snap = None
        self.bg = False


class Sched:
    def __init__(self, nc, n_dma_sems=48):
        self.nc = nc
        self.pending = {e: [] for e in ENGS}
        self.nops = {e: 0 for e in ENGS}
        self.last = {e: None for e in ENGS}
        self.seen = {e: {} for e in ENGS}
        self.n_dma_sems = n_dma_sems
        self.dma_rr = 0
        self.dma_rr_sw = 0
        self.n_hw = 28
        self.dma_last = [None] * n_dma_sems
        self.dma_last_nb = [None] * n_dma_sems
        self.dma_val = [0] * n_dma_sems
        self.cnt = {e: 0 for e in ENGS}
        self.sems = {e: nc.alloc_semaphore(name=f"sem_{e}") for e in ENGS}
        self.dsems = [nc.alloc_semaphore(name=f"dsem_{i}") for i in range(n_dma_sems)]

    def _need(self, o, d):
        e = o.eng
        if d.dma:
            key = ("d", d.dsem)
            if self.seen[e].get(key, 0) >= d.dval:
                return
            self.seen[e][key] = d.dval
            o.waits.append(d)
        else:
            if d.eng == e and e == "pe":
                return
            key = d.eng
            if self.seen[e].get(key, -1) >= d.idx:
                return
            self.seen[e][key] = d.idx
            d.signal = True
            o.waits.append(d)
        if d.snap is not None:
            se = self.seen[e]
            for k, v in d.snap.items():
                if k == e:
                    continue
                if se.get(k, -1) < v:
                    se[k] = v

    def add(self, eng, fn, reads=(), writes=(), dma=False, bg=False):
        o = Op(eng, self.nops[eng], fn, dma)
        o.bg = bg
        self.nops[eng] += 1
        deps = []
        for b in reads:
            if b.w is not None:
                deps.append(b.w)
        for b in writes:
            if b.w is not None:
                deps.append(b.w)
            deps.extend(b.r)
        if dma:
            if eng == "pool":
                s = self.n_hw + self.dma_rr_sw
                self.dma_rr_sw = (self.dma_rr_sw + 1) % (self.n_dma_sems - self.n_hw)
            else:
                s = self.dma_rr
                self.dma_rr = (self.dma_rr + 1) % self.n_hw
            prev = self.dma_last[s]
            if prev is not None:
                deps.append(prev)
            self.dma_val[s] += 16
            o.dsem = s
            o.dval = self.dma_val[s]
            self.dma_last[s] = o
            if not bg:
                self.dma_last_nb[s] = o
        for d in deps:
            if d is not o:
                self._need(o, d)
        for b in reads:
            b.r.append(o)
        for b in writes:
            b.w = o
            b.r = []
        self.pending[eng].append(o)
        if not dma:
            self.last[eng] = o
        o.snap = dict(self.seen[eng])
        return o

    def flush(self):
        lasts = [self.last[e] for e in ENGS if self.last[e] is not None]
        dlasts = [(self.dma_last_nb[i] if d.bg else d) for i, d in enumerate(self.dma_last) if d is not None]
        dlasts = [d for d in dlasts if d is not None]
        for e in ENGS:
            o = Op(e, self.nops[e], None, False)
            self.nops[e] += 1
            for d in lasts + dlasts:
                self._need(o, d)
            self.pending[e].append(o)
            o.snap = dict(self.seen[e])
        nc = self.nc
        for e in ENGS:
            for o in self.pending[e]:
                if o.signal and not o.dma:
                    self.cnt[e] += 1
                    o.count = self.cnt[e]
        sems, dsems = self.sems, self.dsems
        with nc.Block() as block:
            for e in ENGS:
                ops = self.pending[e]

                def body(eng, ops=ops, e=e):
                    for o in ops:
                        for d in o.waits:
                            if d.dma:
                                eng.wait_ge(dsems[d.dsem], d.dval)
                            else:
                                eng.wait_ge(sems[d.eng], d.count)
                        if o.fn is None:
                            continue
                        ins = o.fn(eng)
                        if o.dma:
                            ins.then_inc(dsems[o.dsem], 16)
                        elif o.signal:
                            ins.then_inc(sems[e], 1)

                getattr(block, ENG_ATTR[e])(body)
        self.pending = {e: [] for e in ENGS}


class Ctx:
    pass


def dma(S, q, out, in_, reads=(), writes=(), bg=False):
    return S.add(q, lambda e: e.dma_start(out=out, in_=in_), reads, writes, dma=True, bg=bg)


VEC_ROWS = {}


def build(debug=()):
    nc = bass.Bass("TRN2", target_bir_lowering=False)
    C = Ctx()
    C.nc = nc
    C.debug = set(debug)
    C.S = S = Sched(nc)

    def din(name, shape):
        return nc.dram_tensor(name, list(shape), F32, kind="ExternalInput").ap()

    I = C.I = {}
    for name, shape in [
        ("x", (L, D)), ("c", (D,)), ("w_ada", (D, 6 * D)), ("b_ada", (6 * D,)), ("g_norm_mix", (D,)),
        ("w_in", (D, 5632)), ("b_in", (5632,)),
        ("s5_a_re", (32, 64)), ("s5_a_im", (32, 64)), ("s5_b_re", (32, 64, 16)), ("s5_b_im", (32, 64, 16)),
        ("s5_c_re", (32, 16, 64)), ("s5_c_im", (32, 16, 64)), ("s5_d", (512,)), ("s5_log_dt", (32,)),
        ("w_glu", (512, 512)), ("b_glu", (512,)),
        ("lambda_q1", (64,)), ("lambda_k1", (64,)), ("lambda_q2", (64,)), ("lambda_k2", (64,)), ("g_subln", (128,)),
        ("w_br_ssm", (512, D)), ("w_br_attn", (D, D)), ("w_out", (D, D)), ("g_norm_ffn", (D,)),
        ("w_router_grp", (D, 4)), ("b_router_grp", (4,)), ("w_router_exp", (D, 32)), ("b_router_exp", (32,)),
        ("w_exp_gate", (NEXP, D, 256)), ("w_exp_up", (NEXP, D, 256)), ("w_exp_down", (NEXP, 256, D)),
        ("g_final", (D,)),
    ]:
        I[name] = din(name, shape)
    C.out = nc.dram_tensor("out", [L, D], F32, kind="ExternalOutput").ap()

    def scratch(name, shape, dt):
        if name in C.debug:
            return nc.dram_tensor(name, list(shape), dt, kind="ExternalOutput").ap()
        return nc.dram_tensor(name, list(shape), dt, kind="Internal").ap()

    C.scratch = scratch
    W = C.W = {}
    W["qT"] = scratch("qT", (D, L), BF16)
    W["kT"] = scratch("kT", (D, L), BF16)
    W["vS"] = scratch("vS", (L, D), BF16)
    W["gT"] = scratch("gT", (2 * D, L), BF16)
    W["s5S"] = scratch("s5S", (L, 512), F32)
    W["y2T"] = scratch("y2T", (512, L), BF16)
    W["onT"] = scratch("onT", (D, L), BF16)
    W["hS"] = scratch("hS", (L, D), F32)
    W["u2T"] = scratch("u2T", (D, L), BF16)
    W["combS"] = scratch("combS", (L, NEXP), F32)
    W["wgS"] = scratch("wgS", (NEXP, D, 256), BF16)
    W["wuS"] = scratch("wuS", (NEXP, D, 256), BF16)
    W["wdS"] = scratch("wdS", (NEXP, 256, D), BF16)
    NSLOT = 6144
    W["XN"] = scratch("XN", (L, D), BF16)
    W["CG"] = scratch("CG", (L, 8), F32)
    W["XS"] = scratch("XS", (NSLOT, D), BF16)
    W["CGS"] = scratch("CGS", (NSLOT, 8), F32)
    W["YS"] = scratch("YS", (NSLOT, D), F32)
    W["WALL"] = scratch("WALL", (NEXP * 128, 6144), BF16)
    C.WB = {k: Buf(k) for k in W}
    C.sparse = "dense" not in C.debug
    if "dbg_cols" in C.debug:
        C.dbg_cols = nc.dram_tensor("dbg_cols", [128, 256], F32, kind="ExternalOutput").ap()
    if "dbg_s5y" in C.debug:
        C.dbg_s5y = nc.dram_tensor("dbg_s5y", [L, 512], F32, kind="ExternalOutput").ap()

    with ExitStack() as _es:
        p0 = _es.enter_context(nc.psum_tensor("ps0", [128, 512], F32))
        p1 = _es.enter_context(nc.psum_tensor("ps1", [128, 512], F32))
        p2 = _es.enter_context(nc.psum_tensor("ps2", [128, 512], F32))
        p3 = _es.enter_context(nc.psum_tensor("ps3", [128, 512], F32))
        p4 = _es.enter_context(nc.psum_tensor("ps4", [128, 512], F32))
        p5 = _es.enter_context(nc.psum_tensor("ps5", [128, 512], F32))
        p6 = _es.enter_context(nc.psum_tensor("ps6", [128, 512], F32))
        p7 = _es.enter_context(nc.psum_tensor("ps7", [128, 512], F32))
        ident_f = _es.enter_context(nc.sbuf_tensor("ident_f", [128, 128], F32))
        ident_b = _es.enter_context(nc.sbuf_tensor("ident_b", [128, 128], BF16))
        ones_b = _es.enter_context(nc.sbuf_tensor("ones_b", [128, 128], BF16))
        cols = _es.enter_context(nc.sbuf_tensor("cols", [128, 256], F32))
        gtm_bc = _es.enter_context(nc.sbuf_tensor("gtm_bc", [128, D], F32))
        gtf_bc = _es.enter_context(nc.sbuf_tensor("gtf_bc", [128, D], F32))
        C.RK = _es.enter_context(nc.sbuf_tensor("RK", [128, 32], F32))
        C.OH = _es.enter_context(nc.sbuf_tensor("OH", [128, 32, 4], F32))
        C.tot = _es.enter_context(nc.sbuf_tensor("tot", [128, 4], F32))
        C.SLOT_I = _es.enter_context(nc.sbuf_tensor("SLOT_I", [128, 32], mybir.dt.int32))
        C.TG_I = _es.enter_context(nc.sbuf_tensor("TG_I", [128, 12], mybir.dt.int32))
        C.WIDX = _es.enter_context(nc.sbuf_tensor("WIDX", [128, 96], mybir.dt.int32))
        C.b_w4 = Buf("w4g")
        C.b_rk = Buf("rk")
        C.PS = [p0, p1, p2, p3, p4, p5, p6, p7]
        C.PB = [Buf(f"ps{i}") for i in range(8)]
        C.ident_f, C.ident_b, C.ones_b, C.cols = ident_f, ident_b, ones_b, cols
        C.gtm_bc, C.gtf_bc = gtm_bc, gtf_bc
        C.b_const = Buf("const")
        C.b_cols = Buf("cols")
        C.b_gt = Buf("gt")
        with ExitStack() as _es01:
            C.w_in = _es01.enter_context(nc.sbuf_tensor("sb_w_in", [128, 8, 5632], BF16))
            C.b_win = [Buf(f"win{i}") for i in range(11)]
            phase0(C)
            S.flush()
            if "stop0" not in C.debug and "skip1" not in C.debug:
                phase1(C)
        if "stop0" not in C.debug:
            if "skip2" not in C.debug and "stop1" not in C.debug:
                phase2(C)
            if "heads" in C.debug:
                C.heads = [0, 5]
            for f_ in C.debug:
                if f_.startswith("epi="):
                    C.epi_step = int(f_[4:])
            with ExitStack() as _es34:
                C.wbs = _es34.enter_context(nc.sbuf_tensor("wbs", [128, 4, D], BF16))
                C.wba = _es34.enter_context(nc.sbuf_tensor("wba", [128, 8, D], BF16))
                C.wout = _es34.enter_context(nc.sbuf_tensor("wout", [128, 8, D], BF16))
                if "skip3" not in C.debug and "stop1" not in C.debug:
                    phase3(C)
                if "stop3" not in C.debug and "stop1" not in C.debug:
                    phase4a(C)
            if "stop3" not in C.debug and "stop1" not in C.debug:
                if "n_exp" in C.debug:
                    C.n_exp = 2
                if "stop4a" not in C.debug:
                    if C.sparse:
                        phase4p(C)
                        phase4b_sparse(C)
                    else:
                        phase4b(C)
        if "dbg_cols" in C.debug:
            dma(S, "sp", C.dbg_cols, cols[:], reads=[C.b_cols])
            S.flush()
    return nc


COL_C = 0
COL_BADA = 8
COL_BIN = 56
COL_GNM = 100
COL_GNF = 108
COL_BGLU = 116
COL_GSUB = 120
COL_MOD = 128
COL_G1 = 176
COL_G2 = 184
COL_GSUBS = 192


def phase0(C):
    nc, S, I = C.nc, C.S, C.I
    cols = C.cols
    bc = C.b_const
    S.add("pool", lambda e: e.memset(C.ident_f[:], 0.0), writes=[bc])
    S.add("pool", lambda e: e.affine_select(out=C.ident_f[:], in_=C.ident_f[:], pattern=[[-1, 128]],
                                            compare_op=ALU.not_equal, fill=1.0, base=0, channel_multiplier=1),
          reads=[bc], writes=[bc])
    S.add("dve", lambda e: e.tensor_copy(out=C.ident_b[:], in_=C.ident_f[:]), reads=[bc], writes=[bc])
    S.add("dve", lambda e: e.memset(C.ones_b[:], 1.0), writes=[bc])
    with ExitStack() as _es:
        rows = _es.enter_context(nc.sbuf_tensor("rows", [128, 128], F32))
        cs_b = _es.enter_context(nc.sbuf_tensor("cs_b", [128, 8], BF16))
        cs_rep = _es.enter_context(nc.sbuf_tensor("cs_rep", [128, 8, 128], BF16))
        wa0 = _es.enter_context(nc.sbuf_tensor("wa0", [128, 8, 512], BF16))
        wa1 = _es.enter_context(nc.sbuf_tensor("wa1", [128, 8, 512], BF16))
        bada_bc = _es.enter_context(nc.sbuf_tensor("bada_bc", [128, 2, D], F32))
        b_rows = Buf("rows")
        S.add("dve", lambda e: e.memset(rows[:], 0.0), writes=[b_rows])
        for name, base, n in [("c", COL_C, 8), ("b_ada", COL_BADA, 48), ("b_in", COL_BIN, 44), ("g_norm_mix", COL_GNM, 8),
                              ("g_norm_ffn", COL_GNF, 8), ("b_glu", COL_BGLU, 4), ("g_subln", COL_GSUB, 1)]:
            dma(S, "sp", rows[base:base + n, :], I[name].rearrange("(k p) -> k p", p=128), writes=[b_rows])
        b_bada = Buf("bada_bc")
        dma(S, "sp", bada_bc[:, 0, :], I["b_ada"][2 * D:3 * D].partition_broadcast(128), writes=[b_bada])
        dma(S, "sp", bada_bc[:, 1, :], I["b_ada"][5 * D:6 * D].partition_broadcast(128), writes=[b_bada])
        pT = C.PS[0]
        S.add("pe", lambda e: e.transpose(out=pT[:, 0:128], in_=rows[:], identity=C.ident_f[:]), reads=[b_rows, bc], writes=[C.PB[0]])
        S.add("dve", lambda e: e.tensor_copy(out=cols[:, 0:128], in_=pT[:, 0:128]), reads=[C.PB[0]], writes=[C.b_cols])
        b_cs = Buf("cs")
        S.add("act", lambda e: e.activation(out=cols[:, COL_C:COL_C + 8], in_=cols[:, COL_C:COL_C + 8], func=AF.Silu),
              reads=[C.b_cols], writes=[C.b_cols])
        S.add("dve", lambda e: e.tensor_copy(out=cs_b[:], in_=cols[:, COL_C:COL_C + 8]), reads=[C.b_cols], writes=[b_cs])
        S.add("dve", lambda e: e.tensor_copy(out=cs_rep[:], in_=cs_b[:].unsqueeze(2).broadcast_to([128, 8, 128])),
              reads=[b_cs], writes=[b_cs])
        was = [wa0, wa1]
        waf = [_es.enter_context(nc.sbuf_tensor(f"waf{i}", [128, 8, 512], F32)) for i in range(2)]
        b_waf = [Buf("waf0"), Buf("waf1")]
        b_wa = [Buf("wa0"), Buf("wa1")]
        pcol = C.PS[1]
        for n in range(12):
            wa = was[n % 2]
            bw = b_wa[n % 2]
            waf_ = waf[n % 2]
            dma(S, "sp", waf_[:], I["w_ada"][:, n * 512:(n + 1) * 512].rearrange("(k p) n -> p k n", p=128), writes=[b_waf[n % 2]])
            S.add("act", lambda e, wa=wa, waf_=waf_: e.activation(out=wa[:], in_=waf_[:], func=AF.Copy), reads=[b_waf[n % 2]], writes=[bw])
            if n >= 1:
                i_ = n - 1
                dma(S, "pool", C.w_in[:, :, i_ * 512:(i_ + 1) * 512], I["w_in"][:, i_ * 512:(i_ + 1) * 512].rearrange("(k p) n -> p k n", p=128),
                    writes=[C.b_win[i_]], bg=True)
            for s in range(4):
                j = n * 4 + s
                for k in range(8):
                    S.add("pe", lambda e, wa=wa, s=s, k=k, j=j: e.matmul(pcol[:, j:j + 1], lhsT=wa[:, k, s * 128:(s + 1) * 128],
                                                                      rhs=cs_b[:, k:k + 1], start=(k == 0), stop=(k == 7)),
                          reads=[bw, b_cs], writes=[C.PB[1]])
            if n in (4, 5, 10, 11):
                prow = C.PS[2 + (n % 2)]
                pbb = C.PB[2 + (n % 2)]
                for k in range(8):
                    S.add("pe", lambda e, wa=wa, k=k, prow=prow: e.matmul(prow[:], lhsT=cs_rep[:, k, :], rhs=wa[:, k, :],
                                                                       start=(k == 0), stop=(k == 7)),
                          reads=[bw, b_cs], writes=[pbb])
                dst = C.gtm_bc if n < 6 else C.gtf_bc
                hh = n % 2
                wi = 0 if n < 6 else 1
                S.add("dve", lambda e, dst=dst, hh=hh, wi=wi, prow=prow: e.tensor_tensor(
                    out=dst[:, hh * 512:(hh + 1) * 512], in0=prow[:], in1=bada_bc[:, wi, hh * 512:(hh + 1) * 512], op=ALU.add),
                    reads=[pbb, b_bada], writes=[C.b_gt])
        S.add("dve", lambda e: e.tensor_tensor(out=cols[:, COL_MOD:COL_MOD + 48], in0=pcol[:, 0:48],
                                               in1=cols[:, COL_BADA:COL_BADA + 48], op=ALU.add),
              reads=[C.PB[1], C.b_cols], writes=[C.b_cols])
        S.add("dve", lambda e: e.scalar_tensor_tensor(out=cols[:, COL_G1:COL_G1 + 8], in0=cols[:, COL_MOD + 8:COL_MOD + 16], scalar=1.0,
                                                      in1=cols[:, COL_GNM:COL_GNM + 8], op0=ALU.add, op1=ALU.mult),
              reads=[C.b_cols], writes=[C.b_cols])
        S.add("dve", lambda e: e.scalar_tensor_tensor(out=cols[:, COL_G2:COL_G2 + 8], in0=cols[:, COL_MOD + 32:COL_MOD + 40], scalar=1.0,
                                                      in1=cols[:, COL_GNF:COL_GNF + 8], op0=ALU.add, op1=ALU.mult),
              reads=[C.b_cols], writes=[C.b_cols])
        S.add("dve", lambda e: e.tensor_scalar(out=cols[:, COL_GSUBS:COL_GSUBS + 1], in0=cols[:, COL_GSUB:COL_GSUB + 1],
                                               scalar1=(1.0 - LAMBDA_INIT), scalar2=None, op0=ALU.mult),
              reads=[C.b_cols], writes=[C.b_cols])
        S.flush()


def phase1(C):
    nc, S, I, W, WB = C.nc, C.S, C.I, C.W, C.WB
    cols = C.cols
    with ExitStack() as _es:
        w_in = C.w_in
        xt0 = _es.enter_context(nc.sbuf_tensor("xt0", [128, 4, D], F32))
        xt1 = _es.enter_context(nc.sbuf_tensor("xt1", [128, 4, D], F32))
        xn = _es.enter_context(nc.sbuf_tensor("xn", [128, 4, D], BF16))
        junk = _es.enter_context(nc.sbuf_tensor("junk", [128, D], BF16))
        ssq = _es.enter_context(nc.sbuf_tensor("ssq", [128, 8], F32))
        uT0 = _es.enter_context(nc.sbuf_tensor("uT0", [128, 8, 512], BF16))
        uT1 = _es.enter_context(nc.sbuf_tensor("uT1", [128, 8, 512], BF16))
        stg0 = _es.enter_context(nc.sbuf_tensor("stg0", [128, 4, 512], BF16))
        stg1 = _es.enter_context(nc.sbuf_tensor("stg1", [128, 4, 512], BF16))
        vstg = _es.enter_context(nc.sbuf_tensor("vstg", [128, 4, D], BF16))
        s5stg = _es.enter_context(nc.sbuf_tensor("s5stg", [128, 4, 512], F32))
        bias_bc = _es.enter_context(nc.sbuf_tensor("bias_bc", [128, 1536], F32))
        b_win = C.b_win
        b_bias = Buf("bias_bc")
        dma(S, "sp", bias_bc[:, 0:512], I["b_in"][0:512].partition_broadcast(128), writes=[b_bias])
        dma(S, "sp", bias_bc[:, 512:1536], I["b_in"][2560:3584].partition_broadcast(128), writes=[b_bias])
        xts = [xt0, xt1]
        b_xt = [Buf("xt0"), Buf("xt1")]
        uTs = [uT0, uT1]
        b_uT = [Buf("uT0"), Buf("uT1")]
        stgs = [stg0, stg1]
        b_stg = [Buf("stg0"), Buf("stg1")]
        b_xn, b_ssq, b_junk, b_vstg, b_s5stg = Buf("xn"), Buf("ssq"), Buf("junk"), Buf("vstg"), Buf("s5stg")
        PS, PB = C.PS, C.PB

        def load_x(t):
            dma(S, "sp", xts[t % 2][:], I["x"][t * 512:(t + 1) * 512, :].rearrange("(s p) d -> p s d", p=128), writes=[b_xt[t % 2]])

        load_x(0)
        rot = 0
        stg_i = 0
        for t in range(NTT):
            if t + 1 < NTT:
                load_x(t + 1)
            xt = xts[t % 2]
            bx = b_xt[t % 2]
            uT = uTs[t % 2]
            bu = b_uT[t % 2]
            S.add("dve", lambda e: e.memset(ssq[:], 0.0), writes=[b_ssq])
            for s in range(4):
                S.add("act", lambda e, s=s, xt=xt: e.activation(out=junk[:], in_=xt[:, s, :], func=AF.Square, accum_out=ssq[:, s:s + 1]),
                      reads=[bx, b_ssq], writes=[b_junk, b_ssq])
            S.add("dve", lambda e: e.tensor_scalar(out=ssq[:, 4:8], in0=ssq[:, 0:4], scalar1=1.0 / D, scalar2=EPS, op0=ALU.mult, op1=ALU.add),
                  reads=[b_ssq], writes=[b_ssq])
            S.add("act", lambda e: e.activation(out=ssq[:, 4:8], in_=ssq[:, 4:8], func=AF.Sqrt), reads=[b_ssq], writes=[b_ssq])
            S.add("dve", lambda e: e.reciprocal(out=ssq[:, 4:8], in_=ssq[:, 4:8]), reads=[b_ssq], writes=[b_ssq])
            for s in range(4):
                S.add("dve", lambda e, s=s, xt=xt: e.tensor_scalar(out=xn[:, s, :], in0=xt[:, s, :], scalar1=ssq[:, 4 + s:5 + s], scalar2=None,
                                                                op0=ALU.mult),
                      reads=[bx, b_ssq], writes=[b_xn])
            for k in range(8):
                pt = PS[k % 2]
                ptb = pt[:].bitcast(BF16)
                for s in range(4):
                    S.add("pe", lambda e, s=s, k=k, ptb=ptb: e.transpose(out=ptb[:, s * 128:(s + 1) * 128], in_=xn[:, s, k * 128:(k + 1) * 128],
                                                                      identity=C.ident_b[:]),
                          reads=[b_xn, C.b_const], writes=[PB[k % 2]])
                S.add("dve", lambda e, k=k, ptb=ptb, uT=uT: e.tensor_scalar(out=uT[:, k, :], in0=ptb[:, 0:512], scalar1=cols[:, COL_G1 + k:COL_G1 + k + 1],
                                                                         scalar2=cols[:, COL_MOD + k:COL_MOD + k + 1], op0=ALU.mult, op1=ALU.add),
                      reads=[PB[k % 2], C.b_cols], writes=[bu])
            groups = [("qT", 4, 0), ("qT", 8, 4), ("kT", 12, 0), ("kT", 16, 4), ("gT", 28, 0), ("gT", 32, 4), ("gT", 36, 8), ("gT", 40, 12)]
            for (dst, ct0, r0) in groups:
                stg = stgs[stg_i % 2]
                bs = b_stg[stg_i % 2]
                stg_i += 1
                for cc in range(4):
                    ct = ct0 + cc
                    bank = 2 + (rot % 4)
                    rot += 1
                    pp = PS[bank]
                    for k in range(8):
                        S.add("pe", lambda e, pp=pp, k=k, ct=ct, uT=uT: e.matmul(pp[:], lhsT=w_in[:, k, ct * 128:(ct + 1) * 128], rhs=uT[:, k, :],
                                                                             start=(k == 0), stop=(k == 7)),
                              reads=[b_win[ct // 4], bu], writes=[PB[bank]])
                    fn = AF.Sigmoid if dst == "gT" else AF.Identity
                    S.add("act", lambda e, pp=pp, cc=cc, ct=ct, stg=stg, fn=fn: e.activation(out=stg[:, cc, :], in_=pp[:], func=fn,
                                                                                        bias=cols[:, COL_BIN + ct:COL_BIN + ct + 1], scale=1.0),
                          reads=[PB[bank], C.b_cols], writes=[bs])
                dma(S, "sp", W[dst][r0 * 128:(r0 + 4) * 128, t * 512:(t + 1) * 512].rearrange("(c p) n -> p c n", p=128), stg[:],
                    reads=[bs])
            for s in range(4):
                for hh in range(3):
                    bank = 6 + (rot % 2)
                    rot += 1
                    pp = PS[bank]
                    c0 = 0 if hh == 2 else 2560 + hh * 512
                    for k in range(8):
                        S.add("pe", lambda e, pp=pp, k=k, s=s, c0=c0, uT=uT: e.matmul(pp[:], lhsT=uT[:, k, s * 128:(s + 1) * 128], rhs=w_in[:, k, c0:c0 + 512],
                                                                                  start=(k == 0), stop=(k == 7)),
                              reads=[b_win[c0 // 512], bu], writes=[PB[bank]])
                    if hh < 2:
                        S.add("dve", lambda e, pp=pp, s=s, hh=hh: e.tensor_tensor(out=vstg[:, s, hh * 512:(hh + 1) * 512], in0=pp[:],
                                                                               in1=bias_bc[:, 512 + hh * 512:1024 + hh * 512], op=ALU.add),
                              reads=[PB[bank], b_bias], writes=[b_vstg])
                    else:
                        S.add("dve", lambda e, pp=pp, s=s: e.tensor_tensor(out=s5stg[:, s, :], in0=pp[:], in1=bias_bc[:, 0:512], op=ALU.add),
                              reads=[PB[bank], b_bias], writes=[b_s5stg])
            dma(S, "sp", W["vS"][t * 512:(t + 1) * 512, :].rearrange("(s p) d -> p s d", p=128), vstg[:], reads=[b_vstg])
            dma(S, "sp", W["s5S"][t * 512:(t + 1) * 512, :].rearrange("(s p) d -> p s d", p=128), s5stg[:], reads=[b_s5stg])
        S.flush()


def phase4p(C):
    nc, S, W = C.nc, C.S, C.W
    with ExitStack() as _es:
        sb = lambda name, shape, dt: _es.enter_context(nc.sbuf_tensor(name, shape, dt))
        NB4 = 6
        xb = [sb(f"xb{i}", [128, D], BF16) for i in range(NB4)]
        cgb = [sb(f"cgb{i}", [128, 8], F32) for i in range(NB4)]
        b_xb = [Buf(f"xb{i}") for i in range(NB4)]
        b_cgb = [Buf(f"cgb{i}") for i in range(NB4)]
        for blk in range(32):
            sl = blk % NB4
            dma(S, "sp", xb[sl][:], W["XN"][blk * 128:(blk + 1) * 128, :], writes=[b_xb[sl]])
            dma(S, "sp", cgb[sl][:], W["CG"][blk * 128:(blk + 1) * 128, :], writes=[b_cgb[sl]])
            S.add("pool", lambda e, sl=sl, blk=blk: e.indirect_dma_start(
                out=W["XS"], out_offset=bass.IndirectOffsetOnAxis(ap=C.SLOT_I[:, blk:blk + 1], axis=0), in_=xb[sl][:], in_offset=None), reads=[b_xb[sl]], writes=[], dma=True)
            S.add("pool", lambda e, sl=sl, blk=blk: e.indirect_dma_start(
                out=W["CGS"], out_offset=bass.IndirectOffsetOnAxis(ap=C.SLOT_I[:, blk:blk + 1], axis=0), in_=cgb[sl][:], in_offset=None), reads=[b_cgb[sl]], writes=[], dma=True)
        S.flush()


def phase4b_sparse(C):
    nc, S, I, W = C.nc, C.S, C.I, C.W
    PS, PB = C.PS, C.PB
    cols = C.cols
    bc = C.b_const
    NT = 11
    regs = {}
    with ExitStack() as _es:
        sb = lambda name, shape, dt: _es.enter_context(nc.sbuf_tensor(name, shape, dt))
        xs = [sb(f"xs{i}", [128, 4, D], BF16) for i in range(2)]
        cgs = [sb(f"cgs{i}", [128, 4, 8], F32) for i in range(2)]
        u2t = sb("u2ts", [128, 8, 512], BF16)
        yacc = [sb(f"yaccs{i}", [128, 4, D], F32) for i in range(2)]
        wall = [sb(f"wall{i}", [128, 6144], BF16) for i in range(3)]
        wg = [wall[i][:, 0:2048].rearrange("p (k f) -> p k f", f=256) for i in range(3)]
        wu = [wall[i][:, 2048:4096].rearrange("p (k f) -> p k f", f=256) for i in range(3)]
        wd = [wall[i][:, 4096:6144].rearrange("p (k d) -> p k d", d=D) for i in range(3)]
        sg = [sb(f"sgs{i}", [128, 2, 512], BF16) for i in range(2)]
        hd = [sb(f"hds{i}", [128, 2, 512], BF16) for i in range(2)]
        b_xs = [Buf("xs0"), Buf("xs1")]
        b_cgs = [Buf("cgs0"), Buf("cgs1")]
        b_u2t = Buf("u2ts")
        b_ya = [Buf("ya0"), Buf("ya1")]
        b_wg = [Buf("wg0"), Buf("wg1"), Buf("wg2")]
        b_wu = [Buf("wu0"), Buf("wu1"), Buf("wu2")]
        b_wd = [Buf("wd0"), Buf("wd1"), Buf("wd2")]
        b_sg = [Buf("sg0"), Buf("sg1")]
        b_hd = [Buf("hd0"), Buf("hd1")]

        def load_tile(st):
            sl = st % 2
            dma(S, "sp", xs[sl][:], W["XS"][st * 512:(st + 1) * 512, :].rearrange("(s p) d -> p s d", p=128), writes=[b_xs[sl]])
            dma(S, "sp", cgs[sl][:], W["CGS"][st * 512:(st + 1) * 512, :].rearrange("(s p) e -> p s e", p=128), writes=[b_cgs[sl]])

        def load_w(st, j, sl):
            q = st * 8 + j
            S.add("pool", lambda e, q=q, sl=sl: e.indirect_dma_start(out=wall[sl][:], out_offset=None, in_=W["WALL"],
                                                                  in_offset=bass.IndirectOffsetOnAxis(ap=C.WIDX[:, q:q + 1], axis=0)),
                  reads=[C.b_rk], writes=[b_wg[sl], b_wu[sl], b_wd[sl]], dma=True)

        seq = [(st, j) for st in range(NT) for j in range(8)]
        deferred = []
        load_tile(0)
        load_w(0, 0, 0)
        load_w(0, 1, 1)
        rot = 0
        cnt = 0
        for qi, (st, j) in enumerate(seq):
            sl = qi % 3
            tsl = st % 2
            if j == 0:
                for k in range(8):
                    bk = 6 + k % 2
                    ptb = PS[bk][:].bitcast(BF16)
                    for s_ in range(4):
                        S.add("pe", lambda e, s_=s_, k=k, ptb=ptb, tsl=tsl: e.transpose(out=ptb[:, s_ * 128:(s_ + 1) * 128], in_=xs[tsl][:, s_, k * 128:(k + 1) * 128], identity=C.ident_b[:]),
                              reads=[b_xs[tsl], bc], writes=[PB[bk]])
                    S.add("dve", lambda e, k=k, ptb=ptb: e.tensor_scalar(out=u2t[:, k, :], in0=ptb[:, 0:512], scalar1=cols[:, COL_G2 + k:COL_G2 + k + 1],
                                                                       scalar2=cols[:, COL_MOD + 24 + k:COL_MOD + 25 + k], op0=ALU.mult, op1=ALU.add),
                          reads=[PB[bk], C.b_cols], writes=[b_u2t])
                S.add("pool", lambda e, tsl=tsl: e.memset(yacc[tsl][:], 0.0), writes=[b_ya[tsl]])
            c2 = cnt % 2
            cnt += 1
            for f in range(2):
                for k in range(8):
                    S.add("pe", lambda e, f=f, k=k, sl=sl: e.matmul(PS[f][:], lhsT=wg[sl][:, k, f * 128:(f + 1) * 128], rhs=u2t[:, k, :], start=(k == 0), stop=(k == 7)),
                          reads=[b_wg[sl], b_u2t], writes=[PB[f]])
            for f in range(2):
                for k in range(8):
                    S.add("pe", lambda e, f=f, k=k, sl=sl: e.matmul(PS[2 + f][:], lhsT=wu[sl][:, k, f * 128:(f + 1) * 128], rhs=u2t[:, k, :], start=(k == 0), stop=(k == 7)),
                          reads=[b_wu[sl], b_u2t], writes=[PB[2 + f]])
            for f in range(2):
                S.add("act", lambda e, f=f, c2=c2: e.activation(out=sg[c2][:, f, :], in_=PS[f][:], func=AF.Silu), reads=[PB[f]], writes=[b_sg[c2]])
                S.add("dve", lambda e, f=f, c2=c2: e.tensor_tensor(out=hd[c2][:, f, :], in0=PS[2 + f][:], in1=sg[c2][:, f, :], op=ALU.mult),
                      reads=[PB[2 + f], b_sg[c2]], writes=[b_hd[c2]])

            def down(st=st, j=j, sl=sl, tsl=tsl, c2=c2):
                nonlocal rot
                for sub in range(4):
                    for dh in range(2):
                        bk = 4 + rot % 4
                        rot += 1
                        for f in range(2):
                            S.add("pe", lambda e, bk=bk, f=f, sub=sub, dh=dh: e.matmul(
                                PS[bk][:], lhsT=hd[c2][:, f, sub * 128:(sub + 1) * 128], rhs=wd[sl][:, f, dh * 512:(dh + 1) * 512], start=(f == 0), stop=(f == 1)),
                                reads=[b_hd[c2], b_wd[sl]], writes=[PB[bk]])
                        S.add("dve", lambda e, bk=bk, sub=sub, dh=dh: e.scalar_tensor_tensor(
                            out=yacc[tsl][:, sub, dh * 512:(dh + 1) * 512], in0=PS[bk][:], scalar=cgs[tsl][:, sub, j:j + 1], in1=yacc[tsl][:, sub, dh * 512:(dh + 1) * 512],
                            op0=ALU.mult, op1=ALU.add), reads=[PB[bk], b_cgs[tsl], b_ya[tsl]], writes=[b_ya[tsl]])
                if j == 7:
                    dma(S, "sp", W["YS"][st * 512:(st + 1) * 512, :].rearrange("(s p) d -> p s d", p=128), yacc[tsl][:], reads=[b_ya[tsl]])

            if "nodefer" in C.debug:
                down()
            else:
                if deferred:
                    deferred.pop()()
                deferred.append(down)
            if qi + 2 < len(seq):
                load_w(seq[qi + 2][0], seq[qi + 2][1], (qi + 2) % 3)
            if j == 0 and st + 1 < NT:
                load_tile(st + 1)
        while deferred:
            deferred.pop()()
        S.flush()
    with ExitStack() as _es:
        sb = lambda name, shape, dt: _es.enter_context(nc.sbuf_tensor(name, shape, dt))
        yg = [sb(f"yg{i}", [128, 4, D], F32) for i in range(3)]
        hp = [sb(f"hpf{i}", [128, 4, D], F32) for i in range(3)]
        gfin = sb("gfin_s", [128, D], F32)
        junk = sb("junk6", [128, D], BF16)
        ssq = sb("ssq6", [128, 8], F32)
        b_yg = [Buf("yg0"), Buf("yg1"), Buf("yg2")]
        b_hp = [Buf("hp0"), Buf("hp1"), Buf("hp2")]
        b_gfin, b_junk, b_ssq = Buf("gfin"), Buf("junk"), Buf("ssq")
        dma(S, "sp", gfin[:], I["g_final"].partition_broadcast(128), writes=[b_gfin])

        def fetch(t):
            sl = t % 3
            for s_ in range(4):
                blk = t * 4 + s_
                S.add("pool", lambda e, sl=sl, s_=s_, blk=blk: e.indirect_dma_start(
                    out=yg[sl][:, s_, :], out_offset=None, in_=W["YS"], in_offset=bass.IndirectOffsetOnAxis(ap=C.SLOT_I[:, blk:blk + 1], axis=0)),
                    reads=[C.b_rk], writes=[b_yg[sl]], dma=True)
            dma(S, "sp", hp[sl][:], W["hS"][t * 512:(t + 1) * 512, :].rearrange("(s p) d -> p s d", p=128), writes=[b_hp[sl]])

        fetch(0)
        fetch(1)
        for t in range(NTT):
            sl = t % 3
            if t + 2 < NTT:
                fetch(t + 2)
            r0 = t * 512
            S.add("dve", lambda e, sl=sl: e.tensor_tensor(out=yg[sl][:], in0=yg[sl][:], in1=C.gtf_bc[:].unsqueeze(1).broadcast_to([128, 4, D]), op=ALU.mult),
                  reads=[b_yg[sl], C.b_gt], writes=[b_yg[sl]])
            S.add("dve", lambda e, sl=sl: e.tensor_tensor(out=hp[sl][:], in0=hp[sl][:], in1=yg[sl][:], op=ALU.add), reads=[b_yg[sl], b_hp[sl]], writes=[b_hp[sl]])
            S.add("dve", lambda e: e.memset(ssq[:], 0.0), writes=[b_ssq])
            for s_ in range(4):
                S.add("act", lambda e, s_=s_, sl=sl: e.activation(out=junk[:], in_=hp[sl][:, s_, :], func=AF.Square, accum_out=ssq[:, s_:s_ + 1]),
                      reads=[b_hp[sl], b_ssq], writes=[b_junk, b_ssq])
            S.add("dve", lambda e: e.tensor_scalar(out=ssq[:, 4:8], in0=ssq[:, 0:4], scalar1=1.0 / D, scalar2=EPS, op0=ALU.mult, op1=ALU.add),
                  reads=[b_ssq], writes=[b_ssq])
            S.add("act", lambda e: e.activation(out=ssq[:, 4:8], in_=ssq[:, 4:8], func=AF.Sqrt), reads=[b_ssq], writes=[b_ssq])
            S.add("dve", lambda e: e.reciprocal(out=ssq[:, 4:8], in_=ssq[:, 4:8]), reads=[b_ssq], writes=[b_ssq])
            for s_ in range(4):
                S.add("dve", lambda e, s_=s_, sl=sl: e.scalar_tensor_tensor(out=hp[sl][:, s_, :], in0=hp[sl][:, s_, :], scalar=ssq[:, 4 + s_:5 + s_], in1=gfin[:], op0=ALU.mult, op1=ALU.mult),
                      reads=[b_hp[sl], b_ssq, b_gfin], writes=[b_hp[sl]])
            dma(S, "sp", C.out[r0:r0 + 512, :].rearrange("(s p) d -> p s d", p=128), hp[sl][:], reads=[b_hp[sl]])
        S.flush()


_NC_CACHE = {}


def make_in_maps(inputs):
    maps = []
    shared = {}
    for k, v in inputs.items():
        if k in ("x", "c"):
            continue
        a = np.asarray(v)
        if k != "g_final":
            a = a[0]
        shared[k] = np.ascontiguousarray(a, dtype=np.float32)
    for b in range(8):
        m = dict(shared)
        m["x"] = np.ascontiguousarray(np.asarray(inputs["x"])[b], dtype=np.float32)
        m["c"] = np.ascontiguousarray(np.asarray(inputs["c"])[b], dtype=np.float32)
        maps.append(m)
    return maps


def kernel(**inputs):
    if "nc" not in _NC_CACHE:
        _NC_CACHE["nc"] = build()
    nc = _NC_CACHE["nc"]
    res = run_bass_kernel_spmd(nc, make_in_maps(inputs), core_ids=list(range(8)))
    return np.stack([np.asarray(r["out"], dtype=np.float32) for r in res.results], axis=0)


def _pstep(t):
    n = 1
    for s in list(t.shape)[1:]:
        n *= s
    return n


def view(t, off, dims, parts=128, p0=0):
    ps = _pstep(t)
    return bass.AP(t, p0 * ps + off, [[ps, parts]] + [list(d) for d in dims])


def phase2(C):
    nc, S, I, W = C.nc, C.S, C.I, C.W
    PS, PB = C.PS, C.PB
    cols = C.cols
    ident_f, ident_b = C.ident_f, C.ident_b
    bc = C.b_const
    with ExitStack() as _es:
        Btr = _es.enter_context(nc.sbuf_tensor("Btr", [128, 32, 64], BF16))
        Bti = _es.enter_context(nc.sbuf_tensor("Bti", [128, 32, 64], BF16))
        Ctr = _es.enter_context(nc.sbuf_tensor("Ctr", [64, 32, 128], F32))
        Cti = _es.enter_context(nc.sbuf_tensor("Cti", [64, 32, 128], F32))
        Dt = _es.enter_context(nc.sbuf_tensor("Dt", [128, 32, 128], BF16))
        P12 = _es.enter_context(nc.sbuf_tensor("P12", [64, 2, 64], F32))
        PP = _es.enter_context(nc.sbuf_tensor("PP", [64, 9, 2, 64], F32))
        apw = _es.enter_context(nc.sbuf_tensor("apw", [64, 2, 9, 32], F32))
        dbc = _es.enter_context(nc.sbuf_tensor("dbc", [128, 512], F32))
        wglu = _es.enter_context(nc.sbuf_tensor("wglu", [128, 4, 512], BF16))
        b_wt = Buf("s5w")
        b_wglu = Buf("wglu")
        b_dbc = Buf("dbc")
        dma(S, "pool", wglu[:], I["w_glu"].rearrange("(k p) n -> p k n", p=128), writes=[b_wglu])
        dma(S, "sp", dbc[:], I["s5_d"].partition_broadcast(128), writes=[b_dbc])
        with ExitStack() as _es:
            nat = _es.enter_context(nc.sbuf_tensor("nat", [128, 2, 64], F32))
            cnat = _es.enter_context(nc.sbuf_tensor("cnat", [128, 8, 64], F32))
            sm = _es.enter_context(nc.sbuf_tensor("sm", [64, 24, 32], F32))
            pw = _es.enter_context(nc.sbuf_tensor("pw", [64, 2, 9, 32], F32))
            Bn = _es.enter_context(nc.sbuf_tensor("Bn", [64, 2, 32, 16], F32))
            Bb = _es.enter_context(nc.sbuf_tensor("Bb", [64, 2, 32, 16], F32))
            CT = _es.enter_context(nc.sbuf_tensor("CT", [64, 3, 32, 16], F32))
            Ere = _es.enter_context(nc.sbuf_tensor("Ere", [64, 32, 15, 16], F32))
            Eim = _es.enter_context(nc.sbuf_tensor("Eim", [64, 32, 15, 16], F32))
            tmpB = _es.enter_context(nc.sbuf_tensor("tmpB", [64, 2, 32, 16], F32))
            halfpi = _es.enter_context(nc.sbuf_tensor("halfpi", [64, 1], F32))
            b_nat, b_cnat, b_sm, b_pw, b_Bn, b_Bb, b_CT, b_E, b_tmp = [Buf(n) for n in "nat cnat sm pw Bn Bb CT E tmp".split()]
            S.add("dve", lambda e: e.memset(nat[:], 0.0), writes=[b_nat])
            dma(S, "sp", nat[0:32, 0, :], I["s5_a_re"], writes=[b_nat])
            dma(S, "sp", nat[0:32, 1, :], I["s5_a_im"], writes=[b_nat])
            dma(S, "sp", cnat[:, 0:4, :], I["s5_c_re"].rearrange("g c p -> (g c) p").rearrange("(t q) p -> q t p", q=128), writes=[b_cnat])
            dma(S, "sp", cnat[:, 4:8, :], I["s5_c_im"].rearrange("g c p -> (g c) p").rearrange("(t q) p -> q t p", q=128), writes=[b_cnat])
            dma(S, "sp", Bn[:, 0, :, :], I["s5_b_re"].rearrange("g p c -> p g c"), writes=[b_Bn])
            dma(S, "sp", Bn[:, 1, :, :], I["s5_b_im"].rearrange("g p c -> p g c"), writes=[b_Bn])
            LR, LI, DT, AR, AI, T0, T1, T2, FR, FI, DEN, NR = range(12)
            dma(S, "sp", sm[:, DT, :], I["s5_log_dt"].partition_broadcast(64), writes=[b_sm])
            S.add("dve", lambda e: e.memset(halfpi[:], math.pi / 2), writes=[b_sm])
            for i, slot in ((0, LR), (1, LI)):
                S.add("pe", lambda e, i=i: e.transpose(out=PS[0][0:64, i * 128:(i + 1) * 128], in_=nat[:, i, :], identity=ident_f[:]),
                      reads=[b_nat, bc], writes=[PB[0]])
                S.add("dve", lambda e, i=i, slot=slot: e.tensor_copy(out=sm[:, slot, :], in_=PS[0][0:64, i * 128:i * 128 + 32]),
                      reads=[PB[0]], writes=[b_sm])
            for ri in range(2):
                for t in range(4):
                    S.add("pe", lambda e, ri=ri, t=t: e.transpose(out=PS[1][0:64, t * 128:(t + 1) * 128], in_=cnat[:, ri * 4 + t, :], identity=ident_f[:]),
                          reads=[b_cnat, bc], writes=[PB[1]])
                S.add("dve", lambda e, ri=ri: e.tensor_copy(out=CT[:, ri, :, :], in_=PS[1][0:64, :].rearrange("p (g c) -> p g c", c=16)),
                      reads=[PB[1]], writes=[b_CT])
            S.add("dve", lambda e: e.tensor_scalar(out=CT[:, 2, :, :], in0=CT[:, 1, :, :], scalar1=-1.0, scalar2=None, op0=ALU.mult),
                  reads=[b_CT], writes=[b_CT])

            def sv(i):
                return sm[:, i, :]

            def dv(fn, **kw):
                S.add("dve", lambda e: getattr(e, fn)(**kw), reads=[b_sm, b_pw], writes=[b_sm, b_pw])

            def av(**kw):
                S.add("act", lambda e: e.activation(**kw), reads=[b_sm, b_pw], writes=[b_sm, b_pw])

            av(out=sv(DT), in_=sv(DT), func=AF.Exp)
            dv("tensor_tensor", out=sv(T0), in0=sv(LR), in1=sv(DT), op=ALU.mult)
            av(out=sv(T0), in_=sv(T0), func=AF.Exp, scale=1.0 / 16)
            dv("tensor_tensor", out=sv(T1), in0=sv(LI), in1=sv(DT), op=ALU.mult)
            av(out=sv(AI), in_=sv(T1), func=AF.Sin, scale=1.0 / 16)
            av(out=sv(AR), in_=sv(T1), func=AF.Sin, scale=1.0 / 16, bias=halfpi[:, 0:1])
            dv("tensor_tensor", out=sv(AR), in0=sv(AR), in1=sv(T0), op=ALU.mult)
            dv("tensor_tensor", out=sv(AI), in0=sv(AI), in1=sv(T0), op=ALU.mult)
            for _ in range(4):
                dv("tensor_tensor", out=sv(T0), in0=sv(AR), in1=sv(AR), op=ALU.mult)
                dv("tensor_tensor", out=sv(T1), in0=sv(AI), in1=sv(AI), op=ALU.mult)
                dv("tensor_tensor", out=sv(T2), in0=sv(AR), in1=sv(AI), op=ALU.mult)
                dv("tensor_tensor", out=sv(AR), in0=sv(T0), in1=sv(T1), op=ALU.subtract)
                dv("tensor_scalar", out=sv(AI), in0=sv(T2), scalar1=2.0, scalar2=None, op0=ALU.mult)
            dv("tensor_tensor", out=sv(T0), in0=sv(LR), in1=sv(LR), op=ALU.mult)
            dv("tensor_tensor", out=sv(T1), in0=sv(LI), in1=sv(LI), op=ALU.mult)
            dv("tensor_tensor", out=sv(DEN), in0=sv(T0), in1=sv(T1), op=ALU.add)
            dv("reciprocal", out=sv(DEN), in_=sv(DEN))
            dv("tensor_scalar", out=sv(NR), in0=sv(AR), scalar1=-1.0, scalar2=None, op0=ALU.add)
            dv("tensor_tensor", out=sv(T0), in0=sv(NR), in1=sv(LR), op=ALU.mult)
            dv("tensor_tensor", out=sv(T1), in0=sv(AI), in1=sv(LI), op=ALU.mult)
            dv("tensor_tensor", out=sv(FR), in0=sv(T0), in1=sv(T1), op=ALU.add)
            dv("tensor_tensor", out=sv(FR), in0=sv(FR), in1=sv(DEN), op=ALU.mult)
            dv("tensor_tensor", out=sv(T0), in0=sv(AI), in1=sv(LR), op=ALU.mult)
            dv("tensor_tensor", out=sv(T1), in0=sv(NR), in1=sv(LI), op=ALU.mult)
            dv("tensor_tensor", out=sv(FI), in0=sv(T0), in1=sv(T1), op=ALU.subtract)
            dv("tensor_tensor", out=sv(FI), in0=sv(FI), in1=sv(DEN), op=ALU.mult)
            dv("memset", ap=pw[:, 0, 0, :], constant=1.0)
            dv("memset", ap=pw[:, 1, 0, :], constant=0.0)
            for t in range(8):
                dv("tensor_tensor", out=sv(T0), in0=pw[:, 0, t, :], in1=sv(AR), op=ALU.mult)
                dv("tensor_tensor", out=sv(T1), in0=pw[:, 1, t, :], in1=sv(AI), op=ALU.mult)
                dv("tensor_tensor", out=pw[:, 0, t + 1, :], in0=sv(T0), in1=sv(T1), op=ALU.subtract)
                dv("tensor_tensor", out=sv(T0), in0=pw[:, 0, t, :], in1=sv(AI), op=ALU.mult)
                dv("tensor_tensor", out=sv(T1), in0=pw[:, 1, t, :], in1=sv(AR), op=ALU.mult)
                dv("tensor_tensor", out=pw[:, 1, t + 1, :], in0=sv(T0), in1=sv(T1), op=ALU.add)
            dv("tensor_copy", out=P12[:, 0, 0:32], in_=pw[:, 0, 8, :])
            dv("tensor_copy", out=P12[:, 0, 32:64], in_=pw[:, 0, 8, :])
            dv("tensor_scalar", out=P12[:, 1, 0:32], in0=pw[:, 1, 8, :], scalar1=-1.0, scalar2=None, op0=ALU.mult)
            dv("tensor_copy", out=P12[:, 1, 32:64], in_=pw[:, 1, 8, :])
            dv("memset", ap=apw[:, 0, 0, :], constant=1.0)
            dv("memset", ap=apw[:, 1, 0, :], constant=0.0)
            for t in range(8):
                dv("tensor_tensor", out=sv(T0), in0=apw[:, 0, t, :], in1=pw[:, 0, 8, :], op=ALU.mult)
                dv("tensor_tensor", out=sv(T1), in0=apw[:, 1, t, :], in1=pw[:, 1, 8, :], op=ALU.mult)
                dv("tensor_tensor", out=apw[:, 0, t + 1, :], in0=sv(T0), in1=sv(T1), op=ALU.subtract)
                dv("tensor_tensor", out=sv(T0), in0=apw[:, 0, t, :], in1=pw[:, 1, 8, :], op=ALU.mult)
                dv("tensor_tensor", out=sv(T1), in0=apw[:, 1, t, :], in1=pw[:, 0, 8, :], op=ALU.mult)
                dv("tensor_tensor", out=apw[:, 1, t + 1, :], in0=sv(T0), in1=sv(T1), op=ALU.add)
            for i in range(9):
                dv("tensor_copy", out=PP[:, i, 0, 0:32], in_=apw[:, 0, i, :])
                dv("tensor_copy", out=PP[:, i, 0, 32:64], in_=apw[:, 0, i, :])
                dv("tensor_scalar", out=PP[:, i, 1, 0:32], in0=apw[:, 1, i, :], scalar1=-1.0, scalar2=None, op0=ALU.mult)
                dv("tensor_copy", out=PP[:, i, 1, 32:64], in_=apw[:, 1, i, :])

            def bcast_g(ap2):
                return ap2.unsqueeze(2).broadcast_to([64, 32, 16])

            def big(fn, reads, writes, **kw):
                S.add("dve", lambda e: getattr(e, fn)(**kw), reads=reads, writes=writes)

            RB = [b_sm, b_pw, b_Bn, b_Bb, b_tmp, b_CT]
            big("tensor_tensor", RB, [b_tmp], out=tmpB[:, 0], in0=Bn[:, 0], in1=bcast_g(sv(FR)), op=ALU.mult)
            big("tensor_tensor", RB, [b_tmp], out=tmpB[:, 1], in0=Bn[:, 1], in1=bcast_g(sv(FI)), op=ALU.mult)
            big("tensor_tensor", RB, [b_Bb], out=Bb[:, 0], in0=tmpB[:, 0], in1=tmpB[:, 1], op=ALU.subtract)
            big("tensor_tensor", RB, [b_tmp], out=tmpB[:, 0], in0=Bn[:, 1], in1=bcast_g(sv(FR)), op=ALU.mult)
            big("tensor_tensor", RB, [b_tmp], out=tmpB[:, 1], in0=Bn[:, 0], in1=bcast_g(sv(FI)), op=ALU.mult)
            big("tensor_tensor", RB, [b_Bb], out=Bb[:, 1], in0=tmpB[:, 0], in1=tmpB[:, 1], op=ALU.add)
            if "dbg_s5w" in C.debug:
                C.dbg_s5w = nc.dram_tensor("dbg_s5w", [64, 4 * 32 + 2 * 512], F32, kind="ExternalOutput").ap()
                dma(S, "sp", C.dbg_s5w[:, 0:32], pw[:, 0, 1, :], reads=[b_pw])
                dma(S, "sp", C.dbg_s5w[:, 32:64], pw[:, 1, 1, :], reads=[b_pw])
                dma(S, "sp", C.dbg_s5w[:, 64:96], pw[:, 0, 8, :], reads=[b_pw])
                dma(S, "sp", C.dbg_s5w[:, 96:128], pw[:, 1, 8, :], reads=[b_pw])
                dma(S, "sp", C.dbg_s5w[:, 128:640], Bb[:, 0].rearrange("p g c -> p (g c)"), reads=[b_Bb])
                dma(S, "sp", C.dbg_s5w[:, 640:1152], Bb[:, 1].rearrange("p g c -> p (g c)"), reads=[b_Bb])
            S.add("pool", lambda e: e.memset(Ere[:], 0.0), writes=[b_E])
            S.add("pool", lambda e: e.memset(Eim[:], 0.0), writes=[b_E])
            for m in range(8):
                t = 7 - m
                pr, pi = bcast_g(pw[:, 0, t, :]), bcast_g(pw[:, 1, t, :])
                big("tensor_tensor", RB, [b_tmp], out=tmpB[:, 0], in0=Bb[:, 0], in1=pr, op=ALU.mult)
                big("tensor_tensor", RB, [b_tmp], out=tmpB[:, 1], in0=Bb[:, 1], in1=pi, op=ALU.mult)
                big("tensor_tensor", RB + [b_E], [b_E], out=Ere[:, :, m, :], in0=tmpB[:, 0], in1=tmpB[:, 1], op=ALU.subtract)
                big("tensor_tensor", RB, [b_tmp], out=tmpB[:, 0], in0=Bb[:, 0], in1=pi, op=ALU.mult)
                big("tensor_tensor", RB, [b_tmp], out=tmpB[:, 1], in0=Bb[:, 1], in1=pr, op=ALU.mult)
                big("tensor_tensor", RB + [b_E], [b_E], out=Eim[:, :, m, :], in0=tmpB[:, 0], in1=tmpB[:, 1], op=ALU.add)
            Ctr4 = Ctr[:].rearrange("p g (j c) -> p g j c", c=16)
            Cti4 = Cti[:].rearrange("p g (j c) -> p g j c", c=16)
            for j in range(8):
                pr, pi = bcast_g(pw[:, 0, j + 1, :]), bcast_g(pw[:, 1, j + 1, :])
                big("tensor_tensor", RB, [b_tmp], out=tmpB[:, 0], in0=CT[:, 0], in1=pr, op=ALU.mult)
                big("tensor_tensor", RB, [b_tmp], out=tmpB[:, 1], in0=CT[:, 1], in1=pi, op=ALU.mult)
                big("tensor_tensor", RB + [b_wt], [b_wt], out=Ctr4[:, :, j, :], in0=tmpB[:, 0], in1=tmpB[:, 1], op=ALU.subtract)
                big("tensor_tensor", RB, [b_tmp], out=tmpB[:, 0], in0=CT[:, 0], in1=pi, op=ALU.mult)
                big("tensor_tensor", RB, [b_tmp], out=tmpB[:, 1], in0=CT[:, 2], in1=pr, op=ALU.mult)
                big("tensor_tensor", RB + [b_wt], [b_wt], out=Cti4[:, :, j, :], in0=tmpB[:, 1], in1=tmpB[:, 0], op=ALU.subtract)
            rot = 0
            for g0 in range(0, 32, 4):
                for ri, (Et, Bt) in enumerate(((Ere, Btr), (Eim, Bti))):
                    bank = 2 + (rot % 2)
                    rot += 1
                    for gg in range(4):
                        g = g0 + gg
                        S.add("pe", lambda e, Et=Et, g=g, gg=gg, bank=bank: e.transpose(
                            out=PS[bank][:, gg * 64:(gg + 1) * 64], in_=Et[:, g, 0:8, :].rearrange("p m c -> p (m c)"), identity=ident_f[0:64, 0:64]),
                            reads=[b_E, bc], writes=[PB[bank]])
                    S.add("dve", lambda e, Bt=Bt, g0=g0, bank=bank: e.tensor_copy(out=Bt[:, g0:g0 + 4, :],
                                                                             in_=PS[bank][:, 0:256].rearrange("p (g q) -> p g q", q=64)),
                          reads=[PB[bank]], writes=[b_wt])
                bank = 4 + ((g0 // 4) % 2)
                for gg in range(4):
                    g = g0 + gg
                    for j in range(8):
                        o = PS[bank][:, gg * 128 + j * 16: gg * 128 + (j + 1) * 16]
                        S.add("pe", lambda e, o=o, g=g, j=j: e.matmul(o, lhsT=Ere[:, g, 7 - j:15 - j, :].rearrange("p m c -> p (m c)"), rhs=CT[:, 0, g, :],
                                                                    start=True, stop=False),
                              reads=[b_E, b_CT], writes=[PB[bank]])
                        S.add("pe", lambda e, o=o, g=g, j=j: e.matmul(o, lhsT=Eim[:, g, 7 - j:15 - j, :].rearrange("p m c -> p (m c)"), rhs=CT[:, 2, g, :],
                                                                    start=False, stop=True),
                              reads=[b_E, b_CT], writes=[PB[bank]])
                S.add("act", lambda e, g0=g0, bank=bank: e.activation(out=Dt[:, g0:g0 + 4, :], in_=PS[bank][:].rearrange("p (g q) -> p g q", q=128), func=AF.Copy),
                      reads=[PB[bank]], writes=[b_wt])
            S.flush()
        if "stop_s5w" in C.debug:
            return
        with ExitStack() as _es:
            Uc = _es.enter_context(nc.sbuf_tensor("Uc", [128, 8, 512], F32))
            UT = _es.enter_context(nc.sbuf_tensor("UT", [128, 32, 128], BF16))
            Ug = _es.enter_context(nc.sbuf_tensor("Ug", [128, 32, 128], BF16))
            b_Ug = Buf("Ug")
            Xh = _es.enter_context(nc.sbuf_tensor("Xh", [64, 129, 64], F32))
            st = _es.enter_context(nc.sbuf_tensor("st", [64, 2, 64], F32))
            stw = [_es.enter_context(nc.sbuf_tensor(f"stw{i}", [64, 2, 16, 64], F32)) for i in range(2)]
            b_stw = [[Buf("stw00"), Buf("stw01")], [Buf("stw10"), Buf("stw11")]]
            Yc = _es.enter_context(nc.sbuf_tensor("Yc", [128, 8, 512], F32))
            Gc = _es.enter_context(nc.sbuf_tensor("Gc", [128, 8, 512], BF16))
            geT = _es.enter_context(nc.sbuf_tensor("geT", [128, 4, 1024], BF16))
            y2s = _es.enter_context(nc.sbuf_tensor("y2s", [128, 4, 1024], BF16))
            sig = _es.enter_context(nc.sbuf_tensor("sig", [128, 512], BF16))
            b_Uc, b_UT, b_Xh, b_st, b_Yc, b_Gc, b_geT, b_y2s, b_sig, b_st1 = [Buf(n) for n in "Uc UT Xh st Yc Gc geT y2s sig st1".split()]
            s5v = W["s5S"].rearrange("(c i) f -> c (i f)", i=8)
            S.add("dve", lambda e: e.memset(Xh[:, 0, :], 0.0), writes=[b_Xh])
            prev_rest = []
            sig2 = [sig, _es.enter_context(nc.sbuf_tensor("sigB", [128, 512], BF16))]
            b_sig2 = [Buf("sig0"), Buf("sig1")]
            for T in range(4):
                dma(S, "sp", Uc[:].rearrange("p i f -> p (i f)"), s5v[T * 128:(T + 1) * 128, :], writes=[b_Uc])
                S.add("pool", lambda e: e.tensor_copy(out=Ug[:].rearrange("p g (i c) -> p g i c", c=16), in_=view(Uc, 0, [[16, 32], [512, 8], [1, 16]])),
                      reads=[b_Uc], writes=[b_Ug])
                for g0 in range(0, 32, 8):
                    bank = (g0 // 8) % 2
                    pb = PS[bank][:].bitcast(BF16)
                    for gg in range(8):
                        g = g0 + gg
                        S.add("pe", lambda e, g=g, gg=gg, pb=pb: e.transpose(out=pb[:, gg * 128:(gg + 1) * 128], in_=Ug[:, g, :], identity=ident_b[:]),
                              reads=[b_Ug, bc], writes=[PB[bank]])
                    S.add("act", lambda e, g0=g0, pb=pb: e.activation(out=UT[:, g0:g0 + 8, :], in_=pb[:, 0:1024].rearrange("p (g q) -> p g q", q=128), func=AF.Copy),
                          reads=[PB[bank]], writes=[b_UT])
                for g0 in range(0, 32, 4):
                    for ri, Bt in enumerate((Btr, Bti)):
                        bank = 2 + ri
                        for gg in range(4):
                            g = g0 + gg
                            S.add("pe", lambda e, Bt=Bt, g=g, gg=gg, bank=bank: e.matmul(PS[bank][0:64, gg * 128:(gg + 1) * 128], lhsT=Bt[:, g, :], rhs=UT[:, g, :],
                                                                                    start=True, stop=True),
                                  reads=[b_wt, b_UT], writes=[PB[bank]])
                        o = view(Xh, 64 + ri * 32 + g0, [[1, 4], [64, 128]], parts=64)
                        S.add("dve", lambda e, o=o, bank=bank: e.tensor_copy(out=o, in_=PS[bank][0:64, :].rearrange("p (g c) -> p g c", c=128)),
                              reads=[PB[bank]], writes=[b_Xh])
                if len(prev_rest) > 0:
                    prev_rest.pop()()
                def cmul_acc(dst0, src0, nb, i, k):
                    dst = view(Xh, dst0 * 64, [[512, nb], [1, 64]], parts=64)
                    src = view(Xh, src0 * 64, [[512, nb], [1, 64]], parts=64)
                    srcw = view(Xh, src0 * 64 + 32, [[512, nb], [-32, 2], [1, 32]], parts=64)
                    t1 = stw[k][:, 0, 0:nb, :]
                    t2 = stw[k][:, 1, 0:nb, :]
                    S.add("dve", lambda e: e.tensor_tensor(out=t1, in0=src, in1=PP[:, i, 0, :].unsqueeze(1).broadcast_to([64, nb, 64]), op=ALU.mult),
                          reads=[b_Xh], writes=[b_stw[k][0]])
                    S.add("dve", lambda e: e.tensor_tensor(out=t2.rearrange("p b (a c) -> p b a c", a=2), in0=srcw,
                                                           in1=PP[:, i, 1, :].rearrange("p (a c) -> p a c", a=2).unsqueeze(1).broadcast_to([64, nb, 2, 32]), op=ALU.mult),
                          reads=[b_Xh], writes=[b_stw[k][1]])
                    S.add("dve", lambda e: e.tensor_tensor(out=t1, in0=t1, in1=t2, op=ALU.add), reads=[b_stw[k][0], b_stw[k][1]], writes=[b_stw[k][0]])
                    S.add("dve", lambda e: e.tensor_tensor(out=dst, in0=dst, in1=t1, op=ALU.add), reads=[b_stw[k][0], b_Xh], writes=[b_Xh])

                if "s5_noscan" not in C.debug:
                    for i in range(1, 8):
                        cmul_acc(i + 1, i, 16, 1, i % 2)
                    for b in range(16):
                        cmul_acc(b * 8 + 8, b * 8, 1, 8, b % 2)
                    for i in range(1, 8):
                        cmul_acc(i, 0, 16, i, i % 2)
                if "s5_noY" in C.debug:
                    continue
                S.add("pool", lambda e: e.tensor_tensor(out=Yc[:], in0=Uc[:], in1=dbc[:].unsqueeze(1).broadcast_to([128, 8, 512]), op=ALU.mult),
                      reads=[b_Uc, b_dbc], writes=[b_Yc])
                for g0 in range(0, 32, 4):
                    bank = 4 + (g0 // 4) % 2
                    for gg in range(4):
                        g = g0 + gg
                        o = PS[bank][:, gg * 128:(gg + 1) * 128]
                        S.add("pe", lambda e, o=o, g=g: e.matmul(o, lhsT=UT[:, g, :], rhs=Dt[:, g, :], start=True, stop=False),
                              reads=[b_UT, b_wt], writes=[PB[bank]])
                        xr = view(Xh, g, [[64, 128]], parts=64)
                        xi = view(Xh, 32 + g, [[64, 128]], parts=64)
                        S.add("pe", lambda e, o=o, g=g, xr=xr: e.matmul(o, lhsT=xr, rhs=Ctr[:, g, :], start=False, stop=False),
                              reads=[b_Xh, b_wt], writes=[PB[bank]])
                        S.add("pe", lambda e, o=o, g=g, xi=xi: e.matmul(o, lhsT=xi, rhs=Cti[:, g, :], start=False, stop=True),
                              reads=[b_Xh, b_wt], writes=[PB[bank]])
                    yv = view(Yc, g0 * 16, [[16, 4], [512, 8], [1, 16]])
                    S.add("dve", lambda e, yv=yv, bank=bank: e.tensor_tensor(out=yv, in0=PS[bank][:].rearrange("p (g j c) -> p g j c", g=4, j=8), in1=yv, op=ALU.add),
                          reads=[PB[bank], b_Yc], writes=[b_Yc])
                if T < 3:
                    S.add("dve", lambda e: e.tensor_copy(out=Xh[:, 0, :], in_=Xh[:, 128, :]), reads=[b_Xh], writes=[b_Xh])
                if "dbg_s5y" in C.debug:
                    for i in range(8):
                        dma(S, "sp", C.dbg_s5y.rearrange("(c i) f -> c i f", i=8)[T * 128:(T + 1) * 128, i, :], Yc[:, i, :], reads=[b_Yc])
                def rest(T=T):
                    for i in range(8):
                        S.add("act", lambda e, i=i: e.activation(out=Gc[:, i, :], in_=Yc[:, i, :], func=AF.Gelu_apprx_tanh), reads=[b_Yc], writes=[b_Gc])
                    for ct in range(4):
                        bank = 6 + (ct % 2)
                        pb = PS[bank][:].bitcast(BF16)
                        for j in range(8):
                            S.add("pe", lambda e, pb=pb, j=j, ct=ct: e.transpose(out=pb[:, j * 128:(j + 1) * 128], in_=Gc[:, j, ct * 128:(ct + 1) * 128], identity=ident_b[:]),
                                  reads=[b_Gc, bc], writes=[PB[bank]])
                        o = view(geT, ct * 1024, [[1, 8], [8, 128]])
                        S.add("act", lambda e, o=o, pb=pb: e.activation(out=o, in_=pb[:, 0:1024].rearrange("p (j c) -> p j c", j=8), func=AF.Copy), reads=[PB[bank]], writes=[b_geT])
                    for co in range(4):
                        for hh in range(2):
                            q2 = (co * 2 + hh) % 2
                            bank = 6 + q2
                            for ci in range(4):
                                S.add("pe", lambda e, co=co, hh=hh, ci=ci, bank=bank: e.matmul(PS[bank][:], lhsT=wglu[:, ci, co * 128:(co + 1) * 128],
                                                                                          rhs=geT[:, ci, hh * 512:(hh + 1) * 512], start=(ci == 0), stop=(ci == 3)),
                                      reads=[b_wglu, b_geT], writes=[PB[bank]])
                            S.add("act", lambda e, co=co, bank=bank, q2=q2: e.activation(out=sig2[q2][:], in_=PS[bank][:], func=AF.Sigmoid,
                                                                                     bias=cols[:, COL_BGLU + co:COL_BGLU + co + 1], scale=1.0),
                                  reads=[PB[bank], C.b_cols], writes=[b_sig2[q2]])
                            S.add("pool", lambda e, co=co, hh=hh, q2=q2: e.tensor_tensor(out=y2s[:, co, hh * 512:(hh + 1) * 512], in0=geT[:, co, hh * 512:(hh + 1) * 512],
                                                                                      in1=sig2[q2][:], op=ALU.mult),
                                  reads=[b_geT, b_sig2[q2]], writes=[b_y2s])
                    dma(S, "sp", W["y2T"][:, T * 1024:(T + 1) * 1024].rearrange("(c p) n -> p c n", p=128), y2s[:], reads=[b_y2s])

                prev_rest.append(rest)
            while prev_rest:
                prev_rest.pop()()
            S.flush()


_dummy = {}


def b_sm_dummy(C):
    if "b" not in _dummy:
        _dummy["b"] = Buf("dummy")
    return _dummy["b"]


def phase3(C):
    nc, S, I, W = C.nc, C.S, C.I, C.W
    PS, PB = C.PS, C.PB
    cols = C.cols
    bc = C.b_const
    NQ = 8
    heads = C.heads if hasattr(C, "heads") else range(8)
    with ExitStack() as _es:
        sb = lambda name, shape, dt: _es.enter_context(nc.sbuf_tensor(name, shape, dt))
        lqk = sb("lqk", [128, 4, 64], F32)
        lsc = sb("lsc", [128, 8], F32)
        masks = sb("masks", [128, 4, 512], BF16)
        kTh = [sb(f"kTh{i}", [128, L], BF16) for i in range(2)]
        qTh = [sb(f"qTh{i}", [128, L], BF16) for i in range(2)]
        Vh = [sb(f"Vh{i}", [128, 32, 128], BF16) for i in range(2)]
        Et = sb("Et", [128, 8, 512], BF16)
        rr = sb("rr", [128, 2, 512], F32)
        oo = sb("oo", [128, 2, 512], F32)
        sq = sb("sq", [128, 512], BF16)
        ons = [sb(f"ons{i}", [128, 512], BF16) for i in range(2)]
        b_l, b_mask = Buf("lam"), Buf("mask")
        b_k = [Buf("k0"), Buf("k1")]
        b_q = [Buf("q0"), Buf("q1")]
        b_v = [Buf("v0"), Buf("v1")]
        b_E = [Buf(f"E{i}") for i in range(8)]
        b_rr, b_oo, b_sq = Buf("rr"), Buf("oo"), Buf("sq")
        b_ons = [Buf("ons0"), Buf("ons1")]
        for i, n in enumerate(["lambda_q1", "lambda_k1", "lambda_q2", "lambda_k2"]):
            dma(S, "sp", lqk[:, i, :], I[n].partition_broadcast(128), writes=[b_l])
        S.add("dve", lambda e: e.tensor_tensor(out=lqk[:, 0, :], in0=lqk[:, 0, :], in1=lqk[:, 1, :], op=ALU.mult), reads=[b_l], writes=[b_l])
        S.add("dve", lambda e: e.tensor_tensor(out=lqk[:, 2, :], in0=lqk[:, 2, :], in1=lqk[:, 3, :], op=ALU.mult), reads=[b_l], writes=[b_l])
        S.add("dve", lambda e: e.reduce_sum(out=lsc[:, 0:1], in_=lqk[:, 0, :], axis=AX.X), reads=[b_l], writes=[b_l])
        S.add("dve", lambda e: e.reduce_sum(out=lsc[:, 1:2], in_=lqk[:, 2, :], axis=AX.X), reads=[b_l], writes=[b_l])
        S.add("act", lambda e: e.activation(out=lsc[:, 2:4], in_=lsc[:, 0:2], func=AF.Exp), reads=[b_l], writes=[b_l])
        S.add("dve", lambda e: e.tensor_tensor(out=lsc[:, 4:5], in0=lsc[:, 3:4], in1=lsc[:, 2:3], op=ALU.subtract), reads=[b_l], writes=[b_l])
        S.add("dve", lambda e: e.tensor_scalar(out=lsc[:, 5:6], in0=lsc[:, 4:5], scalar1=-LAMBDA_INIT, scalar2=None, op0=ALU.add), reads=[b_l], writes=[b_l])
        nlam = lsc[:, 5:6]
        S.add("pool", lambda e: e.memset(masks[:], 0.0), writes=[b_mask])
        for r in range(4):
            S.add("pool", lambda e, r=r: e.memset(masks[0:64, r, 128 * r:512], 1.0), writes=[b_mask])
            S.add("pool", lambda e, r=r: e.memset(masks[64:128, r, 128 * r + 64:512], 1.0), writes=[b_mask])

        def load_head(h, slot):
            dma(S, "sp", kTh[slot][:], W["kT"][h * 128:(h + 1) * 128, :], writes=[b_k[slot]])
            dma(S, "sp", qTh[slot][:], W["qT"][h * 128:(h + 1) * 128, :], writes=[b_q[slot]])
            dma(S, "sp", Vh[slot][:], W["vS"][:, h * 128:(h + 1) * 128].rearrange("(j p) e -> p j e", p=128), writes=[b_v[slot]])

        hl = list(heads)
        load_head(hl[0], 0)
        ecnt = 0
        ocnt = 0
        lnt = sb("lnt", [128, 2, 512], F32)
        b_ln = Buf("lnt")
        wst = [sb(f"wst{i}", [128, 6144], BF16) for i in range(2)]
        b_wst = [Buf("wst0"), Buf("wst1")]

        def precast_load(ex):
            sl = ex % 2
            dma(S, "pool", wst[sl][:, 0:2048].rearrange("p (k f) -> p k f", f=256), I["w_exp_gate"][ex].rearrange("(k p) f -> p k f", p=128), writes=[b_wst[sl]])
            dma(S, "pool", wst[sl][:, 2048:4096].rearrange("p (k f) -> p k f", f=256), I["w_exp_up"][ex].rearrange("(k p) f -> p k f", p=128), writes=[b_wst[sl]])
            dma(S, "pool", wst[sl][:, 4096:6144].rearrange("p (k d) -> p k d", d=D), I["w_exp_down"][ex].rearrange("(k p) d -> p k d", p=128), writes=[b_wst[sl]])

        def precast_store(ex):
            sl = ex % 2
            for c3 in range(3):
                dma(S, "sp", W["WALL"][ex * 128:(ex + 1) * 128, c3 * 2048:(c3 + 1) * 2048], wst[sl][:, c3 * 2048:(c3 + 1) * 2048], reads=[b_wst[sl]])

        unit = 0
        e2cnt = 0
        EPI_STEP = int(C.epi_step) if hasattr(C, "epi_step") else 8
        Es2 = sb("Es2", [128, 4, 512], BF16)
        b_E2 = [Buf(f"E2_{i}") for i in range(4)]
        pending = []

        def epi_fast(h, Q):
            S.add("act", lambda e: e.activation(out=oo[:, 0, :], in_=PS[4][:], func=AF.Copy), reads=[PB[4]], writes=[b_oo])
            S.add("act", lambda e: e.activation(out=oo[:, 1, :], in_=PS[5][:], func=AF.Copy), reads=[PB[5]], writes=[b_oo])
            S.add("dve", lambda e: e.tensor_copy(out=lnt[:, 0, :], in_=PS[6][:]), reads=[PB[6]], writes=[b_ln])
            S.add("dve", lambda e: e.tensor_copy(out=lnt[:, 1, :], in_=PS[7][:]), reads=[PB[7]], writes=[b_ln])
            S.add("dve", lambda e: e.reciprocal(out=rr[:, 0, :], in_=lnt[:, 1, :]), reads=[b_ln], writes=[b_rr])
            S.add("dve", lambda e: e.tensor_tensor(out=rr[:, 0, :], in0=rr[:, 0, :], in1=lnt[:, 0, :], op=ALU.mult), reads=[b_rr, b_ln], writes=[b_rr])
            S.add("dve", lambda e: e.tensor_tensor(out=oo[:, 1, :], in0=oo[:, 1, :], in1=rr[:, 0, :], op=ALU.mult), reads=[b_oo, b_rr], writes=[b_oo])
            S.add("dve", lambda e: e.scalar_tensor_tensor(out=oo[:, 0, :], in0=oo[:, 1, :], scalar=nlam, in1=oo[:, 0, :], op0=ALU.mult, op1=ALU.add),
                  reads=[b_oo, b_l], writes=[b_oo])
            S.add("pool", lambda e: e.tensor_tensor(out=sq[:], in0=oo[:, 0, :], in1=oo[:, 0, :], op=ALU.mult), reads=[b_oo], writes=[b_sq])
            S.add("dve", lambda e: e.scalar_tensor_tensor(out=rr[:, 1, :], in0=lnt[:, 0, :], scalar=EPS, in1=lnt[:, 0, :], op0=ALU.mult, op1=ALU.mult),
                  reads=[b_ln], writes=[b_rr])

        def epi_slow(h, Q):
            nonlocal ocnt
            S.add("pe", lambda e: e.matmul(PS[0][:], lhsT=C.ones_b[:], rhs=sq[:], start=True, stop=True), reads=[bc, b_sq], writes=[PB[0]])
            S.add("dve", lambda e: e.scalar_tensor_tensor(out=rr[:, 1, :], in0=PS[0][:], scalar=1.0 / 128, in1=rr[:, 1, :], op0=ALU.mult, op1=ALU.add),
                  reads=[PB[0], b_rr], writes=[b_rr])
            S.add("act", lambda e: e.activation(out=rr[:, 1, :], in_=rr[:, 1, :], func=AF.Ln), reads=[b_rr], writes=[b_rr])
            S.add("act", lambda e: e.activation(out=rr[:, 1, :], in_=rr[:, 1, :], func=AF.Exp, scale=-0.5), reads=[b_rr], writes=[b_rr])
            on = ons[ocnt % 2]
            bo = b_ons[ocnt % 2]
            ocnt += 1
            S.add("dve", lambda e, on=on: e.scalar_tensor_tensor(out=on[:], in0=oo[:, 0, :], scalar=cols[:, COL_GSUBS:COL_GSUBS + 1], in1=rr[:, 1, :],
                                                              op0=ALU.mult, op1=ALU.mult),
                  reads=[b_oo, b_rr, C.b_cols], writes=[bo])
            dma(S, "sp", W["onT"][h * 128:(h + 1) * 128, Q * 512:(Q + 1) * 512], on[:], reads=[bo])

        for hi, h in enumerate(hl):
            slot = hi % 2
            if hi + 1 < len(hl):
                load_head(hl[hi + 1], (hi + 1) % 2)
            kt, qt, vt = kTh[slot], qTh[slot], Vh[slot]
            bk, bq, bv = b_k[slot], b_q[slot], b_v[slot]
            for Q in range(NQ):
                nJ = 4 * (Q + 1)
                eslot = {}
                lready, lnext = [], []
                if C.sparse:
                    if 1 <= unit <= NEXP:
                        precast_store(unit - 1)
                    if unit < NEXP:
                        precast_load(unit)
                    if unit == NEXP + 1:
                        dma(S, "pool", C.wbs[:], I["w_br_ssm"].rearrange("(k p) n -> p k n", p=128), writes=[C.b_w4])
                        dma(S, "pool", C.wba[:], I["w_br_attn"].rearrange("(k p) n -> p k n", p=128), writes=[C.b_w4])
                        dma(S, "pool", C.wout[:], I["w_out"].rearrange("(k p) n -> p k n", p=128), writes=[C.b_w4])
                        C.w4_loaded = True
                    unit += 1
                for step in range(nJ + 1):
                    if step < nJ:
                        J = step
                        c0 = 128 * max(0, J - 4 * Q)
                        for m in range(2):
                            bank = 2 * m + (J % 2)
                            S.add("pe", lambda e, bank=bank, m=m, J=J, Q=Q, kt=kt, qt=qt, c0=c0: e.matmul(
                                PS[bank][:, c0:512], lhsT=kt[m * 64:(m + 1) * 64, J * 128:(J + 1) * 128], rhs=qt[m * 64:(m + 1) * 64, Q * 512 + c0:(Q + 1) * 512],
                                start=True, stop=True), reads=[bk, bq], writes=[PB[bank]])
                            es = ecnt % 8
                            ecnt += 1
                            eslot[(J, m)] = es
                            S.add("act", lambda e, bank=bank, es=es, c0=c0: e.activation(out=Et[:, es, c0:512], in_=PS[bank][:, c0:512], func=AF.Exp, scale=0.125),
                                  reads=[PB[bank]], writes=[b_E[es]])
                            if J >= 4 * Q:
                                r = J - 4 * Q
                                S.add("dve", lambda e, es=es, r=r, c0=c0: e.tensor_tensor(out=Et[:, es, c0:512], in0=Et[:, es, c0:512], in1=masks[:, r, c0:512], op=ALU.mult),
                                      reads=[b_E[es], b_mask], writes=[b_E[es]])
                    if step >= 1:
                        J = step - 1
                        c0 = 128 * max(0, J - 4 * Q)
                        for m in range(2):
                            es = eslot[(J, m)]
                            S.add("pe", lambda e, m=m, J=J, es=es, vt=vt, nJ=nJ, c0=c0: e.matmul(PS[4 + m][:, c0:512], lhsT=vt[:, J, :], rhs=Et[:, es, c0:512],
                                                                                     start=(J == 0), stop=(J == nJ - 1)),
                                  reads=[bv, b_E[es]], writes=[PB[4 + m]])
                            S.add("pe", lambda e, m=m, J=J, es=es, nJ=nJ, c0=c0: e.matmul(PS[6 + m][:, c0:512], lhsT=C.ones_b[:], rhs=Et[:, es, c0:512],
                                                                              start=(J == 0), stop=(J == nJ - 1)),
                                  reads=[bc, b_E[es]], writes=[PB[6 + m]])
                    if step == min(EPI_STEP, nJ) and pending:
                        epi_slow(*pending.pop())
                epi_fast(h, Q)
                pending.append((h, Q))
        while pending:
            epi_slow(*pending.pop())
        S.flush()


def phase4a(C):
    nc, S, I, W = C.nc, C.S, C.I, C.W
    PS, PB = C.PS, C.PB
    cols = C.cols
    bc = C.b_const
    with ExitStack() as _es:
        sb = lambda name, shape, dt: _es.enter_context(nc.sbuf_tensor(name, shape, dt))
        wbs, wba, wout = C.wbs, C.wba, C.wout
        wr = sb("wr", [128, 8, 36], BF16)
        brt = sb("brt", [128, 36], F32)
        y2t = [sb(f"y2t{i}", [128, 4, 512], BF16) for i in range(2)]
        ont = [sb(f"ont{i}", [128, 8, 512], BF16) for i in range(2)]
        gtt = [sb(f"gtt{i}", [128, 16, 512], BF16) for i in range(2)]
        xt = sb("xt4", [128, 4, D], F32)
        mT = sb("mT", [128, 8, 512], BF16)
        tA = sb("tA", [128, 512], F32)
        tB = sb("tB", [128, 512], F32)
        hh_ = sb("h4", [128, 4, D], F32)
        xn2 = sb("xn2", [128, 4, D], BF16)
        junk = sb("junk4", [128, D], BF16)
        u2 = [sb(f"u2_{i}", [128, 8, 512], BF16) for i in range(2)]
        ssq = sb("ssq4", [128, 8], F32)
        rt = sb("rt", [128, 4, 36], F32)
        rw = sb("rw", [128, 24, 4], F32)
        ohg = sb("ohg", [128, 4, 4], F32)
        tE = sb("tE", [128, 4, 4, 8], F32)
        es = sb("es", [128, 6, 4, 8], F32)
        comb = [sb(f"comb{i}", [128, 4, 32], F32) for i in range(2)]
        ustr_f = sb("ustr_f", [128, 128], F32)
        ustr = sb("ustr", [128, 128], BF16)
        ohgb = sb("ohgb", [128, 4, 4], BF16)
        base = sb("base4", [128, 4, 4], F32)
        b_ohgb, b_base, b_tot = Buf("ohgb"), Buf("base"), Buf("tot")
        b_w = Buf("w4")
        if C.sparse:
            S.add("pool", lambda e: e.memset(ustr_f[:], 1.0), writes=[b_ohgb])
            S.add("pool", lambda e: e.affine_select(out=ustr_f[:], in_=ustr_f[:], pattern=[[1, 128]], compare_op=ALU.is_gt, fill=0.0, base=0, channel_multiplier=-1),
                  reads=[b_ohgb], writes=[b_ohgb])
            S.add("dve", lambda e: e.tensor_copy(out=ustr[:], in_=ustr_f[:]), reads=[b_ohgb], writes=[b_ohgb])
            S.add("dve", lambda e: e.memset(C.tot[:], 0.0), writes=[b_tot])
        if not getattr(C, "w4_loaded", False):
            dma(S, "pool", wbs[:], I["w_br_ssm"].rearrange("(k p) n -> p k n", p=128), writes=[b_w])
            dma(S, "pool", wba[:], I["w_br_attn"].rearrange("(k p) n -> p k n", p=128), writes=[b_w])
            dma(S, "pool", wout[:], I["w_out"].rearrange("(k p) n -> p k n", p=128), writes=[b_w])
        dma(S, "pool", wr[:, :, 0:4], I["w_router_grp"].rearrange("(k p) n -> p k n", p=128), writes=[b_w])
        dma(S, "pool", wr[:, :, 4:36], I["w_router_exp"].rearrange("(k p) n -> p k n", p=128), writes=[b_w])
        dma(S, "sp", brt[:, 0:4], I["b_router_grp"].partition_broadcast(128), writes=[b_w])
        dma(S, "sp", brt[:, 4:36], I["b_router_exp"].partition_broadcast(128), writes=[b_w])
        b_y2 = [Buf("y2t0"), Buf("y2t1")]
        b_on = [Buf("ont0"), Buf("ont1")]
        b_gt = [Buf("gtt0"), Buf("gtt1")]
        b_u2 = [Buf("u2_0"), Buf("u2_1")]
        b_cb = [Buf("comb0"), Buf("comb1")]
        b_xt, b_mT, b_tA, b_tB, b_h, b_xn2, b_junk, b_ssq, b_rt, b_rw = [Buf(n) for n in "xt mT tA tB h xn2 junk ssq rt rw".split()]

        def load(t):
            sl = t % 2
            dma(S, "sp", y2t[sl][:], W["y2T"][:, t * 512:(t + 1) * 512].rearrange("(c p) n -> p c n", p=128), writes=[b_y2[sl]])
            dma(S, "sp", ont[sl][:], W["onT"][:, t * 512:(t + 1) * 512].rearrange("(c p) n -> p c n", p=128), writes=[b_on[sl]])
            dma(S, "sp", gtt[sl][:], W["gT"][:, t * 512:(t + 1) * 512].rearrange("(c p) n -> p c n", p=128), writes=[b_gt[sl]])

        load(0)
        rot = 0
        routes = []
        for t in range(NTT):
            sl = t % 2
            if t + 1 < NTT:
                load(t + 1)
            dma(S, "sp", xt[:], I["x"][t * 512:(t + 1) * 512, :].rearrange("(s p) d -> p s d", p=128), writes=[b_xt])
            for ct in range(8):
                ba = rot % 2
                bb = 2 + rot % 2
                rot += 1
                for k in range(4):
                    S.add("pe", lambda e, ba=ba, k=k, ct=ct, sl=sl: e.matmul(PS[ba][:], lhsT=wbs[:, k, ct * 128:(ct + 1) * 128], rhs=y2t[sl][:, k, :],
                                                                        start=(k == 0), stop=(k == 3)), reads=[b_w, b_y2[sl]], writes=[PB[ba]])
                for k in range(8):
                    S.add("pe", lambda e, bb=bb, k=k, ct=ct, sl=sl: e.matmul(PS[bb][:], lhsT=wba[:, k, ct * 128:(ct + 1) * 128], rhs=ont[sl][:, k, :],
                                                                        start=(k == 0), stop=(k == 7)), reads=[b_w, b_on[sl]], writes=[PB[bb]])
                S.add("dve", lambda e, ba=ba, ct=ct, sl=sl: e.tensor_tensor(out=tA[:], in0=PS[ba][:], in1=gtt[sl][:, ct, :], op=ALU.mult),
                      reads=[PB[ba], b_gt[sl]], writes=[b_tA])
                S.add("dve", lambda e, bb=bb, ct=ct, sl=sl: e.tensor_tensor(out=tB[:], in0=PS[bb][:], in1=gtt[sl][:, 8 + ct, :], op=ALU.mult),
                      reads=[PB[bb], b_gt[sl]], writes=[b_tB])
                S.add("pool", lambda e, ct=ct: e.tensor_tensor(out=mT[:, ct, :], in0=tA[:], in1=tB[:], op=ALU.add), reads=[b_tA, b_tB], writes=[b_mT])
            if routes:
                routes.pop()()
            for s in range(4):
                for hf in range(2):
                    bk = 4 + rot % 2
                    rot += 1
                    for k in range(8):
                        S.add("pe", lambda e, bk=bk, k=k, s=s, hf=hf: e.matmul(PS[bk][:], lhsT=mT[:, k, s * 128:(s + 1) * 128], rhs=wout[:, k, hf * 512:(hf + 1) * 512],
                                                                          start=(k == 0), stop=(k == 7)), reads=[b_mT, b_w], writes=[PB[bk]])
                    S.add("dve", lambda e, bk=bk, hf=hf: e.tensor_tensor(out=tA[:], in0=PS[bk][:], in1=C.gtm_bc[:, hf * 512:(hf + 1) * 512], op=ALU.mult),
                          reads=[PB[bk], C.b_gt], writes=[b_tA])
                    S.add("pool", lambda e, s=s, hf=hf: e.tensor_tensor(out=hh_[:, s, hf * 512:(hf + 1) * 512], in0=tA[:], in1=xt[:, s, hf * 512:(hf + 1) * 512], op=ALU.add),
                          reads=[b_tA, b_xt], writes=[b_h])
            dma(S, "sp", W["hS"][t * 512:(t + 1) * 512, :].rearrange("(s p) d -> p s d", p=128), hh_[:], reads=[b_h])
            S.add("dve", lambda e: e.memset(ssq[:], 0.0), writes=[b_ssq])
            for s in range(4):
                S.add("act", lambda e, s=s: e.activation(out=junk[:], in_=hh_[:, s, :], func=AF.Square, accum_out=ssq[:, s:s + 1]),
                      reads=[b_h, b_ssq], writes=[b_junk, b_ssq])
            S.add("dve", lambda e: e.tensor_scalar(out=ssq[:, 4:8], in0=ssq[:, 0:4], scalar1=1.0 / D, scalar2=EPS, op0=ALU.mult, op1=ALU.add),
                  reads=[b_ssq], writes=[b_ssq])
            S.add("act", lambda e: e.activation(out=ssq[:, 4:8], in_=ssq[:, 4:8], func=AF.Sqrt), reads=[b_ssq], writes=[b_ssq])
            S.add("dve", lambda e: e.reciprocal(out=ssq[:, 4:8], in_=ssq[:, 4:8]), reads=[b_ssq], writes=[b_ssq])
            for s in range(4):
                S.add("dve", lambda e, s=s: e.tensor_scalar(out=xn2[:, s, :], in0=hh_[:, s, :], scalar1=ssq[:, 4 + s:5 + s], scalar2=None, op0=ALU.mult),
                      reads=[b_h, b_ssq], writes=[b_xn2])
            u2t = u2[sl]
            for k in range(8):
                bk = 6 + k % 2
                ptb = PS[bk][:].bitcast(BF16)
                for s in range(4):
                    S.add("pe", lambda e, s=s, k=k, ptb=ptb: e.transpose(out=ptb[:, s * 128:(s + 1) * 128], in_=xn2[:, s, k * 128:(k + 1) * 128], identity=C.ident_b[:]),
                          reads=[b_xn2, bc], writes=[PB[bk]])
                S.add("dve", lambda e, k=k, ptb=ptb, u2t=u2t: e.tensor_scalar(out=u2t[:, k, :], in0=ptb[:, 0:512], scalar1=cols[:, COL_G2 + k:COL_G2 + k + 1],
                                                                           scalar2=cols[:, COL_MOD + 24 + k:COL_MOD + 25 + k], op0=ALU.mult, op1=ALU.add),
                      reads=[PB[bk], C.b_cols], writes=[b_u2[sl]])
            dma(S, "sp", W["u2T"][:, t * 512:(t + 1) * 512].rearrange("(c p) n -> p c n", p=128), u2t[:], reads=[b_u2[sl]])
            bk = 4 + rot % 2
            rot += 1
            for s in range(4):
                for k in range(8):
                    S.add("pe", lambda e, bk=bk, k=k, s=s, u2t=u2t: e.matmul(PS[bk][:, s * 36:(s + 1) * 36], lhsT=u2t[:, k, s * 128:(s + 1) * 128], rhs=wr[:, k, :],
                                                                        start=(k == 0), stop=(k == 7)), reads=[b_u2[sl], b_w], writes=[PB[bk]])
            def route(t=t, sl=sl, bk=bk, u2t=u2t):
                nonlocal rot
                S.add("dve", lambda e, bk=bk: e.tensor_tensor(out=rt[:], in0=PS[bk][:, 0:144].rearrange("p (s n) -> p s n", n=36),
                                                             in1=brt[:].unsqueeze(1).broadcast_to([128, 4, 36]), op=ALU.add),
                      reads=[PB[bk], b_w], writes=[b_rt])
                RR = [b_rt, b_rw]

                def dv(fn, **kw):
                    S.add("dve", lambda e: getattr(e, fn)(**kw), reads=RR, writes=[b_rw])

                def av(**kw):
                    S.add("act", lambda e: e.activation(**kw), reads=RR, writes=[b_rw])

                G = rt[:, :, 0:4]
                E4 = rt[:, :, 4:36].rearrange("p s (g e) -> p s g e", e=8)
                w_ = lambda i: rw[:, i, :]
                b4 = lambda a: a.unsqueeze(2).broadcast_to([128, 4, 4])
                b8 = lambda a: a.unsqueeze(2).broadcast_to([128, 4, 8])
                GMAX, GSUM, GP, M1, M2, DD, ED, W1, W1G, W2G = range(10)
                dv("tensor_reduce", out=w_(GMAX), in_=G, axis=AX.X, op=ALU.max)
                dv("tensor_tensor", out=ohg[:], in0=G, in1=b4(w_(GMAX)), op=ALU.subtract)
                av(out=tE[:, 0, :, 0:4], in_=ohg[:], func=AF.Exp)
                dv("tensor_reduce", out=w_(GSUM), in_=tE[:, 0, :, 0:4], axis=AX.X, op=ALU.add)
                dv("reciprocal", out=w_(GP), in_=w_(GSUM))
                dv("tensor_tensor", out=ohg[:], in0=G, in1=b4(w_(GMAX)), op=ALU.is_equal)
                dv("tensor_tensor", out=tE[:], in0=E4, in1=ohg[:].unsqueeze(3).broadcast_to([128, 4, 4, 8]), op=ALU.mult)
                dv("tensor_reduce", out=es[:, 0], in_=tE[:].rearrange("p s g e -> p s e g"), axis=AX.X, op=ALU.add)
                dv("tensor_reduce", out=w_(M1), in_=es[:, 0], axis=AX.X, op=ALU.max)
                dv("tensor_tensor", out=es[:, 1], in0=es[:, 0], in1=b8(w_(M1)), op=ALU.is_equal)
                dv("scalar_tensor_tensor", out=es[:, 2], in0=es[:, 1], scalar=-1e30, in1=es[:, 0], op0=ALU.mult, op1=ALU.add)
                dv("tensor_reduce", out=w_(M2), in_=es[:, 2], axis=AX.X, op=ALU.max)
                dv("tensor_tensor", out=es[:, 3], in0=es[:, 2], in1=b8(w_(M2)), op=ALU.is_equal)
                dv("tensor_tensor", out=w_(DD), in0=w_(M2), in1=w_(M1), op=ALU.subtract)
                av(out=w_(ED), in_=w_(DD), func=AF.Exp)
                dv("tensor_scalar", out=w_(W1), in0=w_(ED), scalar1=1.0, scalar2=None, op0=ALU.add)
                dv("reciprocal", out=w_(W1), in_=w_(W1))
                dv("tensor_tensor", out=w_(W1G), in0=w_(W1), in1=w_(GP), op=ALU.mult)
                dv("tensor_tensor", out=w_(W2G), in0=w_(W1G), in1=w_(ED), op=ALU.mult)
                dv("tensor_tensor", out=es[:, 4], in0=es[:, 1], in1=b8(w_(W1G)), op=ALU.mult)
                dv("tensor_tensor", out=es[:, 5], in0=es[:, 3], in1=b8(w_(W2G)), op=ALU.mult)
                dv("tensor_tensor", out=es[:, 4], in0=es[:, 4], in1=es[:, 5], op=ALU.add)
                if C.sparse:
                    dma(S, "sp", W["XN"][t * 512:(t + 1) * 512, :].rearrange("(s p) d -> p s d", p=128), xn2[:], reads=[b_xn2])
                    dma(S, "sp", W["CG"][t * 512:(t + 1) * 512, :].rearrange("(s p) e -> p s e", p=128), es[:, 4], reads=[b_rw])
                    S.add("dve", lambda e: e.tensor_copy(out=ohgb[:], in_=ohg[:]), reads=RR, writes=[b_ohgb])
                    bk2 = 4 + rot % 2
                    rot += 1
                    S.add("pe", lambda e, bk2=bk2: e.matmul(PS[bk2][:, 0:16], lhsT=ustr[:], rhs=ohgb[:].rearrange("p s g -> p (s g)"), start=True, stop=True),
                          reads=[b_ohgb], writes=[PB[bk2]])
                    S.add("pe", lambda e, bk2=bk2: e.matmul(PS[bk2][:, 16:32], lhsT=C.ones_b[:], rhs=ohgb[:].rearrange("p s g -> p (s g)"), start=True, stop=True),
                          reads=[b_ohgb, bc], writes=[PB[bk2]])
                    S.add("dve", lambda e: e.tensor_copy(out=base[:, 0, :], in_=C.tot[:]), reads=[b_tot], writes=[b_base])
                    for s_ in range(1, 4):
                        S.add("dve", lambda e, s_=s_, bk2=bk2: e.tensor_tensor(out=base[:, s_, :], in0=base[:, s_ - 1, :], in1=PS[bk2][:, 16 + 4 * (s_ - 1):16 + 4 * s_], op=ALU.add),
                              reads=[b_base, PB[bk2]], writes=[b_base])
                    S.add("dve", lambda e, bk2=bk2: e.tensor_tensor(out=C.tot[:], in0=base[:, 3, :], in1=PS[bk2][:, 28:32], op=ALU.add),
                          reads=[b_base, PB[bk2]], writes=[b_tot])
                    S.add("dve", lambda e, bk2=bk2: e.tensor_tensor(out=base[:], in0=base[:], in1=PS[bk2][:, 0:16].rearrange("p (s g) -> p s g", g=4), op=ALU.add),
                          reads=[b_base, PB[bk2]], writes=[b_base])
                    S.add("dve", lambda e: e.tensor_tensor(out=base[:], in0=base[:], in1=ohg[:], op=ALU.mult), reads=[b_base] + RR, writes=[b_base])
                    S.add("dve", lambda e, t=t: e.tensor_reduce(out=C.RK[:, t * 4:(t + 1) * 4], in_=base[:], axis=AX.X, op=ALU.add), reads=[b_base], writes=[C.b_rk])
                    S.add("dve", lambda e, t=t: e.tensor_copy(out=C.OH[:, t * 4:(t + 1) * 4, :], in_=ohg[:]), reads=RR, writes=[C.b_rk])
                cb = comb[sl]
                S.add("dve", lambda e, cb=cb: e.tensor_tensor(out=cb[:].rearrange("p s (g e) -> p s g e", e=8), in0=ohg[:].unsqueeze(3).broadcast_to([128, 4, 4, 8]),
                                                             in1=es[:, 4].unsqueeze(2).broadcast_to([128, 4, 4, 8]), op=ALU.mult),
                      reads=RR, writes=[b_cb[sl]])
                dma(S, "sp", W["combS"][t * 512:(t + 1) * 512, :].rearrange("(s p) e -> p s e", p=128), cb[:], reads=[b_cb[sl]])
            routes.append(route)
        while routes:
            routes.pop()()
        if C.sparse:
            TH = sb("TH", [128, 4, 8], F32)
            cmpt = sb("cmpt", [128, 4, 8], F32)
            kg = sb("kg", [128, 4], F32)
            ck = sb("ck", [128, 5], F32)
            bsl = sb("bsl", [128, 4], F32)
            tmp3 = sb("tmp3", [128, 32, 4], F32)
            slf = sb("slf", [128, 32], F32)
            SI = sb("SI", [128, 12], F32)
            cmp2 = sb("cmp2", [128, 12, 3], F32)
            tgf = sb("tgf", [128, 12], F32)
            bq_ = Buf("slotcalc")
            RQ = [bq_, b_tot, C.b_rk]

            def dq(fn, **kw):
                S.add("dve", lambda e: getattr(e, fn)(**kw), reads=RQ, writes=[bq_])

            for j in range(8):
                dq("memset", ap=TH[:, :, j:j + 1], constant=512.0 * j)
            for j in range(12):
                dq("memset", ap=SI[:, j:j + 1], constant=float(j))
            dq("tensor_tensor", out=cmpt[:], in0=C.tot[:].unsqueeze(2).broadcast_to([128, 4, 8]), in1=TH[:], op=ALU.is_gt)
            dq("tensor_reduce", out=kg[:], in_=cmpt[:], axis=AX.X, op=ALU.add)
            dq("memset", ap=ck[:, 0:1], constant=0.0)
            for g in range(4):
                dq("tensor_tensor", out=ck[:, g + 1:g + 2], in0=ck[:, g:g + 1], in1=kg[:, g:g + 1], op=ALU.add)
            dq("tensor_scalar", out=bsl[:], in0=ck[:, 0:4], scalar1=512.0, scalar2=None, op0=ALU.mult)
            dq("tensor_tensor", out=tmp3[:], in0=C.OH[:], in1=bsl[:].unsqueeze(1).broadcast_to([128, 32, 4]), op=ALU.mult)
            dq("tensor_reduce", out=slf[:], in_=tmp3[:], axis=AX.X, op=ALU.add)
            dq("tensor_tensor", out=slf[:], in0=slf[:], in1=C.RK[:], op=ALU.add)
            dq("tensor_copy", out=C.SLOT_I[:], in_=slf[:])
            dq("tensor_tensor", out=cmp2[:], in0=ck[:, 1:4].unsqueeze(1).broadcast_to([128, 12, 3]), in1=SI[:].unsqueeze(2).broadcast_to([128, 12, 3]), op=ALU.is_le)
            dq("tensor_reduce", out=tgf[:], in_=cmp2[:], axis=AX.X, op=ALU.add)
            dq("tensor_copy", out=C.TG_I[:], in_=tgf[:])
            pidx_i = sb("pidx_i", [128, 1], mybir.dt.int32)
            pidx = sb("pidx", [128, 1], F32)
            j128 = sb("j128", [128, 8], F32)
            widf = sb("widf", [128, 12, 8], F32)
            S.add("pool", lambda e: e.iota(pidx_i[:], [[0, 1]], base=0, channel_multiplier=1), writes=[bq_])
            dq("tensor_copy", out=pidx[:], in_=pidx_i[:])
            for j in range(8):
                dq("memset", ap=j128[:, j:j + 1], constant=128.0 * j)
            dq("tensor_scalar", out=j128[:], in0=j128[:], scalar1=pidx[:, 0:1], scalar2=None, op0=ALU.add)
            dq("scalar_tensor_tensor", out=widf[:], in0=tgf[:].unsqueeze(2).broadcast_to([128, 12, 8]), scalar=1024.0,
               in1=j128[:].unsqueeze(1).broadcast_to([128, 12, 8]), op0=ALU.mult, op1=ALU.add)
            dq("tensor_copy", out=C.WIDX[:].rearrange("p (s j) -> p s j", j=8), in_=widf[:])
            if "dbg_slot" in C.debug:
                C.dbg_slot = nc.dram_tensor("dbg_slot", [128, 64], F32, kind="ExternalOutput").ap()
                dma(S, "sp", C.dbg_slot[:, 0:32], slf[:], reads=[bq_])
                dma(S, "sp", C.dbg_slot[:, 32:44], tgf[:], reads=[bq_])
                dma(S, "sp", C.dbg_slot[:, 44:48], C.tot[:], reads=[bq_])
        S.flush()


def phase4b(C):
    nc, S, I, W = C.nc, C.S, C.I, C.W
    PS, PB = C.PS, C.PB
    n_exp = C.n_exp if hasattr(C, "n_exp") else NEXP
    with ExitStack() as _es:
        sb = lambda name, shape, dt: _es.enter_context(nc.sbuf_tensor(name, shape, dt))
        u2t = sb("u2tt", [128, 8, 1024], BF16)
        cbt = sb("cbt", [128, 8, 32], F32)
        yacc = sb("yacc", [128, 8, D], F32)
        wg = [sb(f"wg{i}", [128, 8, 256], BF16) for i in range(2)]
        wu = [sb(f"wu{i}", [128, 8, 256], BF16) for i in range(2)]
        wd = [sb(f"wd{i}", [128, 2, D], BF16) for i in range(2)]
        sg = [sb(f"sg{i}", [128, 2, 512], BF16) for i in range(2)]
        hd = [sb(f"hd{i}", [128, 2, 512], BF16) for i in range(2)]
        hp = sb("hp", [128, 4, D], F32)
        gfin = sb("gfin", [128, D], F32)
        junk = sb("junk5", [128, D], BF16)
        ssq = sb("ssq5", [128, 8], F32)
        b_u2t, b_cbt, b_yacc, b_hp, b_gfin, b_junk, b_ssq = [Buf(n) for n in "u2t cbt yacc hp gfin junk ssq".split()]
        b_wg = [Buf("wg0"), Buf("wg1")]
        b_wu = [Buf("wu0"), Buf("wu1")]
        b_wd = [Buf("wd0"), Buf("wd1")]
        b_sg = [Buf("sg0"), Buf("sg1")]
        b_hd = [Buf("hd0"), Buf("hd1")]
        dma(S, "sp", gfin[:], I["g_final"].partition_broadcast(128), writes=[b_gfin])

        def load_w(TT, e, sl):
            if TT == 0:
                dma(S, "pool", wg[sl][:], I["w_exp_gate"][e].rearrange("(k p) f -> p k f", p=128), writes=[b_wg[sl]])
                dma(S, "pool", wu[sl][:], I["w_exp_up"][e].rearrange("(k p) f -> p k f", p=128), writes=[b_wu[sl]])
                dma(S, "pool", wd[sl][:], I["w_exp_down"][e].rearrange("(k p) d -> p k d", p=128), writes=[b_wd[sl]])
                dma(S, "sp", W["wgS"][e].rearrange("(k p) f -> p k f", p=128), wg[sl][:], reads=[b_wg[sl]])
                dma(S, "sp", W["wuS"][e].rearrange("(k p) f -> p k f", p=128), wu[sl][:], reads=[b_wu[sl]])
                dma(S, "sp", W["wdS"][e].rearrange("(k p) d -> p k d", p=128), wd[sl][:], reads=[b_wd[sl]])
            else:
                dma(S, "sp", wg[sl][:], W["wgS"][e].rearrange("(k p) f -> p k f", p=128), writes=[b_wg[sl]])
                dma(S, "sp", wu[sl][:], W["wuS"][e].rearrange("(k p) f -> p k f", p=128), writes=[b_wu[sl]])
                dma(S, "sp", wd[sl][:], W["wdS"][e].rearrange("(k p) d -> p k d", p=128), writes=[b_wd[sl]])

        rot = 0
        cnt = 0
        for TT in range(4):
            dma(S, "sp", u2t[:], W["u2T"][:, TT * 1024:(TT + 1) * 1024].rearrange("(c p) n -> p c n", p=128), writes=[b_u2t])
            dma(S, "sp", cbt[:], W["combS"][TT * 1024:(TT + 1) * 1024, :].rearrange("(s p) e -> p s e", p=128), writes=[b_cbt])
            S.add("pool", lambda e: e.memset(yacc[:], 0.0), writes=[b_yacc])
            load_w(TT, 0, 0)
            for ex in range(n_exp):
                sl = ex % 2
                if ex + 1 < n_exp:
                    load_w(TT, ex + 1, (ex + 1) % 2)
                for half in range(2):
                    c2 = cnt % 2
                    cnt += 1
                    for f in range(2):
                        for k in range(8):
                            S.add("pe", lambda e, f=f, k=k, sl=sl, half=half: e.matmul(PS[f][:], lhsT=wg[sl][:, k, f * 128:(f + 1) * 128], rhs=u2t[:, k, half * 512:(half + 1) * 512],
                                                                                  start=(k == 0), stop=(k == 7)), reads=[b_wg[sl], b_u2t], writes=[PB[f]])
                    for f in range(2):
                        for k in range(8):
                            S.add("pe", lambda e, f=f, k=k, sl=sl, half=half: e.matmul(PS[2 + f][:], lhsT=wu[sl][:, k, f * 128:(f + 1) * 128], rhs=u2t[:, k, half * 512:(half + 1) * 512],
                                                                                  start=(k == 0), stop=(k == 7)), reads=[b_wu[sl], b_u2t], writes=[PB[2 + f]])
                    for f in range(2):
                        S.add("act", lambda e, f=f, c2=c2: e.activation(out=sg[c2][:, f, :], in_=PS[f][:], func=AF.Silu), reads=[PB[f]], writes=[b_sg[c2]])
                        S.add("dve", lambda e, f=f, c2=c2: e.tensor_tensor(out=hd[c2][:, f, :], in0=PS[2 + f][:], in1=sg[c2][:, f, :], op=ALU.mult),
                              reads=[PB[2 + f], b_sg[c2]], writes=[b_hd[c2]])
                    for sub in range(4):
                        for dh in range(2):
                            bk = 4 + rot % 4
                            rot += 1
                            for f in range(2):
                                S.add("pe", lambda e, bk=bk, f=f, sub=sub, dh=dh, c2=c2, sl=sl: e.matmul(
                                    PS[bk][:], lhsT=hd[c2][:, f, sub * 128:(sub + 1) * 128], rhs=wd[sl][:, f, dh * 512:(dh + 1) * 512], start=(f == 0), stop=(f == 1)),
                                    reads=[b_hd[c2], b_wd[sl]], writes=[PB[bk]])
                            s8 = half * 4 + sub
                            S.add("dve", lambda e, bk=bk, s8=s8, dh=dh, ex=ex: e.scalar_tensor_tensor(
                                out=yacc[:, s8, dh * 512:(dh + 1) * 512], in0=PS[bk][:], scalar=cbt[:, s8, ex:ex + 1], in1=yacc[:, s8, dh * 512:(dh + 1) * 512],
                                op0=ALU.mult, op1=ALU.add), reads=[PB[bk], b_cbt, b_yacc], writes=[b_yacc])
            for half in range(2):
                r0 = TT * 1024 + half * 512
                dma(S, "sp", hp[:], W["hS"][r0:r0 + 512, :].rearrange("(s p) d -> p s d", p=128), writes=[b_hp])
                ya = yacc[:, half * 4:(half + 1) * 4, :]
                S.add("dve", lambda e, ya=ya: e.tensor_tensor(out=ya, in0=ya, in1=C.gtf_bc[:].unsqueeze(1).broadcast_to([128, 4, D]), op=ALU.mult),
                      reads=[b_yacc, C.b_gt], writes=[b_yacc])
                S.add("pool", lambda e, ya=ya: e.tensor_tensor(out=hp[:], in0=hp[:], in1=ya, op=ALU.add), reads=[b_yacc, b_hp], writes=[b_hp])
                S.add("dve", lambda e: e.memset(ssq[:], 0.0), writes=[b_ssq])
                for s in range(4):
                    S.add("act", lambda e, s=s: e.activation(out=junk[:], in_=hp[:, s, :], func=AF.Square, accum_out=ssq[:, s:s + 1]),
                          reads=[b_hp, b_ssq], writes=[b_junk, b_ssq])
                S.add("dve", lambda e: e.tensor_scalar(out=ssq[:, 4:8], in0=ssq[:, 0:4], scalar1=1.0 / D, scalar2=EPS, op0=ALU.mult, op1=ALU.add),
                      reads=[b_ssq], writes=[b_ssq])
                S.add("act", lambda e: e.activation(out=ssq[:, 4:8], in_=ssq[:, 4:8], func=AF.Sqrt), reads=[b_ssq], writes=[b_ssq])
                S.add("dve", lambda e: e.reciprocal(out=ssq[:, 4:8], in_=ssq[:, 4:8]), reads=[b_ssq], writes=[b_ssq])
                for s in range(4):
                    S.add("dve", lambda e, s=s: e.scalar_tensor_tensor(out=hp[:, s, :], in0=hp[:, s, :], scalar=ssq[:, 4 + s:5 + s], in1=gfin[:], op0=ALU.mult, op1=ALU.mult),
                          reads=[b_hp, b_ssq, b_gfin], writes=[b_hp])
                dma(S, "sp", C.out[r0:r0 + 512, :].rearrange("(s p) d -> p s d", p=128), hp[:], reads=[b_hp])
        S.flush()
```
